# Optimizing a Trainium2 kernel written in Bass

```python
import math
import jax
import jax.numpy as jnp
from jax import lax
import numpy as np

D_MODEL = 2048
BATCH = 8
SEQ = 2048
DEPTH = 4

GRID_W = 64
CTX_LEN = 256
N_MIXERS = 3
EPS = 1e-6
ROPE_BASE = 10000.0
Q_BLOCK = 128
NEG_INF = -1e30

DA_HEADS = 16
DA_DIM = D_MODEL // (2 * DA_HEADS)

DN_HK = 16
DN_HV = 32
DN_DK = D_MODEL // DN_HK
DN_DV = DN_DK
DN_CONV = 5
DN_CHUNK = 64
DN_QK = DN_HK * DN_DK
DN_VD = DN_HV * DN_DV
DN_QKV = 2 * DN_QK + DN_VD
DN_IN = DN_QKV + DN_VD + 4 * DN_HV

WA_HQ = 32
WA_HKV = 4
WA_GROUP = WA_HQ // WA_HKV
WA_DIM = D_MODEL // WA_HQ
WINDOW = 128
WA_QKV = (WA_HQ + 2 * WA_HKV) * WA_DIM

N_GROUPS = 4
EXPERTS_PER_GROUP = 8
N_EXPERTS = N_GROUPS * EXPERTS_PER_GROUP
TOP_K = 2
D_EXPERT = 3 * D_MODEL // 8
MOE_BLOCK = 128

kernel_name = 'hybrid_diffusion_block'


def layer_plan():
    counts = [0] * N_MIXERS
    plan = []
    for i in range(DEPTH):
        kind = i % N_MIXERS
        plan.append((kind, counts[kind]))
        counts[kind] += 1
    return plan, counts


def rms_norm(x, w):
    xf = x.astype(jnp.float32)
    y = xf * lax.rsqrt(jnp.mean(xf * xf, axis=-1, keepdims=True) + EPS)
    return (y * w.astype(jnp.float32)).astype(x.dtype)


def l2_normalize(x):
    return x * lax.rsqrt(jnp.sum(x * x, axis=-1, keepdims=True) + EPS)


def axial_rope_tables(n_tokens, dim):
    rows = n_tokens // GRID_W
    row, col = jnp.meshgrid(jnp.arange(rows), jnp.arange(GRID_W), indexing='ij')
    row = row.reshape(-1).astype(jnp.float32)
    col = col.reshape(-1).astype(jnp.float32)
    half = dim // 2
    inv_freq = 1.0 / (ROPE_BASE ** (jnp.arange(0, half, 2, dtype=jnp.float32) / half))
    def table(pos):
        ang = pos[:, None] * inv_freq[None, :]
        ang = jnp.concatenate([ang, ang], axis=-1)
        return jnp.cos(ang), jnp.sin(ang)
    cr, sr = table(row)
    cc, scol = table(col)
    return jnp.concatenate([cr, cc], -1), jnp.concatenate([sr, scol], -1)


def apply_axial_rope(x, cos, sin):
    dim = x.shape[-1]
    half = dim // 2
    shape = (1, cos.shape[0]) + (1,) * (x.ndim - 3) + (dim,)
    cos = cos.reshape(shape)
    sin = sin.reshape(shape)
    xf = x.astype(jnp.float32)
    def rot_half(u):
        u1, u2 = jnp.split(u, 2, axis=-1)
        return jnp.concatenate([-u2, u1], axis=-1)
    rotated = jnp.concatenate([rot_half(xf[..., :half]), rot_half(xf[..., half:])], -1)
    return (xf * cos + rotated * sin).astype(x.dtype)


def diff_attention(hx, hc, w_qkv, lam, subln_w, w_o, lambda_init, cos, sin, need_ctx):
    B, L, _ = hx.shape
    def project(h):
        n = h.shape[1]
        q, k, v = jnp.split(h @ w_qkv, 3, axis=-1)
        return (q.reshape(B, n, DA_HEADS, 2, DA_DIM),
                k.reshape(B, n, DA_HEADS, 2, DA_DIM),
                v.reshape(B, n, DA_HEADS, 2 * DA_DIM))
    qx, kx, vx = project(hx)
    qx = apply_axial_rope(qx, cos, sin)
    kx = apply_axial_rope(kx, cos, sin)
    qc, kc, vc = project(hc)
    lamf = lam.astype(jnp.float32)
    lam_full = (jnp.exp(jnp.sum(lamf[0] * lamf[1])) - jnp.exp(jnp.sum(lamf[2] * lamf[3]))
                + lambda_init)
    scale = DA_DIM ** -0.5
    def attend(q, k, v):
        s = jnp.einsum('bqhcd,bkhcd->bhcqk', q, k).astype(jnp.float32) * scale
        p = jax.nn.softmax(s, axis=-1)
        a = p[:, :, 0] - lam_full * p[:, :, 1]
        return jnp.einsum('bhqk,bkhe->bqhe', a.astype(v.dtype), v)
    k_all = jnp.concatenate([kc, kx], axis=1)
    v_all = jnp.concatenate([vc, vx], axis=1)
    nb = L // Q_BLOCK
    q_blocks = jnp.moveaxis(qx.reshape(B, nb, Q_BLOCK, DA_HEADS, 2, DA_DIM), 1, 0)
    ox = lax.map(lambda qb: attend(qb, k_all, v_all), q_blocks)
    ox = jnp.moveaxis(ox, 0, 1).reshape(B, L, DA_HEADS, 2 * DA_DIM)
    def head_out(o):
        n = o.shape[1]
        o = rms_norm(o, subln_w) * (1.0 - lambda_init)
        return o.reshape(B, n, D_MODEL) @ w_o
    out_c = head_out(attend(qc, kc, vc)) if need_ctx else None
    return head_out(ox), out_c


def depthwise_conv_centred(x, w):
    k, ch = w.shape
    return lax.conv_general_dilated(
        x, w.reshape(k, 1, ch).astype(x.dtype), window_strides=(1,),
        padding=[((k - 1) // 2, k // 2)], dimension_numbers=('NWC', 'WIO', 'NWC'),
        feature_group_count=ch)


def dn_stream(h, w_in, conv_w):
    B, n, _ = h.shape
    proj = h @ w_in
    qkv = jax.nn.silu(depthwise_conv_centred(proj[..., :DN_QKV], conv_w)).astype(jnp.float32)
    z = proj[..., DN_QKV:DN_QKV + DN_VD]
    ba = proj[..., DN_QKV + DN_VD:].astype(jnp.float32).reshape(B, n, 2, 2, DN_HV)
    q = l2_normalize(qkv[..., :DN_QK].reshape(B, n, DN_HK, DN_DK)) * (DN_DK ** -0.5)
    k = l2_normalize(qkv[..., DN_QK:2 * DN_QK].reshape(B, n, DN_HK, DN_DK))
    v = qkv[..., 2 * DN_QK:].reshape(B, n, DN_HV, DN_DV)
    rep = DN_HV // DN_HK
    return jnp.repeat(q, rep, axis=2), jnp.repeat(k, rep, axis=2), v, z, ba


def dn_gates(ba, a_log, dt_bias, d):
    beta = jax.nn.sigmoid(ba[:, :, d, 0])
    g = -jnp.exp(a_log[d].astype(jnp.float32)) * jax.nn.softplus(
        ba[:, :, d, 1] + dt_bias[d].astype(jnp.float32))
    return beta, g


def to_chunks(t):
    B, n, H = t.shape[:3]
    t = jnp.moveaxis(t, 2, 1)
    return t.reshape((B, H, n // DN_CHUNK, DN_CHUNK) + t.shape[3:])


def chunk_gated_delta_rule(q, k, v, beta, g, state):
    B, n, H, _ = q.shape
    q, k, v, beta, g = (to_chunks(t) for t in (q, k, v, beta, g))
    gc = jnp.cumsum(g, axis=-1)
    C = DN_CHUNK
    incl = jnp.tril(jnp.ones((C, C), bool))
    strict = jnp.tril(jnp.ones((C, C), bool), -1)
    decay = jnp.exp(jnp.where(incl, gc[..., :, None] - gc[..., None, :], -jnp.inf))
    kb = k * beta[..., None]
    lower = jnp.where(strict, jnp.einsum('bhnid,bhnjd->bhnij', kb, k) * decay, 0.0)
    a_mat = lower + jnp.eye(C, dtype=lower.dtype)
    rhs = jnp.concatenate([v * beta[..., None], kb * jnp.exp(gc)[..., None]], axis=-1)
    sol = lax.linalg.triangular_solve(a_mat, rhs, left_side=True, lower=True, unit_diagonal=True)
    u, w = sol[..., :DN_DV], sol[..., DN_DV:]
    intra = jnp.einsum('bhnid,bhnjd->bhnij', q, k) * decay
    q_dec = q * jnp.exp(gc)[..., None]
    k_dec = k * jnp.exp(gc[..., -1:] - gc)[..., None]
    g_tot = jnp.exp(gc[..., -1])
    xs = tuple(jnp.moveaxis(t, 2, 0) for t in (u, w, q_dec, k_dec, intra, g_tot))
    def step(S, inp):
        u_i, w_i, qd_i, kd_i, a_i, gt_i = inp
        v_new = u_i - jnp.einsum('bhck,bhkv->bhcv', w_i, S)
        o_i = (jnp.einsum('bhck,bhkv->bhcv', qd_i, S)
               + jnp.einsum('bhij,bhjv->bhiv', a_i, v_new))
        S = S * gt_i[..., None, None] + jnp.einsum('bhck,bhcv->bhkv', kd_i, v_new)
        return S, o_i
    state, o = lax.scan(step, state, xs)
    o = jnp.moveaxis(o, 0, 2).reshape(B, H, n, DN_DV)
    return jnp.moveaxis(o, 1, 2), state


def gated_deltanet(hx, hc, w_in, conv_w, a_log, dt_bias, norm_w, w_o, need_ctx):
    B, L, _ = hx.shape
    qx, kx, vx, zx, bax = dn_stream(hx, w_in, conv_w)
    qc, kc, vc, zc, bac = dn_stream(hc, w_in, conv_w)
    state0 = jnp.zeros((B, DN_HV, DN_DK, DN_DV), jnp.float32)
    outs_x = []
    outs_c = []
    for d in range(2):
        rev = (lambda t: jnp.flip(t, axis=1)) if d == 1 else (lambda t: t)
        beta_c, g_c = dn_gates(bac, a_log, dt_bias, d)
        beta_x, g_x = dn_gates(bax, a_log, dt_bias, d)
        o_c, s_c = chunk_gated_delta_rule(rev(qc), rev(kc), rev(vc), rev(beta_c), rev(g_c), state0)
        o_x, _ = chunk_gated_delta_rule(rev(qx), rev(kx), rev(vx), rev(beta_x), rev(g_x), s_c)
        outs_x.append(rev(o_x))
        if need_ctx:
            outs_c.append(rev(o_c))
    def head_out(o, z):
        n = o.shape[1]
        o = rms_norm(o, norm_w) * jax.nn.silu(z.astype(jnp.float32)).reshape(B, n, DN_HV, DN_DV)
        return o.reshape(B, n, DN_VD).astype(hx.dtype) @ w_o
    out_c = head_out(outs_c[0] + outs_c[1], zc) if need_ctx else None
    return head_out(outs_x[0] + outs_x[1], zx), out_c


def window_sink_gqa(hx, hc, w_qkv, sinks, w_o, cos, sin, need_ctx):
    B, L, _ = hx.shape
    Lc = hc.shape[1]
    def project(h):
        n = h.shape[1]
        qkv = h @ w_qkv
        q = qkv[..., :WA_HQ * WA_DIM].reshape(B, n, WA_HKV, WA_GROUP, WA_DIM)
        k = qkv[..., WA_HQ * WA_DIM:(WA_HQ + WA_HKV) * WA_DIM].reshape(B, n, WA_HKV, WA_DIM)
        v = qkv[..., (WA_HQ + WA_HKV) * WA_DIM:].reshape(B, n, WA_HKV, WA_DIM)
        return q, k, v
    qx, kx, vx = project(hx)
    qx = apply_axial_rope(qx, cos, sin)
    kx = apply_axial_rope(kx, cos, sin)
    qc, kc, vc = project(hc)
    scale = WA_DIM ** -0.5
    sink = sinks.astype(jnp.float32).reshape(1, WA_HKV, WA_GROUP, 1, 1)
    def softmax_with_sink(s):
        s_sink = jnp.broadcast_to(sink, s.shape[:-1] + (1,))
        return jax.nn.softmax(jnp.concatenate([s, s_sink], axis=-1), axis=-1)[..., :-1]
    def ctx_scores(q):
        return jnp.einsum('bqhgd,bkhd->bhgqk', q, kc).astype(jnp.float32) * scale
    band = Q_BLOCK + 2 * WINDOW
    k_pad = jnp.pad(kx, ((0, 0), (WINDOW, WINDOW), (0, 0), (0, 0)))
    v_pad = jnp.pad(vx, ((0, 0), (WINDOW, WINDOW), (0, 0), (0, 0)))
    def block(b):
        start = b * Q_BLOCK
        qb = lax.dynamic_slice_in_dim(qx, start, Q_BLOCK, axis=1)
        kb = lax.dynamic_slice_in_dim(k_pad, start, band, axis=1)
        vb = lax.dynamic_slice_in_dim(v_pad, start, band, axis=1)
        qpos = start + jnp.arange(Q_BLOCK)
        kpos = start - WINDOW + jnp.arange(band)
        valid = ((jnp.abs(qpos[:, None] - kpos[None, :]) <= WINDOW)
                 & (kpos >= 0)[None, :] & (kpos < L)[None, :])
        s_band = jnp.einsum('bqhgd,bkhd->bhgqk', qb, kb).astype(jnp.float32) * scale
        s_band = jnp.where(valid, s_band, NEG_INF)
        p = softmax_with_sink(jnp.concatenate([ctx_scores(qb), s_band], axis=-1)).astype(vb.dtype)
        return (jnp.einsum('bhgqk,bkhd->bqhgd', p[..., :Lc], vc)
                + jnp.einsum('bhgqk,bkhd->bqhgd', p[..., Lc:], vb))
    ox = lax.map(block, jnp.arange(L // Q_BLOCK))
    ox = jnp.moveaxis(ox, 0, 1).reshape(B, L, D_MODEL)
    out_c = None
    if need_ctx:
        p = softmax_with_sink(ctx_scores(qc)).astype(vc.dtype)
        oc = jnp.einsum('bhgqk,bkhd->bqhgd', p, vc).reshape(B, Lc, D_MODEL)
        out_c = oc @ w_o
    return ox @ w_o, out_c


def routed_experts(h, e_flat, g_flat, w13, w2):
    T, D = h.shape
    TK = e_flat.shape[0]
    tok = jnp.arange(TK) // TOP_K
    order = jnp.argsort(e_flat)
    e_sorted = e_flat[order]
    tok_sorted = tok[order]
    counts = jnp.zeros((N_EXPERTS,), jnp.int32).at[e_flat].add(1)
    padded = (counts + MOE_BLOCK - 1) // MOE_BLOCK * MOE_BLOCK
    start = jnp.cumsum(counts) - counts
    pstart = jnp.cumsum(padded) - padded
    dest = pstart[e_sorted] + jnp.arange(TK) - start[e_sorted]
    n_blocks = (TK + MOE_BLOCK - 1) // MOE_BLOCK + N_EXPERTS
    P = n_blocks * MOE_BLOCK
    buf_tok = jnp.zeros((P,), jnp.int32).at[dest].set(tok_sorted)
    xb = h[buf_tok].reshape(n_blocks, MOE_BLOCK, D)
    block_e = jnp.searchsorted(jnp.cumsum(padded), jnp.arange(n_blocks) * MOE_BLOCK, side='right')
    block_e = jnp.minimum(block_e, N_EXPERTS - 1)
    def one_block(args):
        xblk, e = args
        gate, up = jnp.split(xblk @ w13[e], 2, axis=-1)
        return (jax.nn.silu(gate) * up) @ w2[e]
    yb = lax.map(one_block, (xb, block_e)).reshape(P, D)
    y = yb[dest].astype(jnp.float32) * g_flat[order][:, None]
    return jnp.zeros((T, D), jnp.float32).at[tok_sorted].add(y).astype(h.dtype)


def hier_moe(h, wg, bg, we, be, w13, w2):
    T = h.shape[0]
    hf = h.astype(jnp.float32)
    rows = jnp.arange(T)
    pg = jax.nn.softmax(hf @ wg.astype(jnp.float32) + bg.astype(jnp.float32), axis=-1)
    g_sel = jnp.argmax(pg, axis=-1)
    p_grp = pg[rows, g_sel]
    fine = (hf @ we.astype(jnp.float32) + be.astype(jnp.float32)).reshape(
        T, N_GROUPS, EXPERTS_PER_GROUP)[rows, g_sel]
    top_p, top_i = lax.top_k(jax.nn.softmax(fine, axis=-1), TOP_K)
    gates = p_grp[:, None] * top_p / jnp.sum(top_p, axis=-1, keepdims=True)
    expert = g_sel[:, None] * EXPERTS_PER_GROUP + top_i
    return routed_experts(h, expert.reshape(-1), gates.reshape(-1), w13, w2)


def setup_inputs(seed: int = 0) -> dict:
    key = jax.random.key(seed)
    ks = iter(jax.random.split(key, 40))
    _, (n_a, n_b, n_c) = layer_plan()
    D = D_MODEL
    inv = D ** -0.5
    def normal(shape, scale):
        return jax.random.normal(next(ks), shape, jnp.float32) * scale
    def gain(shape):
        return 1.0 + normal(shape, 0.02)
    dt = jnp.exp(jax.random.uniform(next(ks), (n_b, 2, DN_HV), jnp.float32,
                                    minval=math.log(1e-3), maxval=math.log(1e-1)))
    a_init = jax.random.uniform(next(ks), (n_b, 2, DN_HV), jnp.float32, minval=1.0, maxval=16.0)
    return {
        'x': normal((BATCH, SEQ, D), 1.0),
        'c': normal((BATCH, D), 1.0),
        'ctx': normal((BATCH, CTX_LEN, D), 1.0),
        'c_ctx': normal((D,), 1.0),
        'ada_w': normal((DEPTH, D, 6 * D), 0.5 * inv),
        'ada_b': normal((DEPTH, 6 * D), 0.02),
        'norm_mix_w': gain((DEPTH, D)),
        'norm_ffn_w': gain((DEPTH, D)),
        'da_w_qkv': normal((n_a, D, 3 * D), inv),
        'da_lambda': normal((n_a, 4, DA_DIM), 0.1),
        'da_subln_w': gain((n_a, 2 * DA_DIM)),
        'da_w_o': normal((n_a, D, D), inv),
        'dn_w_in': normal((n_b, D, DN_IN), inv),
        'dn_conv_w': normal((n_b, DN_CONV, DN_QKV), DN_CONV ** -0.5),
        'dn_a_log': jnp.log(a_init),
        'dn_dt_bias': dt + jnp.log(-jnp.expm1(-dt)),
        'dn_norm_w': gain((n_b, DN_DV)),
        'dn_w_o': normal((n_b, DN_VD, D), DN_VD ** -0.5),
        'wa_w_qkv': normal((n_c, D, WA_QKV), inv),
        'wa_sinks': normal((n_c, WA_HQ), 0.5),
        'wa_w_o': normal((n_c, D, D), inv),
        'moe_wg': normal((DEPTH, D, N_GROUPS), inv),
        'moe_bg': normal((DEPTH, N_GROUPS), 0.01),
        'moe_we': normal((DEPTH, D, N_EXPERTS), inv),
        'moe_be': normal((DEPTH, N_EXPERTS), 0.01),
        'moe_w13': normal((DEPTH, N_EXPERTS, D, 2 * D_EXPERT), inv),
        'moe_w2': normal((DEPTH, N_EXPERTS, D_EXPERT, D), D_EXPERT ** -0.5),
        'final_norm_w': gain((D,)),
    }


def reference(x, c, ctx, c_ctx, ada_w, ada_b, norm_mix_w, norm_ffn_w,
              da_w_qkv, da_lambda, da_subln_w, da_w_o,
              dn_w_in, dn_conv_w, dn_a_log, dn_dt_bias, dn_norm_w, dn_w_o,
              wa_w_qkv, wa_sinks, wa_w_o,
              moe_wg, moe_bg, moe_we, moe_be, moe_w13, moe_w2, final_norm_w):
    plan, _ = layer_plan()
    B, L, D = x.shape
    Lc = ctx.shape[1]
    cos_a, sin_a = axial_rope_tables(L, DA_DIM)
    cos_w, sin_w = axial_rope_tables(L, WA_DIM)
    silu_c = jax.nn.silu(c.astype(jnp.float32))
    silu_cc = jax.nn.silu(c_ctx.astype(jnp.float32))
    xc = ctx
    for i, (kind, j) in enumerate(plan):
        last = i == DEPTH - 1
        need_ctx = not last
        w_ada = ada_w[i].astype(jnp.float32)
        b_ada = ada_b[i].astype(jnp.float32)
        mod_x = (silu_c @ w_ada + b_ada).astype(x.dtype)[:, None, :]
        mod_c = (silu_cc @ w_ada + b_ada).astype(x.dtype)
        sh1, sc1, g1, sh2, sc2, g2 = jnp.split(mod_x, 6, axis=-1)
        csh1, csc1, cg1, csh2, csc2, cg2 = jnp.split(mod_c, 6, axis=-1)
        hx = rms_norm(x, norm_mix_w[i]) * (1.0 + sc1) + sh1
        hc = rms_norm(xc, norm_mix_w[i]) * (1.0 + csc1) + csh1
        if kind == 0:
            lambda_init = 0.8 - 0.6 * math.exp(-0.3 * i)
            ox, oc = diff_attention(hx, hc, da_w_qkv[j], da_lambda[j], da_subln_w[j], da_w_o[j],
                                    lambda_init, cos_a, sin_a, need_ctx)
        elif kind == 1:
            ox, oc = gated_deltanet(hx, hc, dn_w_in[j], dn_conv_w[j], dn_a_log[j], dn_dt_bias[j],
                                    dn_norm_w[j], dn_w_o[j], need_ctx)
        else:
            ox, oc = window_sink_gqa(hx, hc, wa_w_qkv[j], wa_sinks[j], wa_w_o[j],
                                     cos_w, sin_w, need_ctx)
        x = x + g1 * ox
        if need_ctx:
            xc = xc + cg1 * oc
        hx = (rms_norm(x, norm_ffn_w[i]) * (1.0 + sc2) + sh2).reshape(B * L, D)
        moe = (moe_wg[i], moe_bg[i], moe_we[i], moe_be[i], moe_w13[i], moe_w2[i])
        if need_ctx:
            hc = (rms_norm(xc, norm_ffn_w[i]) * (1.0 + csc2) + csh2).reshape(B * Lc, D)
            y = hier_moe(jnp.concatenate([hx, hc], axis=0), *moe)
            x = x + g2 * y[:B * L].reshape(B, L, D)
            xc = xc + cg2 * y[B * L:].reshape(B, Lc, D)
        else:
            x = x + g2 * hier_moe(hx, *moe).reshape(B, L, D)
    return rms_norm(x, final_norm_w)
```

```python
import math
from contextlib import ExitStack
import numpy as np
import concourse.bass as bass
import concourse.mybir as mybir
from concourse.bass_utils import run_bass_kernel_spmd

F32 = mybir.dt.float32
BF16 = mybir.dt.bfloat16
I32 = mybir.dt.int32
U32 = mybir.dt.uint32
AF = mybir.ActivationFunctionType
ALU = mybir.AluOpType
AX = mybir.AxisListType

D = 2048
KC = 16
L = 2048
LC = 256
NT = L + LC
NTILE = NT // 128
DEPTH = 4
EPS = 1e-6
N_DMA_SEM = 40


class Tk:
    __slots__ = ("name", "w", "r")

    def __init__(self, name):
        self.name = name
        self.w = None
        self.r = []


class Op:
    __slots__ = ("eng", "fn", "deps", "dma", "sem", "val", "cnt", "need", "waits")


class Prog:
    ENGS = ("pe", "act", "dve", "pool", "sp")

    def __init__(self, nc):
        self.nc = nc
        self.ops = []
        self.dma_uses = [0] * N_DMA_SEM
        self.dma_last = [None] * N_DMA_SEM
        self.dma_rr = 0
        self.final_deps = []
        self.since = []
        self.bar = None

    def op(self, eng, fn, r=(), w=(), dma=False):
        o = Op()
        o.eng = eng
        o.fn = fn
        o.dma = dma
        o.need = False
        deps = set()
        for t in r:
            if t.w is not None:
                deps.add(t.w)
        for t in w:
            if t.w is not None:
                deps.add(t.w)
            deps.update(t.r)
        idx = len(self.ops)
        if self.bar is not None:
            deps.add(self.bar)
        self.since.append(idx)
        if dma:
            s = self.dma_rr
            self.dma_rr = (self.dma_rr + 1) % N_DMA_SEM
            if self.dma_last[s] is not None:
                deps.add(self.dma_last[s])
            self.dma_uses[s] += 1
            self.dma_last[s] = idx
            o.sem = s
            o.val = 16 * self.dma_uses[s]
        o.deps = deps
        self.ops.append(o)
        for t in r:
            t.r.append(idx)
        for t in w:
            t.w = idx
            t.r = []
        return idx

    def barrier(self):
        o = Op()
        o.eng = "sp"
        o.fn = lambda e: e.nop()
        o.dma = False
        o.need = False
        o.deps = set(self.since)
        idx = len(self.ops)
        self.ops.append(o)
        self.since = [idx]
        self.bar = idx
        return idx

    def finalize(self):
        ops = self.ops
        for o in ops:
            for d in o.deps:
                if not ops[d].dma:
                    ops[d].need = True
        cnt = {e: 0 for e in self.ENGS}
        for o in ops:
            if o.need:
                cnt[o.eng] += 1
            o.cnt = cnt[o.eng]
        seen = {e: {} for e in self.ENGS}
        for o in ops:
            waits = {}
            for d in o.deps:
                dd = ops[d]
                if dd.dma:
                    key = ("dma", dd.sem)
                    val = dd.val
                else:
                    if dd.eng == "pe" and o.eng == "pe" and not o.dma:
                        continue
                    key = ("eng", dd.eng)
                    val = dd.cnt
                if seen[o.eng].get(key, 0) >= val:
                    continue
                if waits.get(key, 0) < val:
                    waits[key] = val
            for k, v in waits.items():
                seen[o.eng][k] = v
            o.waits = list(waits.items())

    def emit(self, out_ops):
        nc = self.nc
        self.finalize()
        ops = self.ops
        with ExitStack() as es:
            esem = {e: es.enter_context(nc.semaphore("s_" + e)) for e in self.ENGS}
            dsem = [es.enter_context(nc.semaphore("d%d" % i)) for i in range(N_DMA_SEM)]
            block = es.enter_context(nc.Block())

            def run(ename):
                def body(eng):
                    for o in ops:
                        if o.eng != ename:
                            continue
                        for (kind, k), v in o.waits:
                            eng.wait_ge(esem[k] if kind == "eng" else dsem[k], v)
                        ins = o.fn(eng)
                        if o.dma:
                            ins.then_inc(dsem[o.sem], 16)
                        elif o.need:
                            ins.then_inc(esem[ename], 1)
                    if ename == "sp":
                        for d in out_ops:
                            dd = ops[d]
                            eng.wait_ge(dsem[dd.sem], dd.val)
                return body

            block.tensor(run("pe"))
            block.scalar(run("act"))
            block.vector(run("dve"))
            block.gpsimd(run("pool"))
            block.sync(run("sp"))


class Ctx:
    def __init__(self, nc, es):
        self.nc = nc
        self.es = es
        self.p = Prog(nc)
        self.n = 0

    def sb(self, shape, dt, name=None):
        self.n += 1
        t = self.es.enter_context(self.nc.sbuf_tensor(name or ("sb%d" % self.n), list(shape), dt))
        return t, Tk(name or "sb%d" % self.n)

    def arena_init(self, nbytes):
        self.arena_n = nbytes // 2
        self.arena = self.es.enter_context(self.nc.sbuf_tensor("ARENA", [128, self.arena_n], BF16))
        self.arena_off = 0

    def arena_reset(self):
        self.p.barrier()
        self.arena_off = 0

    def asb(self, shape, dt, name=None):
        self.n += 1
        esz = 4 if dt in (F32, I32, U32) else 2
        free = 1
        for d in shape[1:]:
            free *= d
        nel = (free * esz + 1) // 2
        nel = (nel + 31) // 32 * 32
        assert self.arena_off + nel <= self.arena_n, "arena overflow %s" % name
        v = self.arena[0:shape[0], self.arena_off:self.arena_off + free * esz // 2]
        self.arena_off += nel
        if dt != BF16:
            v = v.bitcast(dt)
        if len(shape) == 3:
            v = v.rearrange("p (a b) -> p a b", a=shape[1])
        return v, Tk(name or "a%d" % self.n)

    def ps(self, shape, dt=F32, name=None):
        self.n += 1
        t = self.es.enter_context(self.nc.psum_tensor(name or ("ps%d" % self.n), list(shape), dt))
        return t, Tk(name or "ps%d" % self.n)

    def dram(self, name, shape, dt, kind="Internal"):
        t = self.nc.dram_tensor(name, list(shape), dt, kind=kind)
        return t.ap(), Tk(name)


def _rope_tables():
    GRID_W = 64
    dim = 64
    rows = L // GRID_W
    row, col = np.meshgrid(np.arange(rows), np.arange(GRID_W), indexing="ij")
    row = row.reshape(-1).astype(np.float32)
    col = col.reshape(-1).astype(np.float32)
    half = dim // 2
    inv_freq = (1.0 / (10000.0 ** (np.arange(0, half, 2, dtype=np.float32) / half))).astype(np.float32)

    def table(pos):
        ang = pos[:, None] * inv_freq[None, :]
        ang = np.concatenate([ang, ang], axis=-1)
        return np.cos(ang), np.sin(ang)

    cr, sr = table(row)
    cc, sc = table(col)
    cos = np.concatenate([cr, cc], -1).astype(np.float32)
    sin = np.concatenate([sr, sc], -1).astype(np.float32)
    cosT = np.concatenate([cos.T, cos.T], 0)
    sinT = np.concatenate([sin.T, sin.T], 0)
    R = np.zeros((64, 64), np.float32)
    for base in (0, 32):
        for i in range(16):
            R[base + i, base + 16 + i] = -1.0
            R[base + 16 + i, base + i] = 1.0
    R2 = np.zeros((128, 128), np.float32)
    R2[:64, :64] = R
    R2[64:, 64:] = R
    return cosT, sinT, np.ascontiguousarray(R2.T)


def _consts():
    cosT, sinT, RT = _rope_tables()
    c = {
        "k_cos": cosT, "k_sin": sinT, "k_rt": RT,
        "k_ident": np.eye(128, dtype=np.float32),
        "k_ones": np.ones((128, 128), np.float32),
    }
    wm = np.zeros((128, 6, 512), np.float32)
    kk = np.arange(128)[:, None]
    qq = np.arange(512)[None, :]
    for ri, r in enumerate(range(-1, 5)):
        wm[:, ri, :] = np.where(np.abs(r * 128 + kk - qq) <= 128, 1.0, 0.0)
    c["k_wmask"] = wm.reshape(128, 6 * 512)
    ii = np.arange(128)
    sc = (ii[:, None] // 64) == (ii[None, :] // 64)
    dm = np.zeros((128, 8, 128), np.float32)
    dm[:, 0] = sc & (ii[:, None] <= ii[None, :])
    dm[:, 1] = sc & (ii[:, None] >= ii[None, :])
    dm[:, 2] = sc & (ii[:, None] > ii[None, :])
    dm[:, 3] = sc & (ii[:, None] < ii[None, :])
    dm[:, 4] = sc
    dm[:, 5] = (ii[:, None] < 64) / 64.0 + 0 * ii[None, :]
    dm[:, 6] = (ii[:, None] >= 64) / 64.0 + 0 * ii[None, :]
    c["k_dnmask"] = dm.reshape(128, 8 * 128)
    pc = np.zeros((128, 4), np.float32)
    pc[:, 0] = ((ii // 32) % 2 == 0)
    pc[:, 1] = ((ii // 32) % 2 == 1)
    c["k_dncol"] = pc
    return c


class Builder:
    def __init__(self, nc, es, n_layers=1, dbg=None, do_mixer=True, do_moe=True, wl=1, layer_abs=0, final=False):
        self.wl = wl
        self.layer_abs = layer_abs
        self.final = final
        self.nc = nc
        self.c = Ctx(nc, es)
        self.p = self.c.p
        self.n_layers = n_layers
        self.dbg = dbg or []
        self.do_mixer = do_mixer
        self.do_moe = do_moe
        self.out_ops = []

    def op(self, eng, fn, r=(), w=(), dma=False):
        psk = getattr(self, "_psk", None)
        if psk:
            r2 = [t for t in r if id(t) not in psk]
            w = list(w) + [t for t in r if id(t) in psk]
            r = r2
        return self.p.op(eng, fn, r=r, w=w, dma=dma)

    def dma(self, eng, out, in_, r=(), w=()):
        return self.p.op(eng, lambda e: e.dma_start(out=out, in_=in_), r=r, w=w, dma=True)

    def declare_io(self):
        c = self.c
        kind = [0, 1, 2, 0][self.layer_abs]
        used = {"x_in", "cin", "ada_w", "ada_b", "norm_mix_w", "norm_ffn_w", "final_norm_w",
                "k_cos", "k_sin", "k_rt", "k_ident", "k_ones", "k_wmask", "k_dnmask", "k_dncol"}
        if self.do_mixer and kind == 0:
            used |= {"da_w_qkv", "da_lambda", "da_subln_w", "da_w_o"}
        if self.do_mixer and kind == 2:
            used |= {"wa_w_qkv", "wa_sinks", "wa_w_o"}
        if self.do_mixer and kind == 1:
            used |= {"dn_w_in", "dn_conv_w", "dn_a_log", "dn_dt_bias", "dn_norm_w", "dn_w_o"}
        if self.do_moe:
            used |= {"moe_wg", "moe_bg", "moe_we", "moe_be", "moe_w13", "moe_w2"}
        self.used = used
        self.in_shapes = {}

        def ein(n, s):
            if n not in used:
                s = [1, 1]
            self.in_shapes[n] = list(s)
            return c.dram(n, s, F32, "ExternalInput")
        self.x_in, self.x_in_k = ein("x_in", [NT, D])
        self.cin, self.cin_k = ein("cin", [128, KC, 2])
        self.ada_w, self.ada_w_k = ein("ada_w", [self.wl, D, 6 * D])
        self.ada_b, _ = ein("ada_b", [1, 6 * D])
        self.norm_mix_w, _ = ein("norm_mix_w", [1, D])
        self.norm_ffn_w, _ = ein("norm_ffn_w", [1, D])
        self.da_w_qkv, _ = ein("da_w_qkv", [1, D, 3 * D])
        self.da_lambda, _ = ein("da_lambda", [1, 4, 64])
        self.da_subln_w, _ = ein("da_subln_w", [1, 128])
        self.da_w_o, _ = ein("da_w_o", [1, D, D])
        self.dn_w_in, _ = ein("dn_w_in", [1, D, 12416])
        self.dn_conv_w, _ = ein("dn_conv_w", [1, 5, 8192])
        self.dn_a_log, _ = ein("dn_a_log", [1, 2, 32])
        self.dn_dt_bias, _ = ein("dn_dt_bias", [1, 2, 32])
        self.dn_norm_w, _ = ein("dn_norm_w", [1, 128])
        self.dn_w_o, _ = ein("dn_w_o", [1, 4096, D])
        self.wa_w_qkv, _ = ein("wa_w_qkv", [1, D, 2560])
        self.wa_sinks, _ = ein("wa_sinks", [1, 32])
        self.wa_w_o, _ = ein("wa_w_o", [1, D, D])
        self.moe_wg, _ = ein("moe_wg", [1, D, 4])
        self.moe_bg, _ = ein("moe_bg", [1, 4])
        self.moe_we, _ = ein("moe_we", [1, D, 32])
        self.moe_be, _ = ein("moe_be", [1, 32])
        self.moe_w13, _ = ein("moe_w13", [self.wl, 32, D, 1536])
        self.moe_w2, _ = ein("moe_w2", [self.wl, 32, 768, D])
        self.final_norm_w, _ = ein("final_norm_w", [1, D])
        self.kc = {}
        for name, arr in _consts().items():
            self.kc[name] = ein(name, list(arr.shape))[0]
        if self.final:
            self.y_out, self.y_out_k = c.dram("y_out", [L, D], F32, "ExternalOutput")
        else:
            self.x_out, self.x_out_k = c.dram("x_out", [NT, D], F32, "ExternalOutput")
        self.XR, _ = c.dram("XR", [NT, D], F32)
        self.XR_k = [Tk("xr%d" % t) for t in range(NTILE)]
        self.MOD, self.MOD_k = c.dram("MODS", [DEPTH, 2, 6 * D], F32)
        import os as _os
        dk = "ExternalOutput" if _os.environ.get("MK_DUMP") else "Internal"
        self.QT, self.QT_k = c.dram("QT", [16, 128, NT], BF16, dk)
        self.KT, self.KT_k = c.dram("KT", [16, 128, NT], BF16, dk)
        self.VV, self.VV_k = c.dram("VV", [NT, D], BF16, dk)
        if kind == 1 and self.do_mixer:
            self.DNK, self.DNK_k = c.dram("DNK", [NT, 2048], BF16, dk)
            self.DNV, self.DNV_k = c.dram("DNV", [NT, 4096], BF16, dk)
            self.DNZ, self.DNZ_k = c.dram("DNZ", [NT, 4096], BF16, dk)
            self.ODN, self.ODN_k = c.dram("ODN", [2, NT, 4096], F32, dk)
        self.dbg_out = {}
        for name, shape in self.dbg:
            self.dbg_out[name] = c.dram(name, shape, F32, "ExternalOutput")

    def alloc(self):
        c = self.c
        self.BIGA, _ = c.sb([128, KC, NT], BF16, "BIGA")
        self.BIGA_k = [Tk("biga%d" % t) for t in range(NTILE)]
        self.PS = [c.ps([128, 512], F32, "PS%d" % i) for i in range(8)]
        self._psk = {id(k) for (_, k) in self.PS}
        self.ident_f, self.ident_f_k = c.sb([128, 128], F32, "ident_f")
        self.ident_b, self.ident_b_k = c.sb([128, 128], BF16, "ident_b")
        self.ones_f, self.ones_f_k = c.sb([128, 128], F32, "ones_f")
        self.ones_b, self.ones_b_k = c.sb([128, 128], BF16, "ones_b")
        self.rt_b, self.rt_b_k = c.sb([128, 128], BF16, "rt_b")
        self.WT = [c.sb([128, KC, 512], BF16, "WT%d" % i) for i in range(2)]
        self.wt_i = 0
        self.WB = [c.sb([128, D], F32, "WB%d" % i) for i in range(2)]
        self.SB = [c.sb([128, D], F32, "SB%d" % i) for i in range(2)]
        self.GB = self.WB
        self.XT = [c.sb([128, D], F32, "XT%d" % i) for i in range(2)]
        self.HB = [c.sb([128, D], BF16, "HB%d" % i) for i in range(2)]
        self.small = [c.sb([128, 8], F32, "small%d" % i) for i in range(2)]
        self.misc = c.sb([128, 64], F32, 'misc')
        self.GATE = c.sb([128, NTILE, 32], F32, 'GATE')
        c.arena_init(42 * 1024)

    def load_consts(self):
        for name, dst_f, dst_fk, dst_b, dst_bk in (
            ("k_ident", self.ident_f, self.ident_f_k, self.ident_b, self.ident_b_k),
            ("k_ones", self.ones_f, self.ones_f_k, self.ones_b, self.ones_b_k),
        ):
            self.dma("sp", dst_f[:], self.kc[name][:, :], w=[dst_fk])
            self.op("dve", lambda e, a=dst_b, b=dst_f: e.tensor_copy(out=a[:], in_=b[:]), r=[dst_fk], w=[dst_bk])
        self.dma("pool", self.rt_b[:], self.kc["k_rt"][:, :], w=[self.rt_b_k])
        for t in range(NTILE):
            self.dma("sp", self.XR[t * 128:(t + 1) * 128, :], self.x_in[t * 128:(t + 1) * 128, :],
                     w=[self.XR_k[t]])

    def phase_mod(self):
        c = self.c
        c.arena_reset()
        cs, cs_k = c.asb([128, KC, 2], F32, "cs")
        sig, sig_k = c.asb([128, KC, 2], F32, "sig")
        self.dma("sp", cs[:], self.cin[:, :, :], w=[cs_k])
        self.op("act", lambda e: e.activation(out=sig[:], in_=cs[:], func=AF.Sigmoid), r=[cs_k], w=[sig_k])
        self.op("dve", lambda e: e.tensor_tensor(out=cs[:], in0=cs[:], in1=sig[:], op=ALU.mult), r=[sig_k, cs_k], w=[cs_k])
        AW = [c.asb([128, KC, 256], F32, "AW%d" % i) for i in range(2)]
        bias, bias_k = self.XT[0][0][0:2, :], self.XT[0][1]
        nrm, nrm_k = self.XT[1][0][0:2, :], self.XT[1][1]
        res, res_k = self.SB[0][0][0:2, :], self.SB[0][1]
        ps, ps_k = self.PS[0]
        i = 0
        for l in range(self.n_layers):
            for v in range(6):
                self.dma("sp", bias[:], self.ada_b[l:l + 1, v * D:(v + 1) * D].partition_broadcast(2), w=[bias_k])
                if v in (1, 4):
                    nw = self.norm_mix_w if v == 1 else self.norm_ffn_w
                    self.dma("sp", nrm[:], nw[l:l + 1, :].partition_broadcast(2), w=[nrm_k])
                for b in range(8):
                    aw, aw_k = AW[i % 2]
                    i += 1
                    col = v * D + b * 256
                    self.dma("sp", aw[:], self.ada_w[l, :, col:col + 256].rearrange("(kc p) n -> p kc n", p=128), w=[aw_k])
                    for kc in range(KC):
                        self.op("pe", lambda e, aw=aw, kc=kc: e.matmul(ps[0:2, 0:256], lhsT=cs[:, kc, :], rhs=aw[:, kc, :],
                                                                      start=(kc == 0), stop=(kc == KC - 1)),
                                r=[cs_k, aw_k], w=[ps_k])
                    self.op("dve", lambda e, b=b: e.tensor_tensor(out=res[:, b * 256:(b + 1) * 256], in0=ps[0:2, 0:256],
                                                                 in1=bias[:, b * 256:(b + 1) * 256], op=ALU.add),
                            r=[ps_k, bias_k], w=[res_k])
                if v in (1, 4):
                    self.op("dve", lambda e: e.scalar_tensor_tensor(out=res[:], in0=res[:], scalar=1.0, in1=nrm[:],
                                                                   op0=ALU.add, op1=ALU.mult),
                            r=[res_k, nrm_k], w=[res_k])
                self.dma("sp", self.MOD[l, :, v * D:(v + 1) * D], res[:], r=[res_k], w=[self.MOD_k])

    def load_mod_tiles(self, l, sub):
        for ty in range(2):
            for j, (buf, bk) in enumerate((self.SB[ty], self.WB[ty])):
                v = sub * 3 + j
                self.dma("sp", buf[:], self.MOD[l, ty:ty + 1, v * D:(v + 1) * D].partition_broadcast(128),
                         r=[self.MOD_k], w=[bk])

    def load_gate_tiles(self, l, sub):
        for ty in range(2):
            buf, bk = self.GB[ty]
            v = sub * 3 + 2
            self.dma("sp", buf[:], self.MOD[l, ty:ty + 1, v * D:(v + 1) * D].partition_broadcast(128),
                     r=[self.MOD_k], w=[bk])

    def phase_norm(self, hrow_dram=None, hrow_k=None, router=None):
        ps_t, ps_tk = self.PS[1]
        pst = ps_t[:].bitcast(BF16)
        for t in range(NTILE):
            ty = 1 if t < 2 else 0
            xt, xt_k = self.XT[t % 2]
            hb, hb_k = self.HB[t % 2]
            sm, sm_k = self.small[t % 2]
            self.dma("sp", xt[:], self.XR[t * 128:(t + 1) * 128, :], r=[self.XR_k[t]], w=[xt_k])
            self.op("act", lambda e, xt=xt, sm=sm, hb=hb: e.activation(out=hb[:], in_=xt[:], func=AF.Square, accum_out=sm[:, 0:1]),
                    r=[xt_k], w=[hb_k, sm_k])
            self.op("act", lambda e, sm=sm: e.activation(out=sm[:, 1:2], in_=sm[:, 0:1], func=AF.Sqrt, scale=1.0 / D, bias=EPS),
                    r=[sm_k], w=[sm_k])
            self.op("dve", lambda e, sm=sm: e.reciprocal(out=sm[:, 2:3], in_=sm[:, 1:2]), r=[sm_k], w=[sm_k])
            self.op("dve", lambda e, xt=xt, sm=sm, ty=ty: e.scalar_tensor_tensor(
                out=xt[:], in0=xt[:], scalar=sm[:, 2:3], in1=self.WB[ty][0][:], op0=ALU.mult, op1=ALU.mult),
                r=[xt_k, sm_k, self.WB[ty][1]], w=[xt_k])
            if router is None:
                self.op("pool", lambda e, xt=xt, hb=hb, ty=ty: e.tensor_tensor(out=hb[:], in0=xt[:], in1=self.SB[ty][0][:], op=ALU.add),
                        r=[xt_k, self.SB[ty][1]], w=[hb_k])
            else:
                self.op("pool", lambda e, xt=xt, ty=ty: e.tensor_tensor(out=xt[:], in0=xt[:], in1=self.SB[ty][0][:], op=ALU.add),
                        r=[xt_k, self.SB[ty][1]], w=[xt_k])
                self.op("act", lambda e, xt=xt, hb=hb: e.copy(out=hb[:], in_=xt[:]), r=[xt_k], w=[hb_k])
                router(t, xt, xt_k)
            if hrow_dram is not None:
                self.dma("sp", hrow_dram[t * 128:(t + 1) * 128, :], hb[:], r=[hb_k], w=[hrow_k[t]])
            for g in range(2):
                for j in range(8):
                    kc = g * 8 + j
                    self.op("pe", lambda e, hb=hb, kc=kc, j=j: e.transpose(out=pst[:, j * 128:(j + 1) * 128],
                                                                          in_=hb[:, kc * 128:(kc + 1) * 128], identity=self.ident_b[:]),
                            r=[hb_k, self.ident_b_k], w=[ps_tk])
                eng = "act" if g == 0 else "dve"
                if eng == "act":
                    self.op("act", lambda e, g=g, t=t: e.copy(out=self.BIGA[:, g * 8:(g + 1) * 8, t * 128:(t + 1) * 128],
                                                             in_=pst.rearrange("p (a b) -> p a b", a=8)),
                            r=[ps_tk], w=[self.BIGA_k[t]])
                else:
                    self.op("dve", lambda e, g=g, t=t: e.tensor_copy(out=self.BIGA[:, g * 8:(g + 1) * 8, t * 128:(t + 1) * 128],
                                                                    in_=pst.rearrange("p (a b) -> p a b", a=8)),
                            r=[ps_tk], w=[self.BIGA_k[t]])

    def load_w(self, w_ap_rows_cols, ncols, nk=KC):
        wt, wt_k = self.WT[self.wt_i % 2]
        self.wt_i += 1
        self.dma("pool", wt[:, 0:nk, 0:ncols], w_ap_rows_cols.rearrange("(kc p) n -> p kc n", p=128), w=[wt_k])
        return wt, wt_k

    TOKBLK = [(0, 256), (256, 512), (768, 512), (1280, 512), (1792, 512)]

    def linear_fm(self, w2d, col0, ncols, post):
        psi = 0
        for cb in range(0, ncols, 512):
            nb = min(512, ncols - cb)
            wt, wt_k = self.load_w(w2d[:, col0 + cb:col0 + cb + nb], nb)
            for jj in range(nb // 128):
                j = (cb // 128) + jj
                for bi, (t0, n) in enumerate(self.TOKBLK):
                    ps, ps_k = self.PS[2 + (psi % 2)]
                    psi += 1
                    rk = [self.BIGA_k[t] for t in range(t0 // 128, (t0 + n) // 128)] + [wt_k]
                    for kc in range(KC):
                        self.op("pe", lambda e, ps=ps, wt=wt, kc=kc, jj=jj, t0=t0, n=n: e.matmul(
                            ps[:, 0:n], lhsT=wt[:, kc, jj * 128:(jj + 1) * 128], rhs=self.BIGA[:, kc, t0:t0 + n],
                            start=(kc == 0), stop=(kc == KC - 1)), r=rk, w=[ps_k])
                    post(j, bi, t0, n, ps, ps_k)

    def linear_tm(self, w2d, col0, ncols, post, src=None, src_k=None, nk=KC):
        src = self.BIGA if src is None else src
        src_k = self.BIGA_k if src_k is None else src_k
        psi = 0
        for cb in range(0, ncols, 512):
            nb = min(512, ncols - cb)
            wt, wt_k = self.load_w(w2d[:, col0 + cb:col0 + cb + nb], nb, nk)
            for t in range(NTILE):
                ps, ps_k = self.PS[2 + (psi % 2)]
                psi += 1
                for kc in range(nk):
                    self.op("pe", lambda e, ps=ps, wt=wt, kc=kc, t=t, nb=nb: e.matmul(
                        ps[:, 0:nb], lhsT=src[:, kc, t * 128:(t + 1) * 128], rhs=wt[:, kc, 0:nb],
                        start=(kc == 0), stop=(kc == nk - 1)), r=[src_k[t], wt_k], w=[ps_k])
                post(cb, nb, t, ps, ps_k)

    def phase_oproj_residual(self, w2d, l):
        c = self.c
        self.load_gate_tiles(l, 0)
        c.arena_reset()
        self.RX = [c.asb([128, 512], F32, "RX%d" % i) for i in range(3)]
        self.rxi = 0

        def post(cb, nb, t, ps, ps_k):
            ty = 1 if t < 2 else 0
            rx, rx_k = self.RX[self.rxi % 3]
            self.rxi += 1
            self.dma("sp", rx[:], self.XR[t * 128:(t + 1) * 128, cb:cb + nb], r=[self.XR_k[t]], w=[rx_k])
            self.op("dve", lambda e, rx=rx, ps=ps, ty=ty, cb=cb, nb=nb: e.tensor_tensor(
                out=ps[:, 0:nb], in0=ps[:, 0:nb], in1=self.GB[ty][0][:, cb:cb + nb], op=ALU.mult),
                r=[ps_k, self.GB[ty][1]], w=[ps_k])
            self.op("dve", lambda e, rx=rx, ps=ps, nb=nb: e.tensor_tensor(out=rx[:, 0:nb], in0=ps[:, 0:nb], in1=rx[:, 0:nb], op=ALU.add),
                    r=[ps_k, rx_k], w=[rx_k])
            self.dma("sp", self.XR[t * 128:(t + 1) * 128, cb:cb + nb], rx[:, 0:nb], r=[rx_k], w=[self.XR_k[t]])

        self.linear_tm(w2d, 0, D, post)

    def load_rope_tables(self):
        c = self.c
        self.cosT, self.cosT_k = c.asb([128, L], BF16, "cosT")
        self.sinT, self.sinT_k = c.asb([128, L], BF16, "sinT")
        self.dma("pool", self.cosT[:], self.kc["k_cos"][:, :], w=[self.cosT_k])
        self.dma("pool", self.sinT[:], self.kc["k_sin"][:, :], w=[self.sinT_k])
        self.RP = [(c.asb([128, 512], BF16, "RPa%d" % i), c.asb([128, 512], BF16, "RPb%d" % i)) for i in range(2)]
        self.rpi = 0

    def rope_store(self, ps, ps_k, t0, n, dst_dram, dst_k):
        c = self.c
        (qa, qa_k), (qb, qb_k) = self.RP[self.rpi % 2]
        self.rpi += 1
        self.op("act", lambda e: e.copy(out=qa[:, 0:n], in_=ps[:, 0:n]), r=[ps_k], w=[qa_k])
        if t0 >= LC:
            l0 = t0 - LC
            pr, pr_k = self.PS[4]
            self.op("pe", lambda e: e.matmul(pr[:, 0:n], lhsT=self.rt_b[:], rhs=qa[:, 0:n], start=True, stop=True),
                    r=[qa_k, self.rt_b_k], w=[pr_k])
            self.op("dve", lambda e: e.tensor_tensor(out=qb[:, 0:n], in0=pr[:, 0:n], in1=self.sinT[:, l0:l0 + n], op=ALU.mult),
                    r=[pr_k, self.sinT_k], w=[qb_k])
            self.op("pool", lambda e: e.tensor_tensor(out=qa[:, 0:n], in0=qa[:, 0:n], in1=self.cosT[:, l0:l0 + n], op=ALU.mult),
                    r=[qa_k, self.cosT_k], w=[qa_k])
            self.op("pool", lambda e: e.tensor_tensor(out=qa[:, 0:n], in0=qa[:, 0:n], in1=qb[:, 0:n], op=ALU.add),
                    r=[qa_k, qb_k], w=[qa_k])
        self.dma("sp", dst_dram[:, t0:t0 + n], qa[:, 0:n], r=[qa_k], w=[dst_k])

    def mixer_diff(self, l, j):
        c = self.c
        lam_init = 0.8 - 0.6 * math.exp(-0.3 * l)
        wqkv = self.da_w_qkv[j]
        c.arena_reset()
        self.load_rope_tables()
        self.VS = [c.asb([128, 512], BF16, "VS%d" % i) for i in range(2)]
        self.vsi = 0
        self.lamt = c.asb([1, 4, 64], F32, "lamt")
        self.lamp = c.asb([1, 8], F32, "lamp")
        self.lamc = (self.misc[0][:, 0:2], Tk("lamc"))
        self.subw = (self.misc[0][:, 2:4], Tk("subw"))
        self.linear_fm(wqkv, 0, D, lambda jj, bi, t0, n, ps, ps_k: self.rope_store(ps, ps_k, t0, n, self.QT[jj], self.QT_k))
        self.linear_fm(wqkv, D, D, lambda jj, bi, t0, n, ps, ps_k: self.rope_store(ps, ps_k, t0, n, self.KT[jj], self.KT_k))

        def vpost(cb, nb, t, ps, ps_k):
            vs, vs_k = self.VS[self.vsi % 2]
            self.vsi += 1
            self.op("act", lambda e: e.copy(out=vs[:, 0:nb], in_=ps[:, 0:nb]), r=[ps_k], w=[vs_k])
            self.dma("sp", self.VV[t * 128:(t + 1) * 128, cb:cb + nb], vs[:, 0:nb], r=[vs_k], w=[self.VV_k])

        self.linear_tm(wqkv, 2 * D, D, vpost)
        lamt, lamt_k = self.lamt
        lamp, lamp_k = self.lamp
        lamc, lamc_k = self.lamc
        subw, subw_k = self.subw
        self.dma("sp", lamt[:], self.da_lambda[j:j + 1, :, :], w=[lamt_k])
        self.op("dve", lambda e: e.tensor_tensor(out=lamt[:, 0, :], in0=lamt[:, 0, :], in1=lamt[:, 1, :], op=ALU.mult), r=[lamt_k], w=[lamt_k])
        self.op("dve", lambda e: e.tensor_tensor(out=lamt[:, 2, :], in0=lamt[:, 2, :], in1=lamt[:, 3, :], op=ALU.mult), r=[lamt_k], w=[lamt_k])
        self.op("dve", lambda e: e.reduce_sum(out=lamp[:, 0:1], in_=lamt[:, 0, :], axis=AX.X), r=[lamt_k], w=[lamp_k])
        self.op("dve", lambda e: e.reduce_sum(out=lamp[:, 1:2], in_=lamt[:, 2, :], axis=AX.X), r=[lamt_k], w=[lamp_k])
        self.op("act", lambda e: e.activation(out=lamp[:, 2:4], in_=lamp[:, 0:2], func=AF.Exp), r=[lamp_k], w=[lamp_k])
        self.op("dve", lambda e: e.scalar_tensor_tensor(out=lamp[:, 4:5], in0=lamp[:, 3:4], scalar=-lam_init, in1=lamp[:, 2:3],
                                                       op0=ALU.add, op1=ALU.subtract), r=[lamp_k], w=[lamp_k])
        psl, psl_k = self.PS[4]
        self.op("pe", lambda e: e.matmul(psl[:, 0:1], lhsT=self.ones_f[0:1, :], rhs=lamp[0:1, 4:5], start=True, stop=True),
                r=[self.ones_f_k, lamp_k], w=[psl_k])
        self.op("dve", lambda e: e.tensor_copy(out=lamc[:, 0:1], in_=psl[:, 0:1]), r=[psl_k], w=[lamc_k])
        self.dma("sp", subw[:, 0:1], self.da_subln_w[j:j + 1, :].rearrange("o d -> d o"), w=[subw_k])
        self.op("dve", lambda e: e.tensor_scalar(out=subw[:, 1:2], in0=subw[:, 0:1], scalar1=(1.0 - lam_init), scalar2=None, op0=ALU.mult),
                r=[subw_k], w=[subw_k])
        self.attention_core(16, diff=True)

    def attention_core(self, nheads, diff):
        c = self.c
        c.arena_reset()
        self.AQ = [c.asb([128, NT], BF16, "AQ%d" % i) for i in range(2)]
        self.AK = [c.asb([128, NT], BF16, "AK%d" % i) for i in range(2)]
        self.AV = [c.asb([128, NTILE, 128], BF16, "AV%d" % i) for i in range(2)]
        self.ET = [c.asb([128, 512], BF16, "ET%d" % i) for i in range(4)]
        self.eti = 0
        self.CMB = [c.asb([128, 512], F32, "CMB%d" % i) for i in range(4)]
        self.SQB = c.asb([128, 512], BF16, "SQB")
        scale = 64 ** -0.5
        lamc, lamc_k = self.lamc
        subw, subw_k = self.subw
        for h in range(nheads):
            aq, aq_k = self.AQ[h % 2]
            ak, ak_k = self.AK[h % 2]
            av, av_k = self.AV[h % 2]
            self.dma("sp", aq[:], self.QT[h], r=[self.QT_k], w=[aq_k])
            self.dma("sp", ak[:], self.KT[h], r=[self.KT_k], w=[ak_k])
            self.dma("sp", av[:], self.VV[:, h * 128:(h + 1) * 128].rearrange("(t p) d -> p t d", p=128), r=[self.VV_k], w=[av_k])
            for bi, (t0, n) in enumerate(self.TOKBLK):
                nkt = 2 if bi == 0 else NTILE
                acc = []
                for comp in range(2):
                    po, po_k = self.PS[4 + comp]
                    pz, pz_k = self.PS[6 + comp]
                    p0 = comp * 64
                    for kt in range(nkt):
                        pss, pss_k = self.PS[kt % 2]
                        et, et_k = self.ET[self.eti % 4]
                        self.eti += 1
                        self.op("pe", lambda e, pss=pss, ak=ak, aq=aq, kt=kt, p0=p0, t0=t0, n=n: e.matmul(
                            pss[:, 0:n], lhsT=ak[p0:p0 + 64, kt * 128:(kt + 1) * 128], rhs=aq[p0:p0 + 64, t0:t0 + n],
                            start=True, stop=True), r=[ak_k, aq_k], w=[pss_k])
                        self.op("act", lambda e, pss=pss, et=et, n=n: e.activation(out=et[:, 0:n], in_=pss[:, 0:n], func=AF.Exp, scale=scale),
                                r=[pss_k], w=[et_k])
                        self.op("pe", lambda e, po=po, av=av, et=et, kt=kt, n=n, nkt=nkt: e.matmul(
                            po[:, 0:n], lhsT=av[:, kt, :], rhs=et[:, 0:n], start=(kt == 0), stop=(kt == nkt - 1)),
                            r=[av_k, et_k], w=[po_k])
                        self.op("pe", lambda e, pz=pz, et=et, kt=kt, n=n, nkt=nkt: e.matmul(
                            pz[:, 0:n], lhsT=self.ones_b[:], rhs=et[:, 0:n], start=(kt == 0), stop=(kt == nkt - 1)),
                            r=[self.ones_b_k, et_k], w=[pz_k])
                    acc.append((po, po_k, pz, pz_k))
                (r0, r0_k), (r1, r1_k), (o0, o0_k), (o1, o1_k) = self.CMB
                (po0, po0_k, pz0, pz0_k), (po1, po1_k, pz1, pz1_k) = acc
                self.op("dve", lambda e, n=n: e.reciprocal(out=r0[:, 0:n], in_=pz0[:, 0:n]), r=[pz0_k], w=[r0_k])
                self.op("dve", lambda e, n=n: e.reciprocal(out=r1[:, 0:n], in_=pz1[:, 0:n]), r=[pz1_k], w=[r1_k])
                self.op("dve", lambda e, n=n: e.tensor_tensor(out=o0[:, 0:n], in0=po0[:, 0:n], in1=r0[:, 0:n], op=ALU.mult), r=[po0_k, r0_k], w=[o0_k])
                self.op("dve", lambda e, n=n: e.tensor_tensor(out=o1[:, 0:n], in0=po1[:, 0:n], in1=r1[:, 0:n], op=ALU.mult), r=[po1_k, r1_k], w=[o1_k])
                self.op("dve", lambda e, n=n: e.scalar_tensor_tensor(out=o0[:, 0:n], in0=o1[:, 0:n], scalar=lamc[:, 0:1], in1=o0[:, 0:n],
                                                                    op0=ALU.mult, op1=ALU.add), r=[o0_k, o1_k, lamc_k], w=[o0_k])
                sqb, sqb_k = self.SQB
                self.op("act", lambda e, n=n: e.activation(out=sqb[:, 0:n], in_=o0[:, 0:n], func=AF.Square), r=[o0_k], w=[sqb_k])
                pss, pss_k = self.PS[0]
                self.op("pe", lambda e, n=n, pss=pss: e.matmul(pss[:, 0:n], lhsT=self.ones_b[:], rhs=sqb[:, 0:n], start=True, stop=True),
                        r=[sqb_k, self.ones_b_k], w=[pss_k])
                self.op("act", lambda e, n=n, pss=pss: e.activation(out=r0[:, 0:n], in_=pss[:, 0:n], func=AF.Sqrt, scale=1.0 / 128, bias=EPS),
                        r=[pss_k], w=[r0_k])
                self.op("dve", lambda e, n=n: e.reciprocal(out=r0[:, 0:n], in_=r0[:, 0:n]), r=[r0_k], w=[r0_k])
                wk = [self.BIGA_k[t] for t in range(t0 // 128, (t0 + n) // 128)]
                self.op("dve", lambda e, n=n, t0=t0, h=h: e.scalar_tensor_tensor(
                    out=self.BIGA[:, h, t0:t0 + n], in0=o0[:, 0:n], scalar=subw[:, 1:2], in1=r0[:, 0:n], op0=ALU.mult, op1=ALU.mult),
                    r=[o0_k, r0_k, subw_k], w=wk)

    def phase_final(self):
        fw, fw_k = self.WB[0]
        self.dma("sp", fw[:], self.final_norm_w[0:1, :].partition_broadcast(128), w=[fw_k])
        for t in range(2, NTILE):
            xt, xt_k = self.XT[t % 2]
            sm, sm_k = self.small[t % 2]
            self.dma("sp", xt[:], self.XR[t * 128:(t + 1) * 128, :], r=[self.XR_k[t]], w=[xt_k])
            hb, hb_k = self.HB[t % 2]
            self.op("act", lambda e, xt=xt, sm=sm, hb=hb: e.activation(out=hb[:], in_=xt[:], func=AF.Square, accum_out=sm[:, 0:1]),
                    r=[xt_k], w=[hb_k, sm_k])
            self.op("act", lambda e, sm=sm: e.activation(out=sm[:, 1:2], in_=sm[:, 0:1], func=AF.Sqrt, scale=1.0 / D, bias=EPS),
                    r=[sm_k], w=[sm_k])
            self.op("dve", lambda e, sm=sm: e.reciprocal(out=sm[:, 2:3], in_=sm[:, 1:2]), r=[sm_k], w=[sm_k])
            self.op("dve", lambda e, xt=xt, sm=sm: e.scalar_tensor_tensor(
                out=xt[:], in0=xt[:], scalar=sm[:, 2:3], in1=fw[:], op0=ALU.mult, op1=ALU.mult),
                r=[xt_k, sm_k, fw_k], w=[xt_k])
            self.out_ops.append(self.dma("sp", self.y_out[(t - 2) * 128:(t - 1) * 128, :], xt[:], r=[xt_k], w=[self.y_out_k]))

    def build(self):
        self.declare_io()
        self.alloc()
        self.load_consts()
        self.phase_mod()
        kind = [0, 1, 2, 0][self.layer_abs]
        if self.do_mixer and kind == 0:
            self.load_mod_tiles(0, 0)
            self.phase_norm()
            self.mixer_diff(self.layer_abs, 0)
            self.phase_oproj_residual(self.da_w_o[0], 0)
        if self.do_mixer and kind == 1:
            self.load_mod_tiles(0, 0)
            self.phase_norm()
            self.mixer_dn()
        if self.do_mixer and kind == 2:
            self.load_mod_tiles(0, 0)
            self.phase_norm()
            self.mixer_win()
            self.phase_oproj_residual(self.wa_w_o[0], 0)
        if self.do_moe:
            self.load_mod_tiles(0, 1)
            self.phase_moe(0)
        if self.final:
            self.phase_final()
        else:
            for t in range(NTILE):
                self.out_ops.append(self.dma("sp", self.x_out[t * 128:(t + 1) * 128, :], self.XR[t * 128:(t + 1) * 128, :],
                                             r=[self.XR_k[t]], w=[self.x_out_k]))
        self.p.emit(self.out_ops)


def build_nc(**kw):
    nc = bass.Bass("TRN2", target_bir_lowering=False)
    with ExitStack() as es:
        b = Builder(nc, es, **kw)
        b.build()
    return nc, b


def _launch(inputs, xcur, layer, do_mixer, do_moe, final):
    nc, bld = build_nc(layer_abs=layer, do_mixer=do_mixer, do_moe=do_moe, final=final)
    print('nops', len(bld.p.ops), flush=True)
    j = [0, 0, 0, 1][layer]
    per_layer = {"ada_w": inputs["ada_w"][layer:layer + 1], "ada_b": inputs["ada_b"][layer:layer + 1],
                 "norm_mix_w": inputs["norm_mix_w"][layer:layer + 1], "norm_ffn_w": inputs["norm_ffn_w"][layer:layer + 1],
                 "da_w_qkv": inputs["da_w_qkv"][j:j + 1], "da_lambda": inputs["da_lambda"][j:j + 1],
                 "da_subln_w": inputs["da_subln_w"][j:j + 1], "da_w_o": inputs["da_w_o"][j:j + 1],
                 "dn_w_in": inputs["dn_w_in"], "dn_conv_w": inputs["dn_conv_w"].reshape(1, 5, 8192),
                 "dn_a_log": inputs["dn_a_log"], "dn_dt_bias": inputs["dn_dt_bias"], "dn_norm_w": inputs["dn_norm_w"],
                 "dn_w_o": inputs["dn_w_o"],
                 "wa_w_qkv": inputs["wa_w_qkv"], "wa_sinks": inputs["wa_sinks"], "wa_w_o": inputs["wa_w_o"],
                 "moe_wg": inputs["moe_wg"][layer:layer + 1], "moe_bg": inputs["moe_bg"][layer:layer + 1],
                 "moe_we": inputs["moe_we"][layer:layer + 1], "moe_be": inputs["moe_be"][layer:layer + 1],
                 "moe_w13": inputs["moe_w13"][layer:layer + 1], "moe_w2": inputs["moe_w2"][layer:layer + 1],
                 "final_norm_w": inputs["final_norm_w"].reshape(1, D)}
    consts = _consts()
    dummy = np.zeros((1, 1), np.float32)
    in_maps = []
    for b in range(len(xcur)):
        m = {"x_in": xcur[b]}
        cin = np.stack([inputs["c"][b], inputs["c_ctx"]], -1)
        m["cin"] = np.ascontiguousarray(cin.reshape(KC, 128, 2).transpose(1, 0, 2))
        for name, shape in bld.in_shapes.items():
            if name in m:
                continue
            if name not in bld.used:
                m[name] = dummy
            elif name in consts:
                m[name] = consts[name]
            else:
                m[name] = np.ascontiguousarray(per_layer[name])
        in_maps.append(m)
    res = run_bass_kernel_spmd(nc, in_maps, core_ids=list(range(len(xcur))))
    key = "y_out" if final else "x_out"
    global _last_res
    _last_res = res.results
    return [r[key] for r in res.results]


def kernel(**inputs):
    inputs = {k: np.asarray(v) for k, v in inputs.items()}
    nb = inputs["x"].shape[0]
    xcur = [np.ascontiguousarray(np.concatenate([inputs["ctx"][b], inputs["x"][b]], 0)) for b in range(nb)]
    for layer in range(DEPTH):
        kind = [0, 1, 2, 0][layer]
        xcur = _launch(inputs, xcur, layer, True, False, False)
        xcur = _launch(inputs, xcur, layer, False, True, layer == DEPTH - 1)
    return np.stack(xcur, 0).astype(np.float32)


def _phase_moe(self, l):
    c = self.c
    c.arena_reset()
    gate, gate_k = self.GATE
    ht32, ht32_k = c.asb([128, KC, 128], F32, "ht32")
    wr, wr_k = c.asb([128, KC, 36], F32, "wr")
    br, br_k = c.asb([128, 36], F32, "br")
    lg, lg_k = c.asb([128, 36], F32, "lg")
    rs_, rs_k = c.asb([128, 64], F32, "rsm")
    self.dma("sp", wr[:, :, 0:4], self.moe_wg[l].rearrange("(kc p) n -> p kc n", p=128), w=[wr_k])
    self.dma("sp", wr[:, :, 4:36], self.moe_we[l].rearrange("(kc p) n -> p kc n", p=128), w=[wr_k])
    self.dma("sp", br[:, 0:4], self.moe_bg[l:l + 1, :].partition_broadcast(128), w=[br_k])
    self.dma("sp", br[:, 4:36], self.moe_be[l:l + 1, :].partition_broadcast(128), w=[br_k])
    R = lambda a, b=None: rs_[:, a:(a + 1 if b is None else b)]

    def router(t, xt, xt_k):
        for g in range(4):
            pt, pt_k = self.PS[2 + (g % 2)]
            for jx in range(4):
                kc = g * 4 + jx
                self.op("pe", lambda e, pt=pt, jx=jx, kc=kc, xt=xt: e.transpose(out=pt[:, jx * 128:(jx + 1) * 128],
                                                                              in_=xt[:, kc * 128:(kc + 1) * 128], identity=self.ident_f[:]),
                        r=[xt_k, self.ident_f_k], w=[pt_k])
            self.op("dve", lambda e, pt=pt, g=g: e.tensor_copy(out=ht32[:, g * 4:(g + 1) * 4, :], in_=pt[:].rearrange("p (a b) -> p a b", a=4)),
                    r=[pt_k], w=[ht32_k])
        pl, pl_k = self.PS[4]
        for kc in range(KC):
            self.op("pe", lambda e, kc=kc: e.matmul(pl[:, 0:36], lhsT=ht32[:, kc, :], rhs=wr[:, kc, :], start=(kc == 0), stop=(kc == KC - 1)),
                    r=[ht32_k, wr_k], w=[pl_k])
        V = lambda fn, r=(), w=(): self.op("dve", fn, r=list(r) + [rs_k], w=list(w) + [rs_k])
        self.op("dve", lambda e: e.tensor_tensor(out=lg[:], in0=pl[:, 0:36], in1=br[:], op=ALU.add), r=[pl_k, br_k], w=[lg_k])
        V(lambda e: e.reduce_max(out=R(0), in_=lg[:, 0:4], axis=AX.X), r=[lg_k])
        V(lambda e: e.tensor_scalar(out=R(1), in0=R(0), scalar1=-1.0, scalar2=None, op0=ALU.mult))
        self.op("act", lambda e: e.activation(out=R(56, 60), in_=lg[:, 0:4], func=AF.Exp, bias=R(1), accum_out=R(2)), r=[lg_k, rs_k], w=[rs_k])
        V(lambda e: e.reciprocal(out=R(3), in_=R(2)))
        V(lambda e: e.tensor_scalar(out=R(4, 8), in0=lg[:, 0:4], scalar1=R(0), scalar2=None, op0=ALU.is_equal), r=[lg_k])
        V(lambda e: e.tensor_scalar(out=R(8, 16), in0=lg[:, 4:12], scalar1=R(4), scalar2=None, op0=ALU.mult), r=[lg_k])
        for g in range(1, 4):
            V(lambda e, g=g: e.scalar_tensor_tensor(out=R(8, 16), in0=lg[:, 4 + 8 * g:12 + 8 * g], scalar=R(4 + g), in1=R(8, 16),
                                                   op0=ALU.mult, op1=ALU.add), r=[lg_k])
        V(lambda e: e.reduce_max(out=R(40), in_=R(8, 16), axis=AX.X))
        V(lambda e: e.tensor_scalar(out=R(16, 24), in0=R(8, 16), scalar1=R(40), scalar2=None, op0=ALU.is_equal))
        V(lambda e: e.scalar_tensor_tensor(out=R(24, 32), in0=R(16, 24), scalar=-1e30, in1=R(8, 16), op0=ALU.mult, op1=ALU.add))
        V(lambda e: e.reduce_max(out=R(41), in_=R(24, 32), axis=AX.X))
        V(lambda e: e.tensor_scalar(out=R(32, 40), in0=R(24, 32), scalar1=R(41), scalar2=None, op0=ALU.is_equal))
        V(lambda e: e.tensor_tensor(out=R(42), in0=R(41), in1=R(40), op=ALU.subtract))
        self.op("act", lambda e: e.activation(out=R(43), in_=R(42), func=AF.Exp), r=[rs_k], w=[rs_k])
        V(lambda e: e.tensor_scalar(out=R(44), in0=R(43), scalar1=1.0, scalar2=None, op0=ALU.add))
        V(lambda e: e.reciprocal(out=R(44), in_=R(44)))
        V(lambda e: e.tensor_tensor(out=R(45), in0=R(44), in1=R(3), op=ALU.mult))
        V(lambda e: e.tensor_tensor(out=R(46), in0=R(45), in1=R(43), op=ALU.mult))
        V(lambda e: e.tensor_scalar(out=R(48, 56), in0=R(16, 24), scalar1=R(45), scalar2=None, op0=ALU.mult))
        V(lambda e: e.scalar_tensor_tensor(out=R(48, 56), in0=R(32, 40), scalar=R(46), in1=R(48, 56), op0=ALU.mult, op1=ALU.add))
        for g in range(4):
            self.op("dve", lambda e, g=g, t=t: e.tensor_scalar(out=gate[:, t, g * 8:(g + 1) * 8], in0=R(48, 56), scalar1=R(4 + g),
                                                              scalar2=None, op0=ALU.mult), r=[rs_k], w=[gate_k])

    self.phase_norm(router=router)
    self.load_gate_tiles(l, 1)
    c.arena_reset()
    actt, _ = c.asb([128, 6, NT], BF16, "actt")
    actt_k = [Tk("actt%d" % i) for i in range(5)]
    ysb = [c.asb([128, 512], F32, "ysb%d" % i) for i in range(3)]
    if not hasattr(self, "YACC"):
        self.YACC, _ = c.dram("YACC", [NT, D], F32)
        self.YACC_k = [Tk("yacc%d" % t) for t in range(NTILE)]
    zt, zt_k = self.XT[0]
    self.op("pool", lambda e: e.memset(zt[:], 0.0), w=[zt_k])
    for t in range(NTILE):
        self.dma("sp", self.YACC[t * 128:(t + 1) * 128, :], zt[:], r=[zt_k], w=[self.YACC_k[t]])
    st = {"i": 0}
    blk_of_tile = {}
    for bi, (t0, n) in enumerate(self.TOKBLK):
        for t in range(t0 // 128, (t0 + n) // 128):
            blk_of_tile[t] = bi
    for ex in range(32):
        def post13(j, bi, t0, n, ps, ps_k):
            if j < 6:
                self.op("act", lambda e, j=j, t0=t0, n=n, ps=ps: e.activation(out=actt[:, j, t0:t0 + n], in_=ps[:, 0:n], func=AF.Silu),
                        r=[ps_k], w=[actt_k[bi]])
            else:
                self.op("dve", lambda e, j=j, t0=t0, n=n, ps=ps: e.tensor_tensor(out=actt[:, j - 6, t0:t0 + n], in0=ps[:, 0:n],
                                                                               in1=actt[:, j - 6, t0:t0 + n], op=ALU.mult),
                        r=[ps_k, actt_k[bi]], w=[actt_k[bi]])

        def post2(cb, nb, t, ps, ps_k, ex=ex):
            yb, yb_k = ysb[st["i"] % 3]
            st["i"] += 1
            self.op("dve", lambda e, yb=yb, ps=ps, t=t: e.tensor_scalar(out=yb[:, 0:nb], in0=ps[:, 0:nb], scalar1=gate[:, t, ex:ex + 1],
                                                                       scalar2=None, op0=ALU.mult), r=[ps_k, gate_k], w=[yb_k])
            self.p.op("pool", lambda e, yb=yb, t=t, cb=cb: e.dma_start(out=self.YACC[t * 128:(t + 1) * 128, cb:cb + nb], in_=yb[:, 0:nb],
                                                                       accum_op=ALU.add), r=[yb_k], w=[self.YACC_k[t]], dma=True)

        self.linear_fm(self.moe_w13[l, ex], 0, 1536, post13)
        self.linear_tm(self.moe_w2[l, ex], 0, D, post2, src=actt, src_k=[actt_k[blk_of_tile[t]] for t in range(NTILE)], nk=6)
    for t in range(NTILE):
        ty = 1 if t < 2 else 0
        ya, ya_k = self.XT[t % 2]
        xa, xa_k = self.SB[t % 2]
        self.dma("sp", ya[:], self.YACC[t * 128:(t + 1) * 128, :], r=[self.YACC_k[t]], w=[ya_k])
        self.dma("sp", xa[:], self.XR[t * 128:(t + 1) * 128, :], r=[self.XR_k[t]], w=[xa_k])
        self.op("pool", lambda e, ya=ya, ty=ty: e.tensor_tensor(out=ya[:], in0=ya[:], in1=self.GB[ty][0][:], op=ALU.mult),
                r=[ya_k, self.GB[ty][1]], w=[ya_k])
        self.op("dve", lambda e, ya=ya, xa=xa: e.tensor_tensor(out=xa[:], in0=xa[:], in1=ya[:], op=ALU.add), r=[ya_k, xa_k], w=[xa_k])
        self.dma("sp", self.XR[t * 128:(t + 1) * 128, :], xa[:], r=[xa_k], w=[self.XR_k[t]])


Builder.phase_moe = _phase_moe


def _mixer_win(self):
    c = self.c
    w = self.wa_w_qkv[0]
    c.arena_reset()
    self.load_rope_tables()
    self.VS = [c.asb([128, 512], BF16, "VS%d" % i) for i in range(2)]
    self.vsi = 0
    self.linear_fm(w, 0, D, lambda jj, bi, t0, n, ps, ps_k: self.rope_store(ps, ps_k, t0, n, self.QT[jj], self.QT_k))
    wt, wt_k = self.WT[self.wt_i % 2]
    self.wt_i += 1
    for kvh in range(4):
        for half in range(2):
            self.dma("pool", wt[:, :, (2 * kvh + half) * 64:(2 * kvh + half + 1) * 64],
                     w[:, D + kvh * 64:D + (kvh + 1) * 64].rearrange("(kc p) n -> p kc n", p=128), w=[wt_k])
    psi = 0
    for kvh in range(4):
        for bi, (t0, n) in enumerate(self.TOKBLK):
            ps, ps_k = self.PS[2 + (psi % 2)]
            psi += 1
            rk = [self.BIGA_k[t] for t in range(t0 // 128, (t0 + n) // 128)] + [wt_k]
            for kc in range(KC):
                self.op("pe", lambda e, ps=ps, kc=kc, kvh=kvh, t0=t0, n=n: e.matmul(
                    ps[:, 0:n], lhsT=wt[:, kc, kvh * 128:(kvh + 1) * 128], rhs=self.BIGA[:, kc, t0:t0 + n],
                    start=(kc == 0), stop=(kc == KC - 1)), r=rk, w=[ps_k])
            self.rope_store(ps, ps_k, t0, n, self.KT[kvh], self.KT_k)

    def vpost(cb, nb, t, ps, ps_k):
        vs, vs_k = self.VS[self.vsi % 2]
        self.vsi += 1
        self.op("act", lambda e: e.copy(out=vs[:, 0:nb], in_=ps[:, 0:nb]), r=[ps_k], w=[vs_k])
        self.dma("sp", self.VV[t * 128:(t + 1) * 128, cb:cb + nb], vs[:, 0:nb], r=[vs_k], w=[self.VV_k])

    self.linear_tm(w, D + 256, 256, vpost)
    c.arena_reset()
    AQ = [c.asb([128, NT], BF16, "wAQ%d" % i) for i in range(2)]
    AK = [c.asb([128, NT], BF16, "wAK%d" % i) for i in range(2)]
    VP = [c.asb([128, NTILE, 128], BF16, "wVP%d" % i) for i in range(2)]
    ET = [c.asb([128, 512], BF16, "wET%d" % i) for i in range(4)]
    mk, mk_k = c.asb([128, 6, 512], BF16, "wmask")
    r0, r0_k = c.asb([128, 512], F32, "wr0")
    sx, sx_k = c.asb([128, 32], F32, "wsink")
    self.dma("pool", mk[:], self.kc["k_wmask"][:, :].rearrange("p (a b) -> p a b", a=6), w=[mk_k])
    self.dma("sp", sx[:], self.wa_sinks[0:1, :].partition_broadcast(128), w=[sx_k])
    self.op("act", lambda e: e.activation(out=sx[:], in_=sx[:], func=AF.Exp), r=[sx_k], w=[sx_k])
    for par in range(2):
        vp, vp_k = VP[par]
        self.op("pool", lambda e, vp=vp: e.memset(vp[:], 0.0), w=[vp_k])
    scale = 64 ** -0.5
    eti = 0
    for h in range(32):
        kvh, par = h // 8, h % 2
        aq, aq_k = AQ[(h // 2) % 2]
        ak, ak_k = AK[kvh % 2]
        if par == 0:
            self.dma("sp", aq[:], self.QT[h // 2], r=[self.QT_k], w=[aq_k])
        if h % 8 == 0:
            self.dma("sp", ak[:], self.KT[kvh], r=[self.KT_k], w=[ak_k])
            for p2 in range(2):
                vp, vp_k = VP[p2]
                self.dma("sp", vp[:, :, p2 * 64:(p2 + 1) * 64],
                         self.VV[:, kvh * 64:(kvh + 1) * 64].rearrange("(t p) d -> p t d", p=128), r=[self.VV_k], w=[vp_k])
        vp, vp_k = VP[par]
        p0 = par * 64
        for bi, (t0, n) in enumerate(self.TOKBLK):
            kts = [(0, None), (1, None)]
            if bi > 0:
                sblk = (t0 - LC) // 128
                for ri, r in enumerate(range(-1, 5)):
                    if 0 <= sblk + r < 16:
                        kts.append((2 + sblk + r, ri))
            po, po_k = self.PS[4]
            pz, pz_k = self.PS[6]
            for i, (kt, ri) in enumerate(kts):
                pss, pss_k = self.PS[i % 2]
                et, et_k = ET[eti % 4]
                eti += 1
                self.op("pe", lambda e, pss=pss, ak=ak, aq=aq, kt=kt, t0=t0, n=n, ri=ri, p0=p0: e.matmul(
                    pss[:, 0:n], lhsT=ak[p0:p0 + 64, kt * 128:(kt + 1) * 128], rhs=aq[p0:p0 + 64, t0:t0 + n],
                    start=True, stop=True), r=[ak_k, aq_k], w=[pss_k])
                self.op("act", lambda e, pss=pss, et=et, n=n: e.activation(out=et[:, 0:n], in_=pss[:, 0:n], func=AF.Exp, scale=scale),
                        r=[pss_k], w=[et_k])
                if ri is not None:
                    self.op("pool", lambda e, et=et, n=n, ri=ri: e.tensor_tensor(out=et[:, 0:n], in0=et[:, 0:n], in1=mk[:, ri, 0:n], op=ALU.mult),
                            r=[et_k, mk_k], w=[et_k])
                last = (i == len(kts) - 1)
                self.op("pe", lambda e, et=et, kt=kt, n=n, i=i, last=last, vp=vp: e.matmul(
                    po[:, 0:n], lhsT=vp[:, kt, :], rhs=et[:, 0:n], start=(i == 0), stop=last), r=[vp_k, et_k], w=[po_k])
                self.op("pe", lambda e, et=et, n=n, i=i, last=last: e.matmul(
                    pz[:, 0:n], lhsT=self.ones_b[:], rhs=et[:, 0:n], start=(i == 0), stop=last), r=[self.ones_b_k, et_k], w=[pz_k])
            self.op("dve", lambda e, n=n, h=h: e.tensor_scalar(out=r0[:, 0:n], in0=pz[:, 0:n], scalar1=sx[:, h:h + 1], scalar2=None, op0=ALU.add),
                    r=[pz_k, sx_k], w=[r0_k])
            self.op("dve", lambda e, n=n: e.reciprocal(out=r0[:, 0:n], in_=r0[:, 0:n]), r=[r0_k], w=[r0_k])
            wk = [self.BIGA_k[t] for t in range(t0 // 128, (t0 + n) // 128)]
            self.op("dve", lambda e, n=n, t0=t0, h=h, p0=p0: e.tensor_tensor(out=self.BIGA[p0:p0 + 64, h // 2, t0:t0 + n], in0=po[p0:p0 + 64, 0:n],
                                                                     in1=r0[p0:p0 + 64, 0:n], op=ALU.mult), r=[po_k, r0_k], w=wk)


Builder.mixer_win = _mixer_win


def _dn_proj(self):
    c = self.c
    w = self.dn_w_in[0]
    c.arena_reset()
    P = [c.asb([128, NT], F32, "dnP%d" % i) for i in range(2)]
    Cb, Cb_k = c.asb([128, NT], F32, "dnC")
    OB = [c.asb([128, NT], BF16, "dnOB%d" % i) for i in range(2)]
    rs, rs_k = self.XT[1][0][:, 0:512], self.XT[1][1]
    TS = [c.asb([128, 4, 128], BF16, "dnTS%d" % i) for i in range(2)]
    cw, cw_k = c.asb([128, 320], F32, "dncw")
    cwr, cwr_k = self.XT[0][0][:, 0:384].rearrange("p (a b) -> p a b", a=3), self.XT[0][1]
    cw2d = self.dn_conv_w[0].rearrange("k (j p) -> (k j) p", p=128)
    for i, (r0_, nr) in enumerate(((0, 128), (128, 128), (256, 64))):
        self.dma("sp", cwr[0:nr, i, :], cw2d[r0_:r0_ + nr, :], w=[cwr_k])
    pt, pt_k = self.PS[4]
    for i, (r0_, nr) in enumerate(((0, 128), (128, 128), (256, 64))):
        self.op("pe", lambda e, i=i, nr=nr, r0_=r0_: e.transpose(out=pt[:, r0_:r0_ + nr], in_=cwr[0:nr, i, :], identity=self.ident_f[0:nr, 0:nr]),
                r=[cwr_k, self.ident_f_k], w=[pt_k])
    self.op("dve", lambda e: e.tensor_copy(out=cw[:], in_=pt[:, 0:320]), r=[pt_k], w=[cw_k])
    st = {"ob": 0, "ts": 0, "eng": 0}
    SEGS = ((0, LC), (LC, NT))

    def finish_chunk(j, p, p_k):
        def wcol(k):
            return cw[:, k * 64 + j:k * 64 + j + 1]
        self.op("dve", lambda e: e.tensor_scalar(out=Cb[:], in0=p[:], scalar1=wcol(2), scalar2=None, op0=ALU.mult),
                r=[p_k, cw_k], w=[Cb_k])
        for k in (0, 1, 3, 4):
            sft = k - 2
            for (a, b) in SEGS:
                lo = max(a, a - sft)
                hi = min(b, b - sft)
                self.op("dve", lambda e, k=k, lo=lo, hi=hi, sft=sft: e.scalar_tensor_tensor(
                    out=Cb[:, lo:hi], in0=p[:, lo + sft:hi + sft], scalar=wcol(k), in1=Cb[:, lo:hi], op0=ALU.mult, op1=ALU.add),
                    r=[p_k, cw_k, Cb_k], w=[Cb_k])
        self.op("act", lambda e: e.activation(out=Cb[:], in_=Cb[:], func=AF.Silu), r=[Cb_k], w=[Cb_k])
        ob, ob_k = OB[st["ob"] % 2]
        st["ob"] += 1
        if j < 32:
            self.op("pool", lambda e: e.tensor_tensor(out=ob[:], in0=Cb[:], in1=Cb[:], op=ALU.mult), r=[Cb_k], w=[ob_k])
            qs = (128 ** -0.5) if j < 16 else 1.0
            for (t0, n) in self.TOKBLK:
                pss, pss_k = self.PS[5 + (st["eng"] % 2)]
                st["eng"] += 1
                self.op("pe", lambda e, pss=pss, t0=t0, n=n: e.matmul(pss[:, 0:n], lhsT=self.ones_b[:], rhs=ob[:, t0:t0 + n], start=True, stop=True),
                        r=[ob_k, self.ones_b_k], w=[pss_k])
                self.op("act", lambda e, pss=pss, n=n: e.activation(out=rs[:, 0:n], in_=pss[:, 0:n], func=AF.Sqrt, bias=EPS), r=[pss_k], w=[rs_k])
                self.op("dve", lambda e, n=n: e.reciprocal(out=rs[:, 0:n], in_=rs[:, 0:n]), r=[rs_k], w=[rs_k])
                self.op("dve", lambda e, t0=t0, n=n: e.scalar_tensor_tensor(out=Cb[:, t0:t0 + n], in0=Cb[:, t0:t0 + n], scalar=qs, in1=rs[:, 0:n],
                                                                           op0=ALU.mult, op1=ALU.mult), r=[Cb_k, rs_k], w=[Cb_k])
        ob2, ob2_k = OB[st["ob"] % 2]
        st["ob"] += 1
        self.op("act", lambda e: e.copy(out=ob2[:], in_=Cb[:]), r=[Cb_k], w=[ob2_k])
        if j < 16:
            self.dma("sp", self.QT[j], ob2[:], r=[ob2_k], w=[self.QT_k])
            return
        if j < 32:
            self.dma("sp", self.KT[j - 16], ob2[:], r=[ob2_k], w=[self.KT_k])
            dst, dst_k, col = self.DNK, self.DNK_k, (j - 16) * 128
        else:
            dst, dst_k, col = self.DNV, self.DNV_k, (j - 32) * 128
        ptb_t, ptb_k = self.PS[7]
        ptb = ptb_t[:].bitcast(BF16)
        for g0 in range(0, NTILE, 4):
            ng = min(4, NTILE - g0)
            ts, ts_k = TS[st["ts"] % 2]
            st["ts"] += 1
            for x in range(ng):
                t = g0 + x
                self.op("pe", lambda e, x=x, t=t: e.transpose(out=ptb[:, x * 128:(x + 1) * 128], in_=ob2[:, t * 128:(t + 1) * 128],
                                                              identity=self.ident_b[:]), r=[ob2_k, self.ident_b_k], w=[ptb_k])
            self.op("dve", lambda e, ts=ts, ng=ng: e.tensor_copy(out=ts[:, 0:ng, :], in_=ptb[:, 0:ng * 128].rearrange("p (a b) -> p a b", a=ng)),
                    r=[ptb_k], w=[ts_k])
            self.dma("sp", dst[g0 * 128:(g0 + ng) * 128, col:col + 128].rearrange("(t p) c -> p t c", p=128), ts[:, 0:ng, :],
                     r=[ts_k], w=[dst_k])

    def post(j, bi, t0, n, ps, ps_k):
        p, p_k = P[j % 2]
        self.op("act", lambda e: e.copy(out=p[:, t0:t0 + n], in_=ps[:, 0:n]), r=[ps_k], w=[p_k])
        if bi == len(self.TOKBLK) - 1:
            finish_chunk(j, p, p_k)

    self.linear_fm(w, 0, 8192, post)
    ZS = [c.asb([128, 512], BF16, "dnZS%d" % i) for i in range(2)]
    zi = {"i": 0}

    def zpost(cb, nb, t, ps, ps_k):
        zs, zs_k = ZS[zi["i"] % 2]
        zi["i"] += 1
        self.op("act", lambda e: e.activation(out=zs[:, 0:nb], in_=ps[:, 0:nb], func=AF.Silu), r=[ps_k], w=[zs_k])
        self.dma("sp", self.DNZ[t * 128:(t + 1) * 128, cb:cb + nb], zs[:, 0:nb], r=[zs_k], w=[self.DNZ_k])

    self.linear_tm(w, 8192, 4096, zpost)


def _mixer_dn(self):
    import os as _os
    self.dn_proj()
    if _os.environ.get("MK_DN_STAGE") == "A":
        return
    self.dn_scan()
    if _os.environ.get("MK_DN_STAGE") == "B":
        return
    self.dn_headout()


Builder.dn_proj = _dn_proj
Builder.mixer_dn = _mixer_dn


def _dn_scan(self, dirs=(0, 1)):
    c = self.c
    w = self.dn_w_in[0]
    c.arena_reset()
    E = self.op
    dm, dm_k = self.XT[0][0][:, 0:1024].rearrange("p (a b) -> p a b", a=8), self.XT[0][1]
    self.dma("sp", dm, self.kc["k_dnmask"][:, :].rearrange("p (a b) -> p a b", a=8), w=[dm_k])
    TRI = [dm[:, 0, :], dm[:, 1, :]]
    SGT = [dm[:, 2, :], dm[:, 3, :]]
    BLK = dm[:, 4, :]
    SELC = [dm[:, 5, :], dm[:, 6, :]]
    misc, misc_k = self.misc
    pc = misc[:, 8:12]
    par = misc[:, 12:16]
    self.dma("sp", pc, self.kc["k_dncol"][:, :], w=[misc_k])
    E("pool", lambda e: e.memset(par, 0.0), r=[misc_k], w=[misc_k])
    for d in range(2):
        self.dma("sp", par[d * 64 + 32:d * 64 + 64, 0:1], self.dn_a_log[0, d:d + 1, :].rearrange("o h -> h o"), r=[misc_k], w=[misc_k])
        self.dma("sp", par[d * 64 + 32:d * 64 + 64, 1:2], self.dn_dt_bias[0, d:d + 1, :].rearrange("o h -> h o"), r=[misc_k], w=[misc_k])
    E("act", lambda e: e.activation(out=par[:, 2:3], in_=par[:, 0:1], func=AF.Exp), r=[misc_k], w=[misc_k])
    E("dve", lambda e: e.scalar_tensor_tensor(out=par[:, 2:3], in0=par[:, 2:3], scalar=-1.0, in1=pc[:, 1:2], op0=ALU.mult, op1=ALU.mult),
      r=[misc_k], w=[misc_k])
    bgs, bgs_k = c.asb([128, 512], F32, "bgs")
    bg2, bg2_k = self.XT[1][0][:, 0:512], self.XT[1][1]
    BGT, BGT_k = c.asb([128, NTILE, 128], F32, "BGT")
    GCA, GCA_k = c.asb([128, NTILE, 64], F32, "GCA")
    GLT, GLT_k = c.asb([128, NTILE, 64], F32, "GLT")
    RR, RR_k = c.asb([128, NTILE, 64], F32, "RR")
    BAx, BAx_k = c.asb([128, NTILE, 64], F32, "BAx")
    GTB, GTB_k = c.asb([128, 2 * NTILE, 64], F32, "GTB")

    def bapost(j, bi, t0, n, ps, ps_k):
        E("act", lambda e: e.activation(out=bgs[:, 0:n], in_=ps[:, 0:n], func=AF.Sigmoid), r=[ps_k], w=[bgs_k])
        E("dve", lambda e: e.tensor_scalar(out=bgs[:, 0:n], in0=bgs[:, 0:n], scalar1=pc[:, 0:1], scalar2=None, op0=ALU.mult),
          r=[bgs_k, misc_k], w=[bgs_k])
        E("act", lambda e: e.activation(out=bg2[:, 0:n], in_=ps[:, 0:n], func=AF.Exp, bias=par[:, 1:2]), r=[ps_k, misc_k], w=[bg2_k])
        E("act", lambda e: e.activation(out=bg2[:, 0:n], in_=bg2[:, 0:n], func=AF.Ln, bias=1.0), r=[bg2_k], w=[bg2_k])
        E("dve", lambda e: e.scalar_tensor_tensor(out=bgs[:, 0:n], in0=bg2[:, 0:n], scalar=par[:, 2:3], in1=bgs[:, 0:n], op0=ALU.mult, op1=ALU.add),
          r=[bg2_k, bgs_k, misc_k], w=[bgs_k])
        for x in range(n // 128):
            t = t0 // 128 + x
            pt, pt_k = self.PS[4 + (t % 2)]
            E("pe", lambda e, x=x, pt=pt: e.transpose(out=pt[:, 0:128], in_=bgs[:, x * 128:(x + 1) * 128], identity=self.ident_f[:]),
              r=[bgs_k, self.ident_f_k], w=[pt_k])
            E("dve", lambda e, t=t, pt=pt: e.tensor_copy(out=BGT[:, t, :], in_=pt[:, 0:128]), r=[pt_k], w=[BGT_k])

    self.linear_fm(w, 12288, 128, bapost)
    for t in range(NTILE):
        pg, pg_k = self.PS[4 + (t % 2)]
        for d in range(2):
            E("pe", lambda e, t=t, d=d, pg=pg: e.matmul(pg[:, d * 32:(d + 1) * 32], lhsT=TRI[d], rhs=BGT[:, t, d * 64 + 32:d * 64 + 64],
                                                       start=True, stop=True), r=[dm_k, BGT_k], w=[pg_k])
            E("pe", lambda e, t=t, d=d, pg=pg: e.matmul(pg[:, 64 + d * 32:64 + (d + 1) * 32], lhsT=BLK, rhs=BGT[:, t, d * 64 + 32:d * 64 + 64],
                                                       start=True, stop=True), r=[dm_k, BGT_k], w=[pg_k])
        E("dve", lambda e, t=t, pg=pg: e.tensor_copy(out=GCA[:, t, :], in_=pg[:, 0:64]), r=[pg_k], w=[GCA_k])
        E("act", lambda e, t=t, pg=pg: e.copy(out=GLT[:, t, :], in_=pg[:, 64:128]), r=[pg_k], w=[GLT_k])
    E("dve", lambda e: e.tensor_tensor(out=RR[:], in0=GLT[:], in1=GCA[:], op=ALU.subtract), r=[GLT_k, GCA_k], w=[RR_k])
    E("act", lambda e: e.activation(out=RR[:], in_=RR[:], func=AF.Exp), r=[RR_k], w=[RR_k])
    E("act", lambda e: e.activation(out=GLT[:], in_=GLT[:], func=AF.Exp), r=[GLT_k, RR_k], w=[GLT_k])
    E("act", lambda e: e.activation(out=GCA[:], in_=GCA[:], func=AF.Exp), r=[GCA_k, RR_k], w=[GCA_k])
    for d in range(2):
        E("dve", lambda e, d=d: e.tensor_tensor(out=BAx[:, :, d * 32:(d + 1) * 32], in0=BGT[:, :, d * 64:d * 64 + 32],
                                               in1=GCA[:, :, d * 32:(d + 1) * 32], op=ALU.mult), r=[BGT_k, GCA_k], w=[BAx_k])
    for t in range(NTILE):
        pg, pg_k = self.PS[4 + (t % 2)]
        for hf in range(2):
            E("pe", lambda e, t=t, hf=hf, pg=pg: e.matmul(pg[:, hf * 64:(hf + 1) * 64], lhsT=SELC[hf], rhs=GLT[:, t, :], start=True, stop=True),
              r=[dm_k, GLT_k], w=[pg_k])
        E("dve", lambda e, t=t, pg=pg: e.tensor_copy(out=GTB[:, 2 * t:2 * t + 2, :], in_=pg[:, 0:128].rearrange("p (a b) -> p a b", a=2)),
          r=[pg_k], w=[GTB_k])
    self.p.barrier()

    NS = 2
    PSQ = [(self.PS[b][0][:, 0:128], self.PS[b][1]) for b in range(8)]
    psq_i = {"i": 0}

    def psq():
        r = PSQ[psq_i["i"] % 8]
        psq_i["i"] += 1
        return r

    class Stream:
        pass

    streams = []
    for s_ in range(NS):
        st = Stream()
        wtf = self.WT[s_][0][:].bitcast(F32)
        st.mats = [(wtf[:, a, b * 128:(b + 1) * 128], Tk("m%d_%d_%d" % (s_, a, b))) for a in range(16) for b in range(2)]
        st.mi = 0
        hb = self.HB[s_][0]
        st.ld = [[(hb[:, (q * 4 + x) * 128:(q * 4 + x + 1) * 128], Tk("ld%d_%d_%d" % (s_, q, x))) for x in range(4)] for q in range(4)]
        st.ldi = 0
        streams.append(st)
    ident = self.ident_f

    def unit(st, d, h, t, first):
        hk = h // 2
        M = {}
        names = ["GTRI", "DEC", "DECT", "L", "U", "La", "Ua", "Lb", "Ub", "P", "vb", "kbg", "kd", "u", "wT", "itT", "qTf", "vn", "o", "S"]
        for i, nm in enumerate(names):
            M[nm] = st.mats[i]
        kTb, qTb, ktok, vtok = st.ld[st.ldi % 4]
        st.ldi += 1
        r0_ = t * 128
        self.dma("sp", kTb[0], self.KT[hk][:, r0_:r0_ + 128], r=[self.KT_k], w=[kTb[1]])
        self.dma("sp", qTb[0], self.QT[hk][:, r0_:r0_ + 128], r=[self.QT_k], w=[qTb[1]])
        self.dma("sp", ktok[0], self.DNK[r0_:r0_ + 128, hk * 128:(hk + 1) * 128], r=[self.DNK_k], w=[ktok[1]])
        self.dma("sp", vtok[0], self.DNV[r0_:r0_ + 128, h * 128:(h + 1) * 128], r=[self.DNV_k], w=[vtok[1]])
        gcol = BGT[:, t, d * 64 + 32 + h:d * 64 + 33 + h]
        bcol = BGT[:, t, d * 64 + h:d * 64 + h + 1]
        acol = GCA[:, t, d * 32 + h:d * 32 + h + 1]
        bacol = BAx[:, t, d * 32 + h:d * 32 + h + 1]
        rcol = RR[:, t, d * 32 + h:d * 32 + h + 1]
        (GTRI, GTRI_k), (DEC, DEC_k), (DECT, DECT_k) = M["GTRI"], M["DEC"], M["DECT"]
        (Lm, L_k), (U, U_k), (P, P_k) = M["L"], M["U"], M["P"]
        E("dve", lambda e: e.tensor_scalar(out=GTRI, in0=TRI[d], scalar1=gcol, scalar2=None, op0=ALU.mult), r=[dm_k, BGT_k], w=[GTRI_k])
        pD, pD_k = psq()
        pDT, pDT_k = psq()
        E("pe", lambda e: e.matmul(pD, lhsT=GTRI, rhs=SGT[d], start=True, stop=True), r=[GTRI_k, dm_k], w=[pD_k])
        E("pe", lambda e: e.matmul(pDT, lhsT=SGT[d], rhs=GTRI, start=True, stop=True), r=[GTRI_k, dm_k], w=[pDT_k])
        E("act", lambda e: e.activation(out=DEC, in_=pD, func=AF.Exp), r=[pD_k], w=[DEC_k])
        E("act", lambda e: e.activation(out=DECT, in_=pDT, func=AF.Exp), r=[pDT_k], w=[DECT_k])
        pKK, pKK_k = psq()
        pQK, pQK_k = psq()
        E("pe", lambda e: e.matmul(pKK, lhsT=kTb[0], rhs=kTb[0], start=True, stop=True), r=[kTb[1]], w=[pKK_k])
        E("pe", lambda e: e.matmul(pQK, lhsT=kTb[0], rhs=qTb[0], start=True, stop=True), r=[kTb[1], qTb[1]], w=[pQK_k])
        E("dve", lambda e: e.scalar_tensor_tensor(out=Lm, in0=pKK, scalar=bcol, in1=DEC, op0=ALU.mult, op1=ALU.mult),
          r=[pKK_k, BGT_k, DEC_k], w=[L_k])
        E("pool", lambda e: e.tensor_tensor(out=Lm, in0=Lm, in1=SGT[d], op=ALU.mult), r=[L_k, dm_k], w=[L_k])
        itT, itT_k = M["itT"]
        E("dve", lambda e: e.tensor_tensor(out=itT, in0=pQK, in1=DECT, op=ALU.mult), r=[pQK_k, DECT_k], w=[itT_k])
        E("pool", lambda e: e.tensor_tensor(out=itT, in0=itT, in1=TRI[d], op=ALU.mult), r=[itT_k, dm_k], w=[itT_k])
        pU, pU_k = psq()
        E("pe", lambda e: e.transpose(out=pU, in_=Lm, identity=ident[:]), r=[L_k, self.ident_f_k], w=[pU_k])
        E("act", lambda e: e.copy(out=U, in_=pU), r=[pU_k], w=[U_k])
        E("dve", lambda e: e.scalar_tensor_tensor(out=P, in0=U, scalar=-1.0, in1=ident[:], op0=ALU.mult, op1=ALU.add),
          r=[U_k, self.ident_f_k], w=[P_k])
        curL, curU = M["L"], M["U"]
        pp = [(M["La"], M["Ua"]), (M["Lb"], M["Ub"])]
        for k in range(1, 6):
            nL, nU = pp[k % 2]
            pl, pl_k = psq()
            E("pe", lambda e, pl=pl, curL=curL, curU=curU: e.matmul(pl, lhsT=curU[0], rhs=curL[0], start=True, stop=True),
              r=[curL[1], curU[1]], w=[pl_k])
            E("act", lambda e, pl=pl, nL=nL: e.copy(out=nL[0], in_=pl), r=[pl_k], w=[nL[1]])
            if k < 5:
                pu_, pu_k = psq()
                E("pe", lambda e, pu_=pu_, curL=curL, curU=curU: e.matmul(pu_, lhsT=curL[0], rhs=curU[0], start=True, stop=True),
                  r=[curL[1], curU[1]], w=[pu_k])
                E("dve", lambda e, pu_=pu_, nU=nU: e.tensor_copy(out=nU[0], in_=pu_), r=[pu_k], w=[nU[1]])
            ppn, ppn_k = psq()
            E("pe", lambda e, ppn=ppn, nL=nL: e.matmul(ppn, lhsT=nL[0], rhs=P, start=True, stop=True), r=[nL[1], P_k], w=[ppn_k])
            E("dve", lambda e, ppn=ppn: e.tensor_tensor(out=P, in0=ppn, in1=P, op=ALU.add), r=[ppn_k, P_k], w=[P_k])
            curL, curU = nL, nU
        (vb, vb_k), (kbg, kbg_k), (kd, kd_k) = M["vb"], M["kbg"], M["kd"]
        E("pool", lambda e: e.tensor_scalar(out=vb, in0=vtok[0], scalar1=bcol, scalar2=None, op0=ALU.mult), r=[vtok[1], BGT_k], w=[vb_k])
        E("pool", lambda e: e.tensor_scalar(out=kbg, in0=ktok[0], scalar1=bacol, scalar2=None, op0=ALU.mult), r=[ktok[1], BAx_k], w=[kbg_k])
        E("pool", lambda e: e.tensor_scalar(out=kd, in0=ktok[0], scalar1=rcol, scalar2=None, op0=ALU.mult), r=[ktok[1], RR_k], w=[kd_k])
        (u, u_k), (wT, wT_k), (qTf, qTf_k) = M["u"], M["wT"], M["qTf"]
        pu2, pu2_k = psq()
        pw, pw_k = psq()
        E("pe", lambda e: e.matmul(pu2, lhsT=P, rhs=vb, start=True, stop=True), r=[P_k, vb_k], w=[pu2_k])
        E("pe", lambda e: e.matmul(pw, lhsT=kbg, rhs=P, start=True, stop=True), r=[P_k, kbg_k], w=[pw_k])
        E("act", lambda e: e.copy(out=u, in_=pu2), r=[pu2_k], w=[u_k])
        E("dve", lambda e: e.tensor_copy(out=wT, in_=pw), r=[pw_k], w=[wT_k])
        E("act", lambda e: e.copy(out=qTf, in_=qTb[0]), r=[qTb[1]], w=[qTf_k])
        (vn, vn_k), (o, o_k), (S, S_k) = M["vn"], M["o"], M["S"]
        if first:
            E("pool", lambda e: e.memset(S, 0.0), w=[S_k])
        for hf in ((0, 1) if d == 0 else (1, 0)):
            c0 = hf * 64
            gtcol = GTB[:, 2 * t + hf, d * 32 + h:d * 32 + h + 1]
            p1, p1_k = psq()
            p2a, p2a_k = psq()
            p2b, p2b_k = psq()
            p3, p3_k = psq()
            E("pe", lambda e, c0=c0, p1=p1: e.matmul(p1[c0:c0 + 64, :], lhsT=wT[:, c0:c0 + 64], rhs=S, start=True, stop=True),
              r=[wT_k, S_k], w=[p1_k])
            E("dve", lambda e, c0=c0, p1=p1: e.tensor_tensor(out=vn[c0:c0 + 64, :], in0=u[c0:c0 + 64, :], in1=p1[c0:c0 + 64, :], op=ALU.subtract),
              r=[u_k, p1_k], w=[vn_k])
            E("pe", lambda e, c0=c0, p2a=p2a: e.matmul(p2a[c0:c0 + 64, :], lhsT=qTf[:, c0:c0 + 64], rhs=S, start=True, stop=True),
              r=[qTf_k, S_k], w=[p2a_k])
            E("pe", lambda e, c0=c0, p2b=p2b: e.matmul(p2b[c0:c0 + 64, :], lhsT=itT[c0:c0 + 64, c0:c0 + 64], rhs=vn[c0:c0 + 64, :],
                                                      start=True, stop=True), r=[itT_k, vn_k], w=[p2b_k])
            E("act", lambda e, c0=c0, p2a=p2a: e.activation(out=o[c0:c0 + 64, :], in_=p2a[c0:c0 + 64, :], func=AF.Copy, scale=acol[c0:c0 + 64, :]),
              r=[p2a_k, GCA_k], w=[o_k])
            E("dve", lambda e, c0=c0, p2b=p2b: e.tensor_tensor(out=o[c0:c0 + 64, :], in0=o[c0:c0 + 64, :], in1=p2b[c0:c0 + 64, :], op=ALU.add),
              r=[o_k, p2b_k], w=[o_k])
            E("pe", lambda e, c0=c0, p3=p3: e.matmul(p3, lhsT=kd[c0:c0 + 64, :], rhs=vn[c0:c0 + 64, :], start=True, stop=True),
              r=[kd_k, vn_k], w=[p3_k])
            E("dve", lambda e, p3=p3, gtcol=gtcol: e.scalar_tensor_tensor(out=S, in0=S, scalar=gtcol, in1=p3, op0=ALU.mult, op1=ALU.add),
              r=[S_k, GTB_k, p3_k], w=[S_k])
        self.dma("sp", self.ODN[d, r0_:r0_ + 128, h * 128:(h + 1) * 128], o, r=[o_k], w=[self.ODN_k])

    order = {0: list(range(NTILE)), 1: [1, 0] + list(range(NTILE - 1, 1, -1))}
    import os as _os
    nh_ = int(_os.environ.get('MK_DN_NH', '32'))
    todo = [(d, h) for d in dirs for h in range(nh_)]
    for g0 in range(0, len(todo), NS):
        grp = todo[g0:g0 + NS]
        for step in range(NTILE):
            for si, (d, h) in enumerate(grp):
                unit(streams[si], d, h, order[d][step], step == 0)
    self.p.barrier()


Builder.dn_scan = _dn_scan


def _dn_headout(self):
    c = self.c
    E = self.op
    for half in range(2):
        c.arena_reset()
        nwt, nwt_k = c.asb([128, 128], F32, "nwt")
        ssb, ssb_k = c.asb([128, 32], F32, "ssb")
        self.dma("sp", nwt, self.dn_norm_w[0:1, :].partition_broadcast(128), w=[nwt_k])
        ps_t, ps_tk = self.PS[1]
        pst = ps_t[:].bitcast(BF16)
        c0 = half * 2048
        for t in range(NTILE):
            oa, oa_k = self.XT[0]
            ob_, ob_k = self.XT[1]
            zt, zt_k = self.HB[t % 2]
            hb, hb_k = self.SB[t % 2]
            hbb = hb[:].bitcast(BF16)[:, 0:2048]
            r0_ = t * 128
            self.dma("sp", oa[:], self.ODN[0, r0_:r0_ + 128, c0:c0 + 2048], r=[self.ODN_k], w=[oa_k])
            self.dma("sp", ob_[:], self.ODN[1, r0_:r0_ + 128, c0:c0 + 2048], r=[self.ODN_k], w=[ob_k])
            self.dma("sp", zt[:], self.DNZ[r0_:r0_ + 128, c0:c0 + 2048], r=[self.DNZ_k], w=[zt_k])
            E("dve", lambda e, oa=oa, ob_=ob_: e.tensor_tensor(out=oa[:], in0=oa[:], in1=ob_[:], op=ALU.add), r=[oa_k, ob_k], w=[oa_k])
            E("act", lambda e, oa=oa, ob_=ob_: e.activation(out=ob_[:], in_=oa[:], func=AF.Square), r=[oa_k], w=[ob_k])
            E("dve", lambda e, ob_=ob_: e.reduce_sum(out=ssb[:, 0:16], in_=ob_[:].rearrange("p (a b) -> p a b", a=16), axis=AX.X),
              r=[ob_k], w=[ssb_k])
            E("act", lambda e: e.activation(out=ssb[:, 16:32], in_=ssb[:, 0:16], func=AF.Sqrt, scale=1.0 / 128, bias=EPS), r=[ssb_k], w=[ssb_k])
            E("dve", lambda e: e.reciprocal(out=ssb[:, 16:32], in_=ssb[:, 16:32]), r=[ssb_k], w=[ssb_k])
            for hh in range(16):
                eng = "dve" if hh % 2 == 0 else "pool"
                if eng == "dve":
                    E("dve", lambda e, hh=hh, oa=oa, hbb=hbb: e.scalar_tensor_tensor(
                        out=hbb[:, hh * 128:(hh + 1) * 128], in0=oa[:, hh * 128:(hh + 1) * 128], scalar=ssb[:, 16 + hh:17 + hh], in1=nwt,
                        op0=ALU.mult, op1=ALU.mult), r=[oa_k, ssb_k, nwt_k], w=[hb_k])
                else:
                    E("pool", lambda e, hh=hh, oa=oa: e.tensor_scalar(out=oa[:, hh * 128:(hh + 1) * 128], in0=oa[:, hh * 128:(hh + 1) * 128],
                                                                       scalar1=ssb[:, 16 + hh:17 + hh], scalar2=None, op0=ALU.mult),
                      r=[oa_k, ssb_k], w=[oa_k])
                    E("pool", lambda e, hh=hh, oa=oa, hbb=hbb: e.tensor_tensor(out=hbb[:, hh * 128:(hh + 1) * 128], in0=oa[:, hh * 128:(hh + 1) * 128],
                                                                              in1=nwt, op=ALU.mult), r=[oa_k, nwt_k], w=[hb_k])
            E("dve", lambda e, hbb=hbb, zt=zt: e.tensor_tensor(out=hbb, in0=hbb, in1=zt[:], op=ALU.mult), r=[hb_k, zt_k], w=[hb_k])
            for g in range(2):
                for j in range(8):
                    kc = g * 8 + j
                    E("pe", lambda e, hbb=hbb, kc=kc, j=j: e.transpose(out=pst[:, j * 128:(j + 1) * 128], in_=hbb[:, kc * 128:(kc + 1) * 128],
                                                                      identity=self.ident_b[:]), r=[hb_k, self.ident_b_k], w=[ps_tk])
                E("act", lambda e, g=g, t=t: e.copy(out=self.BIGA[:, g * 8:(g + 1) * 8, t * 128:(t + 1) * 128],
                                                   in_=pst.rearrange("p (a b) -> p a b", a=8)), r=[ps_tk], w=[self.BIGA_k[t]])
        self.phase_oproj_residual(self.dn_w_o[0][half * 2048:(half + 1) * 2048, :], 0)


Builder.dn_headout = _dn_headout
```

```python
import math
from contextlib import ExitStack
import numpy as np
import concourse.bass as bass
import concourse.mybir as mybir
from concourse.bass_utils import run_bass_kernel_spmd

F32 = mybir.dt.float32
BF16 = mybir.dt.bfloat16
I32 = mybir.dt.int32
U32 = mybir.dt.uint32
AF = mybir.ActivationFunctionType
ALU = mybir.AluOpType
AX = mybir.AxisListType

D = 2048
KC = 16
L = 2048
LC = 256
NT = L + LC
NTILE = NT // 128
DEPTH = 4
EPS = 1e-6
N_DMA_SEM = 40


class Tk:
    __slots__ = ("name", "w", "r")

    def __init__(self, name):
        self.name = name
        self.w = None
        self.r = []


class Op:
    __slots__ = ("eng", "fn", "deps", "dma", "sem", "val", "cnt", "need", "waits")


class Prog:
    ENGS = ("pe", "act", "dve", "pool", "sp")

    def __init__(self, nc):
        self.nc = nc
        self.ops = []
        self.dma_uses = [0] * N_DMA_SEM
        self.dma_last = [None] * N_DMA_SEM
        self.dma_rr = 0
        self.final_deps = []
        self.since = []
        self.bar = None

    def op(self, eng, fn, r=(), w=(), dma=False):
        o = Op()
        o.eng = eng
        o.fn = fn
        o.dma = dma
        o.need = False
        deps = set()
        for t in r:
            if t.w is not None:
                deps.add(t.w)
        for t in w:
            if t.w is not None:
                deps.add(t.w)
            deps.update(t.r)
        idx = len(self.ops)
        if self.bar is not None:
            deps.add(self.bar)
        self.since.append(idx)
        if dma:
            s = self.dma_rr
            self.dma_rr = (self.dma_rr + 1) % N_DMA_SEM
            if self.dma_last[s] is not None:
                deps.add(self.dma_last[s])
            self.dma_uses[s] += 1
            self.dma_last[s] = idx
            o.sem = s
            o.val = 16 * self.dma_uses[s]
        o.deps = deps
        self.ops.append(o)
        for t in r:
            t.r.append(idx)
        for t in w:
            t.w = idx
            t.r = []
        return idx

    def barrier(self):
        o = Op()
        o.eng = "sp"
        o.fn = lambda e: e.nop()
        o.dma = False
        o.need = False
        o.deps = set(self.since)
        idx = len(self.ops)
        self.ops.append(o)
        self.since = [idx]
        self.bar = idx
        return idx

    def finalize(self):
        ops = self.ops
        for o in ops:
            for d in o.deps:
                if not ops[d].dma:
                    ops[d].need = True
        cnt = {e: 0 for e in self.ENGS}
        for o in ops:
            if o.need:
                cnt[o.eng] += 1
            o.cnt = cnt[o.eng]
        seen = {e: {} for e in self.ENGS}
        for o in ops:
            waits = {}
            for d in o.deps:
                dd = ops[d]
                if dd.dma:
                    key = ("dma", dd.sem)
                    val = dd.val
                else:
                    if dd.eng == "pe" and o.eng == "pe" and not o.dma:
                        continue
                    key = ("eng", dd.eng)
                    val = dd.cnt
                if seen[o.eng].get(key, 0) >= val:
                    continue
                if waits.get(key, 0) < val:
                    waits[key] = val
            for k, v in waits.items():
                seen[o.eng][k] = v
            o.waits = list(waits.items())

    def emit(self, out_ops):
        nc = self.nc
        self.finalize()
        ops = self.ops
        with ExitStack() as es:
            esem = {e: es.enter_context(nc.semaphore("s_" + e)) for e in self.ENGS}
            dsem = [es.enter_context(nc.semaphore("d%d" % i)) for i in range(N_DMA_SEM)]
            block = es.enter_context(nc.Block())

            def run(ename):
                def body(eng):
                    for o in ops:
                        if o.eng != ename:
                            continue
                        for (kind, k), v in o.waits:
                            eng.wait_ge(esem[k] if kind == "eng" else dsem[k], v)
                        ins = o.fn(eng)
                        if o.dma:
                            ins.then_inc(dsem[o.sem], 16)
                        elif o.need:
                            ins.then_inc(esem[ename], 1)
                    if ename == "sp":
                        for d in out_ops:
                            dd = ops[d]
                            eng.wait_ge(dsem[dd.sem], dd.val)
                return body

            block.tensor(run("pe"))
            block.scalar(run("act"))
            block.vector(run("dve"))
            block.gpsimd(run("pool"))
            block.sync(run("sp"))


class Ctx:
    def __init__(self, nc, es):
        self.nc = nc
        self.es = es
        self.p = Prog(nc)
        self.n = 0

    def sb(self, shape, dt, name=None):
        self.n += 1
        t = self.es.enter_context(self.nc.sbuf_tensor(name or ("sb%d" % self.n), list(shape), dt))
        return t, Tk(name or "sb%d" % self.n)

    def arena_init(self, nbytes):
        self.arena_n = nbytes // 2
        self.arena = self.es.enter_context(self.nc.sbuf_tensor("ARENA", [128, self.arena_n], BF16))
        self.arena_off = 0

    def arena_reset(self):
        self.p.barrier()
        self.arena_off = 0

    def asb(self, shape, dt, name=None):
        self.n += 1
        esz = 4 if dt in (F32, I32, U32) else 2
        free = 1
        for d in shape[1:]:
            free *= d
        nel = (free * esz + 1) // 2
        nel = (nel + 31) // 32 * 32
        assert self.arena_off + nel <= self.arena_n, "arena overflow %s" % name
        v = self.arena[0:shape[0], self.arena_off:self.arena_off + free * esz // 2]
        self.arena_off += nel
        if dt != BF16:
            v = v.bitcast(dt)
        if len(shape) == 3:
            v = v.rearrange("p (a b) -> p a b", a=shape[1])
        return v, Tk(name or "a%d" % self.n)

    def ps(self, shape, dt=F32, name=None):
        self.n += 1
        t = self.es.enter_context(self.nc.psum_tensor(name or ("ps%d" % self.n), list(shape), dt))
        return t, Tk(name or "ps%d" % self.n)

    def dram(self, name, shape, dt, kind="Internal"):
        t = self.nc.dram_tensor(name, list(shape), dt, kind=kind)
        return t.ap(), Tk(name)


def _rope_tables():
    GRID_W = 64
    dim = 64
    rows = L // GRID_W
    row, col = np.meshgrid(np.arange(rows), np.arange(GRID_W), indexing="ij")
    row = row.reshape(-1).astype(np.float32)
    col = col.reshape(-1).astype(np.float32)
    half = dim // 2
    inv_freq = (1.0 / (10000.0 ** (np.arange(0, half, 2, dtype=np.float32) / half))).astype(np.float32)

    def table(pos):
        ang = pos[:, None] * inv_freq[None, :]
        ang = np.concatenate([ang, ang], axis=-1)
        return np.cos(ang), np.sin(ang)

    cr, sr = table(row)
    cc, sc = table(col)
    cos = np.concatenate([cr, cc], -1).astype(np.float32)
    sin = np.concatenate([sr, sc], -1).astype(np.float32)
    cosT = np.concatenate([cos.T, cos.T], 0)
    sinT = np.concatenate([sin.T, sin.T], 0)
    R = np.zeros((64, 64), np.float32)
    for base in (0, 32):
        for i in range(16):
            R[base + i, base + 16 + i] = -1.0
            R[base + 16 + i, base + i] = 1.0
    R2 = np.zeros((128, 128), np.float32)
    R2[:64, :64] = R
    R2[64:, 64:] = R
    return cosT, sinT, np.ascontiguousarray(R2.T)


def _consts():
    cosT, sinT, RT = _rope_tables()
    c = {
        "k_cos": cosT, "k_sin": sinT, "k_rt": RT,
        "k_ident": np.eye(128, dtype=np.float32),
        "k_ones": np.ones((128, 128), np.float32),
    }
    wm = np.zeros((128, 6, 512), np.float32)
    kk = np.arange(128)[:, None]
    qq = np.arange(512)[None, :]
    for ri, r in enumerate(range(-1, 5)):
        wm[:, ri, :] = np.where(np.abs(r * 128 + kk - qq) <= 128, 1.0, 0.0)
    c["k_wmask"] = wm.reshape(128, 6 * 512)
    ii = np.arange(128)
    sc = (ii[:, None] // 64) == (ii[None, :] // 64)
    dm = np.zeros((128, 8, 128), np.float32)
    dm[:, 0] = sc & (ii[:, None] <= ii[None, :])
    dm[:, 1] = sc & (ii[:, None] >= ii[None, :])
    dm[:, 2] = sc & (ii[:, None] > ii[None, :])
    dm[:, 3] = sc & (ii[:, None] < ii[None, :])
    dm[:, 4] = sc
    dm[:, 5] = (ii[:, None] < 64) / 64.0 + 0 * ii[None, :]
    dm[:, 6] = (ii[:, None] >= 64) / 64.0 + 0 * ii[None, :]
    c["k_dnmask"] = dm.reshape(128, 8 * 128)
    pc = np.zeros((128, 4), np.float32)
    pc[:, 0] = ((ii // 32) % 2 == 0)
    pc[:, 1] = ((ii // 32) % 2 == 1)
    c["k_dncol"] = pc
    return c


class Builder:
    def __init__(self, nc, es, n_layers=1, dbg=None, do_mixer=True, do_moe=True, wl=1, layer_abs=0, final=False, fused=False):
        self.fused = fused
        self.layers = list(range(DEPTH)) if fused else [layer_abs]
        self.wl = DEPTH if fused else 1
        self.layer_abs = layer_abs
        self.final = True if fused else final
        self.nc = nc
        self.c = Ctx(nc, es)
        self.p = self.c.p
        self.n_layers = n_layers
        self.dbg = dbg or []
        self.do_mixer = do_mixer
        self.do_moe = do_moe
        self.out_ops = []

    def op(self, eng, fn, r=(), w=(), dma=False):
        psk = getattr(self, "_psk", None)
        if psk:
            r2 = [t for t in r if id(t) not in psk]
            w = list(w) + [t for t in r if id(t) in psk]
            r = r2
        return self.p.op(eng, fn, r=r, w=w, dma=dma)

    def dma(self, eng, out, in_, r=(), w=()):
        return self.p.op(eng, lambda e: e.dma_start(out=out, in_=in_), r=r, w=w, dma=True)

    def declare_io(self):
        c = self.c
        kind = [0, 1, 2, 0][self.layer_abs]
        kinds = {[0, 1, 2, 0][l] for l in self.layers}
        used = {"x_in", "cin", "ada_w", "ada_b", "norm_mix_w", "norm_ffn_w", "final_norm_w",
                "k_cos", "k_sin", "k_rt", "k_ident", "k_ones", "k_wmask", "k_dnmask", "k_dncol"}
        if self.do_mixer and 0 in kinds:
            used |= {"da_w_qkv", "da_lambda", "da_subln_w", "da_w_o"}
        if self.do_mixer and 2 in kinds:
            used |= {"wa_w_qkv", "wa_sinks", "wa_w_o"}
        if self.do_mixer and 1 in kinds:
            used |= {"dn_w_in", "dn_conv_w", "dn_a_log", "dn_dt_bias", "dn_norm_w", "dn_w_o"}
        if self.do_moe:
            used |= {"moe_wg", "moe_bg", "moe_we", "moe_be", "moe_w13", "moe_w2"}
        self.used = used
        self.in_shapes = {}

        def ein(n, s):
            if n not in used:
                s = [1, 1]
            self.in_shapes[n] = list(s)
            return c.dram(n, s, F32, "ExternalInput")
        self.x_in, self.x_in_k = ein("x_in", [NT, D])
        self.cin, self.cin_k = ein("cin", [128, KC, 2])
        self.ada_w, self.ada_w_k = ein("ada_w", [self.wl, D, 6 * D])
        self.ada_b, _ = ein("ada_b", [self.wl, 6 * D])
        self.norm_mix_w, _ = ein("norm_mix_w", [self.wl, D])
        self.norm_ffn_w, _ = ein("norm_ffn_w", [self.wl, D])
        self.da_w_qkv, _ = ein("da_w_qkv", [(2 if self.fused else 1), D, 3 * D])
        self.da_lambda, _ = ein("da_lambda", [(2 if self.fused else 1), 4, 64])
        self.da_subln_w, _ = ein("da_subln_w", [(2 if self.fused else 1), 128])
        self.da_w_o, _ = ein("da_w_o", [(2 if self.fused else 1), D, D])
        self.dn_w_in, _ = ein("dn_w_in", [1, D, 12416])
        self.dn_conv_w, _ = ein("dn_conv_w", [1, 5, 8192])
        self.dn_a_log, _ = ein("dn_a_log", [1, 2, 32])
        self.dn_dt_bias, _ = ein("dn_dt_bias", [1, 2, 32])
        self.dn_norm_w, _ = ein("dn_norm_w", [1, 128])
        self.dn_w_o, _ = ein("dn_w_o", [1, 4096, D])
        self.wa_w_qkv, _ = ein("wa_w_qkv", [1, D, 2560])
        self.wa_sinks, _ = ein("wa_sinks", [1, 32])
        self.wa_w_o, _ = ein("wa_w_o", [1, D, D])
        self.moe_wg, _ = ein("moe_wg", [self.wl, D, 4])
        self.moe_bg, _ = ein("moe_bg", [self.wl, 4])
        self.moe_we, _ = ein("moe_we", [self.wl, D, 32])
        self.moe_be, _ = ein("moe_be", [self.wl, 32])
        self.moe_w13, _ = ein("moe_w13", [self.wl, 32, D, 1536])
        self.moe_w2, _ = ein("moe_w2", [self.wl, 32, 768, D])
        self.final_norm_w, _ = ein("final_norm_w", [1, D])
        self.kc = {}
        for name, arr in _consts().items():
            self.kc[name] = ein(name, list(arr.shape))[0]
        if self.final:
            self.y_out, self.y_out_k = c.dram("y_out", [L, D], F32, "ExternalOutput")
        else:
            self.x_out, self.x_out_k = c.dram("x_out", [NT, D], F32, "ExternalOutput")
        self.XR, _ = c.dram("XR", [NT, D], F32)
        self.XR_k = [Tk("xr%d" % t) for t in range(NTILE)]
        self.MOD, self.MOD_k = c.dram("MODS", [DEPTH, 2, 6 * D], F32)
        import os as _os
        dk = "ExternalOutput" if _os.environ.get("MK_DUMP") else "Internal"
        self.QT, self.QT_k = c.dram("QT", [16, 128, NT], BF16, dk)
        self.KT, self.KT_k = c.dram("KT", [16, 128, NT], BF16, dk)
        self.VV, self.VV_k = c.dram("VV", [NT, D], BF16, dk)
        if 1 in kinds and self.do_mixer:
            self.DNK, self.DNK_k = c.dram("DNK", [NT, 2048], BF16, dk)
            self.DNV, self.DNV_k = c.dram("DNV", [NT, 4096], BF16, dk)
            self.DNZ, self.DNZ_k = c.dram("DNZ", [NT, 4096], BF16, dk)
            self.ODN, self.ODN_k = c.dram("ODN", [2, NT, 4096], F32, dk)
        self.dbg_out = {}
        for name, shape in self.dbg:
            self.dbg_out[name] = c.dram(name, shape, F32, "ExternalOutput")

    def alloc(self):
        c = self.c
        self.BIGA, _ = c.sb([128, KC, NT], BF16, "BIGA")
        self.BIGA_k = [Tk("biga%d" % t) for t in range(NTILE)]
        self.PS = [c.ps([128, 512], F32, "PS%d" % i) for i in range(8)]
        self._psk = {id(k) for (_, k) in self.PS}
        self.ident_f, self.ident_f_k = c.sb([128, 128], F32, "ident_f")
        self.ident_b, self.ident_b_k = c.sb([128, 128], BF16, "ident_b")
        self.ones_f, self.ones_f_k = c.sb([128, 128], F32, "ones_f")
        self.ones_b, self.ones_b_k = c.sb([128, 128], BF16, "ones_b")
        self.rt_b, self.rt_b_k = c.sb([128, 128], BF16, "rt_b")
        self.WT = [c.sb([128, KC, 512], BF16, "WT%d" % i) for i in range(2)]
        self.wt_i = 0
        self.WB = [c.sb([128, D], F32, "WB%d" % i) for i in range(2)]
        self.SB = [c.sb([128, D], F32, "SB%d" % i) for i in range(2)]
        self.GB = self.WB
        self.XT = [c.sb([128, D], F32, "XT%d" % i) for i in range(2)]
        self.HB = [c.sb([128, D], BF16, "HB%d" % i) for i in range(2)]
        self.small = [c.sb([128, 8], F32, "small%d" % i) for i in range(2)]
        self.misc = c.sb([128, 64], F32, 'misc')
        self.GATE = c.sb([128, NTILE, 32], F32, 'GATE')
        c.arena_init(42 * 1024)

    def load_consts(self):
        for name, dst_f, dst_fk, dst_b, dst_bk in (
            ("k_ident", self.ident_f, self.ident_f_k, self.ident_b, self.ident_b_k),
            ("k_ones", self.ones_f, self.ones_f_k, self.ones_b, self.ones_b_k),
        ):
            self.dma("sp", dst_f[:], self.kc[name][:, :], w=[dst_fk])
            self.op("dve", lambda e, a=dst_b, b=dst_f: e.tensor_copy(out=a[:], in_=b[:]), r=[dst_fk], w=[dst_bk])
        self.dma("pool", self.rt_b[:], self.kc["k_rt"][:, :], w=[self.rt_b_k])
        for t in range(NTILE):
            self.dma("sp", self.XR[t * 128:(t + 1) * 128, :], self.x_in[t * 128:(t + 1) * 128, :],
                     w=[self.XR_k[t]])

    def phase_mod(self):
        c = self.c
        c.arena_reset()
        cs, cs_k = c.asb([128, KC, 2], F32, "cs")
        sig, sig_k = c.asb([128, KC, 2], F32, "sig")
        self.dma("sp", cs[:], self.cin[:, :, :], w=[cs_k])
        self.op("act", lambda e: e.activation(out=sig[:], in_=cs[:], func=AF.Sigmoid), r=[cs_k], w=[sig_k])
        self.op("dve", lambda e: e.tensor_tensor(out=cs[:], in0=cs[:], in1=sig[:], op=ALU.mult), r=[sig_k, cs_k], w=[cs_k])
        AW = [c.asb([128, KC, 256], F32, "AW%d" % i) for i in range(2)]
        bias, bias_k = self.XT[0][0][0:2, :], self.XT[0][1]
        nrm, nrm_k = self.XT[1][0][0:2, :], self.XT[1][1]
        res, res_k = self.SB[0][0][0:2, :], self.SB[0][1]
        ps, ps_k = self.PS[0]
        i = 0
        for l in range(len(self.layers)):
            for v in range(6):
                self.dma("sp", bias[:], self.ada_b[l:l + 1, v * D:(v + 1) * D].partition_broadcast(2), w=[bias_k])
                if v in (1, 4):
                    nw = self.norm_mix_w if v == 1 else self.norm_ffn_w
                    self.dma("sp", nrm[:], nw[l:l + 1, :].partition_broadcast(2), w=[nrm_k])
                for b in range(8):
                    aw, aw_k = AW[i % 2]
                    i += 1
                    col = v * D + b * 256
                    self.dma("sp", aw[:], self.ada_w[l, :, col:col + 256].rearrange("(kc p) n -> p kc n", p=128), w=[aw_k])
                    for kc in range(KC):
                        self.op("pe", lambda e, aw=aw, kc=kc: e.matmul(ps[0:2, 0:256], lhsT=cs[:, kc, :], rhs=aw[:, kc, :],
                                                                      start=(kc == 0), stop=(kc == KC - 1)),
                                r=[cs_k, aw_k], w=[ps_k])
                    self.op("dve", lambda e, b=b: e.tensor_tensor(out=res[:, b * 256:(b + 1) * 256], in0=ps[0:2, 0:256],
                                                                 in1=bias[:, b * 256:(b + 1) * 256], op=ALU.add),
                            r=[ps_k, bias_k], w=[res_k])
                if v in (1, 4):
                    self.op("dve", lambda e: e.scalar_tensor_tensor(out=res[:], in0=res[:], scalar=1.0, in1=nrm[:],
                                                                   op0=ALU.add, op1=ALU.mult),
                            r=[res_k, nrm_k], w=[res_k])
                self.dma("sp", self.MOD[l, :, v * D:(v + 1) * D], res[:], r=[res_k], w=[self.MOD_k])

    def load_mod_tiles(self, l, sub):
        for ty in range(2):
            for j, (buf, bk) in enumerate((self.SB[ty], self.WB[ty])):
                v = sub * 3 + j
                self.dma("sp", buf[:], self.MOD[l, ty:ty + 1, v * D:(v + 1) * D].partition_broadcast(128),
                         r=[self.MOD_k], w=[bk])

    def load_gate_tiles(self, l, sub):
        for ty in range(2):
            buf, bk = self.GB[ty]
            v = sub * 3 + 2
            self.dma("sp", buf[:], self.MOD[l, ty:ty + 1, v * D:(v + 1) * D].partition_broadcast(128),
                     r=[self.MOD_k], w=[bk])

    def phase_norm(self, hrow_dram=None, hrow_k=None, router=None):
        ps_t, ps_tk = self.PS[1]
        pst = ps_t[:].bitcast(BF16)
        for t in range(NTILE):
            ty = 1 if t < 2 else 0
            xt, xt_k = self.XT[t % 2]
            hb, hb_k = self.HB[t % 2]
            sm, sm_k = self.small[t % 2]
            self.dma("sp", xt[:], self.XR[t * 128:(t + 1) * 128, :], r=[self.XR_k[t]], w=[xt_k])
            self.op("act", lambda e, xt=xt, sm=sm, hb=hb: e.activation(out=hb[:], in_=xt[:], func=AF.Square, accum_out=sm[:, 0:1]),
                    r=[xt_k], w=[hb_k, sm_k])
            self.op("act", lambda e, sm=sm: e.activation(out=sm[:, 1:2], in_=sm[:, 0:1], func=AF.Sqrt, scale=1.0 / D, bias=EPS),
                    r=[sm_k], w=[sm_k])
            self.op("dve", lambda e, sm=sm: e.reciprocal(out=sm[:, 2:3], in_=sm[:, 1:2]), r=[sm_k], w=[sm_k])
            self.op("dve", lambda e, xt=xt, sm=sm, ty=ty: e.scalar_tensor_tensor(
                out=xt[:], in0=xt[:], scalar=sm[:, 2:3], in1=self.WB[ty][0][:], op0=ALU.mult, op1=ALU.mult),
                r=[xt_k, sm_k, self.WB[ty][1]], w=[xt_k])
            if router is None:
                self.op("pool", lambda e, xt=xt, hb=hb, ty=ty: e.tensor_tensor(out=hb[:], in0=xt[:], in1=self.SB[ty][0][:], op=ALU.add),
                        r=[xt_k, self.SB[ty][1]], w=[hb_k])
            else:
                self.op("pool", lambda e, xt=xt, ty=ty: e.tensor_tensor(out=xt[:], in0=xt[:], in1=self.SB[ty][0][:], op=ALU.add),
                        r=[xt_k, self.SB[ty][1]], w=[xt_k])
                self.op("act", lambda e, xt=xt, hb=hb: e.copy(out=hb[:], in_=xt[:]), r=[xt_k], w=[hb_k])
                router(t, xt, xt_k)
            if hrow_dram is not None:
                self.dma("sp", hrow_dram[t * 128:(t + 1) * 128, :], hb[:], r=[hb_k], w=[hrow_k[t]])
            for g in range(2):
                for j in range(8):
                    kc = g * 8 + j
                    self.op("pe", lambda e, hb=hb, kc=kc, j=j: e.transpose(out=pst[:, j * 128:(j + 1) * 128],
                                                                          in_=hb[:, kc * 128:(kc + 1) * 128], identity=self.ident_b[:]),
                            r=[hb_k, self.ident_b_k], w=[ps_tk])
                eng = "act" if g == 0 else "dve"
                if eng == "act":
                    self.op("act", lambda e, g=g, t=t: e.copy(out=self.BIGA[:, g * 8:(g + 1) * 8, t * 128:(t + 1) * 128],
                                                             in_=pst.rearrange("p (a b) -> p a b", a=8)),
                            r=[ps_tk], w=[self.BIGA_k[t]])
                else:
                    self.op("dve", lambda e, g=g, t=t: e.tensor_copy(out=self.BIGA[:, g * 8:(g + 1) * 8, t * 128:(t + 1) * 128],
                                                                    in_=pst.rearrange("p (a b) -> p a b", a=8)),
                            r=[ps_tk], w=[self.BIGA_k[t]])

    def load_w(self, w_ap_rows_cols, ncols, nk=KC):
        wt, wt_k = self.WT[self.wt_i % 2]
        self.wt_i += 1
        self.dma("pool", wt[:, 0:nk, 0:ncols], w_ap_rows_cols.rearrange("(kc p) n -> p kc n", p=128), w=[wt_k])
        return wt, wt_k

    TOKBLK = [(0, 256), (256, 512), (768, 512), (1280, 512), (1792, 512)]

    def linear_fm(self, w2d, col0, ncols, post):
        psi = 0
        for cb in range(0, ncols, 512):
            nb = min(512, ncols - cb)
            wt, wt_k = self.load_w(w2d[:, col0 + cb:col0 + cb + nb], nb)
            for jj in range(nb // 128):
                j = (cb // 128) + jj
                for bi, (t0, n) in enumerate(self.TOKBLK):
                    ps, ps_k = self.PS[2 + (psi % 2)]
                    psi += 1
                    rk = [self.BIGA_k[t] for t in range(t0 // 128, (t0 + n) // 128)] + [wt_k]
                    for kc in range(KC):
                        self.op("pe", lambda e, ps=ps, wt=wt, kc=kc, jj=jj, t0=t0, n=n: e.matmul(
                            ps[:, 0:n], lhsT=wt[:, kc, jj * 128:(jj + 1) * 128], rhs=self.BIGA[:, kc, t0:t0 + n],
                            start=(kc == 0), stop=(kc == KC - 1)), r=rk, w=[ps_k])
                    post(j, bi, t0, n, ps, ps_k)

    def linear_tm(self, w2d, col0, ncols, post, src=None, src_k=None, nk=KC):
        src = self.BIGA if src is None else src
        src_k = self.BIGA_k if src_k is None else src_k
        psi = 0
        for cb in range(0, ncols, 512):
            nb = min(512, ncols - cb)
            wt, wt_k = self.load_w(w2d[:, col0 + cb:col0 + cb + nb], nb, nk)
            for t in range(NTILE):
                ps, ps_k = self.PS[2 + (psi % 2)]
                psi += 1
                for kc in range(nk):
                    self.op("pe", lambda e, ps=ps, wt=wt, kc=kc, t=t, nb=nb: e.matmul(
                        ps[:, 0:nb], lhsT=src[:, kc, t * 128:(t + 1) * 128], rhs=wt[:, kc, 0:nb],
                        start=(kc == 0), stop=(kc == nk - 1)), r=[src_k[t], wt_k], w=[ps_k])
                post(cb, nb, t, ps, ps_k)

    def phase_oproj_residual(self, w2d, l):
        c = self.c
        self.load_gate_tiles(l, 0)
        c.arena_reset()
        self.RX = [c.asb([128, 512], F32, "RX%d" % i) for i in range(3)]
        self.rxi = 0

        def post(cb, nb, t, ps, ps_k):
            ty = 1 if t < 2 else 0
            rx, rx_k = self.RX[self.rxi % 3]
            self.rxi += 1
            self.dma("sp", rx[:], self.XR[t * 128:(t + 1) * 128, cb:cb + nb], r=[self.XR_k[t]], w=[rx_k])
            self.op("dve", lambda e, rx=rx, ps=ps, ty=ty, cb=cb, nb=nb: e.tensor_tensor(
                out=ps[:, 0:nb], in0=ps[:, 0:nb], in1=self.GB[ty][0][:, cb:cb + nb], op=ALU.mult),
                r=[ps_k, self.GB[ty][1]], w=[ps_k])
            self.op("dve", lambda e, rx=rx, ps=ps, nb=nb: e.tensor_tensor(out=rx[:, 0:nb], in0=ps[:, 0:nb], in1=rx[:, 0:nb], op=ALU.add),
                    r=[ps_k, rx_k], w=[rx_k])
            self.dma("sp", self.XR[t * 128:(t + 1) * 128, cb:cb + nb], rx[:, 0:nb], r=[rx_k], w=[self.XR_k[t]])

        self.linear_tm(w2d, 0, D, post)

    def load_rope_tables(self):
        c = self.c
        self.cosT, self.cosT_k = c.asb([128, L], BF16, "cosT")
        self.sinT, self.sinT_k = c.asb([128, L], BF16, "sinT")
        self.dma("pool", self.cosT[:], self.kc["k_cos"][:, :], w=[self.cosT_k])
        self.dma("pool", self.sinT[:], self.kc["k_sin"][:, :], w=[self.sinT_k])
        self.RP = [(c.asb([128, 512], BF16, "RPa%d" % i), c.asb([128, 512], BF16, "RPb%d" % i)) for i in range(2)]
        self.rpi = 0

    def rope_store(self, ps, ps_k, t0, n, dst_dram, dst_k):
        c = self.c
        (qa, qa_k), (qb, qb_k) = self.RP[self.rpi % 2]
        self.rpi += 1
        self.op("act", lambda e: e.copy(out=qa[:, 0:n], in_=ps[:, 0:n]), r=[ps_k], w=[qa_k])
        if t0 >= LC:
            l0 = t0 - LC
            pr, pr_k = self.PS[4]
            self.op("pe", lambda e: e.matmul(pr[:, 0:n], lhsT=self.rt_b[:], rhs=qa[:, 0:n], start=True, stop=True),
                    r=[qa_k, self.rt_b_k], w=[pr_k])
            self.op("dve", lambda e: e.tensor_tensor(out=qb[:, 0:n], in0=pr[:, 0:n], in1=self.sinT[:, l0:l0 + n], op=ALU.mult),
                    r=[pr_k, self.sinT_k], w=[qb_k])
            self.op("pool", lambda e: e.tensor_tensor(out=qa[:, 0:n], in0=qa[:, 0:n], in1=self.cosT[:, l0:l0 + n], op=ALU.mult),
                    r=[qa_k, self.cosT_k], w=[qa_k])
            self.op("pool", lambda e: e.tensor_tensor(out=qa[:, 0:n], in0=qa[:, 0:n], in1=qb[:, 0:n], op=ALU.add),
                    r=[qa_k, qb_k], w=[qa_k])
        self.dma("sp", dst_dram[:, t0:t0 + n], qa[:, 0:n], r=[qa_k], w=[dst_k])

    def mixer_diff(self, l, j):
        c = self.c
        lam_init = 0.8 - 0.6 * math.exp(-0.3 * l)
        wqkv = self.da_w_qkv[j]
        c.arena_reset()
        self.load_rope_tables()
        self.VS = [c.asb([128, 512], BF16, "VS%d" % i) for i in range(2)]
        self.vsi = 0
        self.lamt = c.asb([1, 4, 64], F32, "lamt")
        self.lamp = c.asb([1, 8], F32, "lamp")
        self.lamc = (self.misc[0][:, 0:2], Tk("lamc"))
        self.subw = (self.misc[0][:, 2:4], Tk("subw"))
        self.linear_fm(wqkv, 0, D, lambda jj, bi, t0, n, ps, ps_k: self.rope_store(ps, ps_k, t0, n, self.QT[jj], self.QT_k))
        self.linear_fm(wqkv, D, D, lambda jj, bi, t0, n, ps, ps_k: self.rope_store(ps, ps_k, t0, n, self.KT[jj], self.KT_k))

        def vpost(cb, nb, t, ps, ps_k):
            vs, vs_k = self.VS[self.vsi % 2]
            self.vsi += 1
            self.op("act", lambda e: e.copy(out=vs[:, 0:nb], in_=ps[:, 0:nb]), r=[ps_k], w=[vs_k])
            self.dma("sp", self.VV[t * 128:(t + 1) * 128, cb:cb + nb], vs[:, 0:nb], r=[vs_k], w=[self.VV_k])

        self.linear_tm(wqkv, 2 * D, D, vpost)
        lamt, lamt_k = self.lamt
        lamp, lamp_k = self.lamp
        lamc, lamc_k = self.lamc
        subw, subw_k = self.subw
        self.dma("sp", lamt[:], self.da_lambda[j:j + 1, :, :], w=[lamt_k])
        self.op("dve", lambda e: e.tensor_tensor(out=lamt[:, 0, :], in0=lamt[:, 0, :], in1=lamt[:, 1, :], op=ALU.mult), r=[lamt_k], w=[lamt_k])
        self.op("dve", lambda e: e.tensor_tensor(out=lamt[:, 2, :], in0=lamt[:, 2, :], in1=lamt[:, 3, :], op=ALU.mult), r=[lamt_k], w=[lamt_k])
        self.op("dve", lambda e: e.reduce_sum(out=lamp[:, 0:1], in_=lamt[:, 0, :], axis=AX.X), r=[lamt_k], w=[lamp_k])
        self.op("dve", lambda e: e.reduce_sum(out=lamp[:, 1:2], in_=lamt[:, 2, :], axis=AX.X), r=[lamt_k], w=[lamp_k])
        self.op("act", lambda e: e.activation(out=lamp[:, 2:4], in_=lamp[:, 0:2], func=AF.Exp), r=[lamp_k], w=[lamp_k])
        self.op("dve", lambda e: e.scalar_tensor_tensor(out=lamp[:, 4:5], in0=lamp[:, 3:4], scalar=-lam_init, in1=lamp[:, 2:3],
                                                       op0=ALU.add, op1=ALU.subtract), r=[lamp_k], w=[lamp_k])
        psl, psl_k = self.PS[4]
        self.op("pe", lambda e: e.matmul(psl[:, 0:1], lhsT=self.ones_f[0:1, :], rhs=lamp[0:1, 4:5], start=True, stop=True),
                r=[self.ones_f_k, lamp_k], w=[psl_k])
        self.op("dve", lambda e: e.tensor_copy(out=lamc[:, 0:1], in_=psl[:, 0:1]), r=[psl_k], w=[lamc_k])
        self.dma("sp", subw[:, 0:1], self.da_subln_w[j:j + 1, :].rearrange("o d -> d o"), w=[subw_k])
        self.op("dve", lambda e: e.tensor_scalar(out=subw[:, 1:2], in0=subw[:, 0:1], scalar1=(1.0 - lam_init), scalar2=None, op0=ALU.mult),
                r=[subw_k], w=[subw_k])
        self.attention_core(16, diff=True)

    def attention_core(self, nheads, diff):
        c = self.c
        c.arena_reset()
        self.AQ = [c.asb([128, NT], BF16, "AQ%d" % i) for i in range(2)]
        self.AK = [c.asb([128, NT], BF16, "AK%d" % i) for i in range(2)]
        self.AV = [c.asb([128, NTILE, 128], BF16, "AV%d" % i) for i in range(2)]
        self.ET = [c.asb([128, 512], BF16, "ET%d" % i) for i in range(4)]
        self.eti = 0
        self.CMB = [c.asb([128, 512], F32, "CMB%d" % i) for i in range(4)]
        self.SQB = c.asb([128, 512], BF16, "SQB")
        scale = 64 ** -0.5
        lamc, lamc_k = self.lamc
        subw, subw_k = self.subw
        for h in range(nheads):
            aq, aq_k = self.AQ[h % 2]
            ak, ak_k = self.AK[h % 2]
            av, av_k = self.AV[h % 2]
            self.dma("sp", aq[:], self.QT[h], r=[self.QT_k], w=[aq_k])
            self.dma("sp", ak[:], self.KT[h], r=[self.KT_k], w=[ak_k])
            self.dma("sp", av[:], self.VV[:, h * 128:(h + 1) * 128].rearrange("(t p) d -> p t d", p=128), r=[self.VV_k], w=[av_k])
            for bi, (t0, n) in enumerate(self.TOKBLK):
                nkt = 2 if bi == 0 else NTILE
                acc = []
                for comp in range(2):
                    po, po_k = self.PS[4 + comp]
                    pz, pz_k = self.PS[6 + comp]
                    p0 = comp * 64
                    for kt in range(nkt):
                        pss, pss_k = self.PS[kt % 2]
                        et, et_k = self.ET[self.eti % 4]
                        self.eti += 1
                        self.op("pe", lambda e, pss=pss, ak=ak, aq=aq, kt=kt, p0=p0, t0=t0, n=n: e.matmul(
                            pss[:, 0:n], lhsT=ak[p0:p0 + 64, kt * 128:(kt + 1) * 128], rhs=aq[p0:p0 + 64, t0:t0 + n],
                            start=True, stop=True), r=[ak_k, aq_k], w=[pss_k])
                        self.op("act", lambda e, pss=pss, et=et, n=n: e.activation(out=et[:, 0:n], in_=pss[:, 0:n], func=AF.Exp, scale=scale),
                                r=[pss_k], w=[et_k])
                        self.op("pe", lambda e, po=po, av=av, et=et, kt=kt, n=n, nkt=nkt: e.matmul(
                            po[:, 0:n], lhsT=av[:, kt, :], rhs=et[:, 0:n], start=(kt == 0), stop=(kt == nkt - 1)),
                            r=[av_k, et_k], w=[po_k])
                        self.op("pe", lambda e, pz=pz, et=et, kt=kt, n=n, nkt=nkt: e.matmul(
                            pz[:, 0:n], lhsT=self.ones_b[:], rhs=et[:, 0:n], start=(kt == 0), stop=(kt == nkt - 1)),
                            r=[self.ones_b_k, et_k], w=[pz_k])
                    acc.append((po, po_k, pz, pz_k))
                (r0, r0_k), (r1, r1_k), (o0, o0_k), (o1, o1_k) = self.CMB
                (po0, po0_k, pz0, pz0_k), (po1, po1_k, pz1, pz1_k) = acc
                self.op("dve", lambda e, n=n: e.reciprocal(out=r0[:, 0:n], in_=pz0[:, 0:n]), r=[pz0_k], w=[r0_k])
                self.op("dve", lambda e, n=n: e.reciprocal(out=r1[:, 0:n], in_=pz1[:, 0:n]), r=[pz1_k], w=[r1_k])
                self.op("dve", lambda e, n=n: e.tensor_tensor(out=o0[:, 0:n], in0=po0[:, 0:n], in1=r0[:, 0:n], op=ALU.mult), r=[po0_k, r0_k], w=[o0_k])
                self.op("dve", lambda e, n=n: e.tensor_tensor(out=o1[:, 0:n], in0=po1[:, 0:n], in1=r1[:, 0:n], op=ALU.mult), r=[po1_k, r1_k], w=[o1_k])
                self.op("dve", lambda e, n=n: e.scalar_tensor_tensor(out=o0[:, 0:n], in0=o1[:, 0:n], scalar=lamc[:, 0:1], in1=o0[:, 0:n],
                                                                    op0=ALU.mult, op1=ALU.add), r=[o0_k, o1_k, lamc_k], w=[o0_k])
                sqb, sqb_k = self.SQB
                self.op("act", lambda e, n=n: e.activation(out=sqb[:, 0:n], in_=o0[:, 0:n], func=AF.Square), r=[o0_k], w=[sqb_k])
                pss, pss_k = self.PS[0]
                self.op("pe", lambda e, n=n, pss=pss: e.matmul(pss[:, 0:n], lhsT=self.ones_b[:], rhs=sqb[:, 0:n], start=True, stop=True),
                        r=[sqb_k, self.ones_b_k], w=[pss_k])
                self.op("act", lambda e, n=n, pss=pss: e.activation(out=r0[:, 0:n], in_=pss[:, 0:n], func=AF.Sqrt, scale=1.0 / 128, bias=EPS),
                        r=[pss_k], w=[r0_k])
                self.op("dve", lambda e, n=n: e.reciprocal(out=r0[:, 0:n], in_=r0[:, 0:n]), r=[r0_k], w=[r0_k])
                wk = [self.BIGA_k[t] for t in range(t0 // 128, (t0 + n) // 128)]
                self.op("dve", lambda e, n=n, t0=t0, h=h: e.scalar_tensor_tensor(
                    out=self.BIGA[:, h, t0:t0 + n], in0=o0[:, 0:n], scalar=subw[:, 1:2], in1=r0[:, 0:n], op0=ALU.mult, op1=ALU.mult),
                    r=[o0_k, r0_k, subw_k], w=wk)

    def phase_final(self):
        fw, fw_k = self.WB[0]
        self.dma("sp", fw[:], self.final_norm_w[0:1, :].partition_broadcast(128), w=[fw_k])
        for t in range(2, NTILE):
            xt, xt_k = self.XT[t % 2]
            sm, sm_k = self.small[t % 2]
            self.dma("sp", xt[:], self.XR[t * 128:(t + 1) * 128, :], r=[self.XR_k[t]], w=[xt_k])
            hb, hb_k = self.HB[t % 2]
            self.op("act", lambda e, xt=xt, sm=sm, hb=hb: e.activation(out=hb[:], in_=xt[:], func=AF.Square, accum_out=sm[:, 0:1]),
                    r=[xt_k], w=[hb_k, sm_k])
            self.op("act", lambda e, sm=sm: e.activation(out=sm[:, 1:2], in_=sm[:, 0:1], func=AF.Sqrt, scale=1.0 / D, bias=EPS),
                    r=[sm_k], w=[sm_k])
            self.op("dve", lambda e, sm=sm: e.reciprocal(out=sm[:, 2:3], in_=sm[:, 1:2]), r=[sm_k], w=[sm_k])
            self.op("dve", lambda e, xt=xt, sm=sm: e.scalar_tensor_tensor(
                out=xt[:], in0=xt[:], scalar=sm[:, 2:3], in1=fw[:], op0=ALU.mult, op1=ALU.mult),
                r=[xt_k, sm_k, fw_k], w=[xt_k])
            self.out_ops.append(self.dma("sp", self.y_out[(t - 2) * 128:(t - 1) * 128, :], xt[:], r=[xt_k], w=[self.y_out_k]))

    def build(self):
        self.declare_io()
        self.alloc()
        self.load_consts()
        self.phase_mod()
        for li, labs in enumerate(self.layers):
            kind = [0, 1, 2, 0][labs]
            ji = ([0, 0, 0, 1][labs]) if self.fused else 0
            if self.do_mixer:
                self.load_mod_tiles(li, 0)
                self.phase_norm()
                if kind == 0:
                    self.mixer_diff(labs, ji)
                    self.phase_oproj_residual(self.da_w_o[ji], li)
                elif kind == 1:
                    self.cur_li = li
                    self.mixer_dn()
                else:
                    self.mixer_win()
                    self.phase_oproj_residual(self.wa_w_o[0], li)
            if self.do_moe:
                self.load_mod_tiles(li, 1)
                self.phase_moe(li)
        if self.final:
            self.phase_final()
        else:
            for t in range(NTILE):
                self.out_ops.append(self.dma("sp", self.x_out[t * 128:(t + 1) * 128, :], self.XR[t * 128:(t + 1) * 128, :],
                                             r=[self.XR_k[t]], w=[self.x_out_k]))
        self.p.emit(self.out_ops)


def build_nc(**kw):
    nc = bass.Bass("TRN2", target_bir_lowering=False)
    with ExitStack() as es:
        b = Builder(nc, es, **kw)
        b.build()
    return nc, b


def _launch(inputs, xcur, layer, do_mixer, do_moe, final):
    nc, bld = build_nc(layer_abs=layer, do_mixer=do_mixer, do_moe=do_moe, final=final)
    print('nops', len(bld.p.ops), flush=True)
    j = [0, 0, 0, 1][layer]
    per_layer = {"ada_w": inputs["ada_w"][layer:layer + 1], "ada_b": inputs["ada_b"][layer:layer + 1],
                 "norm_mix_w": inputs["norm_mix_w"][layer:layer + 1], "norm_ffn_w": inputs["norm_ffn_w"][layer:layer + 1],
                 "da_w_qkv": inputs["da_w_qkv"][j:j + 1], "da_lambda": inputs["da_lambda"][j:j + 1],
                 "da_subln_w": inputs["da_subln_w"][j:j + 1], "da_w_o": inputs["da_w_o"][j:j + 1],
                 "dn_w_in": inputs["dn_w_in"], "dn_conv_w": inputs["dn_conv_w"].reshape(1, 5, 8192),
                 "dn_a_log": inputs["dn_a_log"], "dn_dt_bias": inputs["dn_dt_bias"], "dn_norm_w": inputs["dn_norm_w"],
                 "dn_w_o": inputs["dn_w_o"],
                 "wa_w_qkv": inputs["wa_w_qkv"], "wa_sinks": inputs["wa_sinks"], "wa_w_o": inputs["wa_w_o"],
                 "moe_wg": inputs["moe_wg"][layer:layer + 1], "moe_bg": inputs["moe_bg"][layer:layer + 1],
                 "moe_we": inputs["moe_we"][layer:layer + 1], "moe_be": inputs["moe_be"][layer:layer + 1],
                 "moe_w13": inputs["moe_w13"][layer:layer + 1], "moe_w2": inputs["moe_w2"][layer:layer + 1],
                 "final_norm_w": inputs["final_norm_w"].reshape(1, D)}
    consts = _consts()
    dummy = np.zeros((1, 1), np.float32)
    in_maps = []
    for b in range(len(xcur)):
        m = {"x_in": xcur[b]}
        cin = np.stack([inputs["c"][b], inputs["c_ctx"]], -1)
        m["cin"] = np.ascontiguousarray(cin.reshape(KC, 128, 2).transpose(1, 0, 2))
        for name, shape in bld.in_shapes.items():
            if name in m:
                continue
            if name not in bld.used:
                m[name] = dummy
            elif name in consts:
                m[name] = consts[name]
            else:
                m[name] = np.ascontiguousarray(per_layer[name])
        in_maps.append(m)
    res = run_bass_kernel_spmd(nc, in_maps, core_ids=list(range(len(xcur))))
    key = "y_out" if final else "x_out"
    global _last_res
    _last_res = res.results
    return [r[key] for r in res.results]


def kernel(**inputs):
    inputs = {k: np.asarray(v) for k, v in inputs.items()}
    nb = inputs["x"].shape[0]
    nc, bld = build_nc(fused=True)
    consts = _consts()
    shared = {k: np.ascontiguousarray(inputs[k]) for k in (
        "ada_w", "ada_b", "norm_mix_w", "norm_ffn_w", "da_w_qkv", "da_lambda", "da_subln_w", "da_w_o",
        "dn_w_in", "dn_conv_w", "dn_a_log", "dn_dt_bias", "dn_norm_w", "dn_w_o", "wa_w_qkv", "wa_sinks", "wa_w_o",
        "moe_wg", "moe_bg", "moe_we", "moe_be", "moe_w13", "moe_w2")}
    shared["final_norm_w"] = inputs["final_norm_w"].reshape(1, D)
    shared.update(consts)
    in_maps = []
    for b in range(nb):
        m = dict(shared)
        m["x_in"] = np.ascontiguousarray(np.concatenate([inputs["ctx"][b], inputs["x"][b]], 0))
        cin = np.stack([inputs["c"][b], inputs["c_ctx"]], -1)
        m["cin"] = np.ascontiguousarray(cin.reshape(KC, 128, 2).transpose(1, 0, 2))
        in_maps.append(m)
    res = run_bass_kernel_spmd(nc, in_maps, core_ids=list(range(nb)))
    return np.stack([r["y_out"] for r in res.results], 0).astype(np.float32)


def _phase_moe(self, l):
    c = self.c
    c.arena_reset()
    gate, gate_k = self.GATE
    ht32, ht32_k = c.asb([128, KC, 128], F32, "ht32")
    wr, wr_k = c.asb([128, KC, 36], F32, "wr")
    br, br_k = c.asb([128, 36], F32, "br")
    lg, lg_k = c.asb([128, 36], F32, "lg")
    rs_, rs_k = c.asb([128, 64], F32, "rsm")
    self.dma("sp", wr[:, :, 0:4], self.moe_wg[l].rearrange("(kc p) n -> p kc n", p=128), w=[wr_k])
    self.dma("sp", wr[:, :, 4:36], self.moe_we[l].rearrange("(kc p) n -> p kc n", p=128), w=[wr_k])
    self.dma("sp", br[:, 0:4], self.moe_bg[l:l + 1, :].partition_broadcast(128), w=[br_k])
    self.dma("sp", br[:, 4:36], self.moe_be[l:l + 1, :].partition_broadcast(128), w=[br_k])
    R = lambda a, b=None: rs_[:, a:(a + 1 if b is None else b)]

    def router(t, xt, xt_k):
        for g in range(4):
            pt, pt_k = self.PS[2 + (g % 2)]
            for jx in range(4):
                kc = g * 4 + jx
                self.op("pe", lambda e, pt=pt, jx=jx, kc=kc, xt=xt: e.transpose(out=pt[:, jx * 128:(jx + 1) * 128],
                                                                              in_=xt[:, kc * 128:(kc + 1) * 128], identity=self.ident_f[:]),
                        r=[xt_k, self.ident_f_k], w=[pt_k])
            self.op("dve", lambda e, pt=pt, g=g: e.tensor_copy(out=ht32[:, g * 4:(g + 1) * 4, :], in_=pt[:].rearrange("p (a b) -> p a b", a=4)),
                    r=[pt_k], w=[ht32_k])
        pl, pl_k = self.PS[4]
        for kc in range(KC):
            self.op("pe", lambda e, kc=kc: e.matmul(pl[:, 0:36], lhsT=ht32[:, kc, :], rhs=wr[:, kc, :], start=(kc == 0), stop=(kc == KC - 1)),
                    r=[ht32_k, wr_k], w=[pl_k])
        V = lambda fn, r=(), w=(): self.op("dve", fn, r=list(r) + [rs_k], w=list(w) + [rs_k])
        self.op("dve", lambda e: e.tensor_tensor(out=lg[:], in0=pl[:, 0:36], in1=br[:], op=ALU.add), r=[pl_k, br_k], w=[lg_k])
        V(lambda e: e.reduce_max(out=R(0), in_=lg[:, 0:4], axis=AX.X), r=[lg_k])
        V(lambda e: e.tensor_scalar(out=R(1), in0=R(0), scalar1=-1.0, scalar2=None, op0=ALU.mult))
        self.op("act", lambda e: e.activation(out=R(56, 60), in_=lg[:, 0:4], func=AF.Exp, bias=R(1), accum_out=R(2)), r=[lg_k, rs_k], w=[rs_k])
        V(lambda e: e.reciprocal(out=R(3), in_=R(2)))
        V(lambda e: e.tensor_scalar(out=R(4, 8), in0=lg[:, 0:4], scalar1=R(0), scalar2=None, op0=ALU.is_equal), r=[lg_k])
        V(lambda e: e.tensor_scalar(out=R(8, 16), in0=lg[:, 4:12], scalar1=R(4), scalar2=None, op0=ALU.mult), r=[lg_k])
        for g in range(1, 4):
            V(lambda e, g=g: e.scalar_tensor_tensor(out=R(8, 16), in0=lg[:, 4 + 8 * g:12 + 8 * g], scalar=R(4 + g), in1=R(8, 16),
                                                   op0=ALU.mult, op1=ALU.add), r=[lg_k])
        V(lambda e: e.reduce_max(out=R(40), in_=R(8, 16), axis=AX.X))
        V(lambda e: e.tensor_scalar(out=R(16, 24), in0=R(8, 16), scalar1=R(40), scalar2=None, op0=ALU.is_equal))
        V(lambda e: e.scalar_tensor_tensor(out=R(24, 32), in0=R(16, 24), scalar=-1e30, in1=R(8, 16), op0=ALU.mult, op1=ALU.add))
        V(lambda e: e.reduce_max(out=R(41), in_=R(24, 32), axis=AX.X))
        V(lambda e: e.tensor_scalar(out=R(32, 40), in0=R(24, 32), scalar1=R(41), scalar2=None, op0=ALU.is_equal))
        V(lambda e: e.tensor_tensor(out=R(42), in0=R(41), in1=R(40), op=ALU.subtract))
        self.op("act", lambda e: e.activation(out=R(43), in_=R(42), func=AF.Exp), r=[rs_k], w=[rs_k])
        V(lambda e: e.tensor_scalar(out=R(44), in0=R(43), scalar1=1.0, scalar2=None, op0=ALU.add))
        V(lambda e: e.reciprocal(out=R(44), in_=R(44)))
        V(lambda e: e.tensor_tensor(out=R(45), in0=R(44), in1=R(3), op=ALU.mult))
        V(lambda e: e.tensor_tensor(out=R(46), in0=R(45), in1=R(43), op=ALU.mult))
        V(lambda e: e.tensor_scalar(out=R(48, 56), in0=R(16, 24), scalar1=R(45), scalar2=None, op0=ALU.mult))
        V(lambda e: e.scalar_tensor_tensor(out=R(48, 56), in0=R(32, 40), scalar=R(46), in1=R(48, 56), op0=ALU.mult, op1=ALU.add))
        for g in range(4):
            self.op("dve", lambda e, g=g, t=t: e.tensor_scalar(out=gate[:, t, g * 8:(g + 1) * 8], in0=R(48, 56), scalar1=R(4 + g),
                                                              scalar2=None, op0=ALU.mult), r=[rs_k], w=[gate_k])

    self.phase_norm(router=router)
    self.load_gate_tiles(l, 1)
    c.arena_reset()
    actt, _ = c.asb([128, 6, NT], BF16, "actt")
    actt_k = [Tk("actt%d" % i) for i in range(5)]
    ysb = [c.asb([128, 512], F32, "ysb%d" % i) for i in range(3)]
    if not hasattr(self, "YACC"):
        self.YACC, _ = c.dram("YACC", [NT, D], F32)
        self.YACC_k = [Tk("yacc%d" % t) for t in range(NTILE)]
    zt, zt_k = self.XT[0]
    self.op("pool", lambda e: e.memset(zt[:], 0.0), w=[zt_k])
    for t in range(NTILE):
        self.dma("sp", self.YACC[t * 128:(t + 1) * 128, :], zt[:], r=[zt_k], w=[self.YACC_k[t]])
    st = {"i": 0}
    blk_of_tile = {}
    for bi, (t0, n) in enumerate(self.TOKBLK):
        for t in range(t0 // 128, (t0 + n) // 128):
            blk_of_tile[t] = bi
    for ex in range(32):
        def post13(j, bi, t0, n, ps, ps_k):
            if j < 6:
                self.op("act", lambda e, j=j, t0=t0, n=n, ps=ps: e.activation(out=actt[:, j, t0:t0 + n], in_=ps[:, 0:n], func=AF.Silu),
                        r=[ps_k], w=[actt_k[bi]])
            else:
                self.op("dve", lambda e, j=j, t0=t0, n=n, ps=ps: e.tensor_tensor(out=actt[:, j - 6, t0:t0 + n], in0=ps[:, 0:n],
                                                                               in1=actt[:, j - 6, t0:t0 + n], op=ALU.mult),
                        r=[ps_k, actt_k[bi]], w=[actt_k[bi]])

        def post2(cb, nb, t, ps, ps_k, ex=ex):
            yb, yb_k = ysb[st["i"] % 3]
            st["i"] += 1
            self.op("dve", lambda e, yb=yb, ps=ps, t=t: e.tensor_scalar(out=yb[:, 0:nb], in0=ps[:, 0:nb], scalar1=gate[:, t, ex:ex + 1],
                                                                       scalar2=None, op0=ALU.mult), r=[ps_k, gate_k], w=[yb_k])
            self.p.op("pool", lambda e, yb=yb, t=t, cb=cb: e.dma_start(out=self.YACC[t * 128:(t + 1) * 128, cb:cb + nb], in_=yb[:, 0:nb],
                                                                       accum_op=ALU.add), r=[yb_k], w=[self.YACC_k[t]], dma=True)

        self.linear_fm(self.moe_w13[l, ex], 0, 1536, post13)
        self.linear_tm(self.moe_w2[l, ex], 0, D, post2, src=actt, src_k=[actt_k[blk_of_tile[t]] for t in range(NTILE)], nk=6)
    for t in range(NTILE):
        ty = 1 if t < 2 else 0
        ya, ya_k = self.XT[t % 2]
        xa, xa_k = self.SB[t % 2]
        self.dma("sp", ya[:], self.YACC[t * 128:(t + 1) * 128, :], r=[self.YACC_k[t]], w=[ya_k])
        self.dma("sp", xa[:], self.XR[t * 128:(t + 1) * 128, :], r=[self.XR_k[t]], w=[xa_k])
        self.op("pool", lambda e, ya=ya, ty=ty: e.tensor_tensor(out=ya[:], in0=ya[:], in1=self.GB[ty][0][:], op=ALU.mult),
                r=[ya_k, self.GB[ty][1]], w=[ya_k])
        self.op("dve", lambda e, ya=ya, xa=xa: e.tensor_tensor(out=xa[:], in0=xa[:], in1=ya[:], op=ALU.add), r=[ya_k, xa_k], w=[xa_k])
        self.dma("sp", self.XR[t * 128:(t + 1) * 128, :], xa[:], r=[xa_k], w=[self.XR_k[t]])


Builder.phase_moe = _phase_moe


def _mixer_win(self):
    c = self.c
    w = self.wa_w_qkv[0]
    c.arena_reset()
    self.load_rope_tables()
    self.VS = [c.asb([128, 512], BF16, "VS%d" % i) for i in range(2)]
    self.vsi = 0
    self.linear_fm(w, 0, D, lambda jj, bi, t0, n, ps, ps_k: self.rope_store(ps, ps_k, t0, n, self.QT[jj], self.QT_k))
    wt, wt_k = self.WT[self.wt_i % 2]
    self.wt_i += 1
    for kvh in range(4):
        for half in range(2):
            self.dma("pool", wt[:, :, (2 * kvh + half) * 64:(2 * kvh + half + 1) * 64],
                     w[:, D + kvh * 64:D + (kvh + 1) * 64].rearrange("(kc p) n -> p kc n", p=128), w=[wt_k])
    psi = 0
    for kvh in range(4):
        for bi, (t0, n) in enumerate(self.TOKBLK):
            ps, ps_k = self.PS[2 + (psi % 2)]
            psi += 1
            rk = [self.BIGA_k[t] for t in range(t0 // 128, (t0 + n) // 128)] + [wt_k]
            for kc in range(KC):
                self.op("pe", lambda e, ps=ps, kc=kc, kvh=kvh, t0=t0, n=n: e.matmul(
                    ps[:, 0:n], lhsT=wt[:, kc, kvh * 128:(kvh + 1) * 128], rhs=self.BIGA[:, kc, t0:t0 + n],
                    start=(kc == 0), stop=(kc == KC - 1)), r=rk, w=[ps_k])
            self.rope_store(ps, ps_k, t0, n, self.KT[kvh], self.KT_k)

    def vpost(cb, nb, t, ps, ps_k):
        vs, vs_k = self.VS[self.vsi % 2]
        self.vsi += 1
        self.op("act", lambda e: e.copy(out=vs[:, 0:nb], in_=ps[:, 0:nb]), r=[ps_k], w=[vs_k])
        self.dma("sp", self.VV[t * 128:(t + 1) * 128, cb:cb + nb], vs[:, 0:nb], r=[vs_k], w=[self.VV_k])

    self.linear_tm(w, D + 256, 256, vpost)
    c.arena_reset()
    AQ = [c.asb([128, NT], BF16, "wAQ%d" % i) for i in range(2)]
    AK = [c.asb([128, NT], BF16, "wAK%d" % i) for i in range(2)]
    VP = [c.asb([128, NTILE, 128], BF16, "wVP%d" % i) for i in range(2)]
    ET = [c.asb([128, 512], BF16, "wET%d" % i) for i in range(4)]
    mk, mk_k = c.asb([128, 6, 512], BF16, "wmask")
    r0, r0_k = c.asb([128, 512], F32, "wr0")
    sx, sx_k = c.asb([128, 32], F32, "wsink")
    self.dma("pool", mk[:], self.kc["k_wmask"][:, :].rearrange("p (a b) -> p a b", a=6), w=[mk_k])
    self.dma("sp", sx[:], self.wa_sinks[0:1, :].partition_broadcast(128), w=[sx_k])
    self.op("act", lambda e: e.activation(out=sx[:], in_=sx[:], func=AF.Exp), r=[sx_k], w=[sx_k])
    for par in range(2):
        vp, vp_k = VP[par]
        self.op("pool", lambda e, vp=vp: e.memset(vp[:], 0.0), w=[vp_k])
    scale = 64 ** -0.5
    eti = 0
    for h in range(32):
        kvh, par = h // 8, h % 2
        aq, aq_k = AQ[(h // 2) % 2]
        ak, ak_k = AK[kvh % 2]
        if par == 0:
            self.dma("sp", aq[:], self.QT[h // 2], r=[self.QT_k], w=[aq_k])
        if h % 8 == 0:
            self.dma("sp", ak[:], self.KT[kvh], r=[self.KT_k], w=[ak_k])
            for p2 in range(2):
                vp, vp_k = VP[p2]
                self.dma("sp", vp[:, :, p2 * 64:(p2 + 1) * 64],
                         self.VV[:, kvh * 64:(kvh + 1) * 64].rearrange("(t p) d -> p t d", p=128), r=[self.VV_k], w=[vp_k])
        vp, vp_k = VP[par]
        p0 = par * 64
        for bi, (t0, n) in enumerate(self.TOKBLK):
            kts = [(0, None), (1, None)]
            if bi > 0:
                sblk = (t0 - LC) // 128
                for ri, r in enumerate(range(-1, 5)):
                    if 0 <= sblk + r < 16:
                        kts.append((2 + sblk + r, ri))
            po, po_k = self.PS[4]
            pz, pz_k = self.PS[6]
            for i, (kt, ri) in enumerate(kts):
                pss, pss_k = self.PS[i % 2]
                et, et_k = ET[eti % 4]
                eti += 1
                self.op("pe", lambda e, pss=pss, ak=ak, aq=aq, kt=kt, t0=t0, n=n, ri=ri, p0=p0: e.matmul(
                    pss[:, 0:n], lhsT=ak[p0:p0 + 64, kt * 128:(kt + 1) * 128], rhs=aq[p0:p0 + 64, t0:t0 + n],
                    start=True, stop=True), r=[ak_k, aq_k], w=[pss_k])
                self.op("act", lambda e, pss=pss, et=et, n=n: e.activation(out=et[:, 0:n], in_=pss[:, 0:n], func=AF.Exp, scale=scale),
                        r=[pss_k], w=[et_k])
                if ri is not None:
                    self.op("pool", lambda e, et=et, n=n, ri=ri: e.tensor_tensor(out=et[:, 0:n], in0=et[:, 0:n], in1=mk[:, ri, 0:n], op=ALU.mult),
                            r=[et_k, mk_k], w=[et_k])
                last = (i == len(kts) - 1)
                self.op("pe", lambda e, et=et, kt=kt, n=n, i=i, last=last, vp=vp: e.matmul(
                    po[:, 0:n], lhsT=vp[:, kt, :], rhs=et[:, 0:n], start=(i == 0), stop=last), r=[vp_k, et_k], w=[po_k])
                self.op("pe", lambda e, et=et, n=n, i=i, last=last: e.matmul(
                    pz[:, 0:n], lhsT=self.ones_b[:], rhs=et[:, 0:n], start=(i == 0), stop=last), r=[self.ones_b_k, et_k], w=[pz_k])
            self.op("dve", lambda e, n=n, h=h: e.tensor_scalar(out=r0[:, 0:n], in0=pz[:, 0:n], scalar1=sx[:, h:h + 1], scalar2=None, op0=ALU.add),
                    r=[pz_k, sx_k], w=[r0_k])
            self.op("dve", lambda e, n=n: e.reciprocal(out=r0[:, 0:n], in_=r0[:, 0:n]), r=[r0_k], w=[r0_k])
            wk = [self.BIGA_k[t] for t in range(t0 // 128, (t0 + n) // 128)]
            self.op("dve", lambda e, n=n, t0=t0, h=h, p0=p0: e.tensor_tensor(out=self.BIGA[p0:p0 + 64, h // 2, t0:t0 + n], in0=po[p0:p0 + 64, 0:n],
                                                                     in1=r0[p0:p0 + 64, 0:n], op=ALU.mult), r=[po_k, r0_k], w=wk)


Builder.mixer_win = _mixer_win


def _dn_proj(self):
    c = self.c
    w = self.dn_w_in[0]
    c.arena_reset()
    P = [c.asb([128, NT], F32, "dnP%d" % i) for i in range(2)]
    Cb, Cb_k = c.asb([128, NT], F32, "dnC")
    OB = [c.asb([128, NT], BF16, "dnOB%d" % i) for i in range(2)]
    rs, rs_k = self.XT[1][0][:, 0:512], self.XT[1][1]
    TS = [c.asb([128, 4, 128], BF16, "dnTS%d" % i) for i in range(2)]
    cw, cw_k = c.asb([128, 320], F32, "dncw")
    cwr, cwr_k = self.XT[0][0][:, 0:384].rearrange("p (a b) -> p a b", a=3), self.XT[0][1]
    cw2d = self.dn_conv_w[0].rearrange("k (j p) -> (k j) p", p=128)
    for i, (r0_, nr) in enumerate(((0, 128), (128, 128), (256, 64))):
        self.dma("sp", cwr[0:nr, i, :], cw2d[r0_:r0_ + nr, :], w=[cwr_k])
    pt, pt_k = self.PS[4]
    for i, (r0_, nr) in enumerate(((0, 128), (128, 128), (256, 64))):
        self.op("pe", lambda e, i=i, nr=nr, r0_=r0_: e.transpose(out=pt[:, r0_:r0_ + nr], in_=cwr[0:nr, i, :], identity=self.ident_f[0:nr, 0:nr]),
                r=[cwr_k, self.ident_f_k], w=[pt_k])
    self.op("dve", lambda e: e.tensor_copy(out=cw[:], in_=pt[:, 0:320]), r=[pt_k], w=[cw_k])
    st = {"ob": 0, "ts": 0, "eng": 0}
    SEGS = ((0, LC), (LC, NT))

    def finish_chunk(j, p, p_k):
        def wcol(k):
            return cw[:, k * 64 + j:k * 64 + j + 1]
        self.op("dve", lambda e: e.tensor_scalar(out=Cb[:], in0=p[:], scalar1=wcol(2), scalar2=None, op0=ALU.mult),
                r=[p_k, cw_k], w=[Cb_k])
        for k in (0, 1, 3, 4):
            sft = k - 2
            for (a, b) in SEGS:
                lo = max(a, a - sft)
                hi = min(b, b - sft)
                self.op("dve", lambda e, k=k, lo=lo, hi=hi, sft=sft: e.scalar_tensor_tensor(
                    out=Cb[:, lo:hi], in0=p[:, lo + sft:hi + sft], scalar=wcol(k), in1=Cb[:, lo:hi], op0=ALU.mult, op1=ALU.add),
                    r=[p_k, cw_k, Cb_k], w=[Cb_k])
        self.op("act", lambda e: e.activation(out=Cb[:], in_=Cb[:], func=AF.Silu), r=[Cb_k], w=[Cb_k])
        ob, ob_k = OB[st["ob"] % 2]
        st["ob"] += 1
        if j < 32:
            self.op("pool", lambda e: e.tensor_tensor(out=ob[:], in0=Cb[:], in1=Cb[:], op=ALU.mult), r=[Cb_k], w=[ob_k])
            qs = (128 ** -0.5) if j < 16 else 1.0
            for (t0, n) in self.TOKBLK:
                pss, pss_k = self.PS[5 + (st["eng"] % 2)]
                st["eng"] += 1
                self.op("pe", lambda e, pss=pss, t0=t0, n=n: e.matmul(pss[:, 0:n], lhsT=self.ones_b[:], rhs=ob[:, t0:t0 + n], start=True, stop=True),
                        r=[ob_k, self.ones_b_k], w=[pss_k])
                self.op("act", lambda e, pss=pss, n=n: e.activation(out=rs[:, 0:n], in_=pss[:, 0:n], func=AF.Sqrt, bias=EPS), r=[pss_k], w=[rs_k])
                self.op("dve", lambda e, n=n: e.reciprocal(out=rs[:, 0:n], in_=rs[:, 0:n]), r=[rs_k], w=[rs_k])
                self.op("dve", lambda e, t0=t0, n=n: e.scalar_tensor_tensor(out=Cb[:, t0:t0 + n], in0=Cb[:, t0:t0 + n], scalar=qs, in1=rs[:, 0:n],
                                                                           op0=ALU.mult, op1=ALU.mult), r=[Cb_k, rs_k], w=[Cb_k])
        ob2, ob2_k = OB[st["ob"] % 2]
        st["ob"] += 1
        self.op("act", lambda e: e.copy(out=ob2[:], in_=Cb[:]), r=[Cb_k], w=[ob2_k])
        if j < 16:
            self.dma("sp", self.QT[j], ob2[:], r=[ob2_k], w=[self.QT_k])
            return
        if j < 32:
            self.dma("sp", self.KT[j - 16], ob2[:], r=[ob2_k], w=[self.KT_k])
            dst, dst_k, col = self.DNK, self.DNK_k, (j - 16) * 128
        else:
            dst, dst_k, col = self.DNV, self.DNV_k, (j - 32) * 128
        ptb_t, ptb_k = self.PS[7]
        ptb = ptb_t[:].bitcast(BF16)
        for g0 in range(0, NTILE, 4):
            ng = min(4, NTILE - g0)
            ts, ts_k = TS[st["ts"] % 2]
            st["ts"] += 1
            for x in range(ng):
                t = g0 + x
                self.op("pe", lambda e, x=x, t=t: e.transpose(out=ptb[:, x * 128:(x + 1) * 128], in_=ob2[:, t * 128:(t + 1) * 128],
                                                              identity=self.ident_b[:]), r=[ob2_k, self.ident_b_k], w=[ptb_k])
            self.op("dve", lambda e, ts=ts, ng=ng: e.tensor_copy(out=ts[:, 0:ng, :], in_=ptb[:, 0:ng * 128].rearrange("p (a b) -> p a b", a=ng)),
                    r=[ptb_k], w=[ts_k])
            self.dma("sp", dst[g0 * 128:(g0 + ng) * 128, col:col + 128].rearrange("(t p) c -> p t c", p=128), ts[:, 0:ng, :],
                     r=[ts_k], w=[dst_k])

    def post(j, bi, t0, n, ps, ps_k):
        p, p_k = P[j % 2]
        self.op("act", lambda e: e.copy(out=p[:, t0:t0 + n], in_=ps[:, 0:n]), r=[ps_k], w=[p_k])
        if bi == len(self.TOKBLK) - 1:
            finish_chunk(j, p, p_k)

    self.linear_fm(w, 0, 8192, post)
    ZS = [c.asb([128, 512], BF16, "dnZS%d" % i) for i in range(2)]
    zi = {"i": 0}

    def zpost(cb, nb, t, ps, ps_k):
        zs, zs_k = ZS[zi["i"] % 2]
        zi["i"] += 1
        self.op("act", lambda e: e.activation(out=zs[:, 0:nb], in_=ps[:, 0:nb], func=AF.Silu), r=[ps_k], w=[zs_k])
        self.dma("sp", self.DNZ[t * 128:(t + 1) * 128, cb:cb + nb], zs[:, 0:nb], r=[zs_k], w=[self.DNZ_k])

    self.linear_tm(w, 8192, 4096, zpost)


def _mixer_dn(self):
    import os as _os
    self.dn_proj()
    if _os.environ.get("MK_DN_STAGE") == "A":
        return
    self.dn_scan()
    if _os.environ.get("MK_DN_STAGE") == "B":
        return
    self.dn_headout()


Builder.dn_proj = _dn_proj
Builder.mixer_dn = _mixer_dn


def _dn_scan(self, dirs=(0, 1)):
    c = self.c
    w = self.dn_w_in[0]
    c.arena_reset()
    E = self.op
    dm, dm_k = self.XT[0][0][:, 0:1024].rearrange("p (a b) -> p a b", a=8), self.XT[0][1]
    self.dma("sp", dm, self.kc["k_dnmask"][:, :].rearrange("p (a b) -> p a b", a=8), w=[dm_k])
    TRI = [dm[:, 0, :], dm[:, 1, :]]
    SGT = [dm[:, 2, :], dm[:, 3, :]]
    BLK = dm[:, 4, :]
    SELC = [dm[:, 5, :], dm[:, 6, :]]
    misc, misc_k = self.misc
    pc = misc[:, 8:12]
    par = misc[:, 12:16]
    self.dma("sp", pc, self.kc["k_dncol"][:, :], w=[misc_k])
    E("pool", lambda e: e.memset(par, 0.0), r=[misc_k], w=[misc_k])
    for d in range(2):
        self.dma("sp", par[d * 64 + 32:d * 64 + 64, 0:1], self.dn_a_log[0, d:d + 1, :].rearrange("o h -> h o"), r=[misc_k], w=[misc_k])
        self.dma("sp", par[d * 64 + 32:d * 64 + 64, 1:2], self.dn_dt_bias[0, d:d + 1, :].rearrange("o h -> h o"), r=[misc_k], w=[misc_k])
    E("act", lambda e: e.activation(out=par[:, 2:3], in_=par[:, 0:1], func=AF.Exp), r=[misc_k], w=[misc_k])
    E("dve", lambda e: e.scalar_tensor_tensor(out=par[:, 2:3], in0=par[:, 2:3], scalar=-1.0, in1=pc[:, 1:2], op0=ALU.mult, op1=ALU.mult),
      r=[misc_k], w=[misc_k])
    bgs, bgs_k = c.asb([128, 512], F32, "bgs")
    bg2, bg2_k = self.XT[1][0][:, 0:512], self.XT[1][1]
    BGT, BGT_k = c.asb([128, NTILE, 128], F32, "BGT")
    GCA, GCA_k = c.asb([128, NTILE, 64], F32, "GCA")
    GLT, GLT_k = c.asb([128, NTILE, 64], F32, "GLT")
    RR, RR_k = c.asb([128, NTILE, 64], F32, "RR")
    BAx, BAx_k = c.asb([128, NTILE, 64], F32, "BAx")
    GTB, GTB_k = c.asb([128, 2 * NTILE, 64], F32, "GTB")

    def bapost(j, bi, t0, n, ps, ps_k):
        E("act", lambda e: e.activation(out=bgs[:, 0:n], in_=ps[:, 0:n], func=AF.Sigmoid), r=[ps_k], w=[bgs_k])
        E("dve", lambda e: e.tensor_scalar(out=bgs[:, 0:n], in0=bgs[:, 0:n], scalar1=pc[:, 0:1], scalar2=None, op0=ALU.mult),
          r=[bgs_k, misc_k], w=[bgs_k])
        E("act", lambda e: e.activation(out=bg2[:, 0:n], in_=ps[:, 0:n], func=AF.Exp, bias=par[:, 1:2]), r=[ps_k, misc_k], w=[bg2_k])
        E("act", lambda e: e.activation(out=bg2[:, 0:n], in_=bg2[:, 0:n], func=AF.Ln, bias=1.0), r=[bg2_k], w=[bg2_k])
        E("dve", lambda e: e.scalar_tensor_tensor(out=bgs[:, 0:n], in0=bg2[:, 0:n], scalar=par[:, 2:3], in1=bgs[:, 0:n], op0=ALU.mult, op1=ALU.add),
          r=[bg2_k, bgs_k, misc_k], w=[bgs_k])
        for x in range(n // 128):
            t = t0 // 128 + x
            pt, pt_k = self.PS[4 + (t % 2)]
            E("pe", lambda e, x=x, pt=pt: e.transpose(out=pt[:, 0:128], in_=bgs[:, x * 128:(x + 1) * 128], identity=self.ident_f[:]),
              r=[bgs_k, self.ident_f_k], w=[pt_k])
            E("dve", lambda e, t=t, pt=pt: e.tensor_copy(out=BGT[:, t, :], in_=pt[:, 0:128]), r=[pt_k], w=[BGT_k])

    self.linear_fm(w, 12288, 128, bapost)
    for t in range(NTILE):
        pg, pg_k = self.PS[4 + (t % 2)]
        for d in range(2):
            E("pe", lambda e, t=t, d=d, pg=pg: e.matmul(pg[:, d * 32:(d + 1) * 32], lhsT=TRI[d], rhs=BGT[:, t, d * 64 + 32:d * 64 + 64],
                                                       start=True, stop=True), r=[dm_k, BGT_k], w=[pg_k])
            E("pe", lambda e, t=t, d=d, pg=pg: e.matmul(pg[:, 64 + d * 32:64 + (d + 1) * 32], lhsT=BLK, rhs=BGT[:, t, d * 64 + 32:d * 64 + 64],
                                                       start=True, stop=True), r=[dm_k, BGT_k], w=[pg_k])
        E("dve", lambda e, t=t, pg=pg: e.tensor_copy(out=GCA[:, t, :], in_=pg[:, 0:64]), r=[pg_k], w=[GCA_k])
        E("act", lambda e, t=t, pg=pg: e.copy(out=GLT[:, t, :], in_=pg[:, 64:128]), r=[pg_k], w=[GLT_k])
    E("dve", lambda e: e.tensor_tensor(out=RR[:], in0=GLT[:], in1=GCA[:], op=ALU.subtract), r=[GLT_k, GCA_k], w=[RR_k])
    E("act", lambda e: e.activation(out=RR[:], in_=RR[:], func=AF.Exp), r=[RR_k], w=[RR_k])
    E("act", lambda e: e.activation(out=GLT[:], in_=GLT[:], func=AF.Exp), r=[GLT_k, RR_k], w=[GLT_k])
    E("act", lambda e: e.activation(out=GCA[:], in_=GCA[:], func=AF.Exp), r=[GCA_k, RR_k], w=[GCA_k])
    for d in range(2):
        E("dve", lambda e, d=d: e.tensor_tensor(out=BAx[:, :, d * 32:(d + 1) * 32], in0=BGT[:, :, d * 64:d * 64 + 32],
                                               in1=GCA[:, :, d * 32:(d + 1) * 32], op=ALU.mult), r=[BGT_k, GCA_k], w=[BAx_k])
    for t in range(NTILE):
        pg, pg_k = self.PS[4 + (t % 2)]
        for hf in range(2):
            E("pe", lambda e, t=t, hf=hf, pg=pg: e.matmul(pg[:, hf * 64:(hf + 1) * 64], lhsT=SELC[hf], rhs=GLT[:, t, :], start=True, stop=True),
              r=[dm_k, GLT_k], w=[pg_k])
        E("dve", lambda e, t=t, pg=pg: e.tensor_copy(out=GTB[:, 2 * t:2 * t + 2, :], in_=pg[:, 0:128].rearrange("p (a b) -> p a b", a=2)),
          r=[pg_k], w=[GTB_k])
    self.p.barrier()

    NS = 2
    PSQ = [(self.PS[b][0][:, 0:128], self.PS[b][1]) for b in range(8)]
    psq_i = {"i": 0}

    def psq():
        r = PSQ[psq_i["i"] % 8]
        psq_i["i"] += 1
        return r

    class Stream:
        pass

    streams = []
    for s_ in range(NS):
        st = Stream()
        wtf = self.WT[s_][0][:].bitcast(F32)
        st.mats = [(wtf[:, a, b * 128:(b + 1) * 128], Tk("m%d_%d_%d" % (s_, a, b))) for a in range(16) for b in range(2)]
        st.mi = 0
        hb = self.HB[s_][0]
        st.ld = [[(hb[:, (q * 4 + x) * 128:(q * 4 + x + 1) * 128], Tk("ld%d_%d_%d" % (s_, q, x))) for x in range(4)] for q in range(4)]
        st.ldi = 0
        streams.append(st)
    ident = self.ident_f

    def unit(st, d, h, t, first):
        hk = h // 2
        M = {}
        names = ["GTRI", "DEC", "DECT", "L", "U", "La", "Ua", "Lb", "Ub", "P", "vb", "kbg", "kd", "u", "wT", "itT", "qTf", "vn", "o", "S"]
        for i, nm in enumerate(names):
            M[nm] = st.mats[i]
        kTb, qTb, ktok, vtok = st.ld[st.ldi % 4]
        st.ldi += 1
        r0_ = t * 128
        self.dma("sp", kTb[0], self.KT[hk][:, r0_:r0_ + 128], r=[self.KT_k], w=[kTb[1]])
        self.dma("sp", qTb[0], self.QT[hk][:, r0_:r0_ + 128], r=[self.QT_k], w=[qTb[1]])
        self.dma("sp", ktok[0], self.DNK[r0_:r0_ + 128, hk * 128:(hk + 1) * 128], r=[self.DNK_k], w=[ktok[1]])
        self.dma("sp", vtok[0], self.DNV[r0_:r0_ + 128, h * 128:(h + 1) * 128], r=[self.DNV_k], w=[vtok[1]])
        gcol = BGT[:, t, d * 64 + 32 + h:d * 64 + 33 + h]
        bcol = BGT[:, t, d * 64 + h:d * 64 + h + 1]
        acol = GCA[:, t, d * 32 + h:d * 32 + h + 1]
        bacol = BAx[:, t, d * 32 + h:d * 32 + h + 1]
        rcol = RR[:, t, d * 32 + h:d * 32 + h + 1]
        (GTRI, GTRI_k), (DEC, DEC_k), (DECT, DECT_k) = M["GTRI"], M["DEC"], M["DECT"]
        (Lm, L_k), (U, U_k), (P, P_k) = M["L"], M["U"], M["P"]
        E("dve", lambda e: e.tensor_scalar(out=GTRI, in0=TRI[d], scalar1=gcol, scalar2=None, op0=ALU.mult), r=[dm_k, BGT_k], w=[GTRI_k])
        pD, pD_k = psq()
        pDT, pDT_k = psq()
        E("pe", lambda e: e.matmul(pD, lhsT=GTRI, rhs=SGT[d], start=True, stop=True), r=[GTRI_k, dm_k], w=[pD_k])
        E("pe", lambda e: e.matmul(pDT, lhsT=SGT[d], rhs=GTRI, start=True, stop=True), r=[GTRI_k, dm_k], w=[pDT_k])
        E("act", lambda e: e.activation(out=DEC, in_=pD, func=AF.Exp), r=[pD_k], w=[DEC_k])
        E("act", lambda e: e.activation(out=DECT, in_=pDT, func=AF.Exp), r=[pDT_k], w=[DECT_k])
        pKK, pKK_k = psq()
        pQK, pQK_k = psq()
        E("pe", lambda e: e.matmul(pKK, lhsT=kTb[0], rhs=kTb[0], start=True, stop=True), r=[kTb[1]], w=[pKK_k])
        E("pe", lambda e: e.matmul(pQK, lhsT=kTb[0], rhs=qTb[0], start=True, stop=True), r=[kTb[1], qTb[1]], w=[pQK_k])
        E("dve", lambda e: e.scalar_tensor_tensor(out=Lm, in0=pKK, scalar=bcol, in1=DEC, op0=ALU.mult, op1=ALU.mult),
          r=[pKK_k, BGT_k, DEC_k], w=[L_k])
        E("pool", lambda e: e.tensor_tensor(out=Lm, in0=Lm, in1=SGT[d], op=ALU.mult), r=[L_k, dm_k], w=[L_k])
        itT, itT_k = M["itT"]
        E("dve", lambda e: e.tensor_tensor(out=itT, in0=pQK, in1=DECT, op=ALU.mult), r=[pQK_k, DECT_k], w=[itT_k])
        E("pool", lambda e: e.tensor_tensor(out=itT, in0=itT, in1=TRI[d], op=ALU.mult), r=[itT_k, dm_k], w=[itT_k])
        pU, pU_k = psq()
        E("pe", lambda e: e.transpose(out=pU, in_=Lm, identity=ident[:]), r=[L_k, self.ident_f_k], w=[pU_k])
        E("act", lambda e: e.copy(out=U, in_=pU), r=[pU_k], w=[U_k])
        E("dve", lambda e: e.scalar_tensor_tensor(out=P, in0=U, scalar=-1.0, in1=ident[:], op0=ALU.mult, op1=ALU.add),
          r=[U_k, self.ident_f_k], w=[P_k])
        curL, curU = M["L"], M["U"]
        pp = [(M["La"], M["Ua"]), (M["Lb"], M["Ub"])]
        for k in range(1, 6):
            nL, nU = pp[k % 2]
            pl, pl_k = psq()
            E("pe", lambda e, pl=pl, curL=curL, curU=curU: e.matmul(pl, lhsT=curU[0], rhs=curL[0], start=True, stop=True),
              r=[curL[1], curU[1]], w=[pl_k])
            E("act", lambda e, pl=pl, nL=nL: e.copy(out=nL[0], in_=pl), r=[pl_k], w=[nL[1]])
            if k < 5:
                pu_, pu_k = psq()
                E("pe", lambda e, pu_=pu_, curL=curL, curU=curU: e.matmul(pu_, lhsT=curL[0], rhs=curU[0], start=True, stop=True),
                  r=[curL[1], curU[1]], w=[pu_k])
                E("dve", lambda e, pu_=pu_, nU=nU: e.tensor_copy(out=nU[0], in_=pu_), r=[pu_k], w=[nU[1]])
            ppn, ppn_k = psq()
            E("pe", lambda e, ppn=ppn, nL=nL: e.matmul(ppn, lhsT=nL[0], rhs=P, start=True, stop=True), r=[nL[1], P_k], w=[ppn_k])
            E("dve", lambda e, ppn=ppn: e.tensor_tensor(out=P, in0=ppn, in1=P, op=ALU.add), r=[ppn_k, P_k], w=[P_k])
            curL, curU = nL, nU
        (vb, vb_k), (kbg, kbg_k), (kd, kd_k) = M["vb"], M["kbg"], M["kd"]
        E("pool", lambda e: e.tensor_scalar(out=vb, in0=vtok[0], scalar1=bcol, scalar2=None, op0=ALU.mult), r=[vtok[1], BGT_k], w=[vb_k])
        E("pool", lambda e: e.tensor_scalar(out=kbg, in0=ktok[0], scalar1=bacol, scalar2=None, op0=ALU.mult), r=[ktok[1], BAx_k], w=[kbg_k])
        E("pool", lambda e: e.tensor_scalar(out=kd, in0=ktok[0], scalar1=rcol, scalar2=None, op0=ALU.mult), r=[ktok[1], RR_k], w=[kd_k])
        (u, u_k), (wT, wT_k), (qTf, qTf_k) = M["u"], M["wT"], M["qTf"]
        pu2, pu2_k = psq()
        pw, pw_k = psq()
        E("pe", lambda e: e.matmul(pu2, lhsT=P, rhs=vb, start=True, stop=True), r=[P_k, vb_k], w=[pu2_k])
        E("pe", lambda e: e.matmul(pw, lhsT=kbg, rhs=P, start=True, stop=True), r=[P_k, kbg_k], w=[pw_k])
        E("act", lambda e: e.copy(out=u, in_=pu2), r=[pu2_k], w=[u_k])
        E("dve", lambda e: e.tensor_copy(out=wT, in_=pw), r=[pw_k], w=[wT_k])
        E("act", lambda e: e.copy(out=qTf, in_=qTb[0]), r=[qTb[1]], w=[qTf_k])
        (vn, vn_k), (o, o_k), (S, S_k) = M["vn"], M["o"], M["S"]
        if first:
            E("pool", lambda e: e.memset(S, 0.0), w=[S_k])
        for hf in ((0, 1) if d == 0 else (1, 0)):
            c0 = hf * 64
            gtcol = GTB[:, 2 * t + hf, d * 32 + h:d * 32 + h + 1]
            p1, p1_k = psq()
            p2a, p2a_k = psq()
            p2b, p2b_k = psq()
            p3, p3_k = psq()
            E("pe", lambda e, c0=c0, p1=p1: e.matmul(p1[c0:c0 + 64, :], lhsT=wT[:, c0:c0 + 64], rhs=S, start=True, stop=True),
              r=[wT_k, S_k], w=[p1_k])
            E("dve", lambda e, c0=c0, p1=p1: e.tensor_tensor(out=vn[c0:c0 + 64, :], in0=u[c0:c0 + 64, :], in1=p1[c0:c0 + 64, :], op=ALU.subtract),
              r=[u_k, p1_k], w=[vn_k])
            E("pe", lambda e, c0=c0, p2a=p2a: e.matmul(p2a[c0:c0 + 64, :], lhsT=qTf[:, c0:c0 + 64], rhs=S, start=True, stop=True),
              r=[qTf_k, S_k], w=[p2a_k])
            E("pe", lambda e, c0=c0, p2b=p2b: e.matmul(p2b[c0:c0 + 64, :], lhsT=itT[c0:c0 + 64, c0:c0 + 64], rhs=vn[c0:c0 + 64, :],
                                                      start=True, stop=True), r=[itT_k, vn_k], w=[p2b_k])
            E("act", lambda e, c0=c0, p2a=p2a: e.activation(out=o[c0:c0 + 64, :], in_=p2a[c0:c0 + 64, :], func=AF.Copy, scale=acol[c0:c0 + 64, :]),
              r=[p2a_k, GCA_k], w=[o_k])
            E("dve", lambda e, c0=c0, p2b=p2b: e.tensor_tensor(out=o[c0:c0 + 64, :], in0=o[c0:c0 + 64, :], in1=p2b[c0:c0 + 64, :], op=ALU.add),
              r=[o_k, p2b_k], w=[o_k])
            E("pe", lambda e, c0=c0, p3=p3: e.matmul(p3, lhsT=kd[c0:c0 + 64, :], rhs=vn[c0:c0 + 64, :], start=True, stop=True),
              r=[kd_k, vn_k], w=[p3_k])
            E("dve", lambda e, p3=p3, gtcol=gtcol: e.scalar_tensor_tensor(out=S, in0=S, scalar=gtcol, in1=p3, op0=ALU.mult, op1=ALU.add),
              r=[S_k, GTB_k, p3_k], w=[S_k])
        self.dma("sp", self.ODN[d, r0_:r0_ + 128, h * 128:(h + 1) * 128], o, r=[o_k], w=[self.ODN_k])

    order = {0: list(range(NTILE)), 1: [1, 0] + list(range(NTILE - 1, 1, -1))}
    import os as _os
    nh_ = int(_os.environ.get('MK_DN_NH', '32'))
    todo = [(d, h) for d in dirs for h in range(nh_)]
    for g0 in range(0, len(todo), NS):
        grp = todo[g0:g0 + NS]
        for step in range(NTILE):
            for si, (d, h) in enumerate(grp):
                unit(streams[si], d, h, order[d][step], step == 0)
    self.p.barrier()


Builder.dn_scan = _dn_scan


def _dn_headout(self):
    c = self.c
    E = self.op
    for half in range(2):
        c.arena_reset()
        nwt, nwt_k = c.asb([128, 128], F32, "nwt")
        ssb, ssb_k = c.asb([128, 32], F32, "ssb")
        self.dma("sp", nwt, self.dn_norm_w[0:1, :].partition_broadcast(128), w=[nwt_k])
        ps_t, ps_tk = self.PS[1]
        pst = ps_t[:].bitcast(BF16)
        c0 = half * 2048
        for t in range(NTILE):
            oa, oa_k = self.XT[0]
            ob_, ob_k = self.XT[1]
            zt, zt_k = self.HB[t % 2]
            hb, hb_k = self.SB[t % 2]
            hbb = hb[:].bitcast(BF16)[:, 0:2048]
            r0_ = t * 128
            self.dma("sp", oa[:], self.ODN[0, r0_:r0_ + 128, c0:c0 + 2048], r=[self.ODN_k], w=[oa_k])
            self.dma("sp", ob_[:], self.ODN[1, r0_:r0_ + 128, c0:c0 + 2048], r=[self.ODN_k], w=[ob_k])
            self.dma("sp", zt[:], self.DNZ[r0_:r0_ + 128, c0:c0 + 2048], r=[self.DNZ_k], w=[zt_k])
            E("dve", lambda e, oa=oa, ob_=ob_: e.tensor_tensor(out=oa[:], in0=oa[:], in1=ob_[:], op=ALU.add), r=[oa_k, ob_k], w=[oa_k])
            E("act", lambda e, oa=oa, ob_=ob_: e.activation(out=ob_[:], in_=oa[:], func=AF.Square), r=[oa_k], w=[ob_k])
            E("dve", lambda e, ob_=ob_: e.reduce_sum(out=ssb[:, 0:16], in_=ob_[:].rearrange("p (a b) -> p a b", a=16), axis=AX.X),
              r=[ob_k], w=[ssb_k])
            E("act", lambda e: e.activation(out=ssb[:, 16:32], in_=ssb[:, 0:16], func=AF.Sqrt, scale=1.0 / 128, bias=EPS), r=[ssb_k], w=[ssb_k])
            E("dve", lambda e: e.reciprocal(out=ssb[:, 16:32], in_=ssb[:, 16:32]), r=[ssb_k], w=[ssb_k])
            for hh in range(16):
                eng = "dve" if hh % 2 == 0 else "pool"
                if eng == "dve":
                    E("dve", lambda e, hh=hh, oa=oa, hbb=hbb: e.scalar_tensor_tensor(
                        out=hbb[:, hh * 128:(hh + 1) * 128], in0=oa[:, hh * 128:(hh + 1) * 128], scalar=ssb[:, 16 + hh:17 + hh], in1=nwt,
                        op0=ALU.mult, op1=ALU.mult), r=[oa_k, ssb_k, nwt_k], w=[hb_k])
                else:
                    E("pool", lambda e, hh=hh, oa=oa: e.tensor_scalar(out=oa[:, hh * 128:(hh + 1) * 128], in0=oa[:, hh * 128:(hh + 1) * 128],
                                                                       scalar1=ssb[:, 16 + hh:17 + hh], scalar2=None, op0=ALU.mult),
                      r=[oa_k, ssb_k], w=[oa_k])
                    E("pool", lambda e, hh=hh, oa=oa, hbb=hbb: e.tensor_tensor(out=hbb[:, hh * 128:(hh + 1) * 128], in0=oa[:, hh * 128:(hh + 1) * 128],
                                                                              in1=nwt, op=ALU.mult), r=[oa_k, nwt_k], w=[hb_k])
            E("dve", lambda e, hbb=hbb, zt=zt: e.tensor_tensor(out=hbb, in0=hbb, in1=zt[:], op=ALU.mult), r=[hb_k, zt_k], w=[hb_k])
            for g in range(2):
                for j in range(8):
                    kc = g * 8 + j
                    E("pe", lambda e, hbb=hbb, kc=kc, j=j: e.transpose(out=pst[:, j * 128:(j + 1) * 128], in_=hbb[:, kc * 128:(kc + 1) * 128],
                                                                      identity=self.ident_b[:]), r=[hb_k, self.ident_b_k], w=[ps_tk])
                E("act", lambda e, g=g, t=t: e.copy(out=self.BIGA[:, g * 8:(g + 1) * 8, t * 128:(t + 1) * 128],
                                                   in_=pst.rearrange("p (a b) -> p a b", a=8)), r=[ps_tk], w=[self.BIGA_k[t]])
        self.phase_oproj_residual(self.dn_w_o[0][half * 2048:(half + 1) * 2048, :], self.cur_li)


Builder.dn_headout = _dn_headout
```

```python
import math
from contextlib import ExitStack
import numpy as np
import concourse.bass as bass
import concourse.mybir as mybir
from concourse.bass_utils import run_bass_kernel_spmd

F32 = mybir.dt.float32
BF16 = mybir.dt.bfloat16
I32 = mybir.dt.int32
U32 = mybir.dt.uint32
AF = mybir.ActivationFunctionType
ALU = mybir.AluOpType
AX = mybir.AxisListType

D = 2048
KC = 16
L = 2048
LC = 256
NT = L + LC
NTILE = NT // 128
DEPTH = 4
EPS = 1e-6
N_DMA_SEM = 40


class Tk:
    __slots__ = ("name", "w", "r")

    def __init__(self, name):
        self.name = name
        self.w = None
        self.r = []


class Op:
    __slots__ = ("eng", "fn", "deps", "dma", "sem", "val", "cnt", "need", "waits")


class Prog:
    ENGS = ("pe", "act", "dve", "pool", "sp")

    def __init__(self, nc):
        self.nc = nc
        self.ops = []
        self.dma_uses = [0] * N_DMA_SEM
        self.dma_last = [None] * N_DMA_SEM
        self.dma_rr = 0
        self.final_deps = []
        self.since = []
        self.bar = None

    def op(self, eng, fn, r=(), w=(), dma=False):
        o = Op()
        o.eng = eng
        o.fn = fn
        o.dma = dma
        o.need = False
        deps = set()
        for t in r:
            if t.w is not None:
                deps.add(t.w)
        for t in w:
            if t.w is not None:
                deps.add(t.w)
            deps.update(t.r)
        idx = len(self.ops)
        if self.bar is not None:
            deps.add(self.bar)
        self.since.append(idx)
        if dma:
            s = self.dma_rr
            self.dma_rr = (self.dma_rr + 1) % N_DMA_SEM
            if self.dma_last[s] is not None:
                deps.add(self.dma_last[s])
            self.dma_uses[s] += 1
            self.dma_last[s] = idx
            o.sem = s
            o.val = 16 * self.dma_uses[s]
        o.deps = deps
        self.ops.append(o)
        for t in r:
            t.r.append(idx)
        for t in w:
            t.w = idx
            t.r = []
        return idx

    def barrier(self):
        o = Op()
        o.eng = "sp"
        o.fn = lambda e: e.nop()
        o.dma = False
        o.need = False
        o.deps = set(self.since)
        idx = len(self.ops)
        self.ops.append(o)
        self.since = [idx]
        self.bar = idx
        return idx

    def finalize(self):
        ops = self.ops
        for o in ops:
            for d in o.deps:
                if not ops[d].dma:
                    ops[d].need = True
        cnt = {e: 0 for e in self.ENGS}
        for o in ops:
            if o.need:
                cnt[o.eng] += 1
            o.cnt = cnt[o.eng]
        seen = {e: {} for e in self.ENGS}
        for o in ops:
            waits = {}
            for d in o.deps:
                dd = ops[d]
                if dd.dma:
                    key = ("dma", dd.sem)
                    val = dd.val
                else:
                    if dd.eng == "pe" and o.eng == "pe" and not o.dma:
                        continue
                    key = ("eng", dd.eng)
                    val = dd.cnt
                if seen[o.eng].get(key, 0) >= val:
                    continue
                if waits.get(key, 0) < val:
                    waits[key] = val
            for k, v in waits.items():
                seen[o.eng][k] = v
            o.waits = list(waits.items())

    def emit(self, out_ops):
        nc = self.nc
        self.finalize()
        ops = self.ops
        with ExitStack() as es:
            esem = {e: es.enter_context(nc.semaphore("s_" + e)) for e in self.ENGS}
            dsem = [es.enter_context(nc.semaphore("d%d" % i)) for i in range(N_DMA_SEM)]
            block = es.enter_context(nc.Block())

            def run(ename):
                def body(eng):
                    for o in ops:
                        if o.eng != ename:
                            continue
                        for (kind, k), v in o.waits:
                            eng.wait_ge(esem[k] if kind == "eng" else dsem[k], v)
                        ins = o.fn(eng)
                        if o.dma:
                            ins.then_inc(dsem[o.sem], 16)
                        elif o.need:
                            ins.then_inc(esem[ename], 1)
                    if ename == "sp":
                        for d in out_ops:
                            dd = ops[d]
                            eng.wait_ge(dsem[dd.sem], dd.val)
                return body

            block.tensor(run("pe"))
            block.scalar(run("act"))
            block.vector(run("dve"))
            block.gpsimd(run("pool"))
            block.sync(run("sp"))


class Ctx:
    def __init__(self, nc, es):
        self.nc = nc
        self.es = es
        self.p = Prog(nc)
        self.n = 0

    def sb(self, shape, dt, name=None):
        self.n += 1
        t = self.es.enter_context(self.nc.sbuf_tensor(name or ("sb%d" % self.n), list(shape), dt))
        return t, Tk(name or "sb%d" % self.n)

    def arena_init(self, nbytes):
        self.arena_n = nbytes // 2
        self.arena = self.es.enter_context(self.nc.sbuf_tensor("ARENA", [128, self.arena_n], BF16))
        self.arena_off = 0

    def arena_reset(self):
        self.p.barrier()
        self.arena_off = 0

    def asb(self, shape, dt, name=None):
        self.n += 1
        esz = 4 if dt in (F32, I32, U32) else 2
        free = 1
        for d in shape[1:]:
            free *= d
        nel = (free * esz + 1) // 2
        nel = (nel + 31) // 32 * 32
        assert self.arena_off + nel <= self.arena_n, "arena overflow %s" % name
        v = self.arena[0:shape[0], self.arena_off:self.arena_off + free * esz // 2]
        self.arena_off += nel
        if dt != BF16:
            v = v.bitcast(dt)
        if len(shape) == 3:
            v = v.rearrange("p (a b) -> p a b", a=shape[1])
        return v, Tk(name or "a%d" % self.n)

    def ps(self, shape, dt=F32, name=None):
        self.n += 1
        t = self.es.enter_context(self.nc.psum_tensor(name or ("ps%d" % self.n), list(shape), dt))
        return t, Tk(name or "ps%d" % self.n)

    def dram(self, name, shape, dt, kind="Internal"):
        t = self.nc.dram_tensor(name, list(shape), dt, kind=kind)
        return t.ap(), Tk(name)


def _rope_tables():
    GRID_W = 64
    dim = 64
    rows = L // GRID_W
    row, col = np.meshgrid(np.arange(rows), np.arange(GRID_W), indexing="ij")
    row = row.reshape(-1).astype(np.float32)
    col = col.reshape(-1).astype(np.float32)
    half = dim // 2
    inv_freq = (1.0 / (10000.0 ** (np.arange(0, half, 2, dtype=np.float32) / half))).astype(np.float32)

    def table(pos):
        ang = pos[:, None] * inv_freq[None, :]
        ang = np.concatenate([ang, ang], axis=-1)
        return np.cos(ang), np.sin(ang)

    cr, sr = table(row)
    cc, sc = table(col)
    cos = np.concatenate([cr, cc], -1).astype(np.float32)
    sin = np.concatenate([sr, sc], -1).astype(np.float32)
    cosT = np.concatenate([cos.T, cos.T], 0)
    sinT = np.concatenate([sin.T, sin.T], 0)
    R = np.zeros((64, 64), np.float32)
    for base in (0, 32):
        for i in range(16):
            R[base + i, base + 16 + i] = -1.0
            R[base + 16 + i, base + i] = 1.0
    R2 = np.zeros((128, 128), np.float32)
    R2[:64, :64] = R
    R2[64:, 64:] = R
    return cosT, sinT, np.ascontiguousarray(R2.T)


def _consts():
    cosT, sinT, RT = _rope_tables()
    c = {
        "k_cos": cosT, "k_sin": sinT, "k_rt": RT,
        "k_ident": np.eye(128, dtype=np.float32),
        "k_ones": np.ones((128, 128), np.float32),
    }
    wm = np.zeros((128, 6, 512), np.float32)
    kk = np.arange(128)[:, None]
    qq = np.arange(512)[None, :]
    for ri, r in enumerate(range(-1, 5)):
        wm[:, ri, :] = np.where(np.abs(r * 128 + kk - qq) <= 128, 1.0, 0.0)
    c["k_wmask"] = wm.reshape(128, 6 * 512)
    ii = np.arange(128)
    sc = (ii[:, None] // 64) == (ii[None, :] // 64)
    dm = np.zeros((128, 8, 128), np.float32)
    dm[:, 0] = sc & (ii[:, None] <= ii[None, :])
    dm[:, 1] = sc & (ii[:, None] >= ii[None, :])
    dm[:, 2] = sc & (ii[:, None] > ii[None, :])
    dm[:, 3] = sc & (ii[:, None] < ii[None, :])
    dm[:, 4] = sc
    dm[:, 5] = (ii[:, None] < 64) / 64.0 + 0 * ii[None, :]
    dm[:, 6] = (ii[:, None] >= 64) / 64.0 + 0 * ii[None, :]
    c["k_dnmask"] = dm.reshape(128, 8 * 128)
    mb = np.zeros((128, 224), np.float32)
    mb[:, 0:128] = (ii[:, None] < ii[None, :])
    mb[:, 128:196] = np.arange(68)[None, :]
    mb[:, 196:212] = np.arange(16)[None, :] * 128 + ii[:, None]
    mb[:, 212:218] = np.arange(6)[None, :] * 128 + ii[:, None]
    c["k_moeb"] = mb
    pc = np.zeros((128, 4), np.float32)
    pc[:, 0] = ((ii // 32) % 2 == 0)
    pc[:, 1] = ((ii // 32) % 2 == 1)
    c["k_dncol"] = pc
    return c


class Builder:
    def __init__(self, nc, es, n_layers=1, dbg=None, do_mixer=True, do_moe=True, wl=1, layer_abs=0, final=False, fused=False):
        self.fused = fused
        self.layers = list(range(DEPTH)) if fused else [layer_abs]
        self.wl = DEPTH if fused else 1
        self.layer_abs = layer_abs
        self.final = True if fused else final
        self.nc = nc
        self.c = Ctx(nc, es)
        self.p = self.c.p
        self.n_layers = n_layers
        self.dbg = dbg or []
        self.do_mixer = do_mixer
        self.do_moe = do_moe
        self.out_ops = []

    def op(self, eng, fn, r=(), w=(), dma=False):
        psk = getattr(self, "_psk", None)
        if psk:
            r2 = [t for t in r if id(t) not in psk]
            w = list(w) + [t for t in r if id(t) in psk]
            r = r2
        return self.p.op(eng, fn, r=r, w=w, dma=dma)

    def dma(self, eng, out, in_, r=(), w=()):
        return self.p.op(eng, lambda e: e.dma_start(out=out, in_=in_), r=r, w=w, dma=True)

    def declare_io(self):
        c = self.c
        kind = [0, 1, 2, 0][self.layer_abs]
        kinds = {[0, 1, 2, 0][l] for l in self.layers}
        used = {"x_in", "cin", "ada_w", "ada_b", "norm_mix_w", "norm_ffn_w", "final_norm_w",
                "k_cos", "k_sin", "k_rt", "k_ident", "k_ones", "k_wmask", "k_dnmask", "k_dncol", "k_moeb"}
        if self.do_mixer and 0 in kinds:
            used |= {"da_w_qkv", "da_lambda", "da_subln_w", "da_w_o"}
        if self.do_mixer and 2 in kinds:
            used |= {"wa_w_qkv", "wa_sinks", "wa_w_o"}
        if self.do_mixer and 1 in kinds:
            used |= {"dn_w_in", "dn_conv_w", "dn_a_log", "dn_dt_bias", "dn_norm_w", "dn_w_o"}
        if self.do_moe:
            used |= {"moe_wg", "moe_bg", "moe_we", "moe_be", "moe_w13", "moe_w2"}
        self.used = used
        self.in_shapes = {}

        def ein(n, s):
            if n not in used:
                s = [1, 1]
            self.in_shapes[n] = list(s)
            return c.dram(n, s, F32, "ExternalInput")
        self.x_in, self.x_in_k = ein("x_in", [NT, D])
        self.cin, self.cin_k = ein("cin", [128, KC, 2])
        self.ada_w, self.ada_w_k = ein("ada_w", [self.wl, D, 6 * D])
        self.ada_b, _ = ein("ada_b", [self.wl, 6 * D])
        self.norm_mix_w, _ = ein("norm_mix_w", [self.wl, D])
        self.norm_ffn_w, _ = ein("norm_ffn_w", [self.wl, D])
        self.da_w_qkv, _ = ein("da_w_qkv", [(2 if self.fused else 1), D, 3 * D])
        self.da_lambda, _ = ein("da_lambda", [(2 if self.fused else 1), 4, 64])
        self.da_subln_w, _ = ein("da_subln_w", [(2 if self.fused else 1), 128])
        self.da_w_o, _ = ein("da_w_o", [(2 if self.fused else 1), D, D])
        self.dn_w_in, _ = ein("dn_w_in", [1, D, 12416])
        self.dn_conv_w, _ = ein("dn_conv_w", [1, 5, 8192])
        self.dn_a_log, _ = ein("dn_a_log", [1, 2, 32])
        self.dn_dt_bias, _ = ein("dn_dt_bias", [1, 2, 32])
        self.dn_norm_w, _ = ein("dn_norm_w", [1, 128])
        self.dn_w_o, _ = ein("dn_w_o", [1, 4096, D])
        self.wa_w_qkv, _ = ein("wa_w_qkv", [1, D, 2560])
        self.wa_sinks, _ = ein("wa_sinks", [1, 32])
        self.wa_w_o, _ = ein("wa_w_o", [1, D, D])
        self.moe_wg, _ = ein("moe_wg", [self.wl, D, 4])
        self.moe_bg, _ = ein("moe_bg", [self.wl, 4])
        self.moe_we, _ = ein("moe_we", [self.wl, D, 32])
        self.moe_be, _ = ein("moe_be", [self.wl, 32])
        self.moe_w13, _ = ein("moe_w13", [self.wl, 32, D, 1536])
        self.moe_w2, _ = ein("moe_w2", [self.wl, 32, 768, D])
        self.final_norm_w, _ = ein("final_norm_w", [1, D])
        self.kc = {}
        for name, arr in _consts().items():
            self.kc[name] = ein(name, list(arr.shape))[0]
        if self.final:
            self.y_out, self.y_out_k = c.dram("y_out", [L, D], F32, "ExternalOutput")
        else:
            self.x_out, self.x_out_k = c.dram("x_out", [NT, D], F32, "ExternalOutput")
        self.XR, _ = c.dram("XR", [NT, D], F32)
        self.XR_k = [Tk("xr%d" % t) for t in range(NTILE)]
        self.MOD, self.MOD_k = c.dram("MODS", [DEPTH, 2, 6 * D], F32)
        import os as _os
        dk = "ExternalOutput" if _os.environ.get("MK_DUMP") else "Internal"
        self.QT, self.QT_k = c.dram("QT", [16, 128, NT], BF16, dk)
        self.KT, self.KT_k = c.dram("KT", [16, 128, NT], BF16, dk)
        self.VV, self.VV_k = c.dram("VV", [NT, D], BF16, dk)
        if 1 in kinds and self.do_mixer:
            self.DNK, self.DNK_k = c.dram("DNK", [NT, 2048], BF16, dk)
            self.DNV, self.DNV_k = c.dram("DNV", [NT, 4096], BF16, dk)
            self.DNZ, self.DNZ_k = c.dram("DNZ", [NT, 4096], BF16, dk)
            self.ODN, self.ODN_k = c.dram("ODN", [2, NT, 4096], F32, dk)
        self.dbg_out = {}
        for name, shape in self.dbg:
            self.dbg_out[name] = c.dram(name, shape, F32, "ExternalOutput")

    def alloc(self):
        c = self.c
        self.BIGA, _ = c.sb([128, KC, NT], BF16, "BIGA")
        self.BIGA_k = [Tk("biga%d" % t) for t in range(NTILE)]
        self.PS = [c.ps([128, 512], F32, "PS%d" % i) for i in range(8)]
        self._psk = {id(k) for (_, k) in self.PS}
        self.ident_f, self.ident_f_k = c.sb([128, 128], F32, "ident_f")
        self.ident_b, self.ident_b_k = c.sb([128, 128], BF16, "ident_b")
        self.ones_f, self.ones_f_k = c.sb([128, 128], F32, "ones_f")
        self.ones_b, self.ones_b_k = c.sb([128, 128], BF16, "ones_b")
        self.rt_b, self.rt_b_k = c.sb([128, 128], BF16, "rt_b")
        self.WT = [c.sb([128, KC, 512], BF16, "WT%d" % i) for i in range(2)]
        self.wt_i = 0
        self.WB = [c.sb([128, D], F32, "WB%d" % i) for i in range(2)]
        self.SB = [c.sb([128, D], F32, "SB%d" % i) for i in range(2)]
        self.GB = self.WB
        self.XT = [c.sb([128, D], F32, "XT%d" % i) for i in range(2)]
        self.HB = [c.sb([128, D], BF16, "HB%d" % i) for i in range(2)]
        self.small = [c.sb([128, 8], F32, "small%d" % i) for i in range(2)]
        self.misc = c.sb([128, 64], F32, 'misc')
        self.GATE = c.sb([128, NTILE, 32], F32, 'GATE')
        c.arena_init(42 * 1024)

    def load_consts(self):
        for name, dst_f, dst_fk, dst_b, dst_bk in (
            ("k_ident", self.ident_f, self.ident_f_k, self.ident_b, self.ident_b_k),
            ("k_ones", self.ones_f, self.ones_f_k, self.ones_b, self.ones_b_k),
        ):
            self.dma("sp", dst_f[:], self.kc[name][:, :], w=[dst_fk])
            self.op("dve", lambda e, a=dst_b, b=dst_f: e.tensor_copy(out=a[:], in_=b[:]), r=[dst_fk], w=[dst_bk])
        self.dma("pool", self.rt_b[:], self.kc["k_rt"][:, :], w=[self.rt_b_k])
        for t in range(NTILE):
            self.dma("sp", self.XR[t * 128:(t + 1) * 128, :], self.x_in[t * 128:(t + 1) * 128, :],
                     w=[self.XR_k[t]])

    def phase_mod(self):
        c = self.c
        c.arena_reset()
        cs, cs_k = c.asb([128, KC, 2], F32, "cs")
        sig, sig_k = c.asb([128, KC, 2], F32, "sig")
        self.dma("sp", cs[:], self.cin[:, :, :], w=[cs_k])
        self.op("act", lambda e: e.activation(out=sig[:], in_=cs[:], func=AF.Sigmoid), r=[cs_k], w=[sig_k])
        self.op("dve", lambda e: e.tensor_tensor(out=cs[:], in0=cs[:], in1=sig[:], op=ALU.mult), r=[sig_k, cs_k], w=[cs_k])
        AW = [c.asb([128, KC, 256], F32, "AW%d" % i) for i in range(2)]
        bias, bias_k = self.XT[0][0][0:2, :], self.XT[0][1]
        nrm, nrm_k = self.XT[1][0][0:2, :], self.XT[1][1]
        res, res_k = self.SB[0][0][0:2, :], self.SB[0][1]
        ps, ps_k = self.PS[0]
        i = 0
        for l in range(len(self.layers)):
            for v in range(6):
                self.dma("sp", bias[:], self.ada_b[l:l + 1, v * D:(v + 1) * D].partition_broadcast(2), w=[bias_k])
                if v in (1, 4):
                    nw = self.norm_mix_w if v == 1 else self.norm_ffn_w
                    self.dma("sp", nrm[:], nw[l:l + 1, :].partition_broadcast(2), w=[nrm_k])
                for b in range(8):
                    aw, aw_k = AW[i % 2]
                    i += 1
                    col = v * D + b * 256
                    self.dma("sp", aw[:], self.ada_w[l, :, col:col + 256].rearrange("(kc p) n -> p kc n", p=128), w=[aw_k])
                    for kc in range(KC):
                        self.op("pe", lambda e, aw=aw, kc=kc: e.matmul(ps[0:2, 0:256], lhsT=cs[:, kc, :], rhs=aw[:, kc, :],
                                                                      start=(kc == 0), stop=(kc == KC - 1)),
                                r=[cs_k, aw_k], w=[ps_k])
                    self.op("dve", lambda e, b=b: e.tensor_tensor(out=res[:, b * 256:(b + 1) * 256], in0=ps[0:2, 0:256],
                                                                 in1=bias[:, b * 256:(b + 1) * 256], op=ALU.add),
                            r=[ps_k, bias_k], w=[res_k])
                if v in (1, 4):
                    self.op("dve", lambda e: e.scalar_tensor_tensor(out=res[:], in0=res[:], scalar=1.0, in1=nrm[:],
                                                                   op0=ALU.add, op1=ALU.mult),
                            r=[res_k, nrm_k], w=[res_k])
                self.dma("sp", self.MOD[l, :, v * D:(v + 1) * D], res[:], r=[res_k], w=[self.MOD_k])

    def load_mod_tiles(self, l, sub):
        for ty in range(2):
            for j, (buf, bk) in enumerate((self.SB[ty], self.WB[ty])):
                v = sub * 3 + j
                self.dma("sp", buf[:], self.MOD[l, ty:ty + 1, v * D:(v + 1) * D].partition_broadcast(128),
                         r=[self.MOD_k], w=[bk])

    def load_gate_tiles(self, l, sub):
        for ty in range(2):
            buf, bk = self.GB[ty]
            v = sub * 3 + 2
            self.dma("sp", buf[:], self.MOD[l, ty:ty + 1, v * D:(v + 1) * D].partition_broadcast(128),
                     r=[self.MOD_k], w=[bk])

    def phase_norm(self, hrow_dram=None, hrow_k=None, router=None, skip_T=False):
        ps_t, ps_tk = self.PS[1]
        pst = ps_t[:].bitcast(BF16)
        for t in range(NTILE):
            ty = 1 if t < 2 else 0
            xt, xt_k = self.XT[t % 2]
            hb, hb_k = self.HB[t % 2]
            sm, sm_k = self.small[t % 2]
            self.dma("sp", xt[:], self.XR[t * 128:(t + 1) * 128, :], r=[self.XR_k[t]], w=[xt_k])
            self.op("act", lambda e, xt=xt, sm=sm, hb=hb: e.activation(out=hb[:], in_=xt[:], func=AF.Square, accum_out=sm[:, 0:1]),
                    r=[xt_k], w=[hb_k, sm_k])
            self.op("act", lambda e, sm=sm: e.activation(out=sm[:, 1:2], in_=sm[:, 0:1], func=AF.Sqrt, scale=1.0 / D, bias=EPS),
                    r=[sm_k], w=[sm_k])
            self.op("dve", lambda e, sm=sm: e.reciprocal(out=sm[:, 2:3], in_=sm[:, 1:2]), r=[sm_k], w=[sm_k])
            self.op("dve", lambda e, xt=xt, sm=sm, ty=ty: e.scalar_tensor_tensor(
                out=xt[:], in0=xt[:], scalar=sm[:, 2:3], in1=self.WB[ty][0][:], op0=ALU.mult, op1=ALU.mult),
                r=[xt_k, sm_k, self.WB[ty][1]], w=[xt_k])
            if router is None:
                self.op("pool", lambda e, xt=xt, hb=hb, ty=ty: e.tensor_tensor(out=hb[:], in0=xt[:], in1=self.SB[ty][0][:], op=ALU.add),
                        r=[xt_k, self.SB[ty][1]], w=[hb_k])
            else:
                self.op("pool", lambda e, xt=xt, ty=ty: e.tensor_tensor(out=xt[:], in0=xt[:], in1=self.SB[ty][0][:], op=ALU.add),
                        r=[xt_k, self.SB[ty][1]], w=[xt_k])
                self.op("act", lambda e, xt=xt, hb=hb: e.copy(out=hb[:], in_=xt[:]), r=[xt_k], w=[hb_k])
                router(t, xt, xt_k)
            if hrow_dram is not None:
                self.dma("sp", hrow_dram[t * 128:(t + 1) * 128, :], hb[:], r=[hb_k], w=[hrow_k[t]])
            if skip_T:
                continue
            for g in range(2):
                for j in range(8):
                    kc = g * 8 + j
                    self.op("pe", lambda e, hb=hb, kc=kc, j=j: e.transpose(out=pst[:, j * 128:(j + 1) * 128],
                                                                          in_=hb[:, kc * 128:(kc + 1) * 128], identity=self.ident_b[:]),
                            r=[hb_k, self.ident_b_k], w=[ps_tk])
                eng = "act" if g == 0 else "dve"
                if eng == "act":
                    self.op("act", lambda e, g=g, t=t: e.copy(out=self.BIGA[:, g * 8:(g + 1) * 8, t * 128:(t + 1) * 128],
                                                             in_=pst.rearrange("p (a b) -> p a b", a=8)),
                            r=[ps_tk], w=[self.BIGA_k[t]])
                else:
                    self.op("dve", lambda e, g=g, t=t: e.tensor_copy(out=self.BIGA[:, g * 8:(g + 1) * 8, t * 128:(t + 1) * 128],
                                                                    in_=pst.rearrange("p (a b) -> p a b", a=8)),
                            r=[ps_tk], w=[self.BIGA_k[t]])

    def load_w(self, w_ap_rows_cols, ncols, nk=KC):
        wt, wt_k = self.WT[self.wt_i % 2]
        self.wt_i += 1
        self.dma("pool", wt[:, 0:nk, 0:ncols], w_ap_rows_cols.rearrange("(kc p) n -> p kc n", p=128), w=[wt_k])
        return wt, wt_k

    TOKBLK = [(0, 256), (256, 512), (768, 512), (1280, 512), (1792, 512)]

    def linear_fm(self, w2d, col0, ncols, post):
        psi = 0
        for cb in range(0, ncols, 512):
            nb = min(512, ncols - cb)
            wt, wt_k = self.load_w(w2d[:, col0 + cb:col0 + cb + nb], nb)
            for jj in range(nb // 128):
                j = (cb // 128) + jj
                for bi, (t0, n) in enumerate(self.TOKBLK):
                    ps, ps_k = self.PS[2 + (psi % 2)]
                    psi += 1
                    rk = [self.BIGA_k[t] for t in range(t0 // 128, (t0 + n) // 128)] + [wt_k]
                    for kc in range(KC):
                        self.op("pe", lambda e, ps=ps, wt=wt, kc=kc, jj=jj, t0=t0, n=n: e.matmul(
                            ps[:, 0:n], lhsT=wt[:, kc, jj * 128:(jj + 1) * 128], rhs=self.BIGA[:, kc, t0:t0 + n],
                            start=(kc == 0), stop=(kc == KC - 1)), r=rk, w=[ps_k])
                    post(j, bi, t0, n, ps, ps_k)

    def linear_tm(self, w2d, col0, ncols, post, src=None, src_k=None, nk=KC):
        src = self.BIGA if src is None else src
        src_k = self.BIGA_k if src_k is None else src_k
        psi = 0
        for cb in range(0, ncols, 512):
            nb = min(512, ncols - cb)
            wt, wt_k = self.load_w(w2d[:, col0 + cb:col0 + cb + nb], nb, nk)
            for t in range(NTILE):
                ps, ps_k = self.PS[2 + (psi % 2)]
                psi += 1
                for kc in range(nk):
                    self.op("pe", lambda e, ps=ps, wt=wt, kc=kc, t=t, nb=nb: e.matmul(
                        ps[:, 0:nb], lhsT=src[:, kc, t * 128:(t + 1) * 128], rhs=wt[:, kc, 0:nb],
                        start=(kc == 0), stop=(kc == nk - 1)), r=[src_k[t], wt_k], w=[ps_k])
                post(cb, nb, t, ps, ps_k)

    def phase_oproj_residual(self, w2d, l):
        c = self.c
        self.load_gate_tiles(l, 0)
        c.arena_reset()
        self.RX = [c.asb([128, 512], F32, "RX%d" % i) for i in range(3)]
        self.rxi = 0

        def post(cb, nb, t, ps, ps_k):
            ty = 1 if t < 2 else 0
            rx, rx_k = self.RX[self.rxi % 3]
            self.rxi += 1
            self.dma("sp", rx[:], self.XR[t * 128:(t + 1) * 128, cb:cb + nb], r=[self.XR_k[t]], w=[rx_k])
            self.op("dve", lambda e, rx=rx, ps=ps, ty=ty, cb=cb, nb=nb: e.tensor_tensor(
                out=ps[:, 0:nb], in0=ps[:, 0:nb], in1=self.GB[ty][0][:, cb:cb + nb], op=ALU.mult),
                r=[ps_k, self.GB[ty][1]], w=[ps_k])
            self.op("dve", lambda e, rx=rx, ps=ps, nb=nb: e.tensor_tensor(out=rx[:, 0:nb], in0=ps[:, 0:nb], in1=rx[:, 0:nb], op=ALU.add),
                    r=[ps_k, rx_k], w=[rx_k])
            self.dma("sp", self.XR[t * 128:(t + 1) * 128, cb:cb + nb], rx[:, 0:nb], r=[rx_k], w=[self.XR_k[t]])

        self.linear_tm(w2d, 0, D, post)

    def load_rope_tables(self):
        c = self.c
        self.cosT, self.cosT_k = c.asb([128, L], BF16, "cosT")
        self.sinT, self.sinT_k = c.asb([128, L], BF16, "sinT")
        self.dma("pool", self.cosT[:], self.kc["k_cos"][:, :], w=[self.cosT_k])
        self.dma("pool", self.sinT[:], self.kc["k_sin"][:, :], w=[self.sinT_k])
        self.RP = [(c.asb([128, 512], BF16, "RPa%d" % i), c.asb([128, 512], BF16, "RPb%d" % i)) for i in range(2)]
        self.rpi = 0

    def rope_store(self, ps, ps_k, t0, n, dst_dram, dst_k):
        c = self.c
        (qa, qa_k), (qb, qb_k) = self.RP[self.rpi % 2]
        self.rpi += 1
        self.op("act", lambda e: e.copy(out=qa[:, 0:n], in_=ps[:, 0:n]), r=[ps_k], w=[qa_k])
        if t0 >= LC:
            l0 = t0 - LC
            pr, pr_k = self.PS[4]
            self.op("pe", lambda e: e.matmul(pr[:, 0:n], lhsT=self.rt_b[:], rhs=qa[:, 0:n], start=True, stop=True),
                    r=[qa_k, self.rt_b_k], w=[pr_k])
            self.op("dve", lambda e: e.tensor_tensor(out=qb[:, 0:n], in0=pr[:, 0:n], in1=self.sinT[:, l0:l0 + n], op=ALU.mult),
                    r=[pr_k, self.sinT_k], w=[qb_k])
            self.op("pool", lambda e: e.tensor_tensor(out=qa[:, 0:n], in0=qa[:, 0:n], in1=self.cosT[:, l0:l0 + n], op=ALU.mult),
                    r=[qa_k, self.cosT_k], w=[qa_k])
            self.op("pool", lambda e: e.tensor_tensor(out=qa[:, 0:n], in0=qa[:, 0:n], in1=qb[:, 0:n], op=ALU.add),
                    r=[qa_k, qb_k], w=[qa_k])
        self.dma("sp", dst_dram[:, t0:t0 + n], qa[:, 0:n], r=[qa_k], w=[dst_k])

    def mixer_diff(self, l, j):
        c = self.c
        lam_init = 0.8 - 0.6 * math.exp(-0.3 * l)
        wqkv = self.da_w_qkv[j]
        c.arena_reset()
        self.load_rope_tables()
        self.VS = [c.asb([128, 512], BF16, "VS%d" % i) for i in range(2)]
        self.vsi = 0
        self.lamt = c.asb([1, 4, 64], F32, "lamt")
        self.lamp = c.asb([1, 8], F32, "lamp")
        self.lamc = (self.misc[0][:, 0:2], Tk("lamc"))
        self.subw = (self.misc[0][:, 2:4], Tk("subw"))
        self.linear_fm(wqkv, 0, D, lambda jj, bi, t0, n, ps, ps_k: self.rope_store(ps, ps_k, t0, n, self.QT[jj], self.QT_k))
        self.linear_fm(wqkv, D, D, lambda jj, bi, t0, n, ps, ps_k: self.rope_store(ps, ps_k, t0, n, self.KT[jj], self.KT_k))

        def vpost(cb, nb, t, ps, ps_k):
            vs, vs_k = self.VS[self.vsi % 2]
            self.vsi += 1
            self.op("act", lambda e: e.copy(out=vs[:, 0:nb], in_=ps[:, 0:nb]), r=[ps_k], w=[vs_k])
            self.dma("sp", self.VV[t * 128:(t + 1) * 128, cb:cb + nb], vs[:, 0:nb], r=[vs_k], w=[self.VV_k])

        self.linear_tm(wqkv, 2 * D, D, vpost)
        lamt, lamt_k = self.lamt
        lamp, lamp_k = self.lamp
        lamc, lamc_k = self.lamc
        subw, subw_k = self.subw
        self.dma("sp", lamt[:], self.da_lambda[j:j + 1, :, :], w=[lamt_k])
        self.op("dve", lambda e: e.tensor_tensor(out=lamt[:, 0, :], in0=lamt[:, 0, :], in1=lamt[:, 1, :], op=ALU.mult), r=[lamt_k], w=[lamt_k])
        self.op("dve", lambda e: e.tensor_tensor(out=lamt[:, 2, :], in0=lamt[:, 2, :], in1=lamt[:, 3, :], op=ALU.mult), r=[lamt_k], w=[lamt_k])
        self.op("dve", lambda e: e.reduce_sum(out=lamp[:, 0:1], in_=lamt[:, 0, :], axis=AX.X), r=[lamt_k], w=[lamp_k])
        self.op("dve", lambda e: e.reduce_sum(out=lamp[:, 1:2], in_=lamt[:, 2, :], axis=AX.X), r=[lamt_k], w=[lamp_k])
        self.op("act", lambda e: e.activation(out=lamp[:, 2:4], in_=lamp[:, 0:2], func=AF.Exp), r=[lamp_k], w=[lamp_k])
        self.op("dve", lambda e: e.scalar_tensor_tensor(out=lamp[:, 4:5], in0=lamp[:, 3:4], scalar=-lam_init, in1=lamp[:, 2:3],
                                                       op0=ALU.add, op1=ALU.subtract), r=[lamp_k], w=[lamp_k])
        psl, psl_k = self.PS[4]
        self.op("pe", lambda e: e.matmul(psl[:, 0:1], lhsT=self.ones_f[0:1, :], rhs=lamp[0:1, 4:5], start=True, stop=True),
                r=[self.ones_f_k, lamp_k], w=[psl_k])
        self.op("dve", lambda e: e.tensor_copy(out=lamc[:, 0:1], in_=psl[:, 0:1]), r=[psl_k], w=[lamc_k])
        self.dma("sp", subw[:, 0:1], self.da_subln_w[j:j + 1, :].rearrange("o d -> d o"), w=[subw_k])
        self.op("dve", lambda e: e.tensor_scalar(out=subw[:, 1:2], in0=subw[:, 0:1], scalar1=(1.0 - lam_init), scalar2=None, op0=ALU.mult),
                r=[subw_k], w=[subw_k])
        self.attention_core(16, diff=True)

    def attention_core(self, nheads, diff):
        c = self.c
        c.arena_reset()
        self.AQ = [c.asb([128, NT], BF16, "AQ%d" % i) for i in range(2)]
        self.AK = [c.asb([128, NT], BF16, "AK%d" % i) for i in range(2)]
        self.AV = [c.asb([128, NTILE, 128], BF16, "AV%d" % i) for i in range(2)]
        self.ET = [c.asb([128, 512], BF16, "ET%d" % i) for i in range(4)]
        self.eti = 0
        self.CMB = [c.asb([128, 512], F32, "CMB%d" % i) for i in range(4)]
        self.SQB = c.asb([128, 512], BF16, "SQB")
        scale = 64 ** -0.5
        lamc, lamc_k = self.lamc
        subw, subw_k = self.subw
        for h in range(nheads):
            aq, aq_k = self.AQ[h % 2]
            ak, ak_k = self.AK[h % 2]
            av, av_k = self.AV[h % 2]
            self.dma("sp", aq[:], self.QT[h], r=[self.QT_k], w=[aq_k])
            self.dma("sp", ak[:], self.KT[h], r=[self.KT_k], w=[ak_k])
            self.dma("sp", av[:], self.VV[:, h * 128:(h + 1) * 128].rearrange("(t p) d -> p t d", p=128), r=[self.VV_k], w=[av_k])
            for bi, (t0, n) in enumerate(self.TOKBLK):
                nkt = 2 if bi == 0 else NTILE
                acc = []
                for comp in range(2):
                    po, po_k = self.PS[4 + comp]
                    pz, pz_k = self.PS[6 + comp]
                    p0 = comp * 64
                    for kt in range(nkt):
                        pss, pss_k = self.PS[kt % 2]
                        et, et_k = self.ET[self.eti % 4]
                        self.eti += 1
                        self.op("pe", lambda e, pss=pss, ak=ak, aq=aq, kt=kt, p0=p0, t0=t0, n=n: e.matmul(
                            pss[:, 0:n], lhsT=ak[p0:p0 + 64, kt * 128:(kt + 1) * 128], rhs=aq[p0:p0 + 64, t0:t0 + n],
                            start=True, stop=True), r=[ak_k, aq_k], w=[pss_k])
                        self.op("act", lambda e, pss=pss, et=et, n=n: e.activation(out=et[:, 0:n], in_=pss[:, 0:n], func=AF.Exp, scale=scale),
                                r=[pss_k], w=[et_k])
                        self.op("pe", lambda e, po=po, av=av, et=et, kt=kt, n=n, nkt=nkt: e.matmul(
                            po[:, 0:n], lhsT=av[:, kt, :], rhs=et[:, 0:n], start=(kt == 0), stop=(kt == nkt - 1)),
                            r=[av_k, et_k], w=[po_k])
                        self.op("pe", lambda e, pz=pz, et=et, kt=kt, n=n, nkt=nkt: e.matmul(
                            pz[:, 0:n], lhsT=self.ones_b[:], rhs=et[:, 0:n], start=(kt == 0), stop=(kt == nkt - 1)),
                            r=[self.ones_b_k, et_k], w=[pz_k])
                    acc.append((po, po_k, pz, pz_k))
                (r0, r0_k), (r1, r1_k), (o0, o0_k), (o1, o1_k) = self.CMB
                (po0, po0_k, pz0, pz0_k), (po1, po1_k, pz1, pz1_k) = acc
                self.op("dve", lambda e, n=n: e.reciprocal(out=r0[:, 0:n], in_=pz0[:, 0:n]), r=[pz0_k], w=[r0_k])
                self.op("dve", lambda e, n=n: e.reciprocal(out=r1[:, 0:n], in_=pz1[:, 0:n]), r=[pz1_k], w=[r1_k])
                self.op("dve", lambda e, n=n: e.tensor_tensor(out=o0[:, 0:n], in0=po0[:, 0:n], in1=r0[:, 0:n], op=ALU.mult), r=[po0_k, r0_k], w=[o0_k])
                self.op("dve", lambda e, n=n: e.tensor_tensor(out=o1[:, 0:n], in0=po1[:, 0:n], in1=r1[:, 0:n], op=ALU.mult), r=[po1_k, r1_k], w=[o1_k])
                self.op("dve", lambda e, n=n: e.scalar_tensor_tensor(out=o0[:, 0:n], in0=o1[:, 0:n], scalar=lamc[:, 0:1], in1=o0[:, 0:n],
                                                                    op0=ALU.mult, op1=ALU.add), r=[o0_k, o1_k, lamc_k], w=[o0_k])
                sqb, sqb_k = self.SQB
                self.op("act", lambda e, n=n: e.activation(out=sqb[:, 0:n], in_=o0[:, 0:n], func=AF.Square), r=[o0_k], w=[sqb_k])
                pss, pss_k = self.PS[0]
                self.op("pe", lambda e, n=n, pss=pss: e.matmul(pss[:, 0:n], lhsT=self.ones_b[:], rhs=sqb[:, 0:n], start=True, stop=True),
                        r=[sqb_k, self.ones_b_k], w=[pss_k])
                self.op("act", lambda e, n=n, pss=pss: e.activation(out=r0[:, 0:n], in_=pss[:, 0:n], func=AF.Sqrt, scale=1.0 / 128, bias=EPS),
                        r=[pss_k], w=[r0_k])
                self.op("dve", lambda e, n=n: e.reciprocal(out=r0[:, 0:n], in_=r0[:, 0:n]), r=[r0_k], w=[r0_k])
                wk = [self.BIGA_k[t] for t in range(t0 // 128, (t0 + n) // 128)]
                self.op("dve", lambda e, n=n, t0=t0, h=h: e.scalar_tensor_tensor(
                    out=self.BIGA[:, h, t0:t0 + n], in0=o0[:, 0:n], scalar=subw[:, 1:2], in1=r0[:, 0:n], op0=ALU.mult, op1=ALU.mult),
                    r=[o0_k, r0_k, subw_k], w=wk)

    def phase_final(self):
        fw, fw_k = self.WB[0]
        self.dma("sp", fw[:], self.final_norm_w[0:1, :].partition_broadcast(128), w=[fw_k])
        for t in range(2, NTILE):
            xt, xt_k = self.XT[t % 2]
            sm, sm_k = self.small[t % 2]
            self.dma("sp", xt[:], self.XR[t * 128:(t + 1) * 128, :], r=[self.XR_k[t]], w=[xt_k])
            hb, hb_k = self.HB[t % 2]
            self.op("act", lambda e, xt=xt, sm=sm, hb=hb: e.activation(out=hb[:], in_=xt[:], func=AF.Square, accum_out=sm[:, 0:1]),
                    r=[xt_k], w=[hb_k, sm_k])
            self.op("act", lambda e, sm=sm: e.activation(out=sm[:, 1:2], in_=sm[:, 0:1], func=AF.Sqrt, scale=1.0 / D, bias=EPS),
                    r=[sm_k], w=[sm_k])
            self.op("dve", lambda e, sm=sm: e.reciprocal(out=sm[:, 2:3], in_=sm[:, 1:2]), r=[sm_k], w=[sm_k])
            self.op("dve", lambda e, xt=xt, sm=sm: e.scalar_tensor_tensor(
                out=xt[:], in0=xt[:], scalar=sm[:, 2:3], in1=fw[:], op0=ALU.mult, op1=ALU.mult),
                r=[xt_k, sm_k, fw_k], w=[xt_k])
            self.out_ops.append(self.dma("sp", self.y_out[(t - 2) * 128:(t - 1) * 128, :], xt[:], r=[xt_k], w=[self.y_out_k]))

    def build(self):
        self.declare_io()
        self.alloc()
        self.load_consts()
        self.phase_mod()
        for li, labs in enumerate(self.layers):
            kind = [0, 1, 2, 0][labs]
            ji = ([0, 0, 0, 1][labs]) if self.fused else 0
            if self.do_mixer:
                self.load_mod_tiles(li, 0)
                self.phase_norm()
                if kind == 0:
                    self.mixer_diff(labs, ji)
                    self.phase_oproj_residual(self.da_w_o[ji], li)
                elif kind == 1:
                    self.cur_li = li
                    self.mixer_dn()
                else:
                    self.mixer_win()
                    self.phase_oproj_residual(self.wa_w_o[0], li)
            if self.do_moe:
                self.load_mod_tiles(li, 1)
                self.phase_moe_b(li)
        if self.final:
            self.phase_final()
        else:
            for t in range(NTILE):
                self.out_ops.append(self.dma("sp", self.x_out[t * 128:(t + 1) * 128, :], self.XR[t * 128:(t + 1) * 128, :],
                                             r=[self.XR_k[t]], w=[self.x_out_k]))
        self.p.emit(self.out_ops)


def build_nc(**kw):
    nc = bass.Bass("TRN2", target_bir_lowering=False)
    with ExitStack() as es:
        b = Builder(nc, es, **kw)
        b.build()
    return nc, b


def _launch(inputs, xcur, layer, do_mixer, do_moe, final):
    nc, bld = build_nc(layer_abs=layer, do_mixer=do_mixer, do_moe=do_moe, final=final)
    print('nops', len(bld.p.ops), flush=True)
    j = [0, 0, 0, 1][layer]
    per_layer = {"ada_w": inputs["ada_w"][layer:layer + 1], "ada_b": inputs["ada_b"][layer:layer + 1],
                 "norm_mix_w": inputs["norm_mix_w"][layer:layer + 1], "norm_ffn_w": inputs["norm_ffn_w"][layer:layer + 1],
                 "da_w_qkv": inputs["da_w_qkv"][j:j + 1], "da_lambda": inputs["da_lambda"][j:j + 1],
                 "da_subln_w": inputs["da_subln_w"][j:j + 1], "da_w_o": inputs["da_w_o"][j:j + 1],
                 "dn_w_in": inputs["dn_w_in"], "dn_conv_w": inputs["dn_conv_w"].reshape(1, 5, 8192),
                 "dn_a_log": inputs["dn_a_log"], "dn_dt_bias": inputs["dn_dt_bias"], "dn_norm_w": inputs["dn_norm_w"],
                 "dn_w_o": inputs["dn_w_o"],
                 "wa_w_qkv": inputs["wa_w_qkv"], "wa_sinks": inputs["wa_sinks"], "wa_w_o": inputs["wa_w_o"],
                 "moe_wg": inputs["moe_wg"][layer:layer + 1], "moe_bg": inputs["moe_bg"][layer:layer + 1],
                 "moe_we": inputs["moe_we"][layer:layer + 1], "moe_be": inputs["moe_be"][layer:layer + 1],
                 "moe_w13": inputs["moe_w13"][layer:layer + 1], "moe_w2": inputs["moe_w2"][layer:layer + 1],
                 "final_norm_w": inputs["final_norm_w"].reshape(1, D)}
    consts = _consts()
    dummy = np.zeros((1, 1), np.float32)
    in_maps = []
    for b in range(len(xcur)):
        m = {"x_in": xcur[b]}
        cin = np.stack([inputs["c"][b], inputs["c_ctx"]], -1)
        m["cin"] = np.ascontiguousarray(cin.reshape(KC, 128, 2).transpose(1, 0, 2))
        for name, shape in bld.in_shapes.items():
            if name in m:
                continue
            if name not in bld.used:
                m[name] = dummy
            elif name in consts:
                m[name] = consts[name]
            else:
                m[name] = np.ascontiguousarray(per_layer[name])
        in_maps.append(m)
    res = run_bass_kernel_spmd(nc, in_maps, core_ids=list(range(len(xcur))))
    key = "y_out" if final else "x_out"
    global _last_res
    _last_res = res.results
    return [r[key] for r in res.results]


def kernel(**inputs):
    inputs = {k: np.asarray(v) for k, v in inputs.items()}
    nb = inputs["x"].shape[0]
    nc, bld = build_nc(fused=True)
    consts = _consts()
    shared = {k: np.ascontiguousarray(inputs[k]) for k in (
        "ada_w", "ada_b", "norm_mix_w", "norm_ffn_w", "da_w_qkv", "da_lambda", "da_subln_w", "da_w_o",
        "dn_w_in", "dn_conv_w", "dn_a_log", "dn_dt_bias", "dn_norm_w", "dn_w_o", "wa_w_qkv", "wa_sinks", "wa_w_o",
        "moe_wg", "moe_bg", "moe_we", "moe_be", "moe_w13", "moe_w2")}
    shared["final_norm_w"] = inputs["final_norm_w"].reshape(1, D)
    shared.update(consts)
    in_maps = []
    for b in range(nb):
        m = dict(shared)
        m["x_in"] = np.ascontiguousarray(np.concatenate([inputs["ctx"][b], inputs["x"][b]], 0))
        cin = np.stack([inputs["c"][b], inputs["c_ctx"]], -1)
        m["cin"] = np.ascontiguousarray(cin.reshape(KC, 128, 2).transpose(1, 0, 2))
        in_maps.append(m)
    res = run_bass_kernel_spmd(nc, in_maps, core_ids=list(range(nb)))
    return np.stack([r["y_out"] for r in res.results], 0).astype(np.float32)


def _phase_moe(self, l):
    c = self.c
    c.arena_reset()
    gate, gate_k = self.GATE
    ht32, ht32_k = c.asb([128, KC, 128], F32, "ht32")
    wr, wr_k = c.asb([128, KC, 36], F32, "wr")
    br, br_k = c.asb([128, 36], F32, "br")
    lg, lg_k = c.asb([128, 36], F32, "lg")
    rs_, rs_k = c.asb([128, 64], F32, "rsm")
    self.dma("sp", wr[:, :, 0:4], self.moe_wg[l].rearrange("(kc p) n -> p kc n", p=128), w=[wr_k])
    self.dma("sp", wr[:, :, 4:36], self.moe_we[l].rearrange("(kc p) n -> p kc n", p=128), w=[wr_k])
    self.dma("sp", br[:, 0:4], self.moe_bg[l:l + 1, :].partition_broadcast(128), w=[br_k])
    self.dma("sp", br[:, 4:36], self.moe_be[l:l + 1, :].partition_broadcast(128), w=[br_k])
    R = lambda a, b=None: rs_[:, a:(a + 1 if b is None else b)]

    def router(t, xt, xt_k):
        for g in range(4):
            pt, pt_k = self.PS[2 + (g % 2)]
            for jx in range(4):
                kc = g * 4 + jx
                self.op("pe", lambda e, pt=pt, jx=jx, kc=kc, xt=xt: e.transpose(out=pt[:, jx * 128:(jx + 1) * 128],
                                                                              in_=xt[:, kc * 128:(kc + 1) * 128], identity=self.ident_f[:]),
                        r=[xt_k, self.ident_f_k], w=[pt_k])
            self.op("dve", lambda e, pt=pt, g=g: e.tensor_copy(out=ht32[:, g * 4:(g + 1) * 4, :], in_=pt[:].rearrange("p (a b) -> p a b", a=4)),
                    r=[pt_k], w=[ht32_k])
        pl, pl_k = self.PS[4]
        for kc in range(KC):
            self.op("pe", lambda e, kc=kc: e.matmul(pl[:, 0:36], lhsT=ht32[:, kc, :], rhs=wr[:, kc, :], start=(kc == 0), stop=(kc == KC - 1)),
                    r=[ht32_k, wr_k], w=[pl_k])
        V = lambda fn, r=(), w=(): self.op("dve", fn, r=list(r) + [rs_k], w=list(w) + [rs_k])
        self.op("dve", lambda e: e.tensor_tensor(out=lg[:], in0=pl[:, 0:36], in1=br[:], op=ALU.add), r=[pl_k, br_k], w=[lg_k])
        V(lambda e: e.reduce_max(out=R(0), in_=lg[:, 0:4], axis=AX.X), r=[lg_k])
        V(lambda e: e.tensor_scalar(out=R(1), in0=R(0), scalar1=-1.0, scalar2=None, op0=ALU.mult))
        self.op("act", lambda e: e.activation(out=R(56, 60), in_=lg[:, 0:4], func=AF.Exp, bias=R(1), accum_out=R(2)), r=[lg_k, rs_k], w=[rs_k])
        V(lambda e: e.reciprocal(out=R(3), in_=R(2)))
        V(lambda e: e.tensor_scalar(out=R(4, 8), in0=lg[:, 0:4], scalar1=R(0), scalar2=None, op0=ALU.is_equal), r=[lg_k])
        V(lambda e: e.tensor_scalar(out=R(8, 16), in0=lg[:, 4:12], scalar1=R(4), scalar2=None, op0=ALU.mult), r=[lg_k])
        for g in range(1, 4):
            V(lambda e, g=g: e.scalar_tensor_tensor(out=R(8, 16), in0=lg[:, 4 + 8 * g:12 + 8 * g], scalar=R(4 + g), in1=R(8, 16),
                                                   op0=ALU.mult, op1=ALU.add), r=[lg_k])
        V(lambda e: e.reduce_max(out=R(40), in_=R(8, 16), axis=AX.X))
        V(lambda e: e.tensor_scalar(out=R(16, 24), in0=R(8, 16), scalar1=R(40), scalar2=None, op0=ALU.is_equal))
        V(lambda e: e.scalar_tensor_tensor(out=R(24, 32), in0=R(16, 24), scalar=-1e30, in1=R(8, 16), op0=ALU.mult, op1=ALU.add))
        V(lambda e: e.reduce_max(out=R(41), in_=R(24, 32), axis=AX.X))
        V(lambda e: e.tensor_scalar(out=R(32, 40), in0=R(24, 32), scalar1=R(41), scalar2=None, op0=ALU.is_equal))
        V(lambda e: e.tensor_tensor(out=R(42), in0=R(41), in1=R(40), op=ALU.subtract))
        self.op("act", lambda e: e.activation(out=R(43), in_=R(42), func=AF.Exp), r=[rs_k], w=[rs_k])
        V(lambda e: e.tensor_scalar(out=R(44), in0=R(43), scalar1=1.0, scalar2=None, op0=ALU.add))
        V(lambda e: e.reciprocal(out=R(44), in_=R(44)))
        V(lambda e: e.tensor_tensor(out=R(45), in0=R(44), in1=R(3), op=ALU.mult))
        V(lambda e: e.tensor_tensor(out=R(46), in0=R(45), in1=R(43), op=ALU.mult))
        V(lambda e: e.tensor_scalar(out=R(48, 56), in0=R(16, 24), scalar1=R(45), scalar2=None, op0=ALU.mult))
        V(lambda e: e.scalar_tensor_tensor(out=R(48, 56), in0=R(32, 40), scalar=R(46), in1=R(48, 56), op0=ALU.mult, op1=ALU.add))
        for g in range(4):
            self.op("dve", lambda e, g=g, t=t: e.tensor_scalar(out=gate[:, t, g * 8:(g + 1) * 8], in0=R(48, 56), scalar1=R(4 + g),
                                                              scalar2=None, op0=ALU.mult), r=[rs_k], w=[gate_k])

    self.phase_norm(router=router)
    self.load_gate_tiles(l, 1)
    c.arena_reset()
    actt, _ = c.asb([128, 6, NT], BF16, "actt")
    actt_k = [Tk("actt%d" % i) for i in range(5)]
    ysb = [c.asb([128, 512], F32, "ysb%d" % i) for i in range(3)]
    if not hasattr(self, "YACC"):
        self.YACC, _ = c.dram("YACC", [NT, D], F32)
        self.YACC_k = [Tk("yacc%d" % t) for t in range(NTILE)]
    zt, zt_k = self.XT[0]
    self.op("pool", lambda e: e.memset(zt[:], 0.0), w=[zt_k])
    for t in range(NTILE):
        self.dma("sp", self.YACC[t * 128:(t + 1) * 128, :], zt[:], r=[zt_k], w=[self.YACC_k[t]])
    st = {"i": 0}
    blk_of_tile = {}
    for bi, (t0, n) in enumerate(self.TOKBLK):
        for t in range(t0 // 128, (t0 + n) // 128):
            blk_of_tile[t] = bi
    for ex in range(32):
        def post13(j, bi, t0, n, ps, ps_k):
            if j < 6:
                self.op("act", lambda e, j=j, t0=t0, n=n, ps=ps: e.activation(out=actt[:, j, t0:t0 + n], in_=ps[:, 0:n], func=AF.Silu),
                        r=[ps_k], w=[actt_k[bi]])
            else:
                self.op("dve", lambda e, j=j, t0=t0, n=n, ps=ps: e.tensor_tensor(out=actt[:, j - 6, t0:t0 + n], in0=ps[:, 0:n],
                                                                               in1=actt[:, j - 6, t0:t0 + n], op=ALU.mult),
                        r=[ps_k, actt_k[bi]], w=[actt_k[bi]])

        def post2(cb, nb, t, ps, ps_k, ex=ex):
            yb, yb_k = ysb[st["i"] % 3]
            st["i"] += 1
            self.op("dve", lambda e, yb=yb, ps=ps, t=t: e.tensor_scalar(out=yb[:, 0:nb], in0=ps[:, 0:nb], scalar1=gate[:, t, ex:ex + 1],
                                                                       scalar2=None, op0=ALU.mult), r=[ps_k, gate_k], w=[yb_k])
            self.p.op("pool", lambda e, yb=yb, t=t, cb=cb: e.dma_start(out=self.YACC[t * 128:(t + 1) * 128, cb:cb + nb], in_=yb[:, 0:nb],
                                                                       accum_op=ALU.add), r=[yb_k], w=[self.YACC_k[t]], dma=True)

        self.linear_fm(self.moe_w13[l, ex], 0, 1536, post13)
        self.linear_tm(self.moe_w2[l, ex], 0, D, post2, src=actt, src_k=[actt_k[blk_of_tile[t]] for t in range(NTILE)], nk=6)
    for t in range(NTILE):
        ty = 1 if t < 2 else 0
        ya, ya_k = self.XT[t % 2]
        xa, xa_k = self.SB[t % 2]
        self.dma("sp", ya[:], self.YACC[t * 128:(t + 1) * 128, :], r=[self.YACC_k[t]], w=[ya_k])
        self.dma("sp", xa[:], self.XR[t * 128:(t + 1) * 128, :], r=[self.XR_k[t]], w=[xa_k])
        self.op("pool", lambda e, ya=ya, ty=ty: e.tensor_tensor(out=ya[:], in0=ya[:], in1=self.GB[ty][0][:], op=ALU.mult),
                r=[ya_k, self.GB[ty][1]], w=[ya_k])
        self.op("dve", lambda e, ya=ya, xa=xa: e.tensor_tensor(out=xa[:], in0=xa[:], in1=ya[:], op=ALU.add), r=[ya_k, xa_k], w=[xa_k])
        self.dma("sp", self.XR[t * 128:(t + 1) * 128, :], xa[:], r=[xa_k], w=[self.XR_k[t]])


Builder.phase_moe = _phase_moe


def _mixer_win(self):
    c = self.c
    w = self.wa_w_qkv[0]
    c.arena_reset()
    self.load_rope_tables()
    self.VS = [c.asb([128, 512], BF16, "VS%d" % i) for i in range(2)]
    self.vsi = 0
    self.linear_fm(w, 0, D, lambda jj, bi, t0, n, ps, ps_k: self.rope_store(ps, ps_k, t0, n, self.QT[jj], self.QT_k))
    wt, wt_k = self.WT[self.wt_i % 2]
    self.wt_i += 1
    for kvh in range(4):
        for half in range(2):
            self.dma("pool", wt[:, :, (2 * kvh + half) * 64:(2 * kvh + half + 1) * 64],
                     w[:, D + kvh * 64:D + (kvh + 1) * 64].rearrange("(kc p) n -> p kc n", p=128), w=[wt_k])
    psi = 0
    for kvh in range(4):
        for bi, (t0, n) in enumerate(self.TOKBLK):
            ps, ps_k = self.PS[2 + (psi % 2)]
            psi += 1
            rk = [self.BIGA_k[t] for t in range(t0 // 128, (t0 + n) // 128)] + [wt_k]
            for kc in range(KC):
                self.op("pe", lambda e, ps=ps, kc=kc, kvh=kvh, t0=t0, n=n: e.matmul(
                    ps[:, 0:n], lhsT=wt[:, kc, kvh * 128:(kvh + 1) * 128], rhs=self.BIGA[:, kc, t0:t0 + n],
                    start=(kc == 0), stop=(kc == KC - 1)), r=rk, w=[ps_k])
            self.rope_store(ps, ps_k, t0, n, self.KT[kvh], self.KT_k)

    def vpost(cb, nb, t, ps, ps_k):
        vs, vs_k = self.VS[self.vsi % 2]
        self.vsi += 1
        self.op("act", lambda e: e.copy(out=vs[:, 0:nb], in_=ps[:, 0:nb]), r=[ps_k], w=[vs_k])
        self.dma("sp", self.VV[t * 128:(t + 1) * 128, cb:cb + nb], vs[:, 0:nb], r=[vs_k], w=[self.VV_k])

    self.linear_tm(w, D + 256, 256, vpost)
    c.arena_reset()
    AQ = [c.asb([128, NT], BF16, "wAQ%d" % i) for i in range(2)]
    AK = [c.asb([128, NT], BF16, "wAK%d" % i) for i in range(2)]
    VP = [c.asb([128, NTILE, 128], BF16, "wVP%d" % i) for i in range(2)]
    ET = [c.asb([128, 512], BF16, "wET%d" % i) for i in range(4)]
    mk, mk_k = c.asb([128, 6, 512], BF16, "wmask")
    r0, r0_k = c.asb([128, 512], F32, "wr0")
    sx, sx_k = c.asb([128, 32], F32, "wsink")
    self.dma("pool", mk[:], self.kc["k_wmask"][:, :].rearrange("p (a b) -> p a b", a=6), w=[mk_k])
    self.dma("sp", sx[:], self.wa_sinks[0:1, :].partition_broadcast(128), w=[sx_k])
    self.op("act", lambda e: e.activation(out=sx[:], in_=sx[:], func=AF.Exp), r=[sx_k], w=[sx_k])
    for par in range(2):
        vp, vp_k = VP[par]
        self.op("pool", lambda e, vp=vp: e.memset(vp[:], 0.0), w=[vp_k])
    scale = 64 ** -0.5
    eti = 0
    for h in range(32):
        kvh, par = h // 8, h % 2
        aq, aq_k = AQ[(h // 2) % 2]
        ak, ak_k = AK[kvh % 2]
        if par == 0:
            self.dma("sp", aq[:], self.QT[h // 2], r=[self.QT_k], w=[aq_k])
        if h % 8 == 0:
            self.dma("sp", ak[:], self.KT[kvh], r=[self.KT_k], w=[ak_k])
            for p2 in range(2):
                vp, vp_k = VP[p2]
                self.dma("sp", vp[:, :, p2 * 64:(p2 + 1) * 64],
                         self.VV[:, kvh * 64:(kvh + 1) * 64].rearrange("(t p) d -> p t d", p=128), r=[self.VV_k], w=[vp_k])
        vp, vp_k = VP[par]
        p0 = par * 64
        for bi, (t0, n) in enumerate(self.TOKBLK):
            kts = [(0, None), (1, None)]
            if bi > 0:
                sblk = (t0 - LC) // 128
                for ri, r in enumerate(range(-1, 5)):
                    if 0 <= sblk + r < 16:
                        kts.append((2 + sblk + r, ri))
            po, po_k = self.PS[4]
            pz, pz_k = self.PS[6]
            for i, (kt, ri) in enumerate(kts):
                pss, pss_k = self.PS[i % 2]
                et, et_k = ET[eti % 4]
                eti += 1
                self.op("pe", lambda e, pss=pss, ak=ak, aq=aq, kt=kt, t0=t0, n=n, ri=ri, p0=p0: e.matmul(
                    pss[:, 0:n], lhsT=ak[p0:p0 + 64, kt * 128:(kt + 1) * 128], rhs=aq[p0:p0 + 64, t0:t0 + n],
                    start=True, stop=True), r=[ak_k, aq_k], w=[pss_k])
                self.op("act", lambda e, pss=pss, et=et, n=n: e.activation(out=et[:, 0:n], in_=pss[:, 0:n], func=AF.Exp, scale=scale),
                        r=[pss_k], w=[et_k])
                if ri is not None:
                    self.op("pool", lambda e, et=et, n=n, ri=ri: e.tensor_tensor(out=et[:, 0:n], in0=et[:, 0:n], in1=mk[:, ri, 0:n], op=ALU.mult),
                            r=[et_k, mk_k], w=[et_k])
                last = (i == len(kts) - 1)
                self.op("pe", lambda e, et=et, kt=kt, n=n, i=i, last=last, vp=vp: e.matmul(
                    po[:, 0:n], lhsT=vp[:, kt, :], rhs=et[:, 0:n], start=(i == 0), stop=last), r=[vp_k, et_k], w=[po_k])
                self.op("pe", lambda e, et=et, n=n, i=i, last=last: e.matmul(
                    pz[:, 0:n], lhsT=self.ones_b[:], rhs=et[:, 0:n], start=(i == 0), stop=last), r=[self.ones_b_k, et_k], w=[pz_k])
            self.op("dve", lambda e, n=n, h=h: e.tensor_scalar(out=r0[:, 0:n], in0=pz[:, 0:n], scalar1=sx[:, h:h + 1], scalar2=None, op0=ALU.add),
                    r=[pz_k, sx_k], w=[r0_k])
            self.op("dve", lambda e, n=n: e.reciprocal(out=r0[:, 0:n], in_=r0[:, 0:n]), r=[r0_k], w=[r0_k])
            wk = [self.BIGA_k[t] for t in range(t0 // 128, (t0 + n) // 128)]
            self.op("dve", lambda e, n=n, t0=t0, h=h, p0=p0: e.tensor_tensor(out=self.BIGA[p0:p0 + 64, h // 2, t0:t0 + n], in0=po[p0:p0 + 64, 0:n],
                                                                     in1=r0[p0:p0 + 64, 0:n], op=ALU.mult), r=[po_k, r0_k], w=wk)


Builder.mixer_win = _mixer_win


def _dn_proj(self):
    c = self.c
    w = self.dn_w_in[0]
    c.arena_reset()
    P = [c.asb([128, NT], F32, "dnP%d" % i) for i in range(2)]
    Cb, Cb_k = c.asb([128, NT], F32, "dnC")
    OB = [c.asb([128, NT], BF16, "dnOB%d" % i) for i in range(2)]
    rs, rs_k = self.XT[1][0][:, 0:512], self.XT[1][1]
    TS = [c.asb([128, 4, 128], BF16, "dnTS%d" % i) for i in range(2)]
    cw, cw_k = c.asb([128, 320], F32, "dncw")
    cwr, cwr_k = self.XT[0][0][:, 0:384].rearrange("p (a b) -> p a b", a=3), self.XT[0][1]
    cw2d = self.dn_conv_w[0].rearrange("k (j p) -> (k j) p", p=128)
    for i, (r0_, nr) in enumerate(((0, 128), (128, 128), (256, 64))):
        self.dma("sp", cwr[0:nr, i, :], cw2d[r0_:r0_ + nr, :], w=[cwr_k])
    pt, pt_k = self.PS[4]
    for i, (r0_, nr) in enumerate(((0, 128), (128, 128), (256, 64))):
        self.op("pe", lambda e, i=i, nr=nr, r0_=r0_: e.transpose(out=pt[:, r0_:r0_ + nr], in_=cwr[0:nr, i, :], identity=self.ident_f[0:nr, 0:nr]),
                r=[cwr_k, self.ident_f_k], w=[pt_k])
    self.op("dve", lambda e: e.tensor_copy(out=cw[:], in_=pt[:, 0:320]), r=[pt_k], w=[cw_k])
    st = {"ob": 0, "ts": 0, "eng": 0}
    SEGS = ((0, LC), (LC, NT))

    def finish_chunk(j, p, p_k):
        def wcol(k):
            return cw[:, k * 64 + j:k * 64 + j + 1]
        self.op("dve", lambda e: e.tensor_scalar(out=Cb[:], in0=p[:], scalar1=wcol(2), scalar2=None, op0=ALU.mult),
                r=[p_k, cw_k], w=[Cb_k])
        for k in (0, 1, 3, 4):
            sft = k - 2
            for (a, b) in SEGS:
                lo = max(a, a - sft)
                hi = min(b, b - sft)
                self.op("dve", lambda e, k=k, lo=lo, hi=hi, sft=sft: e.scalar_tensor_tensor(
                    out=Cb[:, lo:hi], in0=p[:, lo + sft:hi + sft], scalar=wcol(k), in1=Cb[:, lo:hi], op0=ALU.mult, op1=ALU.add),
                    r=[p_k, cw_k, Cb_k], w=[Cb_k])
        self.op("act", lambda e: e.activation(out=Cb[:], in_=Cb[:], func=AF.Silu), r=[Cb_k], w=[Cb_k])
        ob, ob_k = OB[st["ob"] % 2]
        st["ob"] += 1
        if j < 32:
            self.op("pool", lambda e: e.tensor_tensor(out=ob[:], in0=Cb[:], in1=Cb[:], op=ALU.mult), r=[Cb_k], w=[ob_k])
            qs = (128 ** -0.5) if j < 16 else 1.0
            for (t0, n) in self.TOKBLK:
                pss, pss_k = self.PS[5 + (st["eng"] % 2)]
                st["eng"] += 1
                self.op("pe", lambda e, pss=pss, t0=t0, n=n: e.matmul(pss[:, 0:n], lhsT=self.ones_b[:], rhs=ob[:, t0:t0 + n], start=True, stop=True),
                        r=[ob_k, self.ones_b_k], w=[pss_k])
                self.op("act", lambda e, pss=pss, n=n: e.activation(out=rs[:, 0:n], in_=pss[:, 0:n], func=AF.Sqrt, bias=EPS), r=[pss_k], w=[rs_k])
                self.op("dve", lambda e, n=n: e.reciprocal(out=rs[:, 0:n], in_=rs[:, 0:n]), r=[rs_k], w=[rs_k])
                self.op("dve", lambda e, t0=t0, n=n: e.scalar_tensor_tensor(out=Cb[:, t0:t0 + n], in0=Cb[:, t0:t0 + n], scalar=qs, in1=rs[:, 0:n],
                                                                           op0=ALU.mult, op1=ALU.mult), r=[Cb_k, rs_k], w=[Cb_k])
        ob2, ob2_k = OB[st["ob"] % 2]
        st["ob"] += 1
        self.op("act", lambda e: e.copy(out=ob2[:], in_=Cb[:]), r=[Cb_k], w=[ob2_k])
        if j < 16:
            self.dma("sp", self.QT[j], ob2[:], r=[ob2_k], w=[self.QT_k])
            return
        if j < 32:
            self.dma("sp", self.KT[j - 16], ob2[:], r=[ob2_k], w=[self.KT_k])
            dst, dst_k, col = self.DNK, self.DNK_k, (j - 16) * 128
        else:
            dst, dst_k, col = self.DNV, self.DNV_k, (j - 32) * 128
        ptb_t, ptb_k = self.PS[7]
        ptb = ptb_t[:].bitcast(BF16)
        for g0 in range(0, NTILE, 4):
            ng = min(4, NTILE - g0)
            ts, ts_k = TS[st["ts"] % 2]
            st["ts"] += 1
            for x in range(ng):
                t = g0 + x
                self.op("pe", lambda e, x=x, t=t: e.transpose(out=ptb[:, x * 128:(x + 1) * 128], in_=ob2[:, t * 128:(t + 1) * 128],
                                                              identity=self.ident_b[:]), r=[ob2_k, self.ident_b_k], w=[ptb_k])
            self.op("dve", lambda e, ts=ts, ng=ng: e.tensor_copy(out=ts[:, 0:ng, :], in_=ptb[:, 0:ng * 128].rearrange("p (a b) -> p a b", a=ng)),
                    r=[ptb_k], w=[ts_k])
            self.dma("sp", dst[g0 * 128:(g0 + ng) * 128, col:col + 128].rearrange("(t p) c -> p t c", p=128), ts[:, 0:ng, :],
                     r=[ts_k], w=[dst_k])

    def post(j, bi, t0, n, ps, ps_k):
        p, p_k = P[j % 2]
        self.op("act", lambda e: e.copy(out=p[:, t0:t0 + n], in_=ps[:, 0:n]), r=[ps_k], w=[p_k])
        if bi == len(self.TOKBLK) - 1:
            finish_chunk(j, p, p_k)

    self.linear_fm(w, 0, 8192, post)
    ZS = [c.asb([128, 512], BF16, "dnZS%d" % i) for i in range(2)]
    zi = {"i": 0}

    def zpost(cb, nb, t, ps, ps_k):
        zs, zs_k = ZS[zi["i"] % 2]
        zi["i"] += 1
        self.op("act", lambda e: e.activation(out=zs[:, 0:nb], in_=ps[:, 0:nb], func=AF.Silu), r=[ps_k], w=[zs_k])
        self.dma("sp", self.DNZ[t * 128:(t + 1) * 128, cb:cb + nb], zs[:, 0:nb], r=[zs_k], w=[self.DNZ_k])

    self.linear_tm(w, 8192, 4096, zpost)


def _mixer_dn(self):
    import os as _os
    self.dn_proj()
    if _os.environ.get("MK_DN_STAGE") == "A":
        return
    self.dn_scan()
    if _os.environ.get("MK_DN_STAGE") == "B":
        return
    self.dn_headout()


Builder.dn_proj = _dn_proj
Builder.mixer_dn = _mixer_dn


def _dn_scan(self, dirs=(0, 1)):
    c = self.c
    w = self.dn_w_in[0]
    c.arena_reset()
    E = self.op
    dm, dm_k = self.XT[0][0][:, 0:1024].rearrange("p (a b) -> p a b", a=8), self.XT[0][1]
    self.dma("sp", dm, self.kc["k_dnmask"][:, :].rearrange("p (a b) -> p a b", a=8), w=[dm_k])
    TRI = [dm[:, 0, :], dm[:, 1, :]]
    SGT = [dm[:, 2, :], dm[:, 3, :]]
    BLK = dm[:, 4, :]
    SELC = [dm[:, 5, :], dm[:, 6, :]]
    misc, misc_k = self.misc
    pc = misc[:, 8:12]
    par = misc[:, 12:16]
    self.dma("sp", pc, self.kc["k_dncol"][:, :], w=[misc_k])
    E("pool", lambda e: e.memset(par, 0.0), r=[misc_k], w=[misc_k])
    for d in range(2):
        self.dma("sp", par[d * 64 + 32:d * 64 + 64, 0:1], self.dn_a_log[0, d:d + 1, :].rearrange("o h -> h o"), r=[misc_k], w=[misc_k])
        self.dma("sp", par[d * 64 + 32:d * 64 + 64, 1:2], self.dn_dt_bias[0, d:d + 1, :].rearrange("o h -> h o"), r=[misc_k], w=[misc_k])
    E("act", lambda e: e.activation(out=par[:, 2:3], in_=par[:, 0:1], func=AF.Exp), r=[misc_k], w=[misc_k])
    E("dve", lambda e: e.scalar_tensor_tensor(out=par[:, 2:3], in0=par[:, 2:3], scalar=-1.0, in1=pc[:, 1:2], op0=ALU.mult, op1=ALU.mult),
      r=[misc_k], w=[misc_k])
    bgs, bgs_k = c.asb([128, 512], F32, "bgs")
    bg2, bg2_k = self.XT[1][0][:, 0:512], self.XT[1][1]
    BGT, BGT_k = c.asb([128, NTILE, 128], F32, "BGT")
    GCA, GCA_k = c.asb([128, NTILE, 64], F32, "GCA")
    GLT, GLT_k = c.asb([128, NTILE, 64], F32, "GLT")
    RR, RR_k = c.asb([128, NTILE, 64], F32, "RR")
    BAx, BAx_k = c.asb([128, NTILE, 64], F32, "BAx")
    GTB, GTB_k = c.asb([128, 2 * NTILE, 64], F32, "GTB")

    def bapost(j, bi, t0, n, ps, ps_k):
        E("act", lambda e: e.activation(out=bgs[:, 0:n], in_=ps[:, 0:n], func=AF.Sigmoid), r=[ps_k], w=[bgs_k])
        E("dve", lambda e: e.tensor_scalar(out=bgs[:, 0:n], in0=bgs[:, 0:n], scalar1=pc[:, 0:1], scalar2=None, op0=ALU.mult),
          r=[bgs_k, misc_k], w=[bgs_k])
        E("act", lambda e: e.activation(out=bg2[:, 0:n], in_=ps[:, 0:n], func=AF.Exp, bias=par[:, 1:2]), r=[ps_k, misc_k], w=[bg2_k])
        E("act", lambda e: e.activation(out=bg2[:, 0:n], in_=bg2[:, 0:n], func=AF.Ln, bias=1.0), r=[bg2_k], w=[bg2_k])
        E("dve", lambda e: e.scalar_tensor_tensor(out=bgs[:, 0:n], in0=bg2[:, 0:n], scalar=par[:, 2:3], in1=bgs[:, 0:n], op0=ALU.mult, op1=ALU.add),
          r=[bg2_k, bgs_k, misc_k], w=[bgs_k])
        for x in range(n // 128):
            t = t0 // 128 + x
            pt, pt_k = self.PS[4 + (t % 2)]
            E("pe", lambda e, x=x, pt=pt: e.transpose(out=pt[:, 0:128], in_=bgs[:, x * 128:(x + 1) * 128], identity=self.ident_f[:]),
              r=[bgs_k, self.ident_f_k], w=[pt_k])
            E("dve", lambda e, t=t, pt=pt: e.tensor_copy(out=BGT[:, t, :], in_=pt[:, 0:128]), r=[pt_k], w=[BGT_k])

    self.linear_fm(w, 12288, 128, bapost)
    for t in range(NTILE):
        pg, pg_k = self.PS[4 + (t % 2)]
        for d in range(2):
            E("pe", lambda e, t=t, d=d, pg=pg: e.matmul(pg[:, d * 32:(d + 1) * 32], lhsT=TRI[d], rhs=BGT[:, t, d * 64 + 32:d * 64 + 64],
                                                       start=True, stop=True), r=[dm_k, BGT_k], w=[pg_k])
            E("pe", lambda e, t=t, d=d, pg=pg: e.matmul(pg[:, 64 + d * 32:64 + (d + 1) * 32], lhsT=BLK, rhs=BGT[:, t, d * 64 + 32:d * 64 + 64],
                                                       start=True, stop=True), r=[dm_k, BGT_k], w=[pg_k])
        E("dve", lambda e, t=t, pg=pg: e.tensor_copy(out=GCA[:, t, :], in_=pg[:, 0:64]), r=[pg_k], w=[GCA_k])
        E("act", lambda e, t=t, pg=pg: e.copy(out=GLT[:, t, :], in_=pg[:, 64:128]), r=[pg_k], w=[GLT_k])
    E("dve", lambda e: e.tensor_tensor(out=RR[:], in0=GLT[:], in1=GCA[:], op=ALU.subtract), r=[GLT_k, GCA_k], w=[RR_k])
    E("act", lambda e: e.activation(out=RR[:], in_=RR[:], func=AF.Exp), r=[RR_k], w=[RR_k])
    E("act", lambda e: e.activation(out=GLT[:], in_=GLT[:], func=AF.Exp), r=[GLT_k, RR_k], w=[GLT_k])
    E("act", lambda e: e.activation(out=GCA[:], in_=GCA[:], func=AF.Exp), r=[GCA_k, RR_k], w=[GCA_k])
    for d in range(2):
        E("dve", lambda e, d=d: e.tensor_tensor(out=BAx[:, :, d * 32:(d + 1) * 32], in0=BGT[:, :, d * 64:d * 64 + 32],
                                               in1=GCA[:, :, d * 32:(d + 1) * 32], op=ALU.mult), r=[BGT_k, GCA_k], w=[BAx_k])
    for t in range(NTILE):
        pg, pg_k = self.PS[4 + (t % 2)]
        for hf in range(2):
            E("pe", lambda e, t=t, hf=hf, pg=pg: e.matmul(pg[:, hf * 64:(hf + 1) * 64], lhsT=SELC[hf], rhs=GLT[:, t, :], start=True, stop=True),
              r=[dm_k, GLT_k], w=[pg_k])
        E("dve", lambda e, t=t, pg=pg: e.tensor_copy(out=GTB[:, 2 * t:2 * t + 2, :], in_=pg[:, 0:128].rearrange("p (a b) -> p a b", a=2)),
          r=[pg_k], w=[GTB_k])
    self.p.barrier()

    NS = 2
    PSQ = [(self.PS[b][0][:, 0:128], self.PS[b][1]) for b in range(8)]
    psq_i = {"i": 0}

    def psq():
        r = PSQ[psq_i["i"] % 8]
        psq_i["i"] += 1
        return r

    class Stream:
        pass

    streams = []
    for s_ in range(NS):
        st = Stream()
        wtf = self.WT[s_][0][:].bitcast(F32)
        st.mats = [(wtf[:, a, b * 128:(b + 1) * 128], Tk("m%d_%d_%d" % (s_, a, b))) for a in range(16) for b in range(2)]
        st.mi = 0
        hb = self.HB[s_][0]
        st.ld = [[(hb[:, (q * 4 + x) * 128:(q * 4 + x + 1) * 128], Tk("ld%d_%d_%d" % (s_, q, x))) for x in range(4)] for q in range(4)]
        st.ldi = 0
        streams.append(st)
    ident = self.ident_f

    def unit(st, d, h, t, first):
        hk = h // 2
        M = {}
        names = ["GTRI", "DEC", "DECT", "L", "U", "La", "Ua", "Lb", "Ub", "P", "vb", "kbg", "kd", "u", "wT", "itT", "qTf", "vn", "o", "S"]
        for i, nm in enumerate(names):
            M[nm] = st.mats[i]
        kTb, qTb, ktok, vtok = st.ld[st.ldi % 4]
        st.ldi += 1
        r0_ = t * 128
        self.dma("sp", kTb[0], self.KT[hk][:, r0_:r0_ + 128], r=[self.KT_k], w=[kTb[1]])
        self.dma("sp", qTb[0], self.QT[hk][:, r0_:r0_ + 128], r=[self.QT_k], w=[qTb[1]])
        self.dma("sp", ktok[0], self.DNK[r0_:r0_ + 128, hk * 128:(hk + 1) * 128], r=[self.DNK_k], w=[ktok[1]])
        self.dma("sp", vtok[0], self.DNV[r0_:r0_ + 128, h * 128:(h + 1) * 128], r=[self.DNV_k], w=[vtok[1]])
        gcol = BGT[:, t, d * 64 + 32 + h:d * 64 + 33 + h]
        bcol = BGT[:, t, d * 64 + h:d * 64 + h + 1]
        acol = GCA[:, t, d * 32 + h:d * 32 + h + 1]
        bacol = BAx[:, t, d * 32 + h:d * 32 + h + 1]
        rcol = RR[:, t, d * 32 + h:d * 32 + h + 1]
        (GTRI, GTRI_k), (DEC, DEC_k), (DECT, DECT_k) = M["GTRI"], M["DEC"], M["DECT"]
        (Lm, L_k), (U, U_k), (P, P_k) = M["L"], M["U"], M["P"]
        E("dve", lambda e: e.tensor_scalar(out=GTRI, in0=TRI[d], scalar1=gcol, scalar2=None, op0=ALU.mult), r=[dm_k, BGT_k], w=[GTRI_k])
        pD, pD_k = psq()
        pDT, pDT_k = psq()
        E("pe", lambda e: e.matmul(pD, lhsT=GTRI, rhs=SGT[d], start=True, stop=True), r=[GTRI_k, dm_k], w=[pD_k])
        E("pe", lambda e: e.matmul(pDT, lhsT=SGT[d], rhs=GTRI, start=True, stop=True), r=[GTRI_k, dm_k], w=[pDT_k])
        E("act", lambda e: e.activation(out=DEC, in_=pD, func=AF.Exp), r=[pD_k], w=[DEC_k])
        E("act", lambda e: e.activation(out=DECT, in_=pDT, func=AF.Exp), r=[pDT_k], w=[DECT_k])
        pKK, pKK_k = psq()
        pQK, pQK_k = psq()
        E("pe", lambda e: e.matmul(pKK, lhsT=kTb[0], rhs=kTb[0], start=True, stop=True), r=[kTb[1]], w=[pKK_k])
        E("pe", lambda e: e.matmul(pQK, lhsT=kTb[0], rhs=qTb[0], start=True, stop=True), r=[kTb[1], qTb[1]], w=[pQK_k])
        E("dve", lambda e: e.scalar_tensor_tensor(out=Lm, in0=pKK, scalar=bcol, in1=DEC, op0=ALU.mult, op1=ALU.mult),
          r=[pKK_k, BGT_k, DEC_k], w=[L_k])
        E("pool", lambda e: e.tensor_tensor(out=Lm, in0=Lm, in1=SGT[d], op=ALU.mult), r=[L_k, dm_k], w=[L_k])
        itT, itT_k = M["itT"]
        E("dve", lambda e: e.tensor_tensor(out=itT, in0=pQK, in1=DECT, op=ALU.mult), r=[pQK_k, DECT_k], w=[itT_k])
        E("pool", lambda e: e.tensor_tensor(out=itT, in0=itT, in1=TRI[d], op=ALU.mult), r=[itT_k, dm_k], w=[itT_k])
        pU, pU_k = psq()
        E("pe", lambda e: e.transpose(out=pU, in_=Lm, identity=ident[:]), r=[L_k, self.ident_f_k], w=[pU_k])
        E("act", lambda e: e.copy(out=U, in_=pU), r=[pU_k], w=[U_k])
        E("dve", lambda e: e.scalar_tensor_tensor(out=P, in0=U, scalar=-1.0, in1=ident[:], op0=ALU.mult, op1=ALU.add),
          r=[U_k, self.ident_f_k], w=[P_k])
        curL, curU = M["L"], M["U"]
        pp = [(M["La"], M["Ua"]), (M["Lb"], M["Ub"])]
        for k in range(1, 6):
            nL, nU = pp[k % 2]
            pl, pl_k = psq()
            E("pe", lambda e, pl=pl, curL=curL, curU=curU: e.matmul(pl, lhsT=curU[0], rhs=curL[0], start=True, stop=True),
              r=[curL[1], curU[1]], w=[pl_k])
            E("act", lambda e, pl=pl, nL=nL: e.copy(out=nL[0], in_=pl), r=[pl_k], w=[nL[1]])
            if k < 5:
                pu_, pu_k = psq()
                E("pe", lambda e, pu_=pu_, curL=curL, curU=curU: e.matmul(pu_, lhsT=curL[0], rhs=curU[0], start=True, stop=True),
                  r=[curL[1], curU[1]], w=[pu_k])
                E("dve", lambda e, pu_=pu_, nU=nU: e.tensor_copy(out=nU[0], in_=pu_), r=[pu_k], w=[nU[1]])
            ppn, ppn_k = psq()
            E("pe", lambda e, ppn=ppn, nL=nL: e.matmul(ppn, lhsT=nL[0], rhs=P, start=True, stop=True), r=[nL[1], P_k], w=[ppn_k])
            E("dve", lambda e, ppn=ppn: e.tensor_tensor(out=P, in0=ppn, in1=P, op=ALU.add), r=[ppn_k, P_k], w=[P_k])
            curL, curU = nL, nU
        (vb, vb_k), (kbg, kbg_k), (kd, kd_k) = M["vb"], M["kbg"], M["kd"]
        E("pool", lambda e: e.tensor_scalar(out=vb, in0=vtok[0], scalar1=bcol, scalar2=None, op0=ALU.mult), r=[vtok[1], BGT_k], w=[vb_k])
        E("pool", lambda e: e.tensor_scalar(out=kbg, in0=ktok[0], scalar1=bacol, scalar2=None, op0=ALU.mult), r=[ktok[1], BAx_k], w=[kbg_k])
        E("pool", lambda e: e.tensor_scalar(out=kd, in0=ktok[0], scalar1=rcol, scalar2=None, op0=ALU.mult), r=[ktok[1], RR_k], w=[kd_k])
        (u, u_k), (wT, wT_k), (qTf, qTf_k) = M["u"], M["wT"], M["qTf"]
        pu2, pu2_k = psq()
        pw, pw_k = psq()
        E("pe", lambda e: e.matmul(pu2, lhsT=P, rhs=vb, start=True, stop=True), r=[P_k, vb_k], w=[pu2_k])
        E("pe", lambda e: e.matmul(pw, lhsT=kbg, rhs=P, start=True, stop=True), r=[P_k, kbg_k], w=[pw_k])
        E("act", lambda e: e.copy(out=u, in_=pu2), r=[pu2_k], w=[u_k])
        E("dve", lambda e: e.tensor_copy(out=wT, in_=pw), r=[pw_k], w=[wT_k])
        E("act", lambda e: e.copy(out=qTf, in_=qTb[0]), r=[qTb[1]], w=[qTf_k])
        (vn, vn_k), (o, o_k), (S, S_k) = M["vn"], M["o"], M["S"]
        if first:
            E("pool", lambda e: e.memset(S, 0.0), w=[S_k])
        for hf in ((0, 1) if d == 0 else (1, 0)):
            c0 = hf * 64
            gtcol = GTB[:, 2 * t + hf, d * 32 + h:d * 32 + h + 1]
            p1, p1_k = psq()
            p2a, p2a_k = psq()
            p2b, p2b_k = psq()
            p3, p3_k = psq()
            E("pe", lambda e, c0=c0, p1=p1: e.matmul(p1[c0:c0 + 64, :], lhsT=wT[:, c0:c0 + 64], rhs=S, start=True, stop=True),
              r=[wT_k, S_k], w=[p1_k])
            E("dve", lambda e, c0=c0, p1=p1: e.tensor_tensor(out=vn[c0:c0 + 64, :], in0=u[c0:c0 + 64, :], in1=p1[c0:c0 + 64, :], op=ALU.subtract),
              r=[u_k, p1_k], w=[vn_k])
            E("pe", lambda e, c0=c0, p2a=p2a: e.matmul(p2a[c0:c0 + 64, :], lhsT=qTf[:, c0:c0 + 64], rhs=S, start=True, stop=True),
              r=[qTf_k, S_k], w=[p2a_k])
            E("pe", lambda e, c0=c0, p2b=p2b: e.matmul(p2b[c0:c0 + 64, :], lhsT=itT[c0:c0 + 64, c0:c0 + 64], rhs=vn[c0:c0 + 64, :],
                                                      start=True, stop=True), r=[itT_k, vn_k], w=[p2b_k])
            E("act", lambda e, c0=c0, p2a=p2a: e.activation(out=o[c0:c0 + 64, :], in_=p2a[c0:c0 + 64, :], func=AF.Copy, scale=acol[c0:c0 + 64, :]),
              r=[p2a_k, GCA_k], w=[o_k])
            E("dve", lambda e, c0=c0, p2b=p2b: e.tensor_tensor(out=o[c0:c0 + 64, :], in0=o[c0:c0 + 64, :], in1=p2b[c0:c0 + 64, :], op=ALU.add),
              r=[o_k, p2b_k], w=[o_k])
            E("pe", lambda e, c0=c0, p3=p3: e.matmul(p3, lhsT=kd[c0:c0 + 64, :], rhs=vn[c0:c0 + 64, :], start=True, stop=True),
              r=[kd_k, vn_k], w=[p3_k])
            E("dve", lambda e, p3=p3, gtcol=gtcol: e.scalar_tensor_tensor(out=S, in0=S, scalar=gtcol, in1=p3, op0=ALU.mult, op1=ALU.add),
              r=[S_k, GTB_k, p3_k], w=[S_k])
        self.dma("sp", self.ODN[d, r0_:r0_ + 128, h * 128:(h + 1) * 128], o, r=[o_k], w=[self.ODN_k])

    order = {0: list(range(NTILE)), 1: [1, 0] + list(range(NTILE - 1, 1, -1))}
    import os as _os
    nh_ = int(_os.environ.get('MK_DN_NH', '32'))
    todo = [(d, h) for d in dirs for h in range(nh_)]
    for g0 in range(0, len(todo), NS):
        grp = todo[g0:g0 + NS]
        for step in range(NTILE):
            for si, (d, h) in enumerate(grp):
                unit(streams[si], d, h, order[d][step], step == 0)
    self.p.barrier()


Builder.dn_scan = _dn_scan


def _dn_headout(self):
    c = self.c
    E = self.op
    for half in range(2):
        c.arena_reset()
        nwt, nwt_k = c.asb([128, 128], F32, "nwt")
        ssb, ssb_k = c.asb([128, 32], F32, "ssb")
        self.dma("sp", nwt, self.dn_norm_w[0:1, :].partition_broadcast(128), w=[nwt_k])
        ps_t, ps_tk = self.PS[1]
        pst = ps_t[:].bitcast(BF16)
        c0 = half * 2048
        for t in range(NTILE):
            oa, oa_k = self.XT[0]
            ob_, ob_k = self.XT[1]
            zt, zt_k = self.HB[t % 2]
            hb, hb_k = self.SB[t % 2]
            hbb = hb[:].bitcast(BF16)[:, 0:2048]
            r0_ = t * 128
            self.dma("sp", oa[:], self.ODN[0, r0_:r0_ + 128, c0:c0 + 2048], r=[self.ODN_k], w=[oa_k])
            self.dma("sp", ob_[:], self.ODN[1, r0_:r0_ + 128, c0:c0 + 2048], r=[self.ODN_k], w=[ob_k])
            self.dma("sp", zt[:], self.DNZ[r0_:r0_ + 128, c0:c0 + 2048], r=[self.DNZ_k], w=[zt_k])
            E("dve", lambda e, oa=oa, ob_=ob_: e.tensor_tensor(out=oa[:], in0=oa[:], in1=ob_[:], op=ALU.add), r=[oa_k, ob_k], w=[oa_k])
            E("act", lambda e, oa=oa, ob_=ob_: e.activation(out=ob_[:], in_=oa[:], func=AF.Square), r=[oa_k], w=[ob_k])
            E("dve", lambda e, ob_=ob_: e.reduce_sum(out=ssb[:, 0:16], in_=ob_[:].rearrange("p (a b) -> p a b", a=16), axis=AX.X),
              r=[ob_k], w=[ssb_k])
            E("act", lambda e: e.activation(out=ssb[:, 16:32], in_=ssb[:, 0:16], func=AF.Sqrt, scale=1.0 / 128, bias=EPS), r=[ssb_k], w=[ssb_k])
            E("dve", lambda e: e.reciprocal(out=ssb[:, 16:32], in_=ssb[:, 16:32]), r=[ssb_k], w=[ssb_k])
            for hh in range(16):
                eng = "dve" if hh % 2 == 0 else "pool"
                if eng == "dve":
                    E("dve", lambda e, hh=hh, oa=oa, hbb=hbb: e.scalar_tensor_tensor(
                        out=hbb[:, hh * 128:(hh + 1) * 128], in0=oa[:, hh * 128:(hh + 1) * 128], scalar=ssb[:, 16 + hh:17 + hh], in1=nwt,
                        op0=ALU.mult, op1=ALU.mult), r=[oa_k, ssb_k, nwt_k], w=[hb_k])
                else:
                    E("pool", lambda e, hh=hh, oa=oa: e.tensor_scalar(out=oa[:, hh * 128:(hh + 1) * 128], in0=oa[:, hh * 128:(hh + 1) * 128],
                                                                       scalar1=ssb[:, 16 + hh:17 + hh], scalar2=None, op0=ALU.mult),
                      r=[oa_k, ssb_k], w=[oa_k])
                    E("pool", lambda e, hh=hh, oa=oa, hbb=hbb: e.tensor_tensor(out=hbb[:, hh * 128:(hh + 1) * 128], in0=oa[:, hh * 128:(hh + 1) * 128],
                                                                              in1=nwt, op=ALU.mult), r=[oa_k, nwt_k], w=[hb_k])
            E("dve", lambda e, hbb=hbb, zt=zt: e.tensor_tensor(out=hbb, in0=hbb, in1=zt[:], op=ALU.mult), r=[hb_k, zt_k], w=[hb_k])
            for g in range(2):
                for j in range(8):
                    kc = g * 8 + j
                    E("pe", lambda e, hbb=hbb, kc=kc, j=j: e.transpose(out=pst[:, j * 128:(j + 1) * 128], in_=hbb[:, kc * 128:(kc + 1) * 128],
                                                                      identity=self.ident_b[:]), r=[hb_k, self.ident_b_k], w=[ps_tk])
                E("act", lambda e, g=g, t=t: e.copy(out=self.BIGA[:, g * 8:(g + 1) * 8, t * 128:(t + 1) * 128],
                                                   in_=pst.rearrange("p (a b) -> p a b", a=8)), r=[ps_tk], w=[self.BIGA_k[t]])
        self.phase_oproj_residual(self.dn_w_o[0][half * 2048:(half + 1) * 2048, :], self.cur_li)


Builder.dn_headout = _dn_headout


NBLK = 2 * NT // 128 + 32


def _phase_moe_b(self, l):
    c = self.c
    E = self.op
    c.arena_reset()
    if not hasattr(self, "HROW"):
        self.HROW, _ = c.dram("HROW", [NT, D], BF16)
        self.HROW_k = [Tk("hrow%d" % t) for t in range(NTILE)]
        self.XS, self.XS_k = c.dram("XS", [NBLK * 128, D], BF16)
        self.YS, self.YS_k = c.dram("YS", [NBLK * 128, D], F32)
    ht32, ht32_k = self.WT[0][0][:].bitcast(F32)[:, :, 0:128], self.WT[0][1]
    wr, wr_k = c.asb([128, KC, 36], F32, "wr")
    br, br_k = c.asb([128, 36], F32, "br")
    lg, lg_k = c.asb([128, 36], F32, "lg")
    rs_, rs_k = c.asb([128, 64], F32, "rsm")
    OH1, OH1_k = c.asb([128, NTILE, 32], F32, "OH1")
    OH2, OH2_k = c.asb([128, NTILE, 32], F32, "OH2")
    GAB, GAB_k = c.asb([128, NTILE, 2], F32, "GAB")
    self.dma("sp", wr[:, :, 0:4], self.moe_wg[l].rearrange("(kc p) n -> p kc n", p=128), w=[wr_k])
    self.dma("sp", wr[:, :, 4:36], self.moe_we[l].rearrange("(kc p) n -> p kc n", p=128), w=[wr_k])
    self.dma("sp", br[:, 0:4], self.moe_bg[l:l + 1, :].partition_broadcast(128), w=[br_k])
    self.dma("sp", br[:, 4:36], self.moe_be[l:l + 1, :].partition_broadcast(128), w=[br_k])
    R = lambda a, b=None: rs_[:, a:(a + 1 if b is None else b)]

    def router(t, xt, xt_k):
        for g in range(4):
            pt, pt_k = self.PS[2 + (g % 2)]
            for jx in range(4):
                kc = g * 4 + jx
                E("pe", lambda e, pt=pt, jx=jx, kc=kc, xt=xt: e.transpose(out=pt[:, jx * 128:(jx + 1) * 128],
                                                                        in_=xt[:, kc * 128:(kc + 1) * 128], identity=self.ident_f[:]),
                  r=[xt_k, self.ident_f_k], w=[pt_k])
            E("dve", lambda e, pt=pt, g=g: e.tensor_copy(out=ht32[:, g * 4:(g + 1) * 4, :], in_=pt[:].rearrange("p (a b) -> p a b", a=4)),
              r=[pt_k], w=[ht32_k])
        pl, pl_k = self.PS[4]
        for kc in range(KC):
            E("pe", lambda e, kc=kc: e.matmul(pl[:, 0:36], lhsT=ht32[:, kc, :], rhs=wr[:, kc, :], start=(kc == 0), stop=(kc == KC - 1)),
              r=[ht32_k, wr_k], w=[pl_k])
        V = lambda fn, r=(), w=(): E("dve", fn, r=list(r) + [rs_k], w=list(w) + [rs_k])
        E("dve", lambda e: e.tensor_tensor(out=lg[:], in0=pl[:, 0:36], in1=br[:], op=ALU.add), r=[pl_k, br_k], w=[lg_k])
        V(lambda e: e.reduce_max(out=R(0), in_=lg[:, 0:4], axis=AX.X), r=[lg_k])
        V(lambda e: e.tensor_scalar(out=R(1), in0=R(0), scalar1=-1.0, scalar2=None, op0=ALU.mult))
        E("act", lambda e: e.activation(out=R(56, 60), in_=lg[:, 0:4], func=AF.Exp, bias=R(1), accum_out=R(2)), r=[lg_k, rs_k], w=[rs_k])
        V(lambda e: e.reciprocal(out=R(3), in_=R(2)))
        V(lambda e: e.tensor_scalar(out=R(4, 8), in0=lg[:, 0:4], scalar1=R(0), scalar2=None, op0=ALU.is_equal), r=[lg_k])
        V(lambda e: e.tensor_scalar(out=R(8, 16), in0=lg[:, 4:12], scalar1=R(4), scalar2=None, op0=ALU.mult), r=[lg_k])
        for g in range(1, 4):
            V(lambda e, g=g: e.scalar_tensor_tensor(out=R(8, 16), in0=lg[:, 4 + 8 * g:12 + 8 * g], scalar=R(4 + g), in1=R(8, 16),
                                                   op0=ALU.mult, op1=ALU.add), r=[lg_k])
        V(lambda e: e.reduce_max(out=R(40), in_=R(8, 16), axis=AX.X))
        V(lambda e: e.tensor_scalar(out=R(16, 24), in0=R(8, 16), scalar1=R(40), scalar2=None, op0=ALU.is_equal))
        V(lambda e: e.scalar_tensor_tensor(out=R(24, 32), in0=R(16, 24), scalar=-1e30, in1=R(8, 16), op0=ALU.mult, op1=ALU.add))
        V(lambda e: e.reduce_max(out=R(41), in_=R(24, 32), axis=AX.X))
        V(lambda e: e.tensor_scalar(out=R(32, 40), in0=R(24, 32), scalar1=R(41), scalar2=None, op0=ALU.is_equal))
        V(lambda e: e.tensor_tensor(out=R(42), in0=R(41), in1=R(40), op=ALU.subtract))
        E("act", lambda e: e.activation(out=R(43), in_=R(42), func=AF.Exp), r=[rs_k], w=[rs_k])
        V(lambda e: e.tensor_scalar(out=R(44), in0=R(43), scalar1=1.0, scalar2=None, op0=ALU.add))
        V(lambda e: e.reciprocal(out=R(44), in_=R(44)))
        E("dve", lambda e, t=t: e.tensor_tensor(out=GAB[:, t, 0:1], in0=R(44), in1=R(3), op=ALU.mult), r=[rs_k], w=[GAB_k])
        E("dve", lambda e, t=t: e.tensor_tensor(out=GAB[:, t, 1:2], in0=GAB[:, t, 0:1], in1=R(43), op=ALU.mult), r=[rs_k, GAB_k], w=[GAB_k])
        for g in range(4):
            E("dve", lambda e, g=g, t=t: e.tensor_scalar(out=OH1[:, t, g * 8:(g + 1) * 8], in0=R(16, 24), scalar1=R(4 + g),
                                                        scalar2=None, op0=ALU.mult), r=[rs_k], w=[OH1_k])
            E("pool", lambda e, g=g, t=t: e.tensor_scalar(out=OH2[:, t, g * 8:(g + 1) * 8], in0=R(32, 40), scalar1=R(4 + g),
                                                         scalar2=None, op0=ALU.mult), r=[rs_k], w=[OH2_k])

    self.phase_norm(hrow_dram=self.HROW, hrow_k=self.HROW_k, router=router, skip_T=True)
    self.load_gate_tiles(l, 1)
    mbc, mbc_k = c.asb([128, 224], F32, "mbc")
    self.dma("sp", mbc, self.kc["k_moeb"][:, :], w=[mbc_k])
    SLT, BROW, OFF13, OFF2 = mbc[:, 0:128], mbc[:, 128:196], mbc[:, 196:212], mbc[:, 212:218]
    MM, MM_k = c.asb([128, NTILE, 32], F32, "MM")
    RANK, RANK_k = c.asb([128, NTILE, 32], F32, "RANK")
    sm, sm_k = c.asb([128, 5, 32], F32, "moesm")
    BEa, BEa_k = c.asb([128, NBLK], F32, "BEa")
    IDXf, IDXf_k = c.asb([128, NBLK, 22], F32, "IDXf")
    IDXi, IDXi_k = c.asb([128, NBLK, 22], I32, "IDXi")
    DSTf, DSTf_k = c.asb([128, NTILE, 2], F32, "DSTf")
    DSTi, DSTi_k = c.asb([128, NTILE, 2], I32, "DSTi")
    tmp32, tmp32_k = c.asb([128, 32], F32, "tmp32")
    E("dve", lambda e: e.tensor_tensor(out=MM[:], in0=OH1[:], in1=OH2[:], op=ALU.add), r=[OH1_k, OH2_k], w=[MM_k])
    for t in range(NTILE):
        pg, pg_k = self.PS[2 + (t % 2)]
        E("pe", lambda e, t=t, pg=pg: e.matmul(pg[:, 0:32], lhsT=SLT, rhs=MM[:, t, :], start=True, stop=(t == 0)), r=[mbc_k, MM_k], w=[pg_k])
        for t2 in range(t):
            E("pe", lambda e, t=t, t2=t2, pg=pg: e.matmul(pg[:, 0:32], lhsT=self.ones_f[:], rhs=MM[:, t2, :], start=False, stop=(t2 == t - 1)),
              r=[self.ones_f_k, MM_k], w=[pg_k])
        E("dve", lambda e, t=t, pg=pg: e.tensor_copy(out=RANK[:, t, :], in_=pg[:, 0:32]), r=[pg_k], w=[RANK_k])
    pc_, pc_k = self.PS[4]
    for t in range(NTILE):
        E("pe", lambda e, t=t: e.matmul(pc_[:, 0:32], lhsT=self.ones_f[:], rhs=MM[:, t, :], start=(t == 0), stop=(t == NTILE - 1)),
          r=[self.ones_f_k, MM_k], w=[pc_k])
    CNT, PB, CA, CB, PST = sm[:, 0, :], sm[:, 1, :], sm[:, 2, :], sm[:, 3, :], sm[:, 4, :]
    S_ = lambda fn: E("dve", fn, r=[sm_k], w=[sm_k])
    E("dve", lambda e: e.tensor_copy(out=CNT, in_=pc_[:, 0:32]), r=[pc_k], w=[sm_k])
    S_(lambda e: e.tensor_scalar(out=PB, in0=CNT, scalar1=0.0, scalar2=None, op0=ALU.is_gt))
    for m in range(1, NTILE):
        S_(lambda e, m=m: e.scalar_tensor_tensor(out=PB, in0=CNT, scalar=float(128 * m), in1=PB, op0=ALU.is_gt, op1=ALU.add))
    S_(lambda e: e.tensor_copy(out=CA, in_=PB))
    cur, nxt = CA, CB
    for sft in (1, 2, 4, 8, 16):
        S_(lambda e, cur=cur, nxt=nxt, sft=sft: e.tensor_copy(out=nxt[:, 0:sft], in_=cur[:, 0:sft]))
        S_(lambda e, cur=cur, nxt=nxt, sft=sft: e.tensor_tensor(out=nxt[:, sft:32], in0=cur[:, sft:32], in1=cur[:, 0:32 - sft], op=ALU.add))
        cur, nxt = nxt, cur
    PEND = cur
    S_(lambda e: e.tensor_tensor(out=PST, in0=PEND, in1=PB, op=ALU.subtract))
    S_(lambda e: e.tensor_scalar(out=PST, in0=PST, scalar1=128.0, scalar2=None, op0=ALU.mult))
    E("pool", lambda e: e.memset(BEa, 0.0), w=[BEa_k])
    for ex in range(32):
        E("dve", lambda e, ex=ex: e.scalar_tensor_tensor(out=BEa, in0=BROW, scalar=PEND[:, ex:ex + 1], in1=BEa, op0=ALU.is_ge, op1=ALU.add),
          r=[mbc_k, sm_k, BEa_k], w=[BEa_k])
    E("dve", lambda e: e.tensor_scalar(out=BEa, in0=BEa, scalar1=31.0, scalar2=None, op0=ALU.min), r=[BEa_k], w=[BEa_k])
    for kc in range(16):
        E("dve", lambda e, kc=kc: e.tensor_scalar(out=IDXf[:, :, kc], in0=BEa, scalar1=2048.0, scalar2=OFF13[:, kc:kc + 1], op0=ALU.mult, op1=ALU.add),
          r=[BEa_k, mbc_k], w=[IDXf_k])
    for fc in range(6):
        E("dve", lambda e, fc=fc: e.tensor_scalar(out=IDXf[:, :, 16 + fc], in0=BEa, scalar1=768.0, scalar2=OFF2[:, fc:fc + 1], op0=ALU.mult, op1=ALU.add),
          r=[BEa_k, mbc_k], w=[IDXf_k])
    if l > 0:
        E("dve", lambda e: e.tensor_scalar(out=IDXf[:, :, 0:16], in0=IDXf[:, :, 0:16], scalar1=float(l * 32 * 2048), scalar2=None, op0=ALU.add),
          r=[IDXf_k], w=[IDXf_k])
        E("dve", lambda e: e.tensor_scalar(out=IDXf[:, :, 16:22], in0=IDXf[:, :, 16:22], scalar1=float(l * 32 * 768), scalar2=None, op0=ALU.add),
          r=[IDXf_k], w=[IDXf_k])
    E("dve", lambda e: e.tensor_copy(out=IDXi[:], in_=IDXf[:]), r=[IDXf_k], w=[IDXi_k])
    for t in range(NTILE):
        for k, (OH, OH_k) in enumerate(((OH1, OH1_k), (OH2, OH2_k))):
            E("dve", lambda e, t=t: e.tensor_tensor(out=tmp32, in0=RANK[:, t, :], in1=PST, op=ALU.add), r=[RANK_k, sm_k], w=[tmp32_k])
            E("dve", lambda e, t=t, OH=OH: e.tensor_tensor(out=tmp32, in0=tmp32, in1=OH[:, t, :], op=ALU.mult), r=[tmp32_k, OH_k], w=[tmp32_k])
            E("dve", lambda e, t=t, k=k: e.reduce_sum(out=DSTf[:, t, k:k + 1], in_=tmp32, axis=AX.X), r=[tmp32_k], w=[DSTf_k])
    E("dve", lambda e: e.tensor_copy(out=DSTi[:], in_=DSTf[:]), r=[DSTf_k], w=[DSTi_k])
    for t in range(NTILE):
        hb, hb_k = self.HB[t % 2]
        self.dma("sp", hb[:], self.HROW[t * 128:(t + 1) * 128, :], r=[self.HROW_k[t]], w=[hb_k])
        for k in range(2):
            self.p.op("pool", lambda e, hb=hb, t=t, k=k: e.indirect_dma_start(
                out=self.XS[:, :], out_offset=bass.IndirectOffsetOnAxis(ap=DSTi[:, t, k:k + 1], axis=0), in_=hb[:], in_offset=None),
                r=[hb_k, DSTi_k], w=[self.XS_k], dma=True)
    w13sb = self.BIGA[:].rearrange("p a b -> p (a b)")[:, 0:16 * 1536].rearrange("p (a b) -> p a b", a=16)
    w2sb = self.BIGA[:].rearrange("p a b -> p (a b)")[:, 16 * 1536:16 * 1536 + 6 * 2048].rearrange("p (a b) -> p a b", a=6)
    w13_k, w2_k = Tk("w13sb"), Tk("w2sb")
    biga_all = list(self.BIGA_k)
    w13rows = self.moe_w13.rearrange("l e r n -> (l e r) n")
    w2rows = self.moe_w2.rearrange("l e r n -> (l e r) n")
    XTb = [(self.WT[1][0][:, :, i * 128:(i + 1) * 128], Tk("xTb%d" % i)) for i in range(2)]
    GT_ = [c.asb([128, 768], BF16, "gact%d" % i) for i in range(2)]
    AT_ = [c.asb([128, 6, 128], BF16, "actT%d" % i) for i in range(2)]
    ps_t, ps_tk = self.PS[1]
    pst = ps_t[:].bitcast(BF16)
    first = True
    for b in range(NBLK):
        xr, xr_k = self.HB[b % 2]
        xT, xT_k = XTb[b % 2]
        ga, ga_k = GT_[b % 2]
        aT, aT_k = AT_[b % 2]
        yr, yr_k = self.XT[b % 2]
        for kc in range(16):
            self.p.op("pool", lambda e, b=b, kc=kc: e.indirect_dma_start(
                out=w13sb[:, kc, :], out_offset=None, in_=w13rows, in_offset=bass.IndirectOffsetOnAxis(ap=IDXi[:, b, kc:kc + 1], axis=0)),
                r=[IDXi_k], w=[w13_k] + (biga_all if first else []), dma=True)
            first = False
        for fc in range(6):
            self.p.op("pool", lambda e, b=b, fc=fc: e.indirect_dma_start(
                out=w2sb[:, fc, :], out_offset=None, in_=w2rows, in_offset=bass.IndirectOffsetOnAxis(ap=IDXi[:, b, 16 + fc:17 + fc], axis=0)),
                r=[IDXi_k], w=[w2_k], dma=True)
        self.dma("sp", xr[:], self.XS[b * 128:(b + 1) * 128, :], r=[self.XS_k], w=[xr_k])
        for g in range(2):
            for j in range(8):
                kc = g * 8 + j
                E("pe", lambda e, xr=xr, kc=kc, j=j: e.transpose(out=pst[:, j * 128:(j + 1) * 128], in_=xr[:, kc * 128:(kc + 1) * 128],
                                                                identity=self.ident_b[:]), r=[xr_k, self.ident_b_k], w=[ps_tk])
            E("act" if g == 0 else "dve", (lambda e, g=g, xT=xT: e.copy(out=xT[:, g * 8:(g + 1) * 8, :], in_=pst.rearrange("p (a b) -> p a b", a=8)))
              if g == 0 else (lambda e, g=g, xT=xT: e.tensor_copy(out=xT[:, g * 8:(g + 1) * 8, :], in_=pst.rearrange("p (a b) -> p a b", a=8))),
              r=[ps_tk], w=[xT_k])
        pss = [self.PS[2], self.PS[3], self.PS[4]]
        for cb in range(3):
            ps, ps_k = pss[cb]
            for kc in range(16):
                E("pe", lambda e, ps=ps, xT=xT, kc=kc, cb=cb: e.matmul(ps[:, :], lhsT=xT[:, kc, :], rhs=w13sb[:, kc, cb * 512:(cb + 1) * 512],
                                                                      start=(kc == 0), stop=(kc == 15)), r=[xT_k, w13_k], w=[ps_k])
        (pA, pA_k), (pB, pB_k), (pC, pC_k) = pss
        E("act", lambda e, ga=ga: e.activation(out=ga[:, 0:512], in_=pA[:, :], func=AF.Silu), r=[pA_k], w=[ga_k])
        E("act", lambda e, ga=ga: e.activation(out=ga[:, 512:768], in_=pB[:, 0:256], func=AF.Silu), r=[pB_k], w=[ga_k])
        E("dve", lambda e, ga=ga: e.tensor_tensor(out=ga[:, 0:256], in0=pB[:, 256:512], in1=ga[:, 0:256], op=ALU.mult), r=[pB_k, ga_k], w=[ga_k])
        E("dve", lambda e, ga=ga: e.tensor_tensor(out=ga[:, 256:768], in0=pC[:, :], in1=ga[:, 256:768], op=ALU.mult), r=[pC_k, ga_k], w=[ga_k])
        for j in range(6):
            E("pe", lambda e, ga=ga, j=j: e.transpose(out=pst[:, j * 128:(j + 1) * 128], in_=ga[:, j * 128:(j + 1) * 128], identity=self.ident_b[:]),
              r=[ga_k, self.ident_b_k], w=[ps_tk])
        E("act", lambda e, aT=aT: e.copy(out=aT[:], in_=pst[:, 0:768].rearrange("p (a b) -> p a b", a=6)), r=[ps_tk], w=[aT_k])
        for cb in range(4):
            ps, ps_k = self.PS[5 + (cb % 2)]
            for fc in range(6):
                E("pe", lambda e, ps=ps, aT=aT, fc=fc, cb=cb: e.matmul(ps[:, :], lhsT=aT[:, fc, :], rhs=w2sb[:, fc, cb * 512:(cb + 1) * 512],
                                                                      start=(fc == 0), stop=(fc == 5)), r=[aT_k, w2_k], w=[ps_k])
            if cb % 2 == 0:
                E("act", lambda e, ps=ps, yr=yr, cb=cb: e.copy(out=yr[:, cb * 512:(cb + 1) * 512], in_=ps[:, :]), r=[ps_k], w=[yr_k])
            else:
                E("dve", lambda e, ps=ps, yr=yr, cb=cb: e.tensor_copy(out=yr[:, cb * 512:(cb + 1) * 512], in_=ps[:, :]), r=[ps_k], w=[yr_k])
        self.dma("sp", self.YS[b * 128:(b + 1) * 128, :], yr[:], r=[yr_k], w=[self.YS_k])
    for t in range(NTILE):
        ty = 1 if t < 2 else 0
        y1, y1_k = self.XT[0]
        y2, y2_k = self.XT[1]
        xa, xa_k = self.SB[t % 2]
        self.p.op("pool", lambda e, t=t: e.indirect_dma_start(out=y1[:], out_offset=None, in_=self.YS[:, :],
                                                              in_offset=bass.IndirectOffsetOnAxis(ap=DSTi[:, t, 0:1], axis=0)),
                  r=[self.YS_k, DSTi_k], w=[y1_k], dma=True)
        self.p.op("pool", lambda e, t=t: e.indirect_dma_start(out=y2[:], out_offset=None, in_=self.YS[:, :],
                                                              in_offset=bass.IndirectOffsetOnAxis(ap=DSTi[:, t, 1:2], axis=0)),
                  r=[self.YS_k, DSTi_k], w=[y2_k], dma=True)
        self.dma("sp", xa[:], self.XR[t * 128:(t + 1) * 128, :], r=[self.XR_k[t]], w=[xa_k])
        E("dve", lambda e, t=t: e.tensor_scalar(out=y1[:], in0=y1[:], scalar1=GAB[:, t, 0:1], scalar2=None, op0=ALU.mult), r=[y1_k, GAB_k], w=[y1_k])
        E("dve", lambda e, t=t: e.scalar_tensor_tensor(out=y1[:], in0=y2[:], scalar=GAB[:, t, 1:2], in1=y1[:], op0=ALU.mult, op1=ALU.add),
          r=[y1_k, y2_k, GAB_k], w=[y1_k])
        E("pool", lambda e, ty=ty: e.tensor_tensor(out=y1[:], in0=y1[:], in1=self.GB[ty][0][:], op=ALU.mult), r=[y1_k, self.GB[ty][1]], w=[y1_k])
        E("dve", lambda e, xa=xa: e.tensor_tensor(out=xa[:], in0=xa[:], in1=y1[:], op=ALU.add), r=[y1_k, xa_k], w=[xa_k])
        self.dma("sp", self.XR[t * 128:(t + 1) * 128, :], xa[:], r=[xa_k], w=[self.XR_k[t]])
    self.p.barrier()


Builder.phase_moe_b = _phase_moe_b
```

```python
import math
from contextlib import ExitStack
import numpy as np
import concourse.bass as bass
import concourse.mybir as mybir
from concourse.bass_utils import run_bass_kernel_spmd

F32 = mybir.dt.float32
BF16 = mybir.dt.bfloat16
I32 = mybir.dt.int32
U32 = mybir.dt.uint32
AF = mybir.ActivationFunctionType
ALU = mybir.AluOpType
AX = mybir.AxisListType

D = 2048
KC = 16
L = 2048
LC = 256
NT = L + LC
NTILE = NT // 128
DEPTH = 4
EPS = 1e-6
N_DMA_SEM = 40


class Tk:
    __slots__ = ("name", "w", "r")

    def __init__(self, name):
        self.name = name
        self.w = None
        self.r = []


class Op:
    __slots__ = ("eng", "fn", "deps", "dma", "sem", "val", "cnt", "need", "waits")


class Prog:
    ENGS = ("pe", "act", "dve", "pool", "sp")

    def __init__(self, nc):
        self.nc = nc
        self.ops = []
        self.dma_uses = [0] * N_DMA_SEM
        self.dma_last = [None] * N_DMA_SEM
        self.dma_rr = 0
        self.final_deps = []
        self.since = []
        self.bar = None

    def op(self, eng, fn, r=(), w=(), dma=False):
        o = Op()
        o.eng = eng
        o.fn = fn
        o.dma = dma
        o.need = False
        deps = set()
        for t in r:
            if t.w is not None:
                deps.add(t.w)
        for t in w:
            if t.w is not None:
                deps.add(t.w)
            deps.update(t.r)
        idx = len(self.ops)
        if self.bar is not None:
            deps.add(self.bar)
        self.since.append(idx)
        if dma:
            s = self.dma_rr
            self.dma_rr = (self.dma_rr + 1) % N_DMA_SEM
            if self.dma_last[s] is not None:
                deps.add(self.dma_last[s])
            self.dma_uses[s] += 1
            self.dma_last[s] = idx
            o.sem = s
            o.val = 16 * self.dma_uses[s]
        o.deps = deps
        self.ops.append(o)
        for t in r:
            t.r.append(idx)
        for t in w:
            t.w = idx
            t.r = []
        return idx

    def barrier(self):
        o = Op()
        o.eng = "sp"
        o.fn = lambda e: e.nop()
        o.dma = False
        o.need = False
        o.deps = set(self.since)
        idx = len(self.ops)
        self.ops.append(o)
        self.since = [idx]
        self.bar = idx
        return idx

    def finalize(self):
        ops = self.ops
        for o in ops:
            for d in o.deps:
                if not ops[d].dma:
                    ops[d].need = True
        cnt = {e: 0 for e in self.ENGS}
        for o in ops:
            if o.need:
                cnt[o.eng] += 1
            o.cnt = cnt[o.eng]
        seen = {e: {} for e in self.ENGS}
        for o in ops:
            waits = {}
            for d in o.deps:
                dd = ops[d]
                if dd.dma:
                    key = ("dma", dd.sem)
                    val = dd.val
                else:
                    if dd.eng == "pe" and o.eng == "pe" and not o.dma:
                        continue
                    key = ("eng", dd.eng)
                    val = dd.cnt
                if seen[o.eng].get(key, 0) >= val:
                    continue
                if waits.get(key, 0) < val:
                    waits[key] = val
            for k, v in waits.items():
                seen[o.eng][k] = v
            o.waits = list(waits.items())

    def emit(self, out_ops):
        nc = self.nc
        self.finalize()
        ops = self.ops
        with ExitStack() as es:
            esem = {e: es.enter_context(nc.semaphore("s_" + e)) for e in self.ENGS}
            dsem = [es.enter_context(nc.semaphore("d%d" % i)) for i in range(N_DMA_SEM)]
            block = es.enter_context(nc.Block())

            def run(ename):
                def body(eng):
                    for o in ops:
                        if o.eng != ename:
                            continue
                        for (kind, k), v in o.waits:
                            eng.wait_ge(esem[k] if kind == "eng" else dsem[k], v)
                        ins = o.fn(eng)
                        if o.dma:
                            ins.then_inc(dsem[o.sem], 16)
                        elif o.need:
                            ins.then_inc(esem[ename], 1)
                    if ename == "sp":
                        for d in out_ops:
                            dd = ops[d]
                            eng.wait_ge(dsem[dd.sem], dd.val)
                return body

            block.tensor(run("pe"))
            block.scalar(run("act"))
            block.vector(run("dve"))
            block.gpsimd(run("pool"))
            block.sync(run("sp"))


class Ctx:
    def __init__(self, nc, es):
        self.nc = nc
        self.es = es
        self.p = Prog(nc)
        self.n = 0

    def sb(self, shape, dt, name=None):
        self.n += 1
        t = self.es.enter_context(self.nc.sbuf_tensor(name or ("sb%d" % self.n), list(shape), dt))
        return t, Tk(name or "sb%d" % self.n)

    def arena_init(self, nbytes):
        self.arena_n = nbytes // 2
        self.arena = self.es.enter_context(self.nc.sbuf_tensor("ARENA", [128, self.arena_n], BF16))
        self.arena_off = 0

    def arena_reset(self):
        self.p.barrier()
        self.arena_off = 0

    def asb(self, shape, dt, name=None):
        self.n += 1
        esz = 4 if dt in (F32, I32, U32) else 2
        free = 1
        for d in shape[1:]:
            free *= d
        nel = (free * esz + 1) // 2
        nel = (nel + 31) // 32 * 32
        assert self.arena_off + nel <= self.arena_n, "arena overflow %s" % name
        v = self.arena[0:shape[0], self.arena_off:self.arena_off + free * esz // 2]
        self.arena_off += nel
        if dt != BF16:
            v = v.bitcast(dt)
        if len(shape) == 3:
            v = v.rearrange("p (a b) -> p a b", a=shape[1])
        return v, Tk(name or "a%d" % self.n)

    def ps(self, shape, dt=F32, name=None):
        self.n += 1
        t = self.es.enter_context(self.nc.psum_tensor(name or ("ps%d" % self.n), list(shape), dt))
        return t, Tk(name or "ps%d" % self.n)

    def dram(self, name, shape, dt, kind="Internal"):
        t = self.nc.dram_tensor(name, list(shape), dt, kind=kind)
        return t.ap(), Tk(name)


def _rope_tables():
    GRID_W = 64
    dim = 64
    rows = L // GRID_W
    row, col = np.meshgrid(np.arange(rows), np.arange(GRID_W), indexing="ij")
    row = row.reshape(-1).astype(np.float32)
    col = col.reshape(-1).astype(np.float32)
    half = dim // 2
    inv_freq = (1.0 / (10000.0 ** (np.arange(0, half, 2, dtype=np.float32) / half))).astype(np.float32)

    def table(pos):
        ang = pos[:, None] * inv_freq[None, :]
        ang = np.concatenate([ang, ang], axis=-1)
        return np.cos(ang), np.sin(ang)

    cr, sr = table(row)
    cc, sc = table(col)
    cos = np.concatenate([cr, cc], -1).astype(np.float32)
    sin = np.concatenate([sr, sc], -1).astype(np.float32)
    cosT = np.concatenate([cos.T, cos.T], 0)
    sinT = np.concatenate([sin.T, sin.T], 0)
    R = np.zeros((64, 64), np.float32)
    for base in (0, 32):
        for i in range(16):
            R[base + i, base + 16 + i] = -1.0
            R[base + 16 + i, base + i] = 1.0
    R2 = np.zeros((128, 128), np.float32)
    R2[:64, :64] = R
    R2[64:, 64:] = R
    return cosT, sinT, np.ascontiguousarray(R2.T)


def _consts():
    cosT, sinT, RT = _rope_tables()
    c = {
        "k_cos": cosT, "k_sin": sinT, "k_rt": RT,
        "k_ident": np.eye(128, dtype=np.float32),
        "k_ones": np.ones((128, 128), np.float32),
    }
    wm = np.zeros((128, 6, 512), np.float32)
    kk = np.arange(128)[:, None]
    qq = np.arange(512)[None, :]
    for ri, r in enumerate(range(-1, 5)):
        wm[:, ri, :] = np.where(np.abs(r * 128 + kk - qq) <= 128, 1.0, 0.0)
    c["k_wmask"] = wm.reshape(128, 6 * 512)
    ii = np.arange(128)
    sc = (ii[:, None] // 64) == (ii[None, :] // 64)
    dm = np.zeros((128, 8, 128), np.float32)
    dm[:, 0] = sc & (ii[:, None] <= ii[None, :])
    dm[:, 1] = sc & (ii[:, None] >= ii[None, :])
    dm[:, 2] = sc & (ii[:, None] > ii[None, :])
    dm[:, 3] = sc & (ii[:, None] < ii[None, :])
    dm[:, 4] = sc
    dm[:, 5] = (ii[:, None] < 64) / 64.0 + 0 * ii[None, :]
    dm[:, 6] = (ii[:, None] >= 64) / 64.0 + 0 * ii[None, :]
    c["k_dnmask"] = dm.reshape(128, 8 * 128)
    mb = np.zeros((128, 224), np.float32)
    mb[:, 0:128] = (ii[:, None] < ii[None, :])
    mb[:, 128:196] = np.arange(68)[None, :]
    mb[:, 196:212] = np.arange(16)[None, :] * 128 + ii[:, None]
    mb[:, 212:218] = np.arange(6)[None, :] * 128 + ii[:, None]
    c["k_moeb"] = mb
    pc = np.zeros((128, 4), np.float32)
    pc[:, 0] = ((ii // 32) % 2 == 0)
    pc[:, 1] = ((ii // 32) % 2 == 1)
    c["k_dncol"] = pc
    return c


class Builder:
    def __init__(self, nc, es, n_layers=1, dbg=None, do_mixer=True, do_moe=True, wl=1, layer_abs=0, final=False, fused=False):
        self.fused = fused
        self.layers = list(range(DEPTH)) if fused else [layer_abs]
        self.wl = DEPTH if fused else 1
        self.layer_abs = layer_abs
        self.final = True if fused else final
        self.nc = nc
        self.c = Ctx(nc, es)
        self.p = self.c.p
        self.n_layers = n_layers
        self.dbg = dbg or []
        self.do_mixer = do_mixer
        self.do_moe = do_moe
        self.out_ops = []

    def op(self, eng, fn, r=(), w=(), dma=False):
        psk = getattr(self, "_psk", None)
        if psk:
            r2 = [t for t in r if id(t) not in psk]
            w = list(w) + [t for t in r if id(t) in psk]
            r = r2
        return self.p.op(eng, fn, r=r, w=w, dma=dma)

    def dma(self, eng, out, in_, r=(), w=()):
        return self.p.op(eng, lambda e: e.dma_start(out=out, in_=in_), r=r, w=w, dma=True)

    def declare_io(self):
        c = self.c
        kind = [0, 1, 2, 0][self.layer_abs]
        kinds = {[0, 1, 2, 0][l] for l in self.layers}
        used = {"x_in", "cin", "ada_w", "ada_b", "norm_mix_w", "norm_ffn_w", "final_norm_w",
                "k_cos", "k_sin", "k_rt", "k_ident", "k_ones", "k_wmask", "k_dnmask", "k_dncol", "k_moeb"}
        if self.do_mixer and 0 in kinds:
            used |= {"da_w_qkv", "da_lambda", "da_subln_w", "da_w_o"}
        if self.do_mixer and 2 in kinds:
            used |= {"wa_w_qkv", "wa_sinks", "wa_w_o"}
        if self.do_mixer and 1 in kinds:
            used |= {"dn_w_in", "dn_conv_w", "dn_a_log", "dn_dt_bias", "dn_norm_w", "dn_w_o"}
        if self.do_moe:
            used |= {"moe_wg", "moe_bg", "moe_we", "moe_be", "moe_w13", "moe_w2"}
        self.used = used
        self.in_shapes = {}

        def ein(n, s):
            if n not in used:
                s = [1, 1]
            self.in_shapes[n] = list(s)
            return c.dram(n, s, F32, "ExternalInput")
        self.x_in, self.x_in_k = ein("x_in", [NT, D])
        self.cin, self.cin_k = ein("cin", [128, KC, 2])
        self.ada_w, self.ada_w_k = ein("ada_w", [self.wl, D, 6 * D])
        self.ada_b, _ = ein("ada_b", [self.wl, 6 * D])
        self.norm_mix_w, _ = ein("norm_mix_w", [self.wl, D])
        self.norm_ffn_w, _ = ein("norm_ffn_w", [self.wl, D])
        self.da_w_qkv, _ = ein("da_w_qkv", [(2 if self.fused else 1), D, 3 * D])
        self.da_lambda, _ = ein("da_lambda", [(2 if self.fused else 1), 4, 64])
        self.da_subln_w, _ = ein("da_subln_w", [(2 if self.fused else 1), 128])
        self.da_w_o, _ = ein("da_w_o", [(2 if self.fused else 1), D, D])
        self.dn_w_in, _ = ein("dn_w_in", [1, D, 12416])
        self.dn_conv_w, _ = ein("dn_conv_w", [1, 5, 8192])
        self.dn_a_log, _ = ein("dn_a_log", [1, 2, 32])
        self.dn_dt_bias, _ = ein("dn_dt_bias", [1, 2, 32])
        self.dn_norm_w, _ = ein("dn_norm_w", [1, 128])
        self.dn_w_o, _ = ein("dn_w_o", [1, 4096, D])
        self.wa_w_qkv, _ = ein("wa_w_qkv", [1, D, 2560])
        self.wa_sinks, _ = ein("wa_sinks", [1, 32])
        self.wa_w_o, _ = ein("wa_w_o", [1, D, D])
        self.moe_wg, _ = ein("moe_wg", [self.wl, D, 4])
        self.moe_bg, _ = ein("moe_bg", [self.wl, 4])
        self.moe_we, _ = ein("moe_we", [self.wl, D, 32])
        self.moe_be, _ = ein("moe_be", [self.wl, 32])
        self.moe_w13, _ = ein("moe_w13", [self.wl, 32, D, 1536])
        self.moe_w2, _ = ein("moe_w2", [self.wl, 32, 768, D])
        self.final_norm_w, _ = ein("final_norm_w", [1, D])
        self.kc = {}
        for name, arr in _consts().items():
            self.kc[name] = ein(name, list(arr.shape))[0]
        if self.final:
            self.y_out, self.y_out_k = c.dram("y_out", [L, D], F32, "ExternalOutput")
        else:
            self.x_out, self.x_out_k = c.dram("x_out", [NT, D], F32, "ExternalOutput")
        self.XR, _ = c.dram("XR", [NT, D], F32)
        self.XR_k = [Tk("xr%d" % t) for t in range(NTILE)]
        self.MOD, self.MOD_k = c.dram("MODS", [DEPTH, 2, 6 * D], F32)
        import os as _os
        dk = "ExternalOutput" if _os.environ.get("MK_DUMP") else "Internal"
        self.QT, self.QT_k = c.dram("QT", [16, 128, NT], BF16, dk)
        self.KT, self.KT_k = c.dram("KT", [16, 128, NT], BF16, dk)
        self.VV, self.VV_k = c.dram("VV", [NT, D], BF16, dk)
        if 1 in kinds and self.do_mixer:
            self.DNK, self.DNK_k = c.dram("DNK", [NT, 2048], BF16, dk)
            self.DNV, self.DNV_k = c.dram("DNV", [NT, 4096], BF16, dk)
            self.DNZ, self.DNZ_k = c.dram("DNZ", [NT, 4096], BF16, dk)
            self.ODN, self.ODN_k = c.dram("ODN", [2, NT, 4096], F32, dk)
        self.dbg_out = {}
        for name, shape in self.dbg:
            self.dbg_out[name] = c.dram(name, shape, F32, "ExternalOutput")

    def alloc(self):
        c = self.c
        self.BIGA, _ = c.sb([128, KC, NT], BF16, "BIGA")
        self.BIGA_k = [Tk("biga%d" % t) for t in range(NTILE)]
        self.PS = [c.ps([128, 512], F32, "PS%d" % i) for i in range(8)]
        self._psk = {id(k) for (_, k) in self.PS}
        self.ident_f, self.ident_f_k = c.sb([128, 128], F32, "ident_f")
        self.ident_b, self.ident_b_k = c.sb([128, 128], BF16, "ident_b")
        self.ones_f, self.ones_f_k = c.sb([128, 128], F32, "ones_f")
        self.ones_b, self.ones_b_k = c.sb([128, 128], BF16, "ones_b")
        self.rt_b, self.rt_b_k = c.sb([128, 128], BF16, "rt_b")
        self.WT = [c.sb([128, KC, 512], BF16, "WT%d" % i) for i in range(2)]
        self.wt_i = 0
        self.WB = [c.sb([128, D], F32, "WB%d" % i) for i in range(2)]
        self.SB = [c.sb([128, D], F32, "SB%d" % i) for i in range(2)]
        self.GB = self.WB
        self.XT = [c.sb([128, D], F32, "XT%d" % i) for i in range(2)]
        self.HB = [c.sb([128, D], BF16, "HB%d" % i) for i in range(2)]
        self.small = [c.sb([128, 8], F32, "small%d" % i) for i in range(2)]
        self.misc = c.sb([128, 64], F32, 'misc')
        self.GATE = c.sb([128, NTILE, 32], F32, 'GATE')
        c.arena_init(42 * 1024)

    def load_consts(self):
        for name, dst_f, dst_fk, dst_b, dst_bk in (
            ("k_ident", self.ident_f, self.ident_f_k, self.ident_b, self.ident_b_k),
            ("k_ones", self.ones_f, self.ones_f_k, self.ones_b, self.ones_b_k),
        ):
            self.dma("sp", dst_f[:], self.kc[name][:, :], w=[dst_fk])
            self.op("dve", lambda e, a=dst_b, b=dst_f: e.tensor_copy(out=a[:], in_=b[:]), r=[dst_fk], w=[dst_bk])
        self.dma("pool", self.rt_b[:], self.kc["k_rt"][:, :], w=[self.rt_b_k])
        for t in range(NTILE):
            self.dma("sp", self.XR[t * 128:(t + 1) * 128, :], self.x_in[t * 128:(t + 1) * 128, :],
                     w=[self.XR_k[t]])

    def phase_mod(self):
        c = self.c
        c.arena_reset()
        cs, cs_k = c.asb([128, KC, 2], F32, "cs")
        sig, sig_k = c.asb([128, KC, 2], F32, "sig")
        self.dma("sp", cs[:], self.cin[:, :, :], w=[cs_k])
        self.op("act", lambda e: e.activation(out=sig[:], in_=cs[:], func=AF.Sigmoid), r=[cs_k], w=[sig_k])
        self.op("dve", lambda e: e.tensor_tensor(out=cs[:], in0=cs[:], in1=sig[:], op=ALU.mult), r=[sig_k, cs_k], w=[cs_k])
        AW = [c.asb([128, KC, 256], F32, "AW%d" % i) for i in range(2)]
        bias, bias_k = self.XT[0][0][0:2, :], self.XT[0][1]
        nrm, nrm_k = self.XT[1][0][0:2, :], self.XT[1][1]
        res, res_k = self.SB[0][0][0:2, :], self.SB[0][1]
        ps, ps_k = self.PS[0]
        i = 0
        for l in range(len(self.layers)):
            for v in range(6):
                self.dma("sp", bias[:], self.ada_b[l:l + 1, v * D:(v + 1) * D].partition_broadcast(2), w=[bias_k])
                if v in (1, 4):
                    nw = self.norm_mix_w if v == 1 else self.norm_ffn_w
                    self.dma("sp", nrm[:], nw[l:l + 1, :].partition_broadcast(2), w=[nrm_k])
                for b in range(8):
                    aw, aw_k = AW[i % 2]
                    i += 1
                    col = v * D + b * 256
                    self.dma("sp", aw[:], self.ada_w[l, :, col:col + 256].rearrange("(kc p) n -> p kc n", p=128), w=[aw_k])
                    for kc in range(KC):
                        self.op("pe", lambda e, aw=aw, kc=kc: e.matmul(ps[0:2, 0:256], lhsT=cs[:, kc, :], rhs=aw[:, kc, :],
                                                                      start=(kc == 0), stop=(kc == KC - 1)),
                                r=[cs_k, aw_k], w=[ps_k])
                    self.op("dve", lambda e, b=b: e.tensor_tensor(out=res[:, b * 256:(b + 1) * 256], in0=ps[0:2, 0:256],
                                                                 in1=bias[:, b * 256:(b + 1) * 256], op=ALU.add),
                            r=[ps_k, bias_k], w=[res_k])
                if v in (1, 4):
                    self.op("dve", lambda e: e.scalar_tensor_tensor(out=res[:], in0=res[:], scalar=1.0, in1=nrm[:],
                                                                   op0=ALU.add, op1=ALU.mult),
                            r=[res_k, nrm_k], w=[res_k])
                self.dma("sp", self.MOD[l, :, v * D:(v + 1) * D], res[:], r=[res_k], w=[self.MOD_k])

    def load_mod_tiles(self, l, sub):
        for ty in range(2):
            for j, (buf, bk) in enumerate((self.SB[ty], self.WB[ty])):
                v = sub * 3 + j
                self.dma("sp", buf[:], self.MOD[l, ty:ty + 1, v * D:(v + 1) * D].partition_broadcast(128),
                         r=[self.MOD_k], w=[bk])

    def load_gate_tiles(self, l, sub):
        for ty in range(2):
            buf, bk = self.GB[ty]
            v = sub * 3 + 2
            self.dma("sp", buf[:], self.MOD[l, ty:ty + 1, v * D:(v + 1) * D].partition_broadcast(128),
                     r=[self.MOD_k], w=[bk])

    def phase_norm(self, hrow_dram=None, hrow_k=None, router=None, skip_T=False):
        ps_t, ps_tk = self.PS[1]
        pst = ps_t[:].bitcast(BF16)
        for t in range(NTILE):
            ty = 1 if t < 2 else 0
            xt, xt_k = self.XT[t % 2]
            hb, hb_k = self.HB[t % 2]
            sm, sm_k = self.small[t % 2]
            self.dma("sp", xt[:], self.XR[t * 128:(t + 1) * 128, :], r=[self.XR_k[t]], w=[xt_k])
            self.op("act", lambda e, xt=xt, sm=sm, hb=hb: e.activation(out=hb[:], in_=xt[:], func=AF.Square, accum_out=sm[:, 0:1]),
                    r=[xt_k], w=[hb_k, sm_k])
            self.op("act", lambda e, sm=sm: e.activation(out=sm[:, 1:2], in_=sm[:, 0:1], func=AF.Sqrt, scale=1.0 / D, bias=EPS),
                    r=[sm_k], w=[sm_k])
            self.op("dve", lambda e, sm=sm: e.reciprocal(out=sm[:, 2:3], in_=sm[:, 1:2]), r=[sm_k], w=[sm_k])
            self.op("dve", lambda e, xt=xt, sm=sm, ty=ty: e.scalar_tensor_tensor(
                out=xt[:], in0=xt[:], scalar=sm[:, 2:3], in1=self.WB[ty][0][:], op0=ALU.mult, op1=ALU.mult),
                r=[xt_k, sm_k, self.WB[ty][1]], w=[xt_k])
            if router is None:
                self.op("pool", lambda e, xt=xt, hb=hb, ty=ty: e.tensor_tensor(out=hb[:], in0=xt[:], in1=self.SB[ty][0][:], op=ALU.add),
                        r=[xt_k, self.SB[ty][1]], w=[hb_k])
            else:
                self.op("pool", lambda e, xt=xt, ty=ty: e.tensor_tensor(out=xt[:], in0=xt[:], in1=self.SB[ty][0][:], op=ALU.add),
                        r=[xt_k, self.SB[ty][1]], w=[xt_k])
                self.op("act", lambda e, xt=xt, hb=hb: e.copy(out=hb[:], in_=xt[:]), r=[xt_k], w=[hb_k])
                router(t, xt, xt_k)
            if hrow_dram is not None:
                self.dma("sp", hrow_dram[t * 128:(t + 1) * 128, :], hb[:], r=[hb_k], w=[hrow_k[t]])
            if skip_T:
                continue
            for g in range(2):
                for j in range(8):
                    kc = g * 8 + j
                    self.op("pe", lambda e, hb=hb, kc=kc, j=j: e.transpose(out=pst[:, j * 128:(j + 1) * 128],
                                                                          in_=hb[:, kc * 128:(kc + 1) * 128], identity=self.ident_b[:]),
                            r=[hb_k, self.ident_b_k], w=[ps_tk])
                eng = "act" if g == 0 else "dve"
                if eng == "act":
                    self.op("act", lambda e, g=g, t=t: e.copy(out=self.BIGA[:, g * 8:(g + 1) * 8, t * 128:(t + 1) * 128],
                                                             in_=pst.rearrange("p (a b) -> p a b", a=8)),
                            r=[ps_tk], w=[self.BIGA_k[t]])
                else:
                    self.op("dve", lambda e, g=g, t=t: e.tensor_copy(out=self.BIGA[:, g * 8:(g + 1) * 8, t * 128:(t + 1) * 128],
                                                                    in_=pst.rearrange("p (a b) -> p a b", a=8)),
                            r=[ps_tk], w=[self.BIGA_k[t]])

    def load_w(self, w_ap_rows_cols, ncols, nk=KC):
        wt, wt_k = self.WT[self.wt_i % 2]
        self.wt_i += 1
        self.dma("pool", wt[:, 0:nk, 0:ncols], w_ap_rows_cols.rearrange("(kc p) n -> p kc n", p=128), w=[wt_k])
        return wt, wt_k

    TOKBLK = [(0, 256), (256, 512), (768, 512), (1280, 512), (1792, 512)]

    def linear_fm(self, w2d, col0, ncols, post):
        psi = 0
        for cb in range(0, ncols, 512):
            nb = min(512, ncols - cb)
            wt, wt_k = self.load_w(w2d[:, col0 + cb:col0 + cb + nb], nb)
            for jj in range(nb // 128):
                j = (cb // 128) + jj
                for bi, (t0, n) in enumerate(self.TOKBLK):
                    ps, ps_k = self.PS[2 + (psi % 2)]
                    psi += 1
                    rk = [self.BIGA_k[t] for t in range(t0 // 128, (t0 + n) // 128)] + [wt_k]
                    for kc in range(KC):
                        self.op("pe", lambda e, ps=ps, wt=wt, kc=kc, jj=jj, t0=t0, n=n: e.matmul(
                            ps[:, 0:n], lhsT=wt[:, kc, jj * 128:(jj + 1) * 128], rhs=self.BIGA[:, kc, t0:t0 + n],
                            start=(kc == 0), stop=(kc == KC - 1)), r=rk, w=[ps_k])
                    post(j, bi, t0, n, ps, ps_k)

    def linear_tm(self, w2d, col0, ncols, post, src=None, src_k=None, nk=KC):
        src = self.BIGA if src is None else src
        src_k = self.BIGA_k if src_k is None else src_k
        psi = 0
        for cb in range(0, ncols, 512):
            nb = min(512, ncols - cb)
            wt, wt_k = self.load_w(w2d[:, col0 + cb:col0 + cb + nb], nb, nk)
            for t in range(NTILE):
                ps, ps_k = self.PS[2 + (psi % 2)]
                psi += 1
                for kc in range(nk):
                    self.op("pe", lambda e, ps=ps, wt=wt, kc=kc, t=t, nb=nb: e.matmul(
                        ps[:, 0:nb], lhsT=src[:, kc, t * 128:(t + 1) * 128], rhs=wt[:, kc, 0:nb],
                        start=(kc == 0), stop=(kc == nk - 1)), r=[src_k[t], wt_k], w=[ps_k])
                post(cb, nb, t, ps, ps_k)

    def phase_oproj_residual(self, w2d, l):
        c = self.c
        self.load_gate_tiles(l, 0)
        c.arena_reset()
        self.RX = [c.asb([128, 512], F32, "RX%d" % i) for i in range(3)]
        self.rxi = 0

        def post(cb, nb, t, ps, ps_k):
            ty = 1 if t < 2 else 0
            rx, rx_k = self.RX[self.rxi % 3]
            self.rxi += 1
            self.dma("sp", rx[:], self.XR[t * 128:(t + 1) * 128, cb:cb + nb], r=[self.XR_k[t]], w=[rx_k])
            self.op("dve", lambda e, rx=rx, ps=ps, ty=ty, cb=cb, nb=nb: e.tensor_tensor(
                out=ps[:, 0:nb], in0=ps[:, 0:nb], in1=self.GB[ty][0][:, cb:cb + nb], op=ALU.mult),
                r=[ps_k, self.GB[ty][1]], w=[ps_k])
            self.op("dve", lambda e, rx=rx, ps=ps, nb=nb: e.tensor_tensor(out=rx[:, 0:nb], in0=ps[:, 0:nb], in1=rx[:, 0:nb], op=ALU.add),
                    r=[ps_k, rx_k], w=[rx_k])
            self.dma("sp", self.XR[t * 128:(t + 1) * 128, cb:cb + nb], rx[:, 0:nb], r=[rx_k], w=[self.XR_k[t]])

        self.linear_tm(w2d, 0, D, post)

    def load_rope_tables(self):
        c = self.c
        self.cosT, self.cosT_k = c.asb([128, L], BF16, "cosT")
        self.sinT, self.sinT_k = c.asb([128, L], BF16, "sinT")
        self.dma("pool", self.cosT[:], self.kc["k_cos"][:, :], w=[self.cosT_k])
        self.dma("pool", self.sinT[:], self.kc["k_sin"][:, :], w=[self.sinT_k])
        self.RP = [(c.asb([128, 512], BF16, "RPa%d" % i), c.asb([128, 512], BF16, "RPb%d" % i)) for i in range(2)]
        self.rpi = 0

    def rope_store(self, ps, ps_k, t0, n, dst_dram, dst_k):
        c = self.c
        (qa, qa_k), (qb, qb_k) = self.RP[self.rpi % 2]
        self.rpi += 1
        self.op("act", lambda e: e.copy(out=qa[:, 0:n], in_=ps[:, 0:n]), r=[ps_k], w=[qa_k])
        if t0 >= LC:
            l0 = t0 - LC
            pr, pr_k = self.PS[4]
            self.op("pe", lambda e: e.matmul(pr[:, 0:n], lhsT=self.rt_b[:], rhs=qa[:, 0:n], start=True, stop=True),
                    r=[qa_k, self.rt_b_k], w=[pr_k])
            self.op("dve", lambda e: e.tensor_tensor(out=qb[:, 0:n], in0=pr[:, 0:n], in1=self.sinT[:, l0:l0 + n], op=ALU.mult),
                    r=[pr_k, self.sinT_k], w=[qb_k])
            self.op("pool", lambda e: e.tensor_tensor(out=qa[:, 0:n], in0=qa[:, 0:n], in1=self.cosT[:, l0:l0 + n], op=ALU.mult),
                    r=[qa_k, self.cosT_k], w=[qa_k])
            self.op("pool", lambda e: e.tensor_tensor(out=qa[:, 0:n], in0=qa[:, 0:n], in1=qb[:, 0:n], op=ALU.add),
                    r=[qa_k, qb_k], w=[qa_k])
        self.dma("sp", dst_dram[:, t0:t0 + n], qa[:, 0:n], r=[qa_k], w=[dst_k])

    def mixer_diff(self, l, j):
        c = self.c
        lam_init = 0.8 - 0.6 * math.exp(-0.3 * l)
        wqkv = self.da_w_qkv[j]
        c.arena_reset()
        self.load_rope_tables()
        self.VS = [c.asb([128, 512], BF16, "VS%d" % i) for i in range(2)]
        self.vsi = 0
        self.lamt = c.asb([1, 4, 64], F32, "lamt")
        self.lamp = c.asb([1, 8], F32, "lamp")
        self.lamc = (self.misc[0][:, 0:2], Tk("lamc"))
        self.subw = (self.misc[0][:, 2:4], Tk("subw"))
        self.linear_fm(wqkv, 0, D, lambda jj, bi, t0, n, ps, ps_k: self.rope_store(ps, ps_k, t0, n, self.QT[jj], self.QT_k))
        self.linear_fm(wqkv, D, D, lambda jj, bi, t0, n, ps, ps_k: self.rope_store(ps, ps_k, t0, n, self.KT[jj], self.KT_k))

        def vpost(cb, nb, t, ps, ps_k):
            vs, vs_k = self.VS[self.vsi % 2]
            self.vsi += 1
            self.op("act", lambda e: e.copy(out=vs[:, 0:nb], in_=ps[:, 0:nb]), r=[ps_k], w=[vs_k])
            self.dma("sp", self.VV[t * 128:(t + 1) * 128, cb:cb + nb], vs[:, 0:nb], r=[vs_k], w=[self.VV_k])

        self.linear_tm(wqkv, 2 * D, D, vpost)
        lamt, lamt_k = self.lamt
        lamp, lamp_k = self.lamp
        lamc, lamc_k = self.lamc
        subw, subw_k = self.subw
        self.dma("sp", lamt[:], self.da_lambda[j:j + 1, :, :], w=[lamt_k])
        self.op("dve", lambda e: e.tensor_tensor(out=lamt[:, 0, :], in0=lamt[:, 0, :], in1=lamt[:, 1, :], op=ALU.mult), r=[lamt_k], w=[lamt_k])
        self.op("dve", lambda e: e.tensor_tensor(out=lamt[:, 2, :], in0=lamt[:, 2, :], in1=lamt[:, 3, :], op=ALU.mult), r=[lamt_k], w=[lamt_k])
        self.op("dve", lambda e: e.reduce_sum(out=lamp[:, 0:1], in_=lamt[:, 0, :], axis=AX.X), r=[lamt_k], w=[lamp_k])
        self.op("dve", lambda e: e.reduce_sum(out=lamp[:, 1:2], in_=lamt[:, 2, :], axis=AX.X), r=[lamt_k], w=[lamp_k])
        self.op("act", lambda e: e.activation(out=lamp[:, 2:4], in_=lamp[:, 0:2], func=AF.Exp), r=[lamp_k], w=[lamp_k])
        self.op("dve", lambda e: e.scalar_tensor_tensor(out=lamp[:, 4:5], in0=lamp[:, 3:4], scalar=-lam_init, in1=lamp[:, 2:3],
                                                       op0=ALU.add, op1=ALU.subtract), r=[lamp_k], w=[lamp_k])
        psl, psl_k = self.PS[4]
        self.op("pe", lambda e: e.matmul(psl[:, 0:1], lhsT=self.ones_f[0:1, :], rhs=lamp[0:1, 4:5], start=True, stop=True),
                r=[self.ones_f_k, lamp_k], w=[psl_k])
        self.op("dve", lambda e: e.tensor_copy(out=lamc[:, 0:1], in_=psl[:, 0:1]), r=[psl_k], w=[lamc_k])
        self.dma("sp", subw[:, 0:1], self.da_subln_w[j:j + 1, :].rearrange("o d -> d o"), w=[subw_k])
        self.op("dve", lambda e: e.tensor_scalar(out=subw[:, 1:2], in0=subw[:, 0:1], scalar1=(1.0 - lam_init), scalar2=None, op0=ALU.mult),
                r=[subw_k], w=[subw_k])
        self.attention_core(16, diff=True)

    def attention_core(self, nheads, diff):
        c = self.c
        c.arena_reset()
        self.AQ = [c.asb([128, NT], BF16, "AQ%d" % i) for i in range(2)]
        self.AK = [c.asb([128, NT], BF16, "AK%d" % i) for i in range(2)]
        self.AV = [c.asb([128, NTILE, 128], BF16, "AV%d" % i) for i in range(2)]
        self.ET = [c.asb([128, 512], BF16, "ET%d" % i) for i in range(4)]
        self.eti = 0
        self.CMB = [c.asb([128, 512], F32, "CMB%d" % i) for i in range(4)]
        self.SQB = c.asb([128, 512], BF16, "SQB")
        scale = 64 ** -0.5
        lamc, lamc_k = self.lamc
        subw, subw_k = self.subw
        for h in range(nheads):
            aq, aq_k = self.AQ[h % 2]
            ak, ak_k = self.AK[h % 2]
            av, av_k = self.AV[h % 2]
            self.dma("sp", aq[:], self.QT[h], r=[self.QT_k], w=[aq_k])
            self.dma("sp", ak[:], self.KT[h], r=[self.KT_k], w=[ak_k])
            self.dma("sp", av[:], self.VV[:, h * 128:(h + 1) * 128].rearrange("(t p) d -> p t d", p=128), r=[self.VV_k], w=[av_k])
            for bi, (t0, n) in enumerate(self.TOKBLK):
                nkt = 2 if bi == 0 else NTILE
                acc = []
                for comp in range(2):
                    po, po_k = self.PS[4 + comp]
                    pz, pz_k = self.PS[6 + comp]
                    p0 = comp * 64
                    for kt in range(nkt):
                        pss, pss_k = self.PS[kt % 2]
                        et, et_k = self.ET[self.eti % 4]
                        self.eti += 1
                        self.op("pe", lambda e, pss=pss, ak=ak, aq=aq, kt=kt, p0=p0, t0=t0, n=n: e.matmul(
                            pss[:, 0:n], lhsT=ak[p0:p0 + 64, kt * 128:(kt + 1) * 128], rhs=aq[p0:p0 + 64, t0:t0 + n],
                            start=True, stop=True), r=[ak_k, aq_k], w=[pss_k])
                        self.op("act", lambda e, pss=pss, et=et, n=n: e.activation(out=et[:, 0:n], in_=pss[:, 0:n], func=AF.Exp, scale=scale),
                                r=[pss_k], w=[et_k])
                        self.op("pe", lambda e, po=po, av=av, et=et, kt=kt, n=n, nkt=nkt: e.matmul(
                            po[:, 0:n], lhsT=av[:, kt, :], rhs=et[:, 0:n], start=(kt == 0), stop=(kt == nkt - 1)),
                            r=[av_k, et_k], w=[po_k])
                        self.op("pe", lambda e, pz=pz, et=et, kt=kt, n=n, nkt=nkt: e.matmul(
                            pz[:, 0:n], lhsT=self.ones_b[:], rhs=et[:, 0:n], start=(kt == 0), stop=(kt == nkt - 1)),
                            r=[self.ones_b_k, et_k], w=[pz_k])
                    acc.append((po, po_k, pz, pz_k))
                (r0, r0_k), (r1, r1_k), (o0, o0_k), (o1, o1_k) = self.CMB
                (po0, po0_k, pz0, pz0_k), (po1, po1_k, pz1, pz1_k) = acc
                self.op("dve", lambda e, n=n: e.reciprocal(out=r0[:, 0:n], in_=pz0[:, 0:n]), r=[pz0_k], w=[r0_k])
                self.op("dve", lambda e, n=n: e.reciprocal(out=r1[:, 0:n], in_=pz1[:, 0:n]), r=[pz1_k], w=[r1_k])
                self.op("dve", lambda e, n=n: e.tensor_tensor(out=o0[:, 0:n], in0=po0[:, 0:n], in1=r0[:, 0:n], op=ALU.mult), r=[po0_k, r0_k], w=[o0_k])
                self.op("dve", lambda e, n=n: e.tensor_tensor(out=o1[:, 0:n], in0=po1[:, 0:n], in1=r1[:, 0:n], op=ALU.mult), r=[po1_k, r1_k], w=[o1_k])
                self.op("dve", lambda e, n=n: e.scalar_tensor_tensor(out=o0[:, 0:n], in0=o1[:, 0:n], scalar=lamc[:, 0:1], in1=o0[:, 0:n],
                                                                    op0=ALU.mult, op1=ALU.add), r=[o0_k, o1_k, lamc_k], w=[o0_k])
                sqb, sqb_k = self.SQB
                self.op("act", lambda e, n=n: e.activation(out=sqb[:, 0:n], in_=o0[:, 0:n], func=AF.Square), r=[o0_k], w=[sqb_k])
                pss, pss_k = self.PS[0]
                self.op("pe", lambda e, n=n, pss=pss: e.matmul(pss[:, 0:n], lhsT=self.ones_b[:], rhs=sqb[:, 0:n], start=True, stop=True),
                        r=[sqb_k, self.ones_b_k], w=[pss_k])
                self.op("act", lambda e, n=n, pss=pss: e.activation(out=r0[:, 0:n], in_=pss[:, 0:n], func=AF.Sqrt, scale=1.0 / 128, bias=EPS),
                        r=[pss_k], w=[r0_k])
                self.op("dve", lambda e, n=n: e.reciprocal(out=r0[:, 0:n], in_=r0[:, 0:n]), r=[r0_k], w=[r0_k])
                wk = [self.BIGA_k[t] for t in range(t0 // 128, (t0 + n) // 128)]
                self.op("dve", lambda e, n=n, t0=t0, h=h: e.scalar_tensor_tensor(
                    out=self.BIGA[:, h, t0:t0 + n], in0=o0[:, 0:n], scalar=subw[:, 1:2], in1=r0[:, 0:n], op0=ALU.mult, op1=ALU.mult),
                    r=[o0_k, r0_k, subw_k], w=wk)

    def phase_final(self):
        fw, fw_k = self.WB[0]
        self.dma("sp", fw[:], self.final_norm_w[0:1, :].partition_broadcast(128), w=[fw_k])
        for t in range(2, NTILE):
            xt, xt_k = self.XT[t % 2]
            sm, sm_k = self.small[t % 2]
            self.dma("sp", xt[:], self.XR[t * 128:(t + 1) * 128, :], r=[self.XR_k[t]], w=[xt_k])
            hb, hb_k = self.HB[t % 2]
            self.op("act", lambda e, xt=xt, sm=sm, hb=hb: e.activation(out=hb[:], in_=xt[:], func=AF.Square, accum_out=sm[:, 0:1]),
                    r=[xt_k], w=[hb_k, sm_k])
            self.op("act", lambda e, sm=sm: e.activation(out=sm[:, 1:2], in_=sm[:, 0:1], func=AF.Sqrt, scale=1.0 / D, bias=EPS),
                    r=[sm_k], w=[sm_k])
            self.op("dve", lambda e, sm=sm: e.reciprocal(out=sm[:, 2:3], in_=sm[:, 1:2]), r=[sm_k], w=[sm_k])
            self.op("dve", lambda e, xt=xt, sm=sm: e.scalar_tensor_tensor(
                out=xt[:], in0=xt[:], scalar=sm[:, 2:3], in1=fw[:], op0=ALU.mult, op1=ALU.mult),
                r=[xt_k, sm_k, fw_k], w=[xt_k])
            self.out_ops.append(self.dma("sp", self.y_out[(t - 2) * 128:(t - 1) * 128, :], xt[:], r=[xt_k], w=[self.y_out_k]))

    def build(self):
        self.declare_io()
        self.alloc()
        self.load_consts()
        self.phase_mod()
        for li, labs in enumerate(self.layers):
            kind = [0, 1, 2, 0][labs]
            ji = ([0, 0, 0, 1][labs]) if self.fused else 0
            if self.do_mixer:
                self.load_mod_tiles(li, 0)
                self.phase_norm()
                if kind == 0:
                    self.mixer_diff(labs, ji)
                    self.phase_oproj_residual(self.da_w_o[ji], li)
                elif kind == 1:
                    self.cur_li = li
                    self.mixer_dn()
                else:
                    self.mixer_win()
                    self.phase_oproj_residual(self.wa_w_o[0], li)
            if self.do_moe:
                self.load_mod_tiles(li, 1)
                self.phase_moe_b(li)
        if self.final:
            self.phase_final()
        else:
            for t in range(NTILE):
                self.out_ops.append(self.dma("sp", self.x_out[t * 128:(t + 1) * 128, :], self.XR[t * 128:(t + 1) * 128, :],
                                             r=[self.XR_k[t]], w=[self.x_out_k]))
        self.p.emit(self.out_ops)


def build_nc(**kw):
    nc = bass.Bass("TRN2", target_bir_lowering=False)
    with ExitStack() as es:
        b = Builder(nc, es, **kw)
        b.build()
    return nc, b


def _launch(inputs, xcur, layer, do_mixer, do_moe, final):
    nc, bld = build_nc(layer_abs=layer, do_mixer=do_mixer, do_moe=do_moe, final=final)
    print('nops', len(bld.p.ops), flush=True)
    j = [0, 0, 0, 1][layer]
    per_layer = {"ada_w": inputs["ada_w"][layer:layer + 1], "ada_b": inputs["ada_b"][layer:layer + 1],
                 "norm_mix_w": inputs["norm_mix_w"][layer:layer + 1], "norm_ffn_w": inputs["norm_ffn_w"][layer:layer + 1],
                 "da_w_qkv": inputs["da_w_qkv"][j:j + 1], "da_lambda": inputs["da_lambda"][j:j + 1],
                 "da_subln_w": inputs["da_subln_w"][j:j + 1], "da_w_o": inputs["da_w_o"][j:j + 1],
                 "dn_w_in": inputs["dn_w_in"], "dn_conv_w": inputs["dn_conv_w"].reshape(1, 5, 8192),
                 "dn_a_log": inputs["dn_a_log"], "dn_dt_bias": inputs["dn_dt_bias"], "dn_norm_w": inputs["dn_norm_w"],
                 "dn_w_o": inputs["dn_w_o"],
                 "wa_w_qkv": inputs["wa_w_qkv"], "wa_sinks": inputs["wa_sinks"], "wa_w_o": inputs["wa_w_o"],
                 "moe_wg": inputs["moe_wg"][layer:layer + 1], "moe_bg": inputs["moe_bg"][layer:layer + 1],
                 "moe_we": inputs["moe_we"][layer:layer + 1], "moe_be": inputs["moe_be"][layer:layer + 1],
                 "moe_w13": inputs["moe_w13"][layer:layer + 1], "moe_w2": inputs["moe_w2"][layer:layer + 1],
                 "final_norm_w": inputs["final_norm_w"].reshape(1, D)}
    consts = _consts()
    dummy = np.zeros((1, 1), np.float32)
    in_maps = []
    for b in range(len(xcur)):
        m = {"x_in": xcur[b]}
        cin = np.stack([inputs["c"][b], inputs["c_ctx"]], -1)
        m["cin"] = np.ascontiguousarray(cin.reshape(KC, 128, 2).transpose(1, 0, 2))
        for name, shape in bld.in_shapes.items():
            if name in m:
                continue
            if name not in bld.used:
                m[name] = dummy
            elif name in consts:
                m[name] = consts[name]
            else:
                m[name] = np.ascontiguousarray(per_layer[name])
        in_maps.append(m)
    res = run_bass_kernel_spmd(nc, in_maps, core_ids=list(range(len(xcur))))
    key = "y_out" if final else "x_out"
    global _last_res
    _last_res = res.results
    return [r[key] for r in res.results]


def kernel(**inputs):
    inputs = {k: np.asarray(v) for k, v in inputs.items()}
    nb = inputs["x"].shape[0]
    nc, bld = build_nc(fused=True)
    consts = _consts()
    shared = {k: np.ascontiguousarray(inputs[k]) for k in (
        "ada_w", "ada_b", "norm_mix_w", "norm_ffn_w", "da_w_qkv", "da_lambda", "da_subln_w", "da_w_o",
        "dn_w_in", "dn_conv_w", "dn_a_log", "dn_dt_bias", "dn_norm_w", "dn_w_o", "wa_w_qkv", "wa_sinks", "wa_w_o",
        "moe_wg", "moe_bg", "moe_we", "moe_be", "moe_w13", "moe_w2")}
    shared["final_norm_w"] = inputs["final_norm_w"].reshape(1, D)
    shared.update(consts)
    in_maps = []
    for b in range(nb):
        m = dict(shared)
        m["x_in"] = np.ascontiguousarray(np.concatenate([inputs["ctx"][b], inputs["x"][b]], 0))
        cin = np.stack([inputs["c"][b], inputs["c_ctx"]], -1)
        m["cin"] = np.ascontiguousarray(cin.reshape(KC, 128, 2).transpose(1, 0, 2))
        in_maps.append(m)
    res = run_bass_kernel_spmd(nc, in_maps, core_ids=list(range(nb)))
    return np.stack([r["y_out"] for r in res.results], 0).astype(np.float32)


def _phase_moe(self, l):
    c = self.c
    c.arena_reset()
    gate, gate_k = self.GATE
    ht32, ht32_k = c.asb([128, KC, 128], F32, "ht32")
    wr, wr_k = c.asb([128, KC, 36], F32, "wr")
    br, br_k = c.asb([128, 36], F32, "br")
    lg, lg_k = c.asb([128, 36], F32, "lg")
    rs_, rs_k = c.asb([128, 64], F32, "rsm")
    self.dma("sp", wr[:, :, 0:4], self.moe_wg[l].rearrange("(kc p) n -> p kc n", p=128), w=[wr_k])
    self.dma("sp", wr[:, :, 4:36], self.moe_we[l].rearrange("(kc p) n -> p kc n", p=128), w=[wr_k])
    self.dma("sp", br[:, 0:4], self.moe_bg[l:l + 1, :].partition_broadcast(128), w=[br_k])
    self.dma("sp", br[:, 4:36], self.moe_be[l:l + 1, :].partition_broadcast(128), w=[br_k])
    R = lambda a, b=None: rs_[:, a:(a + 1 if b is None else b)]

    def router(t, xt, xt_k):
        for g in range(4):
            pt, pt_k = self.PS[2 + (g % 2)]
            for jx in range(4):
                kc = g * 4 + jx
                self.op("pe", lambda e, pt=pt, jx=jx, kc=kc, xt=xt: e.transpose(out=pt[:, jx * 128:(jx + 1) * 128],
                                                                              in_=xt[:, kc * 128:(kc + 1) * 128], identity=self.ident_f[:]),
                        r=[xt_k, self.ident_f_k], w=[pt_k])
            self.op("dve", lambda e, pt=pt, g=g: e.tensor_copy(out=ht32[:, g * 4:(g + 1) * 4, :], in_=pt[:].rearrange("p (a b) -> p a b", a=4)),
                    r=[pt_k], w=[ht32_k])
        pl, pl_k = self.PS[4]
        for kc in range(KC):
            self.op("pe", lambda e, kc=kc: e.matmul(pl[:, 0:36], lhsT=ht32[:, kc, :], rhs=wr[:, kc, :], start=(kc == 0), stop=(kc == KC - 1)),
                    r=[ht32_k, wr_k], w=[pl_k])
        V = lambda fn, r=(), w=(): self.op("dve", fn, r=list(r) + [rs_k], w=list(w) + [rs_k])
        self.op("dve", lambda e: e.tensor_tensor(out=lg[:], in0=pl[:, 0:36], in1=br[:], op=ALU.add), r=[pl_k, br_k], w=[lg_k])
        V(lambda e: e.reduce_max(out=R(0), in_=lg[:, 0:4], axis=AX.X), r=[lg_k])
        V(lambda e: e.tensor_scalar(out=R(1), in0=R(0), scalar1=-1.0, scalar2=None, op0=ALU.mult))
        self.op("act", lambda e: e.activation(out=R(56, 60), in_=lg[:, 0:4], func=AF.Exp, bias=R(1), accum_out=R(2)), r=[lg_k, rs_k], w=[rs_k])
        V(lambda e: e.reciprocal(out=R(3), in_=R(2)))
        V(lambda e: e.tensor_scalar(out=R(4, 8), in0=lg[:, 0:4], scalar1=R(0), scalar2=None, op0=ALU.is_equal), r=[lg_k])
        V(lambda e: e.tensor_scalar(out=R(8, 16), in0=lg[:, 4:12], scalar1=R(4), scalar2=None, op0=ALU.mult), r=[lg_k])
        for g in range(1, 4):
            V(lambda e, g=g: e.scalar_tensor_tensor(out=R(8, 16), in0=lg[:, 4 + 8 * g:12 + 8 * g], scalar=R(4 + g), in1=R(8, 16),
                                                   op0=ALU.mult, op1=ALU.add), r=[lg_k])
        V(lambda e: e.reduce_max(out=R(40), in_=R(8, 16), axis=AX.X))
        V(lambda e: e.tensor_scalar(out=R(16, 24), in0=R(8, 16), scalar1=R(40), scalar2=None, op0=ALU.is_equal))
        V(lambda e: e.scalar_tensor_tensor(out=R(24, 32), in0=R(16, 24), scalar=-1e30, in1=R(8, 16), op0=ALU.mult, op1=ALU.add))
        V(lambda e: e.reduce_max(out=R(41), in_=R(24, 32), axis=AX.X))
        V(lambda e: e.tensor_scalar(out=R(32, 40), in0=R(24, 32), scalar1=R(41), scalar2=None, op0=ALU.is_equal))
        V(lambda e: e.tensor_tensor(out=R(42), in0=R(41), in1=R(40), op=ALU.subtract))
        self.op("act", lambda e: e.activation(out=R(43), in_=R(42), func=AF.Exp), r=[rs_k], w=[rs_k])
        V(lambda e: e.tensor_scalar(out=R(44), in0=R(43), scalar1=1.0, scalar2=None, op0=ALU.add))
        V(lambda e: e.reciprocal(out=R(44), in_=R(44)))
        V(lambda e: e.tensor_tensor(out=R(45), in0=R(44), in1=R(3), op=ALU.mult))
        V(lambda e: e.tensor_tensor(out=R(46), in0=R(45), in1=R(43), op=ALU.mult))
        V(lambda e: e.tensor_scalar(out=R(48, 56), in0=R(16, 24), scalar1=R(45), scalar2=None, op0=ALU.mult))
        V(lambda e: e.scalar_tensor_tensor(out=R(48, 56), in0=R(32, 40), scalar=R(46), in1=R(48, 56), op0=ALU.mult, op1=ALU.add))
        for g in range(4):
            self.op("dve", lambda e, g=g, t=t: e.tensor_scalar(out=gate[:, t, g * 8:(g + 1) * 8], in0=R(48, 56), scalar1=R(4 + g),
                                                              scalar2=None, op0=ALU.mult), r=[rs_k], w=[gate_k])

    self.phase_norm(router=router)
    self.load_gate_tiles(l, 1)
    c.arena_reset()
    actt, _ = c.asb([128, 6, NT], BF16, "actt")
    actt_k = [Tk("actt%d" % i) for i in range(5)]
    ysb = [c.asb([128, 512], F32, "ysb%d" % i) for i in range(3)]
    if not hasattr(self, "YACC"):
        self.YACC, _ = c.dram("YACC", [NT, D], F32)
        self.YACC_k = [Tk("yacc%d" % t) for t in range(NTILE)]
    zt, zt_k = self.XT[0]
    self.op("pool", lambda e: e.memset(zt[:], 0.0), w=[zt_k])
    for t in range(NTILE):
        self.dma("sp", self.YACC[t * 128:(t + 1) * 128, :], zt[:], r=[zt_k], w=[self.YACC_k[t]])
    st = {"i": 0}
    blk_of_tile = {}
    for bi, (t0, n) in enumerate(self.TOKBLK):
        for t in range(t0 // 128, (t0 + n) // 128):
            blk_of_tile[t] = bi
    for ex in range(32):
        def post13(j, bi, t0, n, ps, ps_k):
            if j < 6:
                self.op("act", lambda e, j=j, t0=t0, n=n, ps=ps: e.activation(out=actt[:, j, t0:t0 + n], in_=ps[:, 0:n], func=AF.Silu),
                        r=[ps_k], w=[actt_k[bi]])
            else:
                self.op("dve", lambda e, j=j, t0=t0, n=n, ps=ps: e.tensor_tensor(out=actt[:, j - 6, t0:t0 + n], in0=ps[:, 0:n],
                                                                               in1=actt[:, j - 6, t0:t0 + n], op=ALU.mult),
                        r=[ps_k, actt_k[bi]], w=[actt_k[bi]])

        def post2(cb, nb, t, ps, ps_k, ex=ex):
            yb, yb_k = ysb[st["i"] % 3]
            st["i"] += 1
            self.op("dve", lambda e, yb=yb, ps=ps, t=t: e.tensor_scalar(out=yb[:, 0:nb], in0=ps[:, 0:nb], scalar1=gate[:, t, ex:ex + 1],
                                                                       scalar2=None, op0=ALU.mult), r=[ps_k, gate_k], w=[yb_k])
            self.p.op("pool", lambda e, yb=yb, t=t, cb=cb: e.dma_start(out=self.YACC[t * 128:(t + 1) * 128, cb:cb + nb], in_=yb[:, 0:nb],
                                                                       accum_op=ALU.add), r=[yb_k], w=[self.YACC_k[t]], dma=True)

        self.linear_fm(self.moe_w13[l, ex], 0, 1536, post13)
        self.linear_tm(self.moe_w2[l, ex], 0, D, post2, src=actt, src_k=[actt_k[blk_of_tile[t]] for t in range(NTILE)], nk=6)
    for t in range(NTILE):
        ty = 1 if t < 2 else 0
        ya, ya_k = self.XT[t % 2]
        xa, xa_k = self.SB[t % 2]
        self.dma("sp", ya[:], self.YACC[t * 128:(t + 1) * 128, :], r=[self.YACC_k[t]], w=[ya_k])
        self.dma("sp", xa[:], self.XR[t * 128:(t + 1) * 128, :], r=[self.XR_k[t]], w=[xa_k])
        self.op("pool", lambda e, ya=ya, ty=ty: e.tensor_tensor(out=ya[:], in0=ya[:], in1=self.GB[ty][0][:], op=ALU.mult),
                r=[ya_k, self.GB[ty][1]], w=[ya_k])
        self.op("dve", lambda e, ya=ya, xa=xa: e.tensor_tensor(out=xa[:], in0=xa[:], in1=ya[:], op=ALU.add), r=[ya_k, xa_k], w=[xa_k])
        self.dma("sp", self.XR[t * 128:(t + 1) * 128, :], xa[:], r=[xa_k], w=[self.XR_k[t]])


Builder.phase_moe = _phase_moe


def _mixer_win(self):
    c = self.c
    w = self.wa_w_qkv[0]
    c.arena_reset()
    self.load_rope_tables()
    self.VS = [c.asb([128, 512], BF16, "VS%d" % i) for i in range(2)]
    self.vsi = 0
    self.linear_fm(w, 0, D, lambda jj, bi, t0, n, ps, ps_k: self.rope_store(ps, ps_k, t0, n, self.QT[jj], self.QT_k))
    wt, wt_k = self.WT[self.wt_i % 2]
    self.wt_i += 1
    for kvh in range(4):
        for half in range(2):
            self.dma("pool", wt[:, :, (2 * kvh + half) * 64:(2 * kvh + half + 1) * 64],
                     w[:, D + kvh * 64:D + (kvh + 1) * 64].rearrange("(kc p) n -> p kc n", p=128), w=[wt_k])
    psi = 0
    for kvh in range(4):
        for bi, (t0, n) in enumerate(self.TOKBLK):
            ps, ps_k = self.PS[2 + (psi % 2)]
            psi += 1
            rk = [self.BIGA_k[t] for t in range(t0 // 128, (t0 + n) // 128)] + [wt_k]
            for kc in range(KC):
                self.op("pe", lambda e, ps=ps, kc=kc, kvh=kvh, t0=t0, n=n: e.matmul(
                    ps[:, 0:n], lhsT=wt[:, kc, kvh * 128:(kvh + 1) * 128], rhs=self.BIGA[:, kc, t0:t0 + n],
                    start=(kc == 0), stop=(kc == KC - 1)), r=rk, w=[ps_k])
            self.rope_store(ps, ps_k, t0, n, self.KT[kvh], self.KT_k)

    def vpost(cb, nb, t, ps, ps_k):
        vs, vs_k = self.VS[self.vsi % 2]
        self.vsi += 1
        self.op("act", lambda e: e.copy(out=vs[:, 0:nb], in_=ps[:, 0:nb]), r=[ps_k], w=[vs_k])
        self.dma("sp", self.VV[t * 128:(t + 1) * 128, cb:cb + nb], vs[:, 0:nb], r=[vs_k], w=[self.VV_k])

    self.linear_tm(w, D + 256, 256, vpost)
    c.arena_reset()
    AQ = [c.asb([128, NT], BF16, "wAQ%d" % i) for i in range(2)]
    AK = [c.asb([128, NT], BF16, "wAK%d" % i) for i in range(2)]
    VP = [c.asb([128, NTILE, 128], BF16, "wVP%d" % i) for i in range(2)]
    ET = [c.asb([128, 512], BF16, "wET%d" % i) for i in range(4)]
    mk, mk_k = c.asb([128, 6, 512], BF16, "wmask")
    r0, r0_k = c.asb([128, 512], F32, "wr0")
    sx, sx_k = c.asb([128, 32], F32, "wsink")
    self.dma("pool", mk[:], self.kc["k_wmask"][:, :].rearrange("p (a b) -> p a b", a=6), w=[mk_k])
    self.dma("sp", sx[:], self.wa_sinks[0:1, :].partition_broadcast(128), w=[sx_k])
    self.op("act", lambda e: e.activation(out=sx[:], in_=sx[:], func=AF.Exp), r=[sx_k], w=[sx_k])
    for par in range(2):
        vp, vp_k = VP[par]
        self.op("pool", lambda e, vp=vp: e.memset(vp[:], 0.0), w=[vp_k])
    scale = 64 ** -0.5
    eti = 0
    for h in range(32):
        kvh, par = h // 8, h % 2
        aq, aq_k = AQ[(h // 2) % 2]
        ak, ak_k = AK[kvh % 2]
        if par == 0:
            self.dma("sp", aq[:], self.QT[h // 2], r=[self.QT_k], w=[aq_k])
        if h % 8 == 0:
            self.dma("sp", ak[:], self.KT[kvh], r=[self.KT_k], w=[ak_k])
            for p2 in range(2):
                vp, vp_k = VP[p2]
                self.dma("sp", vp[:, :, p2 * 64:(p2 + 1) * 64],
                         self.VV[:, kvh * 64:(kvh + 1) * 64].rearrange("(t p) d -> p t d", p=128), r=[self.VV_k], w=[vp_k])
        vp, vp_k = VP[par]
        p0 = par * 64
        for bi, (t0, n) in enumerate(self.TOKBLK):
            kts = [(0, None), (1, None)]
            if bi > 0:
                sblk = (t0 - LC) // 128
                for ri, r in enumerate(range(-1, 5)):
                    if 0 <= sblk + r < 16:
                        kts.append((2 + sblk + r, ri))
            po, po_k = self.PS[4]
            pz, pz_k = self.PS[6]
            for i, (kt, ri) in enumerate(kts):
                pss, pss_k = self.PS[i % 2]
                et, et_k = ET[eti % 4]
                eti += 1
                self.op("pe", lambda e, pss=pss, ak=ak, aq=aq, kt=kt, t0=t0, n=n, ri=ri, p0=p0: e.matmul(
                    pss[:, 0:n], lhsT=ak[p0:p0 + 64, kt * 128:(kt + 1) * 128], rhs=aq[p0:p0 + 64, t0:t0 + n],
                    start=True, stop=True), r=[ak_k, aq_k], w=[pss_k])
                self.op("act", lambda e, pss=pss, et=et, n=n: e.activation(out=et[:, 0:n], in_=pss[:, 0:n], func=AF.Exp, scale=scale),
                        r=[pss_k], w=[et_k])
                if ri is not None:
                    self.op("pool", lambda e, et=et, n=n, ri=ri: e.tensor_tensor(out=et[:, 0:n], in0=et[:, 0:n], in1=mk[:, ri, 0:n], op=ALU.mult),
                            r=[et_k, mk_k], w=[et_k])
                last = (i == len(kts) - 1)
                self.op("pe", lambda e, et=et, kt=kt, n=n, i=i, last=last, vp=vp: e.matmul(
                    po[:, 0:n], lhsT=vp[:, kt, :], rhs=et[:, 0:n], start=(i == 0), stop=last), r=[vp_k, et_k], w=[po_k])
                self.op("pe", lambda e, et=et, n=n, i=i, last=last: e.matmul(
                    pz[:, 0:n], lhsT=self.ones_b[:], rhs=et[:, 0:n], start=(i == 0), stop=last), r=[self.ones_b_k, et_k], w=[pz_k])
            self.op("dve", lambda e, n=n, h=h: e.tensor_scalar(out=r0[:, 0:n], in0=pz[:, 0:n], scalar1=sx[:, h:h + 1], scalar2=None, op0=ALU.add),
                    r=[pz_k, sx_k], w=[r0_k])
            self.op("dve", lambda e, n=n: e.reciprocal(out=r0[:, 0:n], in_=r0[:, 0:n]), r=[r0_k], w=[r0_k])
            wk = [self.BIGA_k[t] for t in range(t0 // 128, (t0 + n) // 128)]
            self.op("dve", lambda e, n=n, t0=t0, h=h, p0=p0: e.tensor_tensor(out=self.BIGA[p0:p0 + 64, h // 2, t0:t0 + n], in0=po[p0:p0 + 64, 0:n],
                                                                     in1=r0[p0:p0 + 64, 0:n], op=ALU.mult), r=[po_k, r0_k], w=wk)


Builder.mixer_win = _mixer_win


def _dn_proj(self):
    c = self.c
    w = self.dn_w_in[0]
    c.arena_reset()
    P = [c.asb([128, NT], F32, "dnP%d" % i) for i in range(2)]
    Cb, Cb_k = c.asb([128, NT], F32, "dnC")
    OB = [c.asb([128, NT], BF16, "dnOB%d" % i) for i in range(2)]
    rs, rs_k = self.XT[1][0][:, 0:512], self.XT[1][1]
    TS = [c.asb([128, 4, 128], BF16, "dnTS%d" % i) for i in range(2)]
    cw, cw_k = c.asb([128, 320], F32, "dncw")
    cwr, cwr_k = self.XT[0][0][:, 0:384].rearrange("p (a b) -> p a b", a=3), self.XT[0][1]
    cw2d = self.dn_conv_w[0].rearrange("k (j p) -> (k j) p", p=128)
    for i, (r0_, nr) in enumerate(((0, 128), (128, 128), (256, 64))):
        self.dma("sp", cwr[0:nr, i, :], cw2d[r0_:r0_ + nr, :], w=[cwr_k])
    pt, pt_k = self.PS[4]
    for i, (r0_, nr) in enumerate(((0, 128), (128, 128), (256, 64))):
        self.op("pe", lambda e, i=i, nr=nr, r0_=r0_: e.transpose(out=pt[:, r0_:r0_ + nr], in_=cwr[0:nr, i, :], identity=self.ident_f[0:nr, 0:nr]),
                r=[cwr_k, self.ident_f_k], w=[pt_k])
    self.op("dve", lambda e: e.tensor_copy(out=cw[:], in_=pt[:, 0:320]), r=[pt_k], w=[cw_k])
    st = {"ob": 0, "ts": 0, "eng": 0}
    SEGS = ((0, LC), (LC, NT))

    def finish_chunk(j, p, p_k):
        def wcol(k):
            return cw[:, k * 64 + j:k * 64 + j + 1]
        self.op("dve", lambda e: e.tensor_scalar(out=Cb[:], in0=p[:], scalar1=wcol(2), scalar2=None, op0=ALU.mult),
                r=[p_k, cw_k], w=[Cb_k])
        for k in (0, 1, 3, 4):
            sft = k - 2
            for (a, b) in SEGS:
                lo = max(a, a - sft)
                hi = min(b, b - sft)
                self.op("dve", lambda e, k=k, lo=lo, hi=hi, sft=sft: e.scalar_tensor_tensor(
                    out=Cb[:, lo:hi], in0=p[:, lo + sft:hi + sft], scalar=wcol(k), in1=Cb[:, lo:hi], op0=ALU.mult, op1=ALU.add),
                    r=[p_k, cw_k, Cb_k], w=[Cb_k])
        self.op("act", lambda e: e.activation(out=Cb[:], in_=Cb[:], func=AF.Silu), r=[Cb_k], w=[Cb_k])
        ob, ob_k = OB[st["ob"] % 2]
        st["ob"] += 1
        if j < 32:
            self.op("pool", lambda e: e.tensor_tensor(out=ob[:], in0=Cb[:], in1=Cb[:], op=ALU.mult), r=[Cb_k], w=[ob_k])
            qs = (128 ** -0.5) if j < 16 else 1.0
            for (t0, n) in self.TOKBLK:
                pss, pss_k = self.PS[5 + (st["eng"] % 2)]
                st["eng"] += 1
                self.op("pe", lambda e, pss=pss, t0=t0, n=n: e.matmul(pss[:, 0:n], lhsT=self.ones_b[:], rhs=ob[:, t0:t0 + n], start=True, stop=True),
                        r=[ob_k, self.ones_b_k], w=[pss_k])
                self.op("act", lambda e, pss=pss, n=n: e.activation(out=rs[:, 0:n], in_=pss[:, 0:n], func=AF.Sqrt, bias=EPS), r=[pss_k], w=[rs_k])
                self.op("dve", lambda e, n=n: e.reciprocal(out=rs[:, 0:n], in_=rs[:, 0:n]), r=[rs_k], w=[rs_k])
                self.op("dve", lambda e, t0=t0, n=n: e.scalar_tensor_tensor(out=Cb[:, t0:t0 + n], in0=Cb[:, t0:t0 + n], scalar=qs, in1=rs[:, 0:n],
                                                                           op0=ALU.mult, op1=ALU.mult), r=[Cb_k, rs_k], w=[Cb_k])
        ob2, ob2_k = OB[st["ob"] % 2]
        st["ob"] += 1
        self.op("act", lambda e: e.copy(out=ob2[:], in_=Cb[:]), r=[Cb_k], w=[ob2_k])
        if j < 16:
            self.dma("sp", self.QT[j], ob2[:], r=[ob2_k], w=[self.QT_k])
            return
        if j < 32:
            self.dma("sp", self.KT[j - 16], ob2[:], r=[ob2_k], w=[self.KT_k])
            dst, dst_k, col = self.DNK, self.DNK_k, (j - 16) * 128
        else:
            dst, dst_k, col = self.DNV, self.DNV_k, (j - 32) * 128
        ptb_t, ptb_k = self.PS[7]
        ptb = ptb_t[:].bitcast(BF16)
        for g0 in range(0, NTILE, 4):
            ng = min(4, NTILE - g0)
            ts, ts_k = TS[st["ts"] % 2]
            st["ts"] += 1
            for x in range(ng):
                t = g0 + x
                self.op("pe", lambda e, x=x, t=t: e.transpose(out=ptb[:, x * 128:(x + 1) * 128], in_=ob2[:, t * 128:(t + 1) * 128],
                                                              identity=self.ident_b[:]), r=[ob2_k, self.ident_b_k], w=[ptb_k])
            self.op("dve", lambda e, ts=ts, ng=ng: e.tensor_copy(out=ts[:, 0:ng, :], in_=ptb[:, 0:ng * 128].rearrange("p (a b) -> p a b", a=ng)),
                    r=[ptb_k], w=[ts_k])
            self.dma("sp", dst[g0 * 128:(g0 + ng) * 128, col:col + 128].rearrange("(t p) c -> p t c", p=128), ts[:, 0:ng, :],
                     r=[ts_k], w=[dst_k])

    def post(j, bi, t0, n, ps, ps_k):
        p, p_k = P[j % 2]
        self.op("act", lambda e: e.copy(out=p[:, t0:t0 + n], in_=ps[:, 0:n]), r=[ps_k], w=[p_k])
        if bi == len(self.TOKBLK) - 1:
            finish_chunk(j, p, p_k)

    self.linear_fm(w, 0, 8192, post)
    ZS = [c.asb([128, 512], BF16, "dnZS%d" % i) for i in range(2)]
    zi = {"i": 0}

    def zpost(cb, nb, t, ps, ps_k):
        zs, zs_k = ZS[zi["i"] % 2]
        zi["i"] += 1
        self.op("act", lambda e: e.activation(out=zs[:, 0:nb], in_=ps[:, 0:nb], func=AF.Silu), r=[ps_k], w=[zs_k])
        self.dma("sp", self.DNZ[t * 128:(t + 1) * 128, cb:cb + nb], zs[:, 0:nb], r=[zs_k], w=[self.DNZ_k])

    self.linear_tm(w, 8192, 4096, zpost)


def _mixer_dn(self):
    import os as _os
    self.dn_proj()
    if _os.environ.get("MK_DN_STAGE") == "A":
        return
    self.dn_scan()
    if _os.environ.get("MK_DN_STAGE") == "B":
        return
    self.dn_headout()


Builder.dn_proj = _dn_proj
Builder.mixer_dn = _mixer_dn


def _dn_scan(self, dirs=(0, 1)):
    c = self.c
    w = self.dn_w_in[0]
    c.arena_reset()
    E = self.op
    dm, dm_k = self.XT[0][0][:, 0:1024].rearrange("p (a b) -> p a b", a=8), self.XT[0][1]
    self.dma("sp", dm, self.kc["k_dnmask"][:, :].rearrange("p (a b) -> p a b", a=8), w=[dm_k])
    TRI = [dm[:, 0, :], dm[:, 1, :]]
    SGT = [dm[:, 2, :], dm[:, 3, :]]
    BLK = dm[:, 4, :]
    SELC = [dm[:, 5, :], dm[:, 6, :]]
    misc, misc_k = self.misc
    pc = misc[:, 8:12]
    par = misc[:, 12:16]
    self.dma("sp", pc, self.kc["k_dncol"][:, :], w=[misc_k])
    E("pool", lambda e: e.memset(par, 0.0), r=[misc_k], w=[misc_k])
    for d in range(2):
        self.dma("sp", par[d * 64 + 32:d * 64 + 64, 0:1], self.dn_a_log[0, d:d + 1, :].rearrange("o h -> h o"), r=[misc_k], w=[misc_k])
        self.dma("sp", par[d * 64 + 32:d * 64 + 64, 1:2], self.dn_dt_bias[0, d:d + 1, :].rearrange("o h -> h o"), r=[misc_k], w=[misc_k])
    E("act", lambda e: e.activation(out=par[:, 2:3], in_=par[:, 0:1], func=AF.Exp), r=[misc_k], w=[misc_k])
    E("dve", lambda e: e.scalar_tensor_tensor(out=par[:, 2:3], in0=par[:, 2:3], scalar=-1.0, in1=pc[:, 1:2], op0=ALU.mult, op1=ALU.mult),
      r=[misc_k], w=[misc_k])
    bgs, bgs_k = c.asb([128, 512], F32, "bgs")
    bg2, bg2_k = self.XT[1][0][:, 0:512], self.XT[1][1]
    BGT, BGT_k = c.asb([128, NTILE, 128], F32, "BGT")
    GCA, GCA_k = c.asb([128, NTILE, 64], F32, "GCA")
    GLT, GLT_k = c.asb([128, NTILE, 64], F32, "GLT")
    RR, RR_k = c.asb([128, NTILE, 64], F32, "RR")
    BAx, BAx_k = c.asb([128, NTILE, 64], F32, "BAx")
    GTB, GTB_k = c.asb([128, 2 * NTILE, 64], F32, "GTB")

    def bapost(j, bi, t0, n, ps, ps_k):
        E("act", lambda e: e.activation(out=bgs[:, 0:n], in_=ps[:, 0:n], func=AF.Sigmoid), r=[ps_k], w=[bgs_k])
        E("dve", lambda e: e.tensor_scalar(out=bgs[:, 0:n], in0=bgs[:, 0:n], scalar1=pc[:, 0:1], scalar2=None, op0=ALU.mult),
          r=[bgs_k, misc_k], w=[bgs_k])
        E("act", lambda e: e.activation(out=bg2[:, 0:n], in_=ps[:, 0:n], func=AF.Exp, bias=par[:, 1:2]), r=[ps_k, misc_k], w=[bg2_k])
        E("act", lambda e: e.activation(out=bg2[:, 0:n], in_=bg2[:, 0:n], func=AF.Ln, bias=1.0), r=[bg2_k], w=[bg2_k])
        E("dve", lambda e: e.scalar_tensor_tensor(out=bgs[:, 0:n], in0=bg2[:, 0:n], scalar=par[:, 2:3], in1=bgs[:, 0:n], op0=ALU.mult, op1=ALU.add),
          r=[bg2_k, bgs_k, misc_k], w=[bgs_k])
        for x in range(n // 128):
            t = t0 // 128 + x
            pt, pt_k = self.PS[4 + (t % 2)]
            E("pe", lambda e, x=x, pt=pt: e.transpose(out=pt[:, 0:128], in_=bgs[:, x * 128:(x + 1) * 128], identity=self.ident_f[:]),
              r=[bgs_k, self.ident_f_k], w=[pt_k])
            E("dve", lambda e, t=t, pt=pt: e.tensor_copy(out=BGT[:, t, :], in_=pt[:, 0:128]), r=[pt_k], w=[BGT_k])

    self.linear_fm(w, 12288, 128, bapost)
    for t in range(NTILE):
        pg, pg_k = self.PS[4 + (t % 2)]
        for d in range(2):
            E("pe", lambda e, t=t, d=d, pg=pg: e.matmul(pg[:, d * 32:(d + 1) * 32], lhsT=TRI[d], rhs=BGT[:, t, d * 64 + 32:d * 64 + 64],
                                                       start=True, stop=True), r=[dm_k, BGT_k], w=[pg_k])
            E("pe", lambda e, t=t, d=d, pg=pg: e.matmul(pg[:, 64 + d * 32:64 + (d + 1) * 32], lhsT=BLK, rhs=BGT[:, t, d * 64 + 32:d * 64 + 64],
                                                       start=True, stop=True), r=[dm_k, BGT_k], w=[pg_k])
        E("dve", lambda e, t=t, pg=pg: e.tensor_copy(out=GCA[:, t, :], in_=pg[:, 0:64]), r=[pg_k], w=[GCA_k])
        E("act", lambda e, t=t, pg=pg: e.copy(out=GLT[:, t, :], in_=pg[:, 64:128]), r=[pg_k], w=[GLT_k])
    E("dve", lambda e: e.tensor_tensor(out=RR[:], in0=GLT[:], in1=GCA[:], op=ALU.subtract), r=[GLT_k, GCA_k], w=[RR_k])
    E("act", lambda e: e.activation(out=RR[:], in_=RR[:], func=AF.Exp), r=[RR_k], w=[RR_k])
    E("act", lambda e: e.activation(out=GLT[:], in_=GLT[:], func=AF.Exp), r=[GLT_k, RR_k], w=[GLT_k])
    E("act", lambda e: e.activation(out=GCA[:], in_=GCA[:], func=AF.Exp), r=[GCA_k, RR_k], w=[GCA_k])
    for d in range(2):
        E("dve", lambda e, d=d: e.tensor_tensor(out=BAx[:, :, d * 32:(d + 1) * 32], in0=BGT[:, :, d * 64:d * 64 + 32],
                                               in1=GCA[:, :, d * 32:(d + 1) * 32], op=ALU.mult), r=[BGT_k, GCA_k], w=[BAx_k])
    for t in range(NTILE):
        pg, pg_k = self.PS[4 + (t % 2)]
        for hf in range(2):
            E("pe", lambda e, t=t, hf=hf, pg=pg: e.matmul(pg[:, hf * 64:(hf + 1) * 64], lhsT=SELC[hf], rhs=GLT[:, t, :], start=True, stop=True),
              r=[dm_k, GLT_k], w=[pg_k])
        E("dve", lambda e, t=t, pg=pg: e.tensor_copy(out=GTB[:, 2 * t:2 * t + 2, :], in_=pg[:, 0:128].rearrange("p (a b) -> p a b", a=2)),
          r=[pg_k], w=[GTB_k])
    self.p.barrier()

    NS = 2
    PSQ = [(self.PS[b][0][:, 0:128], self.PS[b][1]) for b in range(8)]
    psq_i = {"i": 0}

    def psq():
        r = PSQ[psq_i["i"] % 8]
        psq_i["i"] += 1
        return r

    class Stream:
        pass

    streams = []
    for s_ in range(NS):
        st = Stream()
        wtf = self.WT[s_][0][:].bitcast(F32)
        st.mats = [(wtf[:, a, b * 128:(b + 1) * 128], Tk("m%d_%d_%d" % (s_, a, b))) for a in range(16) for b in range(2)]
        st.mi = 0
        hb = self.HB[s_][0]
        st.ld = [[(hb[:, (q * 4 + x) * 128:(q * 4 + x + 1) * 128], Tk("ld%d_%d_%d" % (s_, q, x))) for x in range(4)] for q in range(4)]
        st.ldi = 0
        streams.append(st)
    ident = self.ident_f

    def unit(st, d, h, t, first):
        hk = h // 2
        M = {}
        names = ["GTRI", "DEC", "DECT", "L", "U", "La", "Ua", "Lb", "Ub", "P", "vb", "kbg", "kd", "u", "wT", "itT", "qTf", "vn", "o", "S"]
        for i, nm in enumerate(names):
            M[nm] = st.mats[i]
        kTb, qTb, ktok, vtok = st.ld[st.ldi % 4]
        st.ldi += 1
        r0_ = t * 128
        self.dma("sp", kTb[0], self.KT[hk][:, r0_:r0_ + 128], r=[self.KT_k], w=[kTb[1]])
        self.dma("sp", qTb[0], self.QT[hk][:, r0_:r0_ + 128], r=[self.QT_k], w=[qTb[1]])
        self.dma("sp", ktok[0], self.DNK[r0_:r0_ + 128, hk * 128:(hk + 1) * 128], r=[self.DNK_k], w=[ktok[1]])
        self.dma("sp", vtok[0], self.DNV[r0_:r0_ + 128, h * 128:(h + 1) * 128], r=[self.DNV_k], w=[vtok[1]])
        gcol = BGT[:, t, d * 64 + 32 + h:d * 64 + 33 + h]
        bcol = BGT[:, t, d * 64 + h:d * 64 + h + 1]
        acol = GCA[:, t, d * 32 + h:d * 32 + h + 1]
        bacol = BAx[:, t, d * 32 + h:d * 32 + h + 1]
        rcol = RR[:, t, d * 32 + h:d * 32 + h + 1]
        (GTRI, GTRI_k), (DEC, DEC_k), (DECT, DECT_k) = M["GTRI"], M["DEC"], M["DECT"]
        (Lm, L_k), (U, U_k), (P, P_k) = M["L"], M["U"], M["P"]
        E("dve", lambda e: e.tensor_scalar(out=GTRI, in0=TRI[d], scalar1=gcol, scalar2=None, op0=ALU.mult), r=[dm_k, BGT_k], w=[GTRI_k])
        pD, pD_k = psq()
        pDT, pDT_k = psq()
        E("pe", lambda e: e.matmul(pD, lhsT=GTRI, rhs=SGT[d], start=True, stop=True), r=[GTRI_k, dm_k], w=[pD_k])
        E("pe", lambda e: e.matmul(pDT, lhsT=SGT[d], rhs=GTRI, start=True, stop=True), r=[GTRI_k, dm_k], w=[pDT_k])
        E("act", lambda e: e.activation(out=DEC, in_=pD, func=AF.Exp), r=[pD_k], w=[DEC_k])
        E("act", lambda e: e.activation(out=DECT, in_=pDT, func=AF.Exp), r=[pDT_k], w=[DECT_k])
        pKK, pKK_k = psq()
        pQK, pQK_k = psq()
        E("pe", lambda e: e.matmul(pKK, lhsT=kTb[0], rhs=kTb[0], start=True, stop=True), r=[kTb[1]], w=[pKK_k])
        E("pe", lambda e: e.matmul(pQK, lhsT=kTb[0], rhs=qTb[0], start=True, stop=True), r=[kTb[1], qTb[1]], w=[pQK_k])
        E("dve", lambda e: e.scalar_tensor_tensor(out=Lm, in0=pKK, scalar=bcol, in1=DEC, op0=ALU.mult, op1=ALU.mult),
          r=[pKK_k, BGT_k, DEC_k], w=[L_k])
        E("pool", lambda e: e.tensor_tensor(out=Lm, in0=Lm, in1=SGT[d], op=ALU.mult), r=[L_k, dm_k], w=[L_k])
        itT, itT_k = M["itT"]
        E("dve", lambda e: e.tensor_tensor(out=itT, in0=pQK, in1=DECT, op=ALU.mult), r=[pQK_k, DECT_k], w=[itT_k])
        E("pool", lambda e: e.tensor_tensor(out=itT, in0=itT, in1=TRI[d], op=ALU.mult), r=[itT_k, dm_k], w=[itT_k])
        pU, pU_k = psq()
        E("pe", lambda e: e.transpose(out=pU, in_=Lm, identity=ident[:]), r=[L_k, self.ident_f_k], w=[pU_k])
        E("act", lambda e: e.copy(out=U, in_=pU), r=[pU_k], w=[U_k])
        E("dve", lambda e: e.scalar_tensor_tensor(out=P, in0=U, scalar=-1.0, in1=ident[:], op0=ALU.mult, op1=ALU.add),
          r=[U_k, self.ident_f_k], w=[P_k])
        curL, curU = M["L"], M["U"]
        pp = [(M["La"], M["Ua"]), (M["Lb"], M["Ub"])]
        for k in range(1, 6):
            nL, nU = pp[k % 2]
            pl, pl_k = psq()
            E("pe", lambda e, pl=pl, curL=curL, curU=curU: e.matmul(pl, lhsT=curU[0], rhs=curL[0], start=True, stop=True),
              r=[curL[1], curU[1]], w=[pl_k])
            E("act", lambda e, pl=pl, nL=nL: e.copy(out=nL[0], in_=pl), r=[pl_k], w=[nL[1]])
            if k < 5:
                pu_, pu_k = psq()
                E("pe", lambda e, pu_=pu_, curL=curL, curU=curU: e.matmul(pu_, lhsT=curL[0], rhs=curU[0], start=True, stop=True),
                  r=[curL[1], curU[1]], w=[pu_k])
                E("dve", lambda e, pu_=pu_, nU=nU: e.tensor_copy(out=nU[0], in_=pu_), r=[pu_k], w=[nU[1]])
            ppn, ppn_k = psq()
            E("pe", lambda e, ppn=ppn, nL=nL: e.matmul(ppn, lhsT=nL[0], rhs=P, start=True, stop=True), r=[nL[1], P_k], w=[ppn_k])
            E("dve", lambda e, ppn=ppn: e.tensor_tensor(out=P, in0=ppn, in1=P, op=ALU.add), r=[ppn_k, P_k], w=[P_k])
            curL, curU = nL, nU
        (vb, vb_k), (kbg, kbg_k), (kd, kd_k) = M["vb"], M["kbg"], M["kd"]
        E("pool", lambda e: e.tensor_scalar(out=vb, in0=vtok[0], scalar1=bcol, scalar2=None, op0=ALU.mult), r=[vtok[1], BGT_k], w=[vb_k])
        E("pool", lambda e: e.tensor_scalar(out=kbg, in0=ktok[0], scalar1=bacol, scalar2=None, op0=ALU.mult), r=[ktok[1], BAx_k], w=[kbg_k])
        E("pool", lambda e: e.tensor_scalar(out=kd, in0=ktok[0], scalar1=rcol, scalar2=None, op0=ALU.mult), r=[ktok[1], RR_k], w=[kd_k])
        (u, u_k), (wT, wT_k), (qTf, qTf_k) = M["u"], M["wT"], M["qTf"]
        pu2, pu2_k = psq()
        pw, pw_k = psq()
        E("pe", lambda e: e.matmul(pu2, lhsT=P, rhs=vb, start=True, stop=True), r=[P_k, vb_k], w=[pu2_k])
        E("pe", lambda e: e.matmul(pw, lhsT=kbg, rhs=P, start=True, stop=True), r=[P_k, kbg_k], w=[pw_k])
        E("act", lambda e: e.copy(out=u, in_=pu2), r=[pu2_k], w=[u_k])
        E("dve", lambda e: e.tensor_copy(out=wT, in_=pw), r=[pw_k], w=[wT_k])
        E("act", lambda e: e.copy(out=qTf, in_=qTb[0]), r=[qTb[1]], w=[qTf_k])
        (vn, vn_k), (o, o_k), (S, S_k) = M["vn"], M["o"], M["S"]
        if first:
            E("pool", lambda e: e.memset(S, 0.0), w=[S_k])
        for hf in ((0, 1) if d == 0 else (1, 0)):
            c0 = hf * 64
            gtcol = GTB[:, 2 * t + hf, d * 32 + h:d * 32 + h + 1]
            p1, p1_k = psq()
            p2a, p2a_k = psq()
            p2b, p2b_k = psq()
            p3, p3_k = psq()
            E("pe", lambda e, c0=c0, p1=p1: e.matmul(p1[c0:c0 + 64, :], lhsT=wT[:, c0:c0 + 64], rhs=S, start=True, stop=True),
              r=[wT_k, S_k], w=[p1_k])
            E("dve", lambda e, c0=c0, p1=p1: e.tensor_tensor(out=vn[c0:c0 + 64, :], in0=u[c0:c0 + 64, :], in1=p1[c0:c0 + 64, :], op=ALU.subtract),
              r=[u_k, p1_k], w=[vn_k])
            E("pe", lambda e, c0=c0, p2a=p2a: e.matmul(p2a[c0:c0 + 64, :], lhsT=qTf[:, c0:c0 + 64], rhs=S, start=True, stop=True),
              r=[qTf_k, S_k], w=[p2a_k])
            E("pe", lambda e, c0=c0, p2b=p2b: e.matmul(p2b[c0:c0 + 64, :], lhsT=itT[c0:c0 + 64, c0:c0 + 64], rhs=vn[c0:c0 + 64, :],
                                                      start=True, stop=True), r=[itT_k, vn_k], w=[p2b_k])
            E("act", lambda e, c0=c0, p2a=p2a: e.activation(out=o[c0:c0 + 64, :], in_=p2a[c0:c0 + 64, :], func=AF.Copy, scale=acol[c0:c0 + 64, :]),
              r=[p2a_k, GCA_k], w=[o_k])
            E("dve", lambda e, c0=c0, p2b=p2b: e.tensor_tensor(out=o[c0:c0 + 64, :], in0=o[c0:c0 + 64, :], in1=p2b[c0:c0 + 64, :], op=ALU.add),
              r=[o_k, p2b_k], w=[o_k])
            E("pe", lambda e, c0=c0, p3=p3: e.matmul(p3, lhsT=kd[c0:c0 + 64, :], rhs=vn[c0:c0 + 64, :], start=True, stop=True),
              r=[kd_k, vn_k], w=[p3_k])
            E("dve", lambda e, p3=p3, gtcol=gtcol: e.scalar_tensor_tensor(out=S, in0=S, scalar=gtcol, in1=p3, op0=ALU.mult, op1=ALU.add),
              r=[S_k, GTB_k, p3_k], w=[S_k])
        self.dma("sp", self.ODN[d, r0_:r0_ + 128, h * 128:(h + 1) * 128], o, r=[o_k], w=[self.ODN_k])

    order = {0: list(range(NTILE)), 1: [1, 0] + list(range(NTILE - 1, 1, -1))}
    import os as _os
    nh_ = int(_os.environ.get('MK_DN_NH', '32'))
    todo = [(d, h) for d in dirs for h in range(nh_)]
    for g0 in range(0, len(todo), NS):
        grp = todo[g0:g0 + NS]
        for step in range(NTILE):
            for si, (d, h) in enumerate(grp):
                unit(streams[si], d, h, order[d][step], step == 0)
    self.p.barrier()


Builder.dn_scan = _dn_scan


def _dn_headout(self):
    c = self.c
    E = self.op
    for half in range(2):
        c.arena_reset()
        nwt, nwt_k = c.asb([128, 128], F32, "nwt")
        ssb, ssb_k = c.asb([128, 32], F32, "ssb")
        self.dma("sp", nwt, self.dn_norm_w[0:1, :].partition_broadcast(128), w=[nwt_k])
        ps_t, ps_tk = self.PS[1]
        pst = ps_t[:].bitcast(BF16)
        c0 = half * 2048
        for t in range(NTILE):
            oa, oa_k = self.XT[0]
            ob_, ob_k = self.XT[1]
            zt, zt_k = self.HB[t % 2]
            hb, hb_k = self.SB[t % 2]
            hbb = hb[:].bitcast(BF16)[:, 0:2048]
            r0_ = t * 128
            self.dma("sp", oa[:], self.ODN[0, r0_:r0_ + 128, c0:c0 + 2048], r=[self.ODN_k], w=[oa_k])
            self.dma("sp", ob_[:], self.ODN[1, r0_:r0_ + 128, c0:c0 + 2048], r=[self.ODN_k], w=[ob_k])
            self.dma("sp", zt[:], self.DNZ[r0_:r0_ + 128, c0:c0 + 2048], r=[self.DNZ_k], w=[zt_k])
            E("dve", lambda e, oa=oa, ob_=ob_: e.tensor_tensor(out=oa[:], in0=oa[:], in1=ob_[:], op=ALU.add), r=[oa_k, ob_k], w=[oa_k])
            E("act", lambda e, oa=oa, ob_=ob_: e.activation(out=ob_[:], in_=oa[:], func=AF.Square), r=[oa_k], w=[ob_k])
            E("dve", lambda e, ob_=ob_: e.reduce_sum(out=ssb[:, 0:16], in_=ob_[:].rearrange("p (a b) -> p a b", a=16), axis=AX.X),
              r=[ob_k], w=[ssb_k])
            E("act", lambda e: e.activation(out=ssb[:, 16:32], in_=ssb[:, 0:16], func=AF.Sqrt, scale=1.0 / 128, bias=EPS), r=[ssb_k], w=[ssb_k])
            E("dve", lambda e: e.reciprocal(out=ssb[:, 16:32], in_=ssb[:, 16:32]), r=[ssb_k], w=[ssb_k])
            for hh in range(16):
                eng = "dve" if hh % 2 == 0 else "pool"
                if eng == "dve":
                    E("dve", lambda e, hh=hh, oa=oa, hbb=hbb: e.scalar_tensor_tensor(
                        out=hbb[:, hh * 128:(hh + 1) * 128], in0=oa[:, hh * 128:(hh + 1) * 128], scalar=ssb[:, 16 + hh:17 + hh], in1=nwt,
                        op0=ALU.mult, op1=ALU.mult), r=[oa_k, ssb_k, nwt_k], w=[hb_k])
                else:
                    E("pool", lambda e, hh=hh, oa=oa: e.tensor_scalar(out=oa[:, hh * 128:(hh + 1) * 128], in0=oa[:, hh * 128:(hh + 1) * 128],
                                                                       scalar1=ssb[:, 16 + hh:17 + hh], scalar2=None, op0=ALU.mult),
                      r=[oa_k, ssb_k], w=[oa_k])
                    E("pool", lambda e, hh=hh, oa=oa, hbb=hbb: e.tensor_tensor(out=hbb[:, hh * 128:(hh + 1) * 128], in0=oa[:, hh * 128:(hh + 1) * 128],
                                                                              in1=nwt, op=ALU.mult), r=[oa_k, nwt_k], w=[hb_k])
            E("dve", lambda e, hbb=hbb, zt=zt: e.tensor_tensor(out=hbb, in0=hbb, in1=zt[:], op=ALU.mult), r=[hb_k, zt_k], w=[hb_k])
            for g in range(2):
                for j in range(8):
                    kc = g * 8 + j
                    E("pe", lambda e, hbb=hbb, kc=kc, j=j: e.transpose(out=pst[:, j * 128:(j + 1) * 128], in_=hbb[:, kc * 128:(kc + 1) * 128],
                                                                      identity=self.ident_b[:]), r=[hb_k, self.ident_b_k], w=[ps_tk])
                E("act", lambda e, g=g, t=t: e.copy(out=self.BIGA[:, g * 8:(g + 1) * 8, t * 128:(t + 1) * 128],
                                                   in_=pst.rearrange("p (a b) -> p a b", a=8)), r=[ps_tk], w=[self.BIGA_k[t]])
        self.phase_oproj_residual(self.dn_w_o[0][half * 2048:(half + 1) * 2048, :], self.cur_li)


Builder.dn_headout = _dn_headout


NBLK = 2 * NT // 128 + 32


def _phase_moe_b(self, l):
    c = self.c
    E = self.op
    c.arena_reset()
    if not hasattr(self, "HROW"):
        self.HROW, _ = c.dram("HROW", [NT, D], BF16)
        self.HROW_k = [Tk("hrow%d" % t) for t in range(NTILE)]
        self.XS, self.XS_k = c.dram("XS", [NBLK * 128, D], BF16)
        self.YS, self.YS_k = c.dram("YS", [NBLK * 128, D], F32)
    ht32, ht32_k = self.WT[0][0][:].bitcast(F32)[:, :, 0:128], self.WT[0][1]
    wr, wr_k = c.asb([128, KC, 36], F32, "wr")
    br, br_k = c.asb([128, 36], F32, "br")
    lg, lg_k = c.asb([128, 36], F32, "lg")
    rs_, rs_k = c.asb([128, 64], F32, "rsm")
    OH1, OH1_k = c.asb([128, NTILE, 32], F32, "OH1")
    OH2, OH2_k = c.asb([128, NTILE, 32], F32, "OH2")
    GAB, GAB_k = c.asb([128, NTILE, 2], F32, "GAB")
    self.dma("sp", wr[:, :, 0:4], self.moe_wg[l].rearrange("(kc p) n -> p kc n", p=128), w=[wr_k])
    self.dma("sp", wr[:, :, 4:36], self.moe_we[l].rearrange("(kc p) n -> p kc n", p=128), w=[wr_k])
    self.dma("sp", br[:, 0:4], self.moe_bg[l:l + 1, :].partition_broadcast(128), w=[br_k])
    self.dma("sp", br[:, 4:36], self.moe_be[l:l + 1, :].partition_broadcast(128), w=[br_k])
    R = lambda a, b=None: rs_[:, a:(a + 1 if b is None else b)]

    def router(t, xt, xt_k):
        for g in range(4):
            pt, pt_k = self.PS[2 + (g % 2)]
            for jx in range(4):
                kc = g * 4 + jx
                E("pe", lambda e, pt=pt, jx=jx, kc=kc, xt=xt: e.transpose(out=pt[:, jx * 128:(jx + 1) * 128],
                                                                        in_=xt[:, kc * 128:(kc + 1) * 128], identity=self.ident_f[:]),
                  r=[xt_k, self.ident_f_k], w=[pt_k])
            E("dve", lambda e, pt=pt, g=g: e.tensor_copy(out=ht32[:, g * 4:(g + 1) * 4, :], in_=pt[:].rearrange("p (a b) -> p a b", a=4)),
              r=[pt_k], w=[ht32_k])
        pl, pl_k = self.PS[4]
        for kc in range(KC):
            E("pe", lambda e, kc=kc: e.matmul(pl[:, 0:36], lhsT=ht32[:, kc, :], rhs=wr[:, kc, :], start=(kc == 0), stop=(kc == KC - 1)),
              r=[ht32_k, wr_k], w=[pl_k])
        V = lambda fn, r=(), w=(): E("dve", fn, r=list(r) + [rs_k], w=list(w) + [rs_k])
        E("dve", lambda e: e.tensor_tensor(out=lg[:], in0=pl[:, 0:36], in1=br[:], op=ALU.add), r=[pl_k, br_k], w=[lg_k])
        V(lambda e: e.reduce_max(out=R(0), in_=lg[:, 0:4], axis=AX.X), r=[lg_k])
        V(lambda e: e.tensor_scalar(out=R(1), in0=R(0), scalar1=-1.0, scalar2=None, op0=ALU.mult))
        E("act", lambda e: e.activation(out=R(56, 60), in_=lg[:, 0:4], func=AF.Exp, bias=R(1), accum_out=R(2)), r=[lg_k, rs_k], w=[rs_k])
        V(lambda e: e.reciprocal(out=R(3), in_=R(2)))
        V(lambda e: e.tensor_scalar(out=R(4, 8), in0=lg[:, 0:4], scalar1=R(0), scalar2=None, op0=ALU.is_equal), r=[lg_k])
        V(lambda e: e.tensor_scalar(out=R(8, 16), in0=lg[:, 4:12], scalar1=R(4), scalar2=None, op0=ALU.mult), r=[lg_k])
        for g in range(1, 4):
            V(lambda e, g=g: e.scalar_tensor_tensor(out=R(8, 16), in0=lg[:, 4 + 8 * g:12 + 8 * g], scalar=R(4 + g), in1=R(8, 16),
                                                   op0=ALU.mult, op1=ALU.add), r=[lg_k])
        V(lambda e: e.reduce_max(out=R(40), in_=R(8, 16), axis=AX.X))
        V(lambda e: e.tensor_scalar(out=R(16, 24), in0=R(8, 16), scalar1=R(40), scalar2=None, op0=ALU.is_equal))
        V(lambda e: e.scalar_tensor_tensor(out=R(24, 32), in0=R(16, 24), scalar=-1e30, in1=R(8, 16), op0=ALU.mult, op1=ALU.add))
        V(lambda e: e.reduce_max(out=R(41), in_=R(24, 32), axis=AX.X))
        V(lambda e: e.tensor_scalar(out=R(32, 40), in0=R(24, 32), scalar1=R(41), scalar2=None, op0=ALU.is_equal))
        V(lambda e: e.tensor_tensor(out=R(42), in0=R(41), in1=R(40), op=ALU.subtract))
        E("act", lambda e: e.activation(out=R(43), in_=R(42), func=AF.Exp), r=[rs_k], w=[rs_k])
        V(lambda e: e.tensor_scalar(out=R(44), in0=R(43), scalar1=1.0, scalar2=None, op0=ALU.add))
        V(lambda e: e.reciprocal(out=R(44), in_=R(44)))
        E("dve", lambda e, t=t: e.tensor_tensor(out=GAB[:, t, 0:1], in0=R(44), in1=R(3), op=ALU.mult), r=[rs_k], w=[GAB_k])
        E("dve", lambda e, t=t: e.tensor_tensor(out=GAB[:, t, 1:2], in0=GAB[:, t, 0:1], in1=R(43), op=ALU.mult), r=[rs_k, GAB_k], w=[GAB_k])
        for g in range(4):
            E("dve", lambda e, g=g, t=t: e.tensor_scalar(out=OH1[:, t, g * 8:(g + 1) * 8], in0=R(16, 24), scalar1=R(4 + g),
                                                        scalar2=None, op0=ALU.mult), r=[rs_k], w=[OH1_k])
            E("pool", lambda e, g=g, t=t: e.tensor_scalar(out=OH2[:, t, g * 8:(g + 1) * 8], in0=R(32, 40), scalar1=R(4 + g),
                                                         scalar2=None, op0=ALU.mult), r=[rs_k], w=[OH2_k])

    self.phase_norm(hrow_dram=self.HROW, hrow_k=self.HROW_k, router=router, skip_T=True)
    self.load_gate_tiles(l, 1)
    mbc, mbc_k = c.asb([128, 224], F32, "mbc")
    self.dma("sp", mbc, self.kc["k_moeb"][:, :], w=[mbc_k])
    SLT, BROW, OFF13, OFF2 = mbc[:, 0:128], mbc[:, 128:196], mbc[:, 196:212], mbc[:, 212:218]
    MM, MM_k = c.asb([128, NTILE, 32], F32, "MM")
    RANK, RANK_k = c.asb([128, NTILE, 32], F32, "RANK")
    sm, sm_k = c.asb([128, 5, 32], F32, "moesm")
    BEa, BEa_k = c.asb([128, NBLK], F32, "BEa")
    IDXf, IDXf_k = c.asb([128, NBLK, 22], F32, "IDXf")
    IDXi, IDXi_k = c.asb([128, NBLK, 22], I32, "IDXi")
    DSTf, DSTf_k = c.asb([128, NTILE, 2], F32, "DSTf")
    DSTi, DSTi_k = c.asb([128, NTILE, 2], I32, "DSTi")
    tmp32, tmp32_k = c.asb([128, 32], F32, "tmp32")
    E("dve", lambda e: e.tensor_tensor(out=MM[:], in0=OH1[:], in1=OH2[:], op=ALU.add), r=[OH1_k, OH2_k], w=[MM_k])
    for t in range(NTILE):
        pg, pg_k = self.PS[2 + (t % 2)]
        E("pe", lambda e, t=t, pg=pg: e.matmul(pg[:, 0:32], lhsT=SLT, rhs=MM[:, t, :], start=True, stop=(t == 0)), r=[mbc_k, MM_k], w=[pg_k])
        for t2 in range(t):
            E("pe", lambda e, t=t, t2=t2, pg=pg: e.matmul(pg[:, 0:32], lhsT=self.ones_f[:], rhs=MM[:, t2, :], start=False, stop=(t2 == t - 1)),
              r=[self.ones_f_k, MM_k], w=[pg_k])
        E("dve", lambda e, t=t, pg=pg: e.tensor_copy(out=RANK[:, t, :], in_=pg[:, 0:32]), r=[pg_k], w=[RANK_k])
    pc_, pc_k = self.PS[4]
    for t in range(NTILE):
        E("pe", lambda e, t=t: e.matmul(pc_[:, 0:32], lhsT=self.ones_f[:], rhs=MM[:, t, :], start=(t == 0), stop=(t == NTILE - 1)),
          r=[self.ones_f_k, MM_k], w=[pc_k])
    CNT, PB, CA, CB, PST = sm[:, 0, :], sm[:, 1, :], sm[:, 2, :], sm[:, 3, :], sm[:, 4, :]
    S_ = lambda fn: E("dve", fn, r=[sm_k], w=[sm_k])
    E("dve", lambda e: e.tensor_copy(out=CNT, in_=pc_[:, 0:32]), r=[pc_k], w=[sm_k])
    S_(lambda e: e.tensor_scalar(out=PB, in0=CNT, scalar1=0.0, scalar2=None, op0=ALU.is_gt))
    for m in range(1, NTILE):
        S_(lambda e, m=m: e.scalar_tensor_tensor(out=PB, in0=CNT, scalar=float(128 * m), in1=PB, op0=ALU.is_gt, op1=ALU.add))
    S_(lambda e: e.tensor_copy(out=CA, in_=PB))
    cur, nxt = CA, CB
    for sft in (1, 2, 4, 8, 16):
        S_(lambda e, cur=cur, nxt=nxt, sft=sft: e.tensor_copy(out=nxt[:, 0:sft], in_=cur[:, 0:sft]))
        S_(lambda e, cur=cur, nxt=nxt, sft=sft: e.tensor_tensor(out=nxt[:, sft:32], in0=cur[:, sft:32], in1=cur[:, 0:32 - sft], op=ALU.add))
        cur, nxt = nxt, cur
    PEND = cur
    S_(lambda e: e.tensor_tensor(out=PST, in0=PEND, in1=PB, op=ALU.subtract))
    S_(lambda e: e.tensor_scalar(out=PST, in0=PST, scalar1=128.0, scalar2=None, op0=ALU.mult))
    E("pool", lambda e: e.memset(BEa, 0.0), w=[BEa_k])
    for ex in range(32):
        E("dve", lambda e, ex=ex: e.scalar_tensor_tensor(out=BEa, in0=BROW, scalar=PEND[:, ex:ex + 1], in1=BEa, op0=ALU.is_ge, op1=ALU.add),
          r=[mbc_k, sm_k, BEa_k], w=[BEa_k])
    E("dve", lambda e: e.tensor_scalar(out=BEa, in0=BEa, scalar1=31.0, scalar2=None, op0=ALU.min), r=[BEa_k], w=[BEa_k])
    for kc in range(16):
        E("dve", lambda e, kc=kc: e.tensor_scalar(out=IDXf[:, :, kc], in0=BEa, scalar1=2048.0, scalar2=OFF13[:, kc:kc + 1], op0=ALU.mult, op1=ALU.add),
          r=[BEa_k, mbc_k], w=[IDXf_k])
    for fc in range(6):
        E("dve", lambda e, fc=fc: e.tensor_scalar(out=IDXf[:, :, 16 + fc], in0=BEa, scalar1=768.0, scalar2=OFF2[:, fc:fc + 1], op0=ALU.mult, op1=ALU.add),
          r=[BEa_k, mbc_k], w=[IDXf_k])
    if l > 0:
        E("dve", lambda e: e.tensor_scalar(out=IDXf[:, :, 0:16], in0=IDXf[:, :, 0:16], scalar1=float(l * 32 * 2048), scalar2=None, op0=ALU.add),
          r=[IDXf_k], w=[IDXf_k])
        E("dve", lambda e: e.tensor_scalar(out=IDXf[:, :, 16:22], in0=IDXf[:, :, 16:22], scalar1=float(l * 32 * 768), scalar2=None, op0=ALU.add),
          r=[IDXf_k], w=[IDXf_k])
    SK, SK_k = c.asb([128, NBLK], F32, "SK")
    E("pool", lambda e: e.memset(SK[:, 0:1], 0.0), w=[SK_k])
    E("dve", lambda e: e.tensor_tensor(out=SK[:, 1:NBLK], in0=BEa[:, 1:NBLK], in1=BEa[:, 0:NBLK - 1], op=ALU.is_equal), r=[BEa_k, SK_k], w=[SK_k])
    E("dve", lambda e: e.scalar_tensor_tensor(out=SK[:, 1:NBLK], in0=BROW[:, 1:NBLK], scalar=PEND[:, 31:32], in1=SK[:, 1:NBLK], op0=ALU.is_ge, op1=ALU.max),
      r=[mbc_k, sm_k, SK_k], w=[SK_k])
    for cc in range(22):
        E("dve", lambda e, cc=cc: e.scalar_tensor_tensor(out=IDXf[:, :, cc], in0=SK, scalar=1.0e7, in1=IDXf[:, :, cc], op0=ALU.mult, op1=ALU.add),
          r=[SK_k, IDXf_k], w=[IDXf_k])
    E("dve", lambda e: e.tensor_copy(out=IDXi[:], in_=IDXf[:]), r=[IDXf_k], w=[IDXi_k])
    if not hasattr(self, "_bregs"):
        self._bregs = {}
    bregs = self._bregs

    def breg(e, key, val):
        if key not in bregs:
            bregs[key] = e.to_reg(val)
        return bregs[key]
    for t in range(NTILE):
        for k, (OH, OH_k) in enumerate(((OH1, OH1_k), (OH2, OH2_k))):
            E("dve", lambda e, t=t: e.tensor_tensor(out=tmp32, in0=RANK[:, t, :], in1=PST, op=ALU.add), r=[RANK_k, sm_k], w=[tmp32_k])
            E("dve", lambda e, t=t, OH=OH: e.tensor_tensor(out=tmp32, in0=tmp32, in1=OH[:, t, :], op=ALU.mult), r=[tmp32_k, OH_k], w=[tmp32_k])
            E("dve", lambda e, t=t, k=k: e.reduce_sum(out=DSTf[:, t, k:k + 1], in_=tmp32, axis=AX.X), r=[tmp32_k], w=[DSTf_k])
    E("dve", lambda e: e.tensor_copy(out=DSTi[:], in_=DSTf[:]), r=[DSTf_k], w=[DSTi_k])
    for t in range(NTILE):
        hb, hb_k = self.HB[t % 2]
        self.dma("sp", hb[:], self.HROW[t * 128:(t + 1) * 128, :], r=[self.HROW_k[t]], w=[hb_k])
        for k in range(2):
            self.p.op("pool", lambda e, hb=hb, t=t, k=k: e.indirect_dma_start(
                out=self.XS[:, :], out_offset=bass.IndirectOffsetOnAxis(ap=DSTi[:, t, k:k + 1], axis=0), in_=hb[:], in_offset=None),
                r=[hb_k, DSTi_k], w=[self.XS_k], dma=True)
    w13sb = self.BIGA[:].rearrange("p a b -> p (a b)")[:, 0:16 * 1536].rearrange("p (a b) -> p a b", a=16)
    w2sb = self.BIGA[:].rearrange("p a b -> p (a b)")[:, 16 * 1536:16 * 1536 + 6 * 2048].rearrange("p (a b) -> p a b", a=6)
    w13_k, w2_k = Tk("w13sb"), Tk("w2sb")
    biga_all = list(self.BIGA_k)
    w13rows = self.moe_w13.rearrange("l e r n -> (l e r) n")
    w2rows = self.moe_w2.rearrange("l e r n -> (l e r) n")
    XTb = [(self.WT[1][0][:, :, i * 128:(i + 1) * 128], Tk("xTb%d" % i)) for i in range(2)]
    GT_ = [c.asb([128, 768], BF16, "gact%d" % i) for i in range(2)]
    AT_ = [c.asb([128, 6, 128], BF16, "actT%d" % i) for i in range(2)]
    ps_t, ps_tk = self.PS[1]
    pst = ps_t[:].bitcast(BF16)
    first = True
    for b in range(NBLK):
        xr, xr_k = self.HB[b % 2]
        xT, xT_k = XTb[b % 2]
        ga, ga_k = GT_[b % 2]
        aT, aT_k = AT_[b % 2]
        yr, yr_k = self.XT[b % 2]
        for kc in range(16):
            self.p.op("pool", lambda e, b=b, kc=kc: e.indirect_dma_start(
                out=w13sb[:, kc, :], out_offset=None, in_=w13rows, in_offset=bass.IndirectOffsetOnAxis(ap=IDXi[:, b, kc:kc + 1], axis=0),
                bounds_check=breg(e, "w13", self.wl * 32 * 2048 - 1), oob_is_err=False),
                r=[IDXi_k], w=[w13_k] + (biga_all if first else []), dma=True)
            first = False
        for fc in range(6):
            self.p.op("pool", lambda e, b=b, fc=fc: e.indirect_dma_start(
                out=w2sb[:, fc, :], out_offset=None, in_=w2rows, in_offset=bass.IndirectOffsetOnAxis(ap=IDXi[:, b, 16 + fc:17 + fc], axis=0),
                bounds_check=breg(e, "w2", self.wl * 32 * 768 - 1), oob_is_err=False),
                r=[IDXi_k], w=[w2_k], dma=True)
        self.dma("sp", xr[:], self.XS[b * 128:(b + 1) * 128, :], r=[self.XS_k], w=[xr_k])
        for g in range(2):
            for j in range(8):
                kc = g * 8 + j
                E("pe", lambda e, xr=xr, kc=kc, j=j: e.transpose(out=pst[:, j * 128:(j + 1) * 128], in_=xr[:, kc * 128:(kc + 1) * 128],
                                                                identity=self.ident_b[:]), r=[xr_k, self.ident_b_k], w=[ps_tk])
            E("act" if g == 0 else "dve", (lambda e, g=g, xT=xT: e.copy(out=xT[:, g * 8:(g + 1) * 8, :], in_=pst.rearrange("p (a b) -> p a b", a=8)))
              if g == 0 else (lambda e, g=g, xT=xT: e.tensor_copy(out=xT[:, g * 8:(g + 1) * 8, :], in_=pst.rearrange("p (a b) -> p a b", a=8))),
              r=[ps_tk], w=[xT_k])
        pss = [self.PS[2], self.PS[3], self.PS[4]]
        for cb in range(3):
            ps, ps_k = pss[cb]
            for kc in range(16):
                E("pe", lambda e, ps=ps, xT=xT, kc=kc, cb=cb: e.matmul(ps[:, :], lhsT=xT[:, kc, :], rhs=w13sb[:, kc, cb * 512:(cb + 1) * 512],
                                                                      start=(kc == 0), stop=(kc == 15)), r=[xT_k, w13_k], w=[ps_k])
        (pA, pA_k), (pB, pB_k), (pC, pC_k) = pss
        E("act", lambda e, ga=ga: e.activation(out=ga[:, 0:512], in_=pA[:, :], func=AF.Silu), r=[pA_k], w=[ga_k])
        E("act", lambda e, ga=ga: e.activation(out=ga[:, 512:768], in_=pB[:, 0:256], func=AF.Silu), r=[pB_k], w=[ga_k])
        E("dve", lambda e, ga=ga: e.tensor_tensor(out=ga[:, 0:256], in0=pB[:, 256:512], in1=ga[:, 0:256], op=ALU.mult), r=[pB_k, ga_k], w=[ga_k])
        E("dve", lambda e, ga=ga: e.tensor_tensor(out=ga[:, 256:768], in0=pC[:, :], in1=ga[:, 256:768], op=ALU.mult), r=[pC_k, ga_k], w=[ga_k])
        for j in range(6):
            E("pe", lambda e, ga=ga, j=j: e.transpose(out=pst[:, j * 128:(j + 1) * 128], in_=ga[:, j * 128:(j + 1) * 128], identity=self.ident_b[:]),
              r=[ga_k, self.ident_b_k], w=[ps_tk])
        E("act", lambda e, aT=aT: e.copy(out=aT[:], in_=pst[:, 0:768].rearrange("p (a b) -> p a b", a=6)), r=[ps_tk], w=[aT_k])
        for cb in range(4):
            ps, ps_k = self.PS[5 + (cb % 2)]
            for fc in range(6):
                E("pe", lambda e, ps=ps, aT=aT, fc=fc, cb=cb: e.matmul(ps[:, :], lhsT=aT[:, fc, :], rhs=w2sb[:, fc, cb * 512:(cb + 1) * 512],
                                                                      start=(fc == 0), stop=(fc == 5)), r=[aT_k, w2_k], w=[ps_k])
            if cb % 2 == 0:
                E("act", lambda e, ps=ps, yr=yr, cb=cb: e.copy(out=yr[:, cb * 512:(cb + 1) * 512], in_=ps[:, :]), r=[ps_k], w=[yr_k])
            else:
                E("dve", lambda e, ps=ps, yr=yr, cb=cb: e.tensor_copy(out=yr[:, cb * 512:(cb + 1) * 512], in_=ps[:, :]), r=[ps_k], w=[yr_k])
        self.dma("sp", self.YS[b * 128:(b + 1) * 128, :], yr[:], r=[yr_k], w=[self.YS_k])
    for t in range(NTILE):
        ty = 1 if t < 2 else 0
        y1, y1_k = self.XT[0]
        y2, y2_k = self.XT[1]
        xa, xa_k = self.SB[t % 2]
        self.p.op("pool", lambda e, t=t: e.indirect_dma_start(out=y1[:], out_offset=None, in_=self.YS[:, :],
                                                              in_offset=bass.IndirectOffsetOnAxis(ap=DSTi[:, t, 0:1], axis=0)),
                  r=[self.YS_k, DSTi_k], w=[y1_k], dma=True)
        self.p.op("pool", lambda e, t=t: e.indirect_dma_start(out=y2[:], out_offset=None, in_=self.YS[:, :],
                                                              in_offset=bass.IndirectOffsetOnAxis(ap=DSTi[:, t, 1:2], axis=0)),
                  r=[self.YS_k, DSTi_k], w=[y2_k], dma=True)
        self.dma("sp", xa[:], self.XR[t * 128:(t + 1) * 128, :], r=[self.XR_k[t]], w=[xa_k])
        E("dve", lambda e, t=t: e.tensor_scalar(out=y1[:], in0=y1[:], scalar1=GAB[:, t, 0:1], scalar2=None, op0=ALU.mult), r=[y1_k, GAB_k], w=[y1_k])
        E("dve", lambda e, t=t: e.scalar_tensor_tensor(out=y1[:], in0=y2[:], scalar=GAB[:, t, 1:2], in1=y1[:], op0=ALU.mult, op1=ALU.add),
          r=[y1_k, y2_k, GAB_k], w=[y1_k])
        E("pool", lambda e, ty=ty: e.tensor_tensor(out=y1[:], in0=y1[:], in1=self.GB[ty][0][:], op=ALU.mult), r=[y1_k, self.GB[ty][1]], w=[y1_k])
        E("dve", lambda e, xa=xa: e.tensor_tensor(out=xa[:], in0=xa[:], in1=y1[:], op=ALU.add), r=[y1_k, xa_k], w=[xa_k])
        self.dma("sp", self.XR[t * 128:(t + 1) * 128, :], xa[:], r=[xa_k], w=[self.XR_k[t]])
    self.p.barrier()


Builder.phase_moe_b = _phase_moe_b
```
